# Optimizing a Trainium2 kernel written in Bass

```python
import math
import jax
import jax.numpy as jnp
from jax import lax
import numpy as np

D_MODEL = 1024
BATCH = 2
SEQ = 16384
DEPTH = 2

HEAD_DIM = 64
BRANCH_WIDTH = 256
N_BRANCHES = 4
A_HEADS = BRANCH_WIDTH // HEAD_DIM
DILATED_PATTERNS = ((128, 1), (512, 4), (2048, 16))
ROPE_THETA = 500000.0
ROPE_DIMS = HEAD_DIM // 4
RWKV_HEADS = BRANCH_WIDTH // HEAD_DIM
RWKV_DECAY_LORA = 64
RWKV_ICLR_LORA = 64
RWKV_GATE_LORA = 128
RWKV_GN_EPS = 64e-5
S5_GROUP_CH = 16
S5_GROUPS = BRANCH_WIDTH // S5_GROUP_CH
S5_STATE = 64
GQA_Q_HEADS = BRANCH_WIDTH // HEAD_DIM
GQA_KV_HEADS = 2
GQA_Q_BLOCK = 128
AXIAL_THETA = 10000.0
GRID_W = 64
DENSE_FF = 2816
N_EXPERTS = 8
TOP_K = 2
EXPERT_FF = 3584
MOE_BLOCK = 512
NORM_EPS = 1e-6
NEG_INF = -1e30
IN_A = 3 * BRANCH_WIDTH
IN_B = 3 * BRANCH_WIDTH
IN_C = BRANCH_WIDTH
IN_DQ = GQA_Q_HEADS * HEAD_DIM
IN_DKV = GQA_KV_HEADS * HEAD_DIM
IN_GATES = N_BRANCHES * D_MODEL
IN_TOTAL = IN_A + IN_B + IN_C + IN_DQ + 2 * IN_DKV + IN_GATES
N_DENSE_LAYERS = (DEPTH + 1) // 2
N_MOE_LAYERS = DEPTH // 2

kernel_name = 'hybrid_gated_dilated_rwkv7_s5_gqa_moe'


def rms_norm(x, g, eps=NORM_EPS):
    xf = x.astype(jnp.float32)
    y = xf * lax.rsqrt(jnp.mean(xf * xf, axis=-1, keepdims=True) + eps)
    return (y * g.astype(jnp.float32)).astype(x.dtype)


def rope_angles(pos, n_freq, theta):
    inv_freq = theta ** (-jnp.arange(n_freq, dtype=jnp.float32) / n_freq)
    return pos.astype(jnp.float32)[:, None] * inv_freq[None, :]


def apply_rotary(x, ang):
    n = ang.shape[-1]
    cos = jnp.cos(ang)[None, :, None, :].astype(x.dtype)
    sin = jnp.sin(ang)[None, :, None, :].astype(x.dtype)
    x1, x2, rest = x[..., :n], x[..., n:2 * n], x[..., 2 * n:]
    return jnp.concatenate([x1 * cos - x2 * sin, x2 * cos + x1 * sin, rest], axis=-1)


def shift_prev(x):
    pad = [(0, 0), (1, 0)] + [(0, 0)] * (x.ndim - 2)
    return jnp.pad(x[:, :-1], pad)


def shift_next(x):
    pad = [(0, 0), (0, 1)] + [(0, 0)] * (x.ndim - 2)
    return jnp.pad(x[:, 1:], pad)


def dilated_window_attention(q, k, v, dilation, radius):
    B, S, H, hd = q.shape
    L = S // dilation
    BD = B * dilation
    blk = radius
    nb = -(-L // blk)
    Lp = nb * blk

    def to_sub(t):
        return t.reshape(B, L, dilation, H, hd).swapaxes(1, 2).reshape(BD, L, H, hd)

    qs = jnp.pad(to_sub(q), ((0, 0), (0, Lp - L), (0, 0), (0, 0))).reshape(BD, nb, blk, H, hd)
    kv_pad = ((0, 0), (blk, Lp - L + blk), (0, 0), (0, 0))
    ks = jnp.pad(to_sub(k), kv_pad).reshape(BD, nb + 2, blk, H, hd)
    vs = jnp.pad(to_sub(v), kv_pad).reshape(BD, nb + 2, blk, H, hd)
    kb = jnp.concatenate([ks[:, :-2], ks[:, 1:-1], ks[:, 2:]], axis=2)
    vb = jnp.concatenate([vs[:, :-2], vs[:, 1:-1], vs[:, 2:]], axis=2)

    qpos = jnp.arange(Lp, dtype=jnp.int32).reshape(nb, blk)
    kpos = qpos[:, :1] - blk + jnp.arange(3 * blk, dtype=jnp.int32)[None, :]
    mask = ((jnp.abs(qpos[:, :, None] - kpos[:, None, :]) <= radius)
            & (kpos[:, None, :] >= 0) & (kpos[:, None, :] < L))

    s = jnp.einsum('znqhd,znkhd->znhqk', qs, kb, preferred_element_type=jnp.float32)
    s = jnp.where(mask[None, :, None], s, NEG_INF)
    m = jnp.max(s, axis=-1, keepdims=True)
    p = jnp.exp(s - m)
    l = jnp.sum(p, axis=-1, keepdims=True)
    o = jnp.einsum('znhqk,znkhd->znqhd', (p / l).astype(v.dtype), vb)
    lse = (m + jnp.log(l))[..., 0].swapaxes(2, 3)

    def from_sub(t):
        t = t.reshape((BD, Lp) + t.shape[3:])[:, :L]
        t = t.reshape((B, dilation, L) + t.shape[2:]).swapaxes(1, 2)
        return t.reshape((B, S) + t.shape[3:])

    return from_sub(o), from_sub(lse)


def dilated_attention_mixer(qkv, ang):
    B, S, _ = qkv.shape
    qkv = qkv.reshape(B, S, 3, A_HEADS, HEAD_DIM)
    q = apply_rotary(qkv[:, :, 0], ang) * (HEAD_DIM ** -0.5)
    k = apply_rotary(qkv[:, :, 1], ang)
    v = qkv[:, :, 2]
    outs, lses = [], []
    for window, dilation in DILATED_PATTERNS:
        o, lse = dilated_window_attention(q, k, v, dilation, window // (2 * dilation))
        outs.append(o)
        lses.append(lse)
    wts = jax.nn.softmax(jnp.stack(lses), axis=0)
    o = jnp.einsum('gbsh,gbshd->bshd', wts.astype(v.dtype), jnp.stack(outs))
    return o.reshape(B, S, A_HEADS * HEAD_DIM)


def wkv7_scan(r, decay, k, v, kappa, iclr, reverse):
    B, S, H, N = r.shape
    xs = tuple(jnp.moveaxis(t.astype(jnp.float32), 1, 0)
               for t in (r, decay, k, v, kappa, iclr * kappa))

    def step(state, inp):
        r_t, w_t, k_t, v_t, kap_t, b_t = inp
        s_kap = jnp.einsum('bhvk,bhk->bhv', state, kap_t)
        state = (state * w_t[:, :, None, :]
                 - s_kap[..., None] * b_t[:, :, None, :]
                 + v_t[..., None] * k_t[:, :, None, :])
        return state, jnp.einsum('bhvk,bhk->bhv', state, r_t)

    _, y = lax.scan(step, jnp.zeros((B, H, N, N), jnp.float32), xs, reverse=reverse)
    return jnp.moveaxis(y, 0, 1)


def rwkv7_mixer(rkv, xn, mu_rkv, mu_x, w0, w1, w2, a0, a1, a2, g1, g2, k_k, k_a, r_k, ln_w, ln_b):
    B, S, _ = rkv.shape
    H, N, W = RWKV_HEADS, HEAD_DIM, BRANCH_WIDTH

    def heads(t):
        return t.reshape(B, S, H, N)

    rkv = rkv.reshape(B, S, 3, W)
    rkv = rkv + mu_rkv[0] * (shift_prev(rkv) - rkv) + mu_rkv[1] * (shift_next(rkv) - rkv)
    r, k, v = rkv[:, :, 0], rkv[:, :, 1], rkv[:, :, 2]
    kap = heads(k * k_k).astype(jnp.float32)
    kap = kap * lax.rsqrt(jnp.sum(kap * kap, axis=-1, keepdims=True) + 1e-12)
    y_state = jnp.zeros((B, S, H, N), jnp.float32)
    bonus = jnp.zeros((B, S, H, N), jnp.float32)
    for d, (x_shift, reverse) in enumerate(((shift_prev(xn), False), (shift_next(xn), True))):
        xd = xn + mu_x[d] * (x_shift - xn)
        w_log = -jax.nn.softplus(-(w0[d] + jnp.tanh(xd @ w1[d]) @ w2[d])) - 0.5
        decay = jnp.exp(-jnp.exp(w_log.astype(jnp.float32)))
        iclr = jax.nn.sigmoid(a0[d] + (xd @ a1[d]) @ a2[d])
        k_d = k * (1.0 + (iclr - 1.0) * k_a)
        y_state = y_state + wkv7_scan(heads(r), heads(decay), heads(k_d), heads(v),
                                      kap, heads(iclr), reverse)
        bonus = bonus + (jnp.sum(heads(r * k_d * r_k), axis=-1, keepdims=True).astype(jnp.float32)
                         * heads(v).astype(jnp.float32))
    mu = jnp.mean(y_state, axis=-1, keepdims=True)
    var = jnp.mean(jnp.square(y_state - mu), axis=-1, keepdims=True)
    y = ((y_state - mu) * lax.rsqrt(var + RWKV_GN_EPS)).reshape(B, S, W) * ln_w + ln_b
    y = y + bonus.reshape(B, S, W)
    g = jax.nn.sigmoid(xn @ g1) @ g2
    return (y * g).astype(xn.dtype)


def _complex_affine_combine(e1, e2):
    a1r, a1i, b1r, b1i = e1
    a2r, a2i, b2r, b2i = e2
    return (a2r * a1r - a2i * a1i, a2r * a1i + a2i * a1r,
            a2r * b1r - a2i * b1i + b2r, a2r * b1i + a2i * b1r + b2i)


def s5_mixer(u, a_re, a_im, log_dt, b_re, b_im, c_re, c_im, d_skip, glu_w, glu_b):
    B, S, W = u.shape
    G, P, C = S5_GROUPS, S5_STATE, S5_GROUP_CH
    ug = u.reshape(B, S, G, C).astype(jnp.float32)
    y = jnp.zeros((B, S, G, C), jnp.float32)
    for d in range(2):
        lr = a_re[d].astype(jnp.float32)
        li = a_im[d].astype(jnp.float32)
        dt = jnp.exp(log_dt[d].astype(jnp.float32))[:, None]
        mag = jnp.exp(lr * dt)
        bar_re, bar_im = mag * jnp.cos(li * dt), mag * jnp.sin(li * dt)
        den = lr * lr + li * li
        f_re = ((bar_re - 1.0) * lr + bar_im * li) / den
        f_im = (bar_im * lr - (bar_re - 1.0) * li) / den
        bb_re = f_re[..., None] * b_re - f_im[..., None] * b_im
        bb_im = f_re[..., None] * b_im + f_im[..., None] * b_re
        bu_re = jnp.einsum('bsgc,gpc->bsgp', ug, bb_re)
        bu_im = jnp.einsum('bsgc,gpc->bsgp', ug, bb_im)
        lam_re = jnp.broadcast_to(bar_re, (1, S, G, P))
        lam_im = jnp.broadcast_to(bar_im, (1, S, G, P))
        _, _, x_re, x_im = lax.associative_scan(
            _complex_affine_combine, (lam_re, lam_im, bu_re, bu_im), reverse=(d == 1), axis=1)
        y = (y + jnp.einsum('bsgp,gcp->bsgc', x_re, c_re[d])
             - jnp.einsum('bsgp,gcp->bsgc', x_im, c_im[d]))
    y = y.reshape(B, S, W) + d_skip * u
    z = jax.nn.gelu(y)
    h = z @ glu_w + glu_b
    return (h[..., :W] * jax.nn.sigmoid(h[..., W:])).astype(u.dtype)


def gqa_axial_mixer(q, k, v, q_norm, k_norm, ang):
    B, S, _ = q.shape
    rep = GQA_Q_HEADS // GQA_KV_HEADS
    q = apply_rotary(rms_norm(q.reshape(B, S, GQA_Q_HEADS, HEAD_DIM), q_norm), ang) * (HEAD_DIM ** -0.5)
    k = apply_rotary(rms_norm(k.reshape(B, S, GQA_KV_HEADS, HEAD_DIM), k_norm), ang)
    v = v.reshape(B, S, GQA_KV_HEADS, HEAD_DIM)
    n_blk = S // GQA_Q_BLOCK
    qb = q.reshape(B, n_blk, GQA_Q_BLOCK, GQA_KV_HEADS, rep, HEAD_DIM).swapaxes(0, 1)

    def attend(q_blk):
        s = jnp.einsum('bqgrd,bkgd->bgrqk', q_blk, k, preferred_element_type=jnp.float32)
        p = jax.nn.softmax(s, axis=-1).astype(v.dtype)
        return jnp.einsum('bgrqk,bkgd->bqgrd', p, v)

    o = lax.map(attend, qb).swapaxes(0, 1)
    return o.reshape(B, S, GQA_Q_HEADS * HEAD_DIM)


def hybrid_mixer(xn, w_in, gate_b, w_branch, w_out, ang_rope, ang_axial,
                 rwkv_mu_rkv, rwkv_mu_x, rwkv_w0, rwkv_w1, rwkv_w2, rwkv_a0, rwkv_a1, rwkv_a2,
                 rwkv_g1, rwkv_g2, rwkv_k_k, rwkv_k_a, rwkv_r_k, rwkv_ln_w, rwkv_ln_b,
                 s5_a_re, s5_a_im, s5_log_dt, s5_b_re, s5_b_im, s5_c_re, s5_c_im, s5_d,
                 s5_glu_w, s5_glu_b, gqa_q_norm, gqa_k_norm):
    B, S, _ = xn.shape
    z = xn @ w_in
    bounds = [int(b) for b in np.cumsum([IN_A, IN_B, IN_C, IN_DQ, IN_DKV, IN_DKV])]
    za, zb, zc, zq, zk, zv, zg = jnp.split(z, bounds, axis=-1)
    ya = dilated_attention_mixer(za, ang_rope)
    yb = rwkv7_mixer(zb, xn, rwkv_mu_rkv, rwkv_mu_x, rwkv_w0, rwkv_w1, rwkv_w2, rwkv_a0, rwkv_a1,
                     rwkv_a2, rwkv_g1, rwkv_g2, rwkv_k_k, rwkv_k_a, rwkv_r_k, rwkv_ln_w, rwkv_ln_b)
    yc = s5_mixer(zc, s5_a_re, s5_a_im, s5_log_dt, s5_b_re, s5_b_im, s5_c_re, s5_c_im, s5_d,
                  s5_glu_w, s5_glu_b)
    yd = gqa_axial_mixer(zq, zk, zv, gqa_q_norm, gqa_k_norm, ang_axial)
    gates = jax.nn.sigmoid(zg.reshape(B, S, N_BRANCHES, D_MODEL) + gate_b)
    branches = (ya, yb, yc, yd)
    merged = sum(gates[:, :, i] * (y_i @ w_branch[i]) for i, y_i in enumerate(branches))
    return merged @ w_out


def swiglu(x, w_gate, w_up, w_down):
    return (jax.nn.silu(x @ w_gate) * (x @ w_up)) @ w_down


def moe_swiglu(x, router, w_gate, w_up, w_down):
    B, S, D = x.shape
    xt = x.reshape(B * S, D)
    N = xt.shape[0]
    NK = N * TOP_K
    logits = jnp.einsum('nd,de->ne', xt, router, preferred_element_type=jnp.float32)
    top_logit, top_e = lax.top_k(logits, TOP_K)
    top_w = jax.nn.softmax(top_logit, axis=-1)
    flat_e = top_e.reshape(NK)
    flat_tok = jnp.arange(NK, dtype=jnp.int32) // TOP_K
    flat_w = top_w.reshape(NK)
    order = jnp.argsort(flat_e)
    se = flat_e[order]
    counts = jnp.zeros((N_EXPERTS,), jnp.int32).at[flat_e].add(1)
    padded = (counts + MOE_BLOCK - 1) // MOE_BLOCK * MOE_BLOCK
    pad_end = jnp.cumsum(padded)
    pad_start = pad_end - padded
    start = jnp.cumsum(counts) - counts
    dest = pad_start[se] + jnp.arange(NK, dtype=jnp.int32) - start[se]
    n_blocks = -(-NK // MOE_BLOCK) + N_EXPERTS
    rows = n_blocks * MOE_BLOCK
    row_tok = jnp.full((rows,), N, jnp.int32).at[dest].set(flat_tok[order])
    row_w = jnp.zeros((rows,), jnp.float32).at[dest].set(flat_w[order])
    blk_e = jnp.minimum(jnp.searchsorted(pad_end, jnp.arange(n_blocks, dtype=jnp.int32) * MOE_BLOCK,
                                         side='right'), N_EXPERTS - 1)
    x_pad = jnp.concatenate([xt, jnp.zeros((1, D), xt.dtype)], axis=0)
    xb = x_pad[row_tok].reshape(n_blocks, MOE_BLOCK, D)

    def expert_block(args):
        x_blk, e = args
        return (jax.nn.silu(x_blk @ w_gate[e]) * (x_blk @ w_up[e])) @ w_down[e]

    yb = lax.map(expert_block, (xb, blk_e)).reshape(rows, D)
    y = jnp.zeros((N + 1, D), yb.dtype).at[row_tok].add(yb * row_w[:, None].astype(yb.dtype))
    return y[:N].reshape(B, S, D)


def setup_inputs(seed: int = 0) -> dict:
    key = jax.random.key(seed)
    keys = iter(jax.random.split(key, 64))

    def normal(shape, scale):
        return scale * jax.random.normal(next(keys), shape, jnp.float32)

    def uniform(shape, lo, hi):
        return jax.random.uniform(next(keys), shape, jnp.float32, lo, hi)

    L, D, W = DEPTH, D_MODEL, BRANCH_WIDTH
    G, P, C = S5_GROUPS, S5_STATE, S5_GROUP_CH
    s5_n = jnp.arange(P, dtype=jnp.float32)
    return {
        'x': normal((BATCH, SEQ, D), 1.0),
        'norm_mix_g': 1.0 + normal((L, D), 0.02),
        'w_in': normal((L, D, IN_TOTAL), D ** -0.5),
        'gate_b': normal((L, N_BRANCHES, D), 0.1),
        'w_branch': normal((L, N_BRANCHES, W, D), W ** -0.5),
        'w_out': normal((L, D, D), D ** -0.5),
        'rwkv_mu_rkv': uniform((L, 2, 3, W), 0.0, 0.5),
        'rwkv_mu_x': uniform((L, 2, D), 0.0, 0.5),
        'rwkv_w0': uniform((L, 2, W), -3.0, 1.0),
        'rwkv_w1': normal((L, 2, D, RWKV_DECAY_LORA), D ** -0.5),
        'rwkv_w2': normal((L, 2, RWKV_DECAY_LORA, W), 0.5 * RWKV_DECAY_LORA ** -0.5),
        'rwkv_a0': normal((L, 2, W), 0.5),
        'rwkv_a1': normal((L, 2, D, RWKV_ICLR_LORA), D ** -0.5),
        'rwkv_a2': normal((L, 2, RWKV_ICLR_LORA, W), 0.5 * RWKV_ICLR_LORA ** -0.5),
        'rwkv_g1': normal((L, D, RWKV_GATE_LORA), D ** -0.5),
        'rwkv_g2': normal((L, RWKV_GATE_LORA, W), RWKV_GATE_LORA ** -0.5),
        'rwkv_k_k': 0.85 + normal((L, W), 0.02),
        'rwkv_k_a': 1.0 + normal((L, W), 0.02),
        'rwkv_r_k': normal((L, W), 0.1),
        'rwkv_ln_w': 1.0 + normal((L, W), 0.02),
        'rwkv_ln_b': normal((L, W), 0.02),
        's5_a_re': -0.5 + normal((L, 2, G, P), 0.01),
        's5_a_im': jnp.pi * s5_n + normal((L, 2, G, P), 0.01),
        's5_log_dt': uniform((L, 2, G), math.log(1e-3), math.log(1e-1)),
        's5_b_re': normal((L, G, P, C), (2 * C) ** -0.5),
        's5_b_im': normal((L, G, P, C), (2 * C) ** -0.5),
        's5_c_re': normal((L, 2, G, C, P), P ** -0.5),
        's5_c_im': normal((L, 2, G, C, P), P ** -0.5),
        's5_d': normal((L, W), 1.0),
        's5_glu_w': normal((L, W, 2 * W), W ** -0.5),
        's5_glu_b': normal((L, 2 * W), 0.01),
        'gqa_q_norm': 1.0 + normal((L, HEAD_DIM), 0.02),
        'gqa_k_norm': 1.0 + normal((L, HEAD_DIM), 0.02),
        'norm_ffn_g': 1.0 + normal((L, D), 0.02),
        'dense_w_gate': normal((N_DENSE_LAYERS, D, DENSE_FF), D ** -0.5),
        'dense_w_up': normal((N_DENSE_LAYERS, D, DENSE_FF), D ** -0.5),
        'dense_w_down': normal((N_DENSE_LAYERS, DENSE_FF, D), DENSE_FF ** -0.5),
        'moe_router': normal((N_MOE_LAYERS, D, N_EXPERTS), D ** -0.5),
        'moe_w_gate': normal((N_MOE_LAYERS, N_EXPERTS, D, EXPERT_FF), D ** -0.5),
        'moe_w_up': normal((N_MOE_LAYERS, N_EXPERTS, D, EXPERT_FF), D ** -0.5),
        'moe_w_down': normal((N_MOE_LAYERS, N_EXPERTS, EXPERT_FF, D), EXPERT_FF ** -0.5),
        'final_norm_g': 1.0 + normal((D,), 0.02),
    }


def reference(x, norm_mix_g, w_in, gate_b, w_branch, w_out,
              rwkv_mu_rkv, rwkv_mu_x, rwkv_w0, rwkv_w1, rwkv_w2, rwkv_a0, rwkv_a1, rwkv_a2,
              rwkv_g1, rwkv_g2, rwkv_k_k, rwkv_k_a, rwkv_r_k, rwkv_ln_w, rwkv_ln_b,
              s5_a_re, s5_a_im, s5_log_dt, s5_b_re, s5_b_im, s5_c_re, s5_c_im, s5_d,
              s5_glu_w, s5_glu_b, gqa_q_norm, gqa_k_norm, norm_ffn_g,
              dense_w_gate, dense_w_up, dense_w_down,
              moe_router, moe_w_gate, moe_w_up, moe_w_down, final_norm_g):
    B, S, D = x.shape
    ROWS = S // GRID_W
    t = jnp.arange(S, dtype=jnp.int32)
    ang_rope = rope_angles(t, ROPE_DIMS // 2, ROPE_THETA)
    row = jnp.repeat(jnp.arange(ROWS, dtype=jnp.int32), GRID_W)
    col = jnp.tile(jnp.arange(GRID_W, dtype=jnp.int32), ROWS)
    ang_axial = jnp.concatenate([rope_angles(row, HEAD_DIM // 4, AXIAL_THETA),
                                 rope_angles(col, HEAD_DIM // 4, AXIAL_THETA)], axis=-1)
    for l in range(DEPTH):
        xn = rms_norm(x, norm_mix_g[l])
        x = x + hybrid_mixer(
            xn, w_in[l], gate_b[l], w_branch[l], w_out[l], ang_rope, ang_axial,
            rwkv_mu_rkv[l], rwkv_mu_x[l], rwkv_w0[l], rwkv_w1[l], rwkv_w2[l], rwkv_a0[l],
            rwkv_a1[l], rwkv_a2[l], rwkv_g1[l], rwkv_g2[l], rwkv_k_k[l], rwkv_k_a[l], rwkv_r_k[l],
            rwkv_ln_w[l], rwkv_ln_b[l],
            s5_a_re[l], s5_a_im[l], s5_log_dt[l], s5_b_re[l], s5_b_im[l], s5_c_re[l], s5_c_im[l],
            s5_d[l], s5_glu_w[l], s5_glu_b[l], gqa_q_norm[l], gqa_k_norm[l])
        xn = rms_norm(x, norm_ffn_g[l])
        i = l // 2
        if l % 2 == 0:
            x = x + swiglu(xn, dense_w_gate[i], dense_w_up[i], dense_w_down[i])
        else:
            x = x + moe_swiglu(xn, moe_router[i], moe_w_gate[i], moe_w_up[i], moe_w_down[i])
    return rms_norm(x, final_norm_g)
```

```python
import os
from concourse.bass_utils import run_bass_kernel_spmd

from contextlib import ExitStack
import numpy as np
import concourse.bass as bass
import concourse.mybir as mybir

F32 = mybir.dt.float32
BF16 = mybir.dt.bfloat16
I32 = mybir.dt.int32
ALU = mybir.AluOpType
AF = mybir.ActivationFunctionType
AX = mybir.AxisListType

COMPUTE = ("tensor", "vector", "scalar", "gpsimd")


class Prog:
    def __init__(self, same_engine_sync=True):
        self.nc = bass.Bass("TRN2", target_bir_lowering=False)
        self.es = ExitStack()
        self.eng = {"tensor": self.nc.tensor, "vector": self.nc.vector, "scalar": self.nc.scalar,
                    "gpsimd": self.nc.gpsimd, "sync": self.nc.sync}
        self.esem = {e: self.es.enter_context(self.nc.semaphore("s_" + e)) for e in COMPUTE}
        self.ecount = {e: 0 for e in COMPUTE}
        self.waited = {}
        self.tsem = {}
        self.writers = {}
        self.readers = {}
        self.gen = {}
        self.ses = True
        self.excl = {}
        self.same_engine_sync = same_engine_sync
        self.n_inst = 0

    def dram(self, name, shape, dtype=F32, kind="ExternalInput"):
        return self.nc.dram_tensor(name, list(shape), dtype, kind=kind).ap()

    def sb(self, name, shape, dtype=F32):
        return self.es.enter_context(self.nc.sbuf_tensor("sb_" + name, list(shape), dtype))

    def ps(self, name, shape, dtype=F32):
        return self.es.enter_context(self.nc.psum_tensor("ps_" + name, list(shape), dtype))

    def _wait(self, eng, ev):
        kind, a, b = ev
        if kind == "e":
            if a == eng and not (self.same_engine_sync and self.ses and eng != "tensor"):
                return
            sem, val, key = self.esem[a], b, (eng, "e" + a)
        else:
            sem, val, key = self.tsem[a][0], b, (eng, "d", a)
        if self.waited.get(key, -1) >= val:
            return
        self.waited[key] = val
        self.eng[eng].wait_ge(sem, val)

    def _deps(self, eng, r, w, wj=()):
        evs = []
        for t in r:
            evs += self.writers.get(t, [])
        for t in w:
            prior = list(self.writers.get(t, ())) + list(self.readers.get(t, ()))
            evs += prior
            self.gen[t] = prior
        for t in wj:
            evs += self.gen.get(t, [])
            evs += self.readers.get(t, [])
            if t in self.excl:
                evs.append(self.excl[t])
        mx = {}
        for (k, a, b) in evs:
            if mx.get((k, a), -1) < b:
                mx[(k, a)] = b
        for (k, a), b in mx.items():
            self._wait(eng, (k, a, b))

    def _commit(self, ev, r, w, wj=()):
        for t in r:
            self.readers.setdefault(t, []).append(ev)
        for t in w:
            self.writers[t] = [ev]
            self.readers[t] = []
            self.excl[t] = ev
        for t in wj:
            self.writers.setdefault(t, []).append(ev)

    def op(self, eng, fn, r=(), w=(), wj=()):
        self._deps(eng, r, w, wj)
        ins = fn()
        self.ecount[eng] += 1
        ins.then_inc(self.esem[eng], 1)
        self._commit(("e", eng, self.ecount[eng]), r, w, wj)
        self.n_inst += 1
        return ins

    def T(self, fn, r=(), w=(), wj=()): return self.op("tensor", fn, r, w, wj)
    def V(self, fn, r=(), w=(), wj=()): return self.op("vector", fn, r, w, wj)
    def A(self, fn, r=(), w=(), wj=()): return self.op("scalar", fn, r, w, wj)
    def G(self, fn, r=(), w=(), wj=()): return self.op("gpsimd", fn, r, w, wj)

    def dma(self, out, in_, r=(), w=(), wj=(), q="sync", key=None, **kw):
        if key is None:
            key = (list(w) + list(wj) + list(r))[0]
        self._deps(q, r, w, wj)
        if key not in self.tsem:
            self.tsem[key] = [self.es.enter_context(self.nc.semaphore("d%d" % len(self.tsem))), 0]
        ent = self.tsem[key]
        ent[1] += 16
        self.eng[q].dma_start(out=out, in_=in_, **kw).then_inc(ent[0], 16)
        self._commit(("d", key, ent[1]), r, w, wj)
        self.n_inst += 1

    def finish(self, q="sync"):
        for key, ent in self.tsem.items():
            self._wait(q, ("d", key, ent[1]))
        return self.nc

    def close(self):
        self.es.close()


NZ = 2944


def rmsnorm_tile(P, nc, xt, xtok, gbc, gtok, out_bf, otok, scr, eps=1e-6, D=1024):
    sq, ss = scr["sq"], scr["ss"]
    P.A(lambda: nc.scalar.activation(out=sq[:], in_=xt, func=AF.Square, accum_out=ss[:]), r=[xtok], w=["sq", "ss"])
    P.V(lambda: nc.vector.tensor_scalar(out=ss[:], in0=ss[:], scalar1=1.0 / D, scalar2=eps, op0=ALU.mult, op1=ALU.add), r=["ss"], w=["ss"])
    P.A(lambda: nc.scalar.sqrt(out=ss[:], in_=ss[:]), r=["ss"], w=["ss"])
    P.V(lambda: nc.vector.reciprocal(out=ss[:], in_=ss[:]), r=["ss"], w=["ss"])
    P.V(lambda: nc.vector.scalar_tensor_tensor(out=out_bf, in0=xt, scalar=ss[:], in1=gbc, op0=ALU.mult, op1=ALU.mult),
        r=[xtok, "ss", gtok], w=[otok])


def load_ident(P, nc, ident_d):
    i32 = P.sb("ident32", [128, 128]); ib = P.sb("identb", [128, 128], BF16)
    P.dma(i32[:], ident_d[:, :], w=["ident32"])
    P.V(lambda: nc.vector.tensor_copy(out=ib[:], in_=i32[:]), r=["ident32"], w=["ident"])
    return ib


def build_P(TC):
    P = Prog(); nc = P.nc
    x = P.dram("x", [TC, 1024]); g = P.dram("g", [1, 1024]); ident_d = P.dram("ident", [128, 128])
    w_mix = P.dram("w_mix", [1024, 2304]); w1 = P.dram("w1", [2, 1024, 64]); a1 = P.dram("a1", [2, 1024, 64])
    g1 = P.dram("g1", [1024, 128]); mu = P.dram("mu", [2, 1024])
    z = P.dram("z", [TC, NZ], kind="ExternalOutput")
    Wsb = P.sb("Wsb", [128, 8, NZ], BF16)
    stg = [P.sb("stg%d" % i, [128, NZ]) for i in range(2)]
    gbc = P.sb("gbc", [128, 1024]); mut = P.sb("mut", [128, 2, 8]); omu = P.sb("omu", [128, 2, 8])
    ident = load_ident(P, nc, ident_d)
    P.dma(gbc[:], g[0:1, :].partition_broadcast(128), w=["gbc"])
    for d in range(2):
        P.dma(mut[:, d, :], mu[d].rearrange("(c p) -> p c", p=128), wj=["mut"], allow_slow_non_contiguous=True)
    P.V(lambda: nc.vector.tensor_scalar(out=omu[:], in0=mut[:], scalar1=-1.0, scalar2=1.0, op0=ALU.mult, op1=ALU.add), r=["mut"], w=["omu"])
    for kc in range(8):
        s = stg[kc % 2]; tk = "stg%d" % (kc % 2)
        rows = slice(kc * 128, (kc + 1) * 128)
        P.dma(s[:, 0:2304], w_mix[rows, :], w=[tk])
        for d in range(2):
            P.dma(s[:, 2304 + d * 256:2304 + d * 256 + 64], w1[d, rows, :], wj=[tk])
            P.dma(s[:, 2304 + d * 256 + 128:2304 + d * 256 + 192], a1[d, rows, :], wj=[tk])
        P.dma(s[:, 2816:2944], g1[rows, :], wj=[tk])
        P.A(lambda: nc.scalar.copy(out=Wsb[:, kc, 0:1152], in_=s[:, 0:1152]), r=[tk], wj=["Wsb"])
        P.V(lambda: nc.vector.tensor_copy(out=Wsb[:, kc, 1152:2304], in_=s[:, 1152:2304]), r=[tk], wj=["Wsb"])
        for d in range(2):
            for wa in range(2):
                b0 = 2304 + d * 256 + wa * 128
                P.V(lambda: nc.vector.tensor_scalar(out=Wsb[:, kc, b0 + 64:b0 + 128], in0=s[:, b0:b0 + 64], scalar1=mut[:, d, kc:kc + 1], scalar2=None, op0=ALU.mult),
                    r=[tk, "mut"], wj=["Wsb"])
                P.V(lambda: nc.vector.tensor_scalar(out=Wsb[:, kc, b0:b0 + 64], in0=s[:, b0:b0 + 64], scalar1=omu[:, d, kc:kc + 1], scalar2=None, op0=ALU.mult),
                    r=[tk, "omu"], wj=["Wsb"])
        P.V(lambda: nc.vector.tensor_copy(out=Wsb[:, kc, 2816:2944], in_=s[:, 2816:2944]), r=[tk], wj=["Wsb"])
    xt = [P.sb("xt%d" % i, [128, 1024]) for i in range(2)]
    xnb = P.sb("xnb", [128, 1024], BF16)
    xnT = P.sb("xnT", [128, 8, 128], BF16)
    zt = [P.sb("zt%d" % i, [128, NZ]) for i in range(2)]
    scr = {"sq": P.sb("sq", [128, 1024]), "ss": P.sb("ss", [128, 1])}
    pst = P.ps("pst", [128, 8, 128], BF16)
    psz = [P.ps("psz%d" % i, [128, 512]) for i in range(4)]
    NT = TC // 128
    P.dma(xt[0][:], x[0:128, :], w=["xt0"])
    ci = 0
    for t in range(NT):
        b = t % 2
        if t + 1 < NT:
            P.dma(xt[1 - b][:], x[(t + 1) * 128:(t + 2) * 128, :], w=["xt%d" % (1 - b)])
        rmsnorm_tile(P, nc, xt[b][:], "xt%d" % b, gbc[:], "gbc", xnb[:], "xnb", scr)
        for kc in range(8):
            P.T(lambda: nc.tensor.transpose(pst[:, kc, :], xnb[:, kc * 128:(kc + 1) * 128], ident[:]), r=["xnb", "ident"],
                w=["pst"] if kc == 0 else [], wj=[] if kc == 0 else ["pst"])
        P.V(lambda: nc.vector.tensor_copy(out=xnT[:], in_=pst[:]), r=["pst"], w=["xnT"])
        for n0 in range(0, NZ, 512):
            n1 = min(NZ, n0 + 512); pz = psz[ci % 4]; pk = "psz%d" % (ci % 4)
            for kc in range(8):
                P.T(lambda: nc.tensor.matmul(pz[:, 0:n1 - n0], lhsT=xnT[:, kc, :], rhs=Wsb[:, kc, n0:n1], start=(kc == 0), stop=(kc == 7)),
                    r=["xnT", "Wsb"], w=[pk] if kc == 0 else [], wj=[] if kc == 0 else [pk])
            first = (n0 == 0)
            if ci % 2 == 0:
                P.A(lambda: nc.scalar.copy(out=zt[b][:, n0:n1], in_=pz[:, 0:n1 - n0]), r=[pk], w=["zt%d" % b] if first else [], wj=[] if first else ["zt%d" % b])
            else:
                P.V(lambda: nc.vector.tensor_copy(out=zt[b][:, n0:n1], in_=pz[:, 0:n1 - n0]), r=[pk], w=["zt%d" % b] if first else [], wj=[] if first else ["zt%d" % b])
            ci += 1
        P.dma(z[t * 128:(t + 1) * 128, :], zt[b][:], r=["zt%d" % b], wj=["z"], key="zt%d" % b)
    P.finish(); P.close()
    return nc


def run_P(xf, prm, l, ncores=8):
    T = xf.shape[0]; TC = T // ncores
    nc = build_P(TC)
    com = {"g": prm["norm_mix_g"][l][None, :], "ident": np.eye(128, dtype=np.float32),
           "w_mix": np.ascontiguousarray(prm["w_in"][l][:, :2304]), "w1": prm["rwkv_w1"][l], "a1": prm["rwkv_a1"][l],
           "g1": prm["rwkv_g1"][l], "mu": prm["rwkv_mu_x"][l]}
    maps = [dict(com, x=xf[c * TC:(c + 1) * TC]) for c in range(ncores)]
    res = run_bass_kernel_spmd(nc, maps, core_ids=list(range(ncores)))
    return np.concatenate([r["z"] for r in res.results], axis=0)


def load_ident32(P, nc, ident_d):
    i32 = P.sb("ident32", [128, 128])
    P.dma(i32[:], ident_d[:, :], w=["ident32"])
    return i32


import os
DBG = int(os.environ.get('DBG', '0'))


def build_F2(TC, F, E, moe, final):
    P = Prog(); nc = P.nc
    x = P.dram("x", [TC, 1024]); g = P.dram("g", [1, 1024]); ident_d = P.dram("ident", [128, 128])
    wg = P.dram("wg", [E, 1024, F]); wu = P.dram("wu", [E, 1024, F]); wd = P.dram("wd", [E, F, 1024])
    if moe:
        router = P.dram("router", [1024, 8])
    if final:
        gf = P.dram("gf", [1, 1024])
    y = P.dram("y", [TC, 1024], kind="ExternalOutput")
    ST = min(TC, 1024); NTT = ST // 128; NST = TC // ST; NTH = max(1, ST // 512); TH = min(512, ST)
    NFC = F // 128
    GC = 11 if NFC % 11 == 0 else 7
    NG = NFC // GC
    ident = load_ident32(P, nc, ident_d)
    gbc = P.sb("gbc", [128, 1024]); P.dma(gbc[:], g[0:1, :].partition_broadcast(128), w=["gbc"])
    if final:
        gfbc = P.sb("gfbc", [128, 1024]); P.dma(gfbc[:], gf[0:1, :].partition_broadcast(128), w=["gfbc"])
    if moe:
        rsb = P.sb("rsb", [128, 8, 8])
        if not (DBG & 8):
            P.dma(rsb[:], router.rearrange("(kc p) e -> p kc e", p=128), w=["rsb"])
        xnT32 = P.sb("xnT32", [128, 8, 128])
        wt = P.sb("wt", [128, NTT, 8]); lg = P.sb("lg", [128, 8]); eq1 = P.sb("eq1", [128, 8]); eq2 = P.sb("eq2", [128, 8])
        m1 = P.sb("m1", [128, 1]); m2 = P.sb("m2", [128, 1])
    acc = P.sb("acc", [128, NTT, 1024])
    xnT = P.sb("xnT", [128, 8, ST], BF16)
    xn32 = P.sb("xn32", [128, 1024]); sq = P.sb("sq", [128, 1024]); ss = P.sb("ss", [128, 1])
    hT = P.sb("hT", [128, GC, ST], BF16)
    wdb = P.sb("wdb", [128, GC, 1024], BF16)
    wds = [P.sb("wds%d" % i, [128, 1024]) for i in range(2)]
    wgs = [P.sb("wgs%d" % i, [128, 8, 128]) for i in range(2)]
    wus = [P.sb("wus%d" % i, [128, 8, 128]) for i in range(2)]
    wgb = [P.sb("wgb%d" % i, [128, 8, 128], BF16) for i in range(2)]
    wub = [P.sb("wub%d" % i, [128, 8, 128], BF16) for i in range(2)]
    sg = [P.sb("sg%d" % i, [128, TH]) for i in range(2)]
    pst = P.ps("pst", [128, 8, 128])
    psg = [P.ps("psg%d" % i, [128, 512]) for i in range(2)]
    psu = [P.ps("psu%d" % i, [128, 512]) for i in range(2)]
    pso = P.ps("pso", [128, 1024])
    wi = 0; di = 0; gi = 0
    for st in range(NST):
        t0 = st * ST
        for tt in range(NTT):
            atok = "acc%d" % tt
            P.dma(acc[:, tt, :], x[t0 + tt * 128:t0 + (tt + 1) * 128, :], w=[atok])
            P.A(lambda: nc.scalar.activation(out=sq[:], in_=acc[:, tt, :], func=AF.Square, accum_out=ss[:]), r=[atok], w=["sq", "ss"])
            P.V(lambda: nc.vector.tensor_scalar(out=ss[:], in0=ss[:], scalar1=1.0 / 1024, scalar2=1e-6, op0=ALU.mult, op1=ALU.add), r=["ss"], w=["ss"])
            P.A(lambda: nc.scalar.sqrt(out=ss[:], in_=ss[:]), r=["ss"], w=["ss"])
            P.V(lambda: nc.vector.reciprocal(out=ss[:], in_=ss[:]), r=["ss"], w=["ss"])
            P.V(lambda: nc.vector.scalar_tensor_tensor(out=xn32[:], in0=acc[:, tt, :], scalar=ss[:], in1=gbc[:], op0=ALU.mult, op1=ALU.mult),
                r=[atok, "ss", "gbc"], w=["xn32"])
            for kc in range(8):
                P.T(lambda: nc.tensor.transpose(pst[:, kc, :], xn32[:, kc * 128:(kc + 1) * 128], ident[:]), r=["xn32", "ident32"],
                    w=["pst"] if kc == 0 else [], wj=[] if kc == 0 else ["pst"])
            P.V(lambda: nc.vector.tensor_copy(out=xnT[:, :, tt * 128:(tt + 1) * 128], in_=pst[:]), r=["pst"], w=["xnT%d" % tt])
            if moe:
                if not (DBG & 16):
                    P.V(lambda: nc.vector.tensor_copy(out=xnT32[:], in_=pst[:]), r=["pst"], w=["xnT32"])
                for kc in range(8 if not (DBG & 1) else 0):
                    P.T(lambda: nc.tensor.matmul(pso[:, 0:8], lhsT=xnT32[:, kc, :], rhs=rsb[:, kc, :], start=(kc == 0), stop=(kc == 7)),
                        r=["xnT32", "rsb"], w=["pso"] if kc == 0 else [], wj=[] if kc == 0 else ["pso"])
                if DBG & 2:
                    P.V(lambda: nc.vector.memset(wt[:, tt, :], 0.5), w=["wt%d" % tt])
                    continue
                if DBG & 1:
                    P.V(lambda: nc.vector.memset(lg[:], 0.5), w=["lg"])
                else:
                    P.V(lambda: nc.vector.tensor_copy(out=lg[:], in_=pso[:, 0:8]), r=["pso"], w=["lg"])
                P.V(lambda: nc.vector.tensor_reduce(out=m1[:], in_=lg[:], axis=AX.X, op=ALU.max), r=["lg"], w=["m1"])
                P.V(lambda: nc.vector.tensor_scalar(out=eq1[:], in0=lg[:], scalar1=m1[:], scalar2=None, op0=ALU.is_equal), r=["lg", "m1"], w=["eq1"])
                P.V(lambda: nc.vector.scalar_tensor_tensor(out=lg[:], in0=eq1[:], scalar=-1e30, in1=lg[:], op0=ALU.mult, op1=ALU.add), r=["eq1", "lg"], w=["lg"])
                P.V(lambda: nc.vector.tensor_reduce(out=m2[:], in_=lg[:], axis=AX.X, op=ALU.max), r=["lg"], w=["m2"])
                P.V(lambda: nc.vector.tensor_scalar(out=eq2[:], in0=lg[:], scalar1=m2[:], scalar2=None, op0=ALU.is_equal), r=["lg", "m2"], w=["eq2"])
                P.V(lambda: nc.vector.tensor_tensor(out=m1[:], in0=m1[:], in1=m2[:], op=ALU.subtract), r=["m1", "m2"], w=["m1"])
                P.A(lambda: nc.scalar.activation(out=m1[:], in_=m1[:], func=AF.Sigmoid), r=["m1"], w=["m1"])
                P.V(lambda: nc.vector.tensor_tensor(out=eq1[:], in0=eq1[:], in1=eq2[:], op=ALU.subtract), r=["eq1", "eq2"], w=["eq1"])
                P.V(lambda: nc.vector.scalar_tensor_tensor(out=wt[:, tt, :], in0=eq1[:], scalar=m1[:], in1=eq2[:], op0=ALU.mult, op1=ALU.add),
                    r=["eq1", "eq2", "m1"], w=["wt%d" % tt])
        xtoks = ["xnT%d" % tt for tt in range(NTT)]
        for e in range(E):
            for grp in range(NG):
                for fci in range(GC):
                    fc = grp * GC + fci; b = wi % 2; wi += 1
                    P.dma(wgs[b][:], wg[e, :, fc * 128:(fc + 1) * 128].rearrange("(kc p) f -> p kc f", p=128), w=["wgs%d" % b])
                    P.dma(wus[b][:], wu[e, :, fc * 128:(fc + 1) * 128].rearrange("(kc p) f -> p kc f", p=128), w=["wus%d" % b])
                    P.G(lambda: nc.gpsimd.tensor_copy(out=wgb[b][:], in_=wgs[b][:]), r=["wgs%d" % b], w=["wgb%d" % b])
                    P.G(lambda: nc.gpsimd.tensor_copy(out=wub[b][:], in_=wus[b][:]), r=["wus%d" % b], w=["wub%d" % b])
                    for th in range(NTH):
                        pb = gi % 2; gi += 1
                        cols = slice(th * TH, (th + 1) * TH)
                        ttk = xtoks[th * (TH // 128):(th + 1) * (TH // 128)]
                        for kc in range(8):
                            P.T(lambda: nc.tensor.matmul(psg[pb][:, 0:TH], lhsT=wgb[b][:, kc, :], rhs=xnT[:, kc, cols], start=(kc == 0), stop=(kc == 7)),
                                r=["wgb%d" % b] + ttk, w=["psg%d" % pb] if kc == 0 else [], wj=[] if kc == 0 else ["psg%d" % pb])
                        for kc in range(8):
                            P.T(lambda: nc.tensor.matmul(psu[pb][:, 0:TH], lhsT=wub[b][:, kc, :], rhs=xnT[:, kc, cols], start=(kc == 0), stop=(kc == 7)),
                                r=["wub%d" % b] + ttk, w=["psu%d" % pb] if kc == 0 else [], wj=[] if kc == 0 else ["psu%d" % pb])
                        P.A(lambda: nc.scalar.activation(out=sg[pb][:], in_=psg[pb][:, 0:TH], func=AF.Silu), r=["psg%d" % pb], w=["sg%d" % pb])
                        P.V(lambda: nc.vector.tensor_tensor(out=hT[:, fci, cols], in0=psu[pb][:, 0:TH], in1=sg[pb][:], op=ALU.mult),
                            r=["psu%d" % pb, "sg%d" % pb], w=["hT%d_%d" % (fci, th)])
                for fci in range(GC):
                    fc = grp * GC + fci; b = di % 2; di += 1
                    P.dma(wds[b][:], wd[e, fc * 128:(fc + 1) * 128, :], w=["wds%d" % b])
                    P.G(lambda: nc.gpsimd.tensor_copy(out=wdb[:, fci, :], in_=wds[b][:]), r=["wds%d" % b], w=["wdb%d" % fci])
                for tt in range(NTT):
                    th = (tt * 128) // TH
                    for half in range(2):
                        for fci in range(GC):
                            P.T(lambda: nc.tensor.matmul(pso[:, half * 512:(half + 1) * 512], lhsT=hT[:, fci, tt * 128:(tt + 1) * 128],
                                                         rhs=wdb[:, fci, half * 512:(half + 1) * 512], start=(fci == 0), stop=(fci == GC - 1)),
                                r=["hT%d_%d" % (fci, th), "wdb%d" % fci], w=["pso"] if (fci == 0 and half == 0) else [], wj=[] if (fci == 0 and half == 0) else ["pso"])
                    atok = "acc%d" % tt
                    if moe and not (DBG & 4):
                        P.V(lambda: nc.vector.scalar_tensor_tensor(out=acc[:, tt, :], in0=pso[:], scalar=wt[:, tt, e:e + 1], in1=acc[:, tt, :], op0=ALU.mult, op1=ALU.add),
                            r=["pso", "wt%d" % tt, atok], w=[atok])
                    else:
                        P.V(lambda: nc.vector.tensor_tensor(out=acc[:, tt, :], in0=pso[:], in1=acc[:, tt, :], op=ALU.add), r=["pso", atok], w=[atok])
        for tt in range(NTT):
            atok = "acc%d" % tt
            if final:
                P.A(lambda: nc.scalar.activation(out=sq[:], in_=acc[:, tt, :], func=AF.Square, accum_out=ss[:]), r=[atok], w=["sq", "ss"])
                P.V(lambda: nc.vector.tensor_scalar(out=ss[:], in0=ss[:], scalar1=1.0 / 1024, scalar2=1e-6, op0=ALU.mult, op1=ALU.add), r=["ss"], w=["ss"])
                P.A(lambda: nc.scalar.sqrt(out=ss[:], in_=ss[:]), r=["ss"], w=["ss"])
                P.V(lambda: nc.vector.reciprocal(out=ss[:], in_=ss[:]), r=["ss"], w=["ss"])
                P.V(lambda: nc.vector.scalar_tensor_tensor(out=acc[:, tt, :], in0=acc[:, tt, :], scalar=ss[:], in1=gfbc[:], op0=ALU.mult, op1=ALU.mult),
                    r=[atok, "ss", "gfbc"], w=[atok])
            P.dma(y[t0 + tt * 128:t0 + (tt + 1) * 128, :], acc[:, tt, :], r=[atok], wj=["y"], key=atok)
    P.finish(); P.close()
    return nc


def run_F2(xf, prm, l, ncores=8):
    T = xf.shape[0]; TC = T // ncores
    moe = (l % 2 == 1); final = (l == 1); i = l // 2
    com = {"g": prm["norm_ffn_g"][l][None, :], "ident": np.eye(128, dtype=np.float32)}
    if moe:
        com.update(wg=prm["moe_w_gate"][i], wu=prm["moe_w_up"][i], wd=prm["moe_w_down"][i], router=prm["moe_router"][i])
        E, F = 8, 3584
    else:
        com.update(wg=prm["dense_w_gate"][i][None], wu=prm["dense_w_up"][i][None], wd=prm["dense_w_down"][i][None])
        E, F = 1, 2816
    if final:
        com["gf"] = prm["final_norm_g"][None, :]
    nc = build_F2(TC, F, E, moe, final)
    maps = [dict(com, x=xf[c * TC:(c + 1) * TC]) for c in range(ncores)]
    res = run_bass_kernel_spmd(nc, maps, core_ids=list(range(ncores)))
    return np.concatenate([r["y"] for r in res.results], axis=0)


def prep_qk(P, nc, src, dstT, PADC, S, nrot, cos_d, sin_d, ident, scale, norm_g, pfx, ps_t):
    NT = S // 128
    xt = [P.sb(pfx + "x%d" % i, [128, 64]) for i in range(2)]
    cs = [P.sb(pfx + "c%d" % i, [128, 2, nrot]) for i in range(2)]
    tmp = P.sb(pfx + "tmp", [128, 4, nrot]); sq = P.sb(pfx + "sq", [128, 64]); ss = P.sb(pfx + "ss", [128, 1])
    if norm_g is not None:
        gbc = P.sb(pfx + "gbc", [128, 64]); P.dma(gbc[:], norm_g[0:1, :].partition_broadcast(128), w=[pfx + "gbc"])
    for t in range(NT):
        b = t % 2; xk = pfx + "x%d" % b; ck = pfx + "c%d" % b
        x = xt[b]; c = cs[b]
        P.dma(x[:], src[t * 128:(t + 1) * 128, :], w=[xk])
        P.dma(c[:, 0, :], cos_d[t * 128:(t + 1) * 128, :], w=[ck])
        P.dma(c[:, 1, :], sin_d[t * 128:(t + 1) * 128, :], wj=[ck])
        if norm_g is not None:
            P.A(lambda: nc.scalar.activation(out=sq[:], in_=x[:], func=AF.Square, accum_out=ss[:]), r=[xk], w=[pfx + "sq", pfx + "ss"])
            P.V(lambda: nc.vector.tensor_scalar(out=ss[:], in0=ss[:], scalar1=1.0 / 64, scalar2=1e-6, op0=ALU.mult, op1=ALU.add), r=[pfx + "ss"], w=[pfx + "ss"])
            P.A(lambda: nc.scalar.sqrt(out=ss[:], in_=ss[:]), r=[pfx + "ss"], w=[pfx + "ss"])
            P.V(lambda: nc.vector.reciprocal(out=ss[:], in_=ss[:]), r=[pfx + "ss"], w=[pfx + "ss"])
            P.V(lambda: nc.vector.scalar_tensor_tensor(out=x[:], in0=x[:], scalar=ss[:], in1=gbc[:], op0=ALU.mult, op1=ALU.mult), r=[xk, pfx + "ss", pfx + "gbc"], w=[xk])
        n = nrot
        x1, x2 = x[:, 0:n], x[:, n:2 * n]
        tk = pfx + "tmp"
        P.V(lambda: nc.vector.tensor_tensor(out=tmp[:, 0, :], in0=x1, in1=c[:, 0, :], op=ALU.mult), r=[xk, ck], w=[tk])
        P.V(lambda: nc.vector.tensor_tensor(out=tmp[:, 1, :], in0=x2, in1=c[:, 1, :], op=ALU.mult), r=[xk, ck], wj=[tk])
        P.V(lambda: nc.vector.tensor_tensor(out=tmp[:, 2, :], in0=x2, in1=c[:, 0, :], op=ALU.mult), r=[xk, ck], wj=[tk])
        P.V(lambda: nc.vector.tensor_tensor(out=tmp[:, 3, :], in0=x1, in1=c[:, 1, :], op=ALU.mult), r=[xk, ck], wj=[tk])
        P.V(lambda: nc.vector.tensor_tensor(out=x1, in0=tmp[:, 0, :], in1=tmp[:, 1, :], op=ALU.subtract), r=[tk], w=[xk])
        P.V(lambda: nc.vector.tensor_tensor(out=x2, in0=tmp[:, 2, :], in1=tmp[:, 3, :], op=ALU.add), r=[tk], wj=[xk])
        P.T(lambda: nc.tensor.transpose(ps_t[0:64, 0:128], x[:], ident[:]), r=[xk, "ident32"], w=["ps_t"])
        P.V(lambda: nc.vector.tensor_scalar(out=dstT[:, PADC + t * 128:PADC + (t + 1) * 128], in0=ps_t[0:64, 0:128], scalar1=scale, scalar2=None, op0=ALU.mult),
            r=["ps_t"], wj=[pfx + "T"])


def normalize_blk(P, nc, acc, acctok, W, yT, c0, ones, ps_bc, rl, ob, obtok):
    P.V(lambda: nc.vector.reciprocal(out=rl[64:65, 0:W], in_=acc[64:65, 0:W]), r=[acctok], w=["rl"])
    P.T(lambda: nc.tensor.matmul(ps_bc[0:64, 0:W], lhsT=ones[64:65, 0:64], rhs=rl[64:65, 0:W], start=True, stop=True), r=["rl", "ones"], w=["ps_bc"])
    P.V(lambda: nc.vector.tensor_tensor(out=ob[:, 0:W], in0=acc[0:64, 0:W], in1=ps_bc[0:64, 0:W], op=ALU.mult), r=[acctok, "ps_bc"], w=[obtok])
    P.dma(yT[:, c0:c0 + W], ob[:, 0:W], r=[obtok], wj=["yT"], key=obtok)


def build_MD(S):
    P = Prog(); nc = P.nc
    q = P.dram("q", [S, 64]); k = P.dram("k", [S, 64]); v = P.dram("v", [S, 64])
    qg = P.dram("qg", [1, 64]); kg = P.dram("kg", [1, 64]); cos_d = P.dram("cos", [S, 32]); sin_d = P.dram("sin", [S, 32])
    ident_d = P.dram("ident", [128, 128])
    yT = P.dram("yT", [64, S], kind="ExternalOutput")
    NT = S // 128
    ident = load_ident32(P, nc, ident_d)
    ones = P.sb("ones", [128, 64]); P.V(lambda: nc.vector.memset(ones[:], 1.0), w=["ones"])
    qT = P.sb("qT", [64, S], BF16); kT = P.sb("kT", [64, S], BF16)
    ps_t = P.ps("ps_t", [128, 512])
    prep_qk(P, nc, q, qT, 0, S, 32, cos_d, sin_d, ident, 0.125, qg, "q", ps_t)
    prep_qk(P, nc, k, kT, 0, S, 32, cos_d, sin_d, ident, 1.0, kg, "k", ps_t)
    Vx = P.sb("Vx", [128, NT, 65], BF16)
    P.V(lambda: nc.vector.memset(Vx[:, :, 64:65], 1.0), w=["Vx"])
    VC = min(16, NT)
    vst = [P.sb("vst%d" % i, [128, VC, 64]) for i in range(2)]
    for ci, m0 in enumerate(range(0, NT, VC)):
        vb = ci % 2
        P.dma(vst[vb][:], v[m0 * 128:(m0 + VC) * 128, :].rearrange("(m p) c -> p m c", p=128), w=["vst%d" % vb])
        P.V(lambda: nc.vector.tensor_copy(out=Vx[:, m0:m0 + VC, 0:64], in_=vst[vb][:]), r=["vst%d" % vb], wj=["Vx"])
    acc = [P.sb("acc%d" % i, [65, 512]) for i in range(2)]
    rl = P.sb("rl", [65, 512]); ob = [P.sb("ob%d" % i, [64, 512]) for i in range(2)]
    NB = 3
    ps_s = [P.ps("ps_s%d" % i, [128, 512]) for i in range(NB)]
    pT = [P.sb("pT%d" % i, [128, 512], BF16) for i in range(NB)]
    po = [P.ps("po%d" % i, [128, 512]) for i in range(2)]
    ps_bc = P.ps("ps_bc", [128, 512])
    W = min(512, S); it = 0
    for qb in range(S // W):
        pb = qb % 2
        for kt in range(NT):
            b = it % NB; it += 1
            P.T(lambda: nc.tensor.matmul(ps_s[b][:, 0:W], lhsT=kT[:, kt * 128:(kt + 1) * 128], rhs=qT[:, qb * W:(qb + 1) * W], start=True, stop=True),
                r=["qT", "kT"], w=["ps_s%d" % b])
            P.A(lambda: nc.scalar.activation(out=pT[b][:, 0:W], in_=ps_s[b][:, 0:W], func=AF.Exp), r=["ps_s%d" % b], w=["pT%d" % b])
            P.T(lambda: nc.tensor.matmul(po[pb][0:65, 0:W], lhsT=Vx[:, kt, :], rhs=pT[b][:, 0:W], start=(kt == 0), stop=(kt == NT - 1)),
                r=["Vx", "pT%d" % b], w=["po%d" % pb] if kt == 0 else [], wj=[] if kt == 0 else ["po%d" % pb])
        P.V(lambda: nc.vector.tensor_copy(out=acc[pb][:, 0:W], in_=po[pb][0:65, 0:W]), r=["po%d" % pb], w=["acc%d" % pb])
        normalize_blk(P, nc, acc[pb], "acc%d" % pb, W, yT, qb * W, ones, ps_bc, rl, ob[pb], "ob%d" % pb)
    P.finish(); P.close()
    return nc


def axial_tables(S):
    def ang(pos, n, theta):
        inv = (np.float32(theta) ** (-np.arange(n, dtype=np.float32) / np.float32(n))).astype(np.float32)
        return pos.astype(np.float32)[:, None] * inv[None, :]
    t = np.arange(S)
    a = np.concatenate([ang(t // 64, 16, 10000.0), ang(t % 64, 16, 10000.0)], axis=-1).astype(np.float32)
    return np.cos(a).astype(np.float32), np.sin(a).astype(np.float32)


def rope_tables(S):
    inv = (np.float32(500000.0) ** (-np.arange(8, dtype=np.float32) / np.float32(8))).astype(np.float32)
    a = (np.arange(S).astype(np.float32)[:, None] * inv[None, :]).astype(np.float32)
    return np.cos(a).astype(np.float32), np.sin(a).astype(np.float32)


def run_MD(zq, zk, zv, prm, l):
    B, S, _ = zq.shape
    nc = build_MD(S)
    cos, sin = axial_tables(S)
    com = {"qg": prm["gqa_q_norm"][l][None, :], "kg": prm["gqa_k_norm"][l][None, :], "cos": cos, "sin": sin, "ident": np.eye(128, dtype=np.float32)}
    maps = []
    for c in range(B * 4):
        b, h = c // 4, c % 4
        maps.append(dict(com, q=np.ascontiguousarray(zq[b, :, h * 64:(h + 1) * 64]), k=np.ascontiguousarray(zk[b, :, (h // 2) * 64:(h // 2 + 1) * 64]),
                         v=np.ascontiguousarray(zv[b, :, (h // 2) * 64:(h // 2 + 1) * 64])))
    res = run_bass_kernel_spmd(nc, maps, core_ids=list(range(B * 4)))
    y = np.zeros((B, S, 256), np.float32)
    for c in range(B * 4):
        b, h = c // 4, c % 4
        y[b, :, h * 64:(h + 1) * 64] = res.results[c]["yT"].T
    return y


def build_MA(S):
    P = Prog(); nc = P.nc
    PADC = 1024
    DIL = (1, 4, 16)
    q = P.dram("q", [S, 64]); k = P.dram("k", [S, 64])
    vx = [P.dram("vx%d" % d, [d * (S // d + 128), 65]) for d in DIL]
    cos_d = P.dram("cos", [S, 8]); sin_d = P.dram("sin", [S, 8])
    ident_d = P.dram("ident", [128, 128]); mask_d = P.dram("mask", [128, 256])
    yT = P.dram("yT", [64, S], kind="ExternalOutput")
    ident = load_ident32(P, nc, ident_d)
    ones = P.sb("ones", [128, 64]); P.V(lambda: nc.vector.memset(ones[:], 1.0), w=["ones"])
    m32 = P.sb("m32", [128, 256]); mask = P.sb("maskb", [128, 256], BF16)
    P.dma(m32[:], mask_d[:, :], w=["m32"]); P.V(lambda: nc.vector.tensor_copy(out=mask[:], in_=m32[:]), r=["m32"], w=["mask"])
    qT = P.sb("qT", [64, S + 2 * PADC], BF16); kT = P.sb("kT", [64, S + 2 * PADC], BF16)
    P.V(lambda: nc.vector.memset(kT[:, 0:PADC], 0.0), w=["kT"]); P.V(lambda: nc.vector.memset(kT[:, PADC + S:], 0.0), wj=["kT"])
    P.V(lambda: nc.vector.memset(qT[:, 0:PADC], 0.0), w=["qT"]); P.V(lambda: nc.vector.memset(qT[:, PADC + S:], 0.0), wj=["qT"])
    ps_t = P.ps("ps_t", [128, 512])
    prep_qk(P, nc, q, qT, PADC, S, 8, cos_d, sin_d, ident, 0.125, None, "q", ps_t)
    prep_qk(P, nc, k, kT, PADC, S, 8, cos_d, sin_d, ident, 1.0, None, "k", ps_t)
    SB = 2048
    accT = P.sb("accT", [65, SB])
    NB = 2
    ps_s = [P.ps("ps_s%d" % i, [128, 512]) for i in range(NB)]
    pT = [P.sb("pT%d" % i, [128, 256], BF16) for i in range(NB)]
    pTm = [P.sb("pTm%d" % i, [128, 256], BF16) for i in range(NB)]
    po = [P.ps("po%d" % i, [128, 512]) for i in range(NB)]
    ps_bc = P.ps("ps_bc", [128, 512])
    rl = P.sb("rl", [65, 512]); ob = [P.sb("ob%d" % i, [64, 512]) for i in range(2)]
    Vxs = {}
    vst = [P.sb("vst%d" % i, [128, 16, 65]) for i in range(2)]
    ci = 0
    for di, d in enumerate(DIL):
        L = S // d; NKT = L // 128 + 1; NTL = d * NKT
        Vx = P.sb("Vx%d" % d, [128, NTL, 65], BF16)
        for m0 in range(0, NTL, 16):
            m1 = min(NTL, m0 + 16); vb = ci % 2; ci += 1
            P.dma(vst[vb][:, 0:m1 - m0, :], vx[di][m0 * 128:m1 * 128, :].rearrange("(m p) c -> p m c", p=128), w=["vst%d" % vb])
            P.V(lambda: nc.vector.tensor_copy(out=Vx[:, m0:m1, :], in_=vst[vb][:, 0:m1 - m0, :]), r=["vst%d" % vb], wj=["Vx%d" % d])
        Vxs[d] = (Vx, NKT)
    it = 0; oi = 0
    for sbi in range(S // SB):
        T0 = sbi * SB
        for di, d in enumerate(DIL):
            Vx, NKT = Vxs[d]
            for r in range(d):
                for bl in range(SB // (128 * d)):
                    i0 = (T0 // d) + bl * 128
                    blk = i0 // 128
                    b = it % NB; it += 1
                    qs = PADC + r + d * i0
                    rhs = qT[:, qs:qs + 127 * d + 1:d]
                    for ab in range(2):
                        ks = PADC + r + d * (i0 - 64 + 128 * ab)
                        P.T(lambda: nc.tensor.matmul(ps_s[b][:, ab * 128:(ab + 1) * 128], lhsT=kT[:, ks:ks + 127 * d + 1:d], rhs=rhs, start=True, stop=True),
                            r=["qT", "kT"], w=["ps_s%d" % b] if ab == 0 else [], wj=[] if ab == 0 else ["ps_s%d" % b])
                    P.A(lambda: nc.scalar.activation(out=pT[b][:], in_=ps_s[b][:, 0:256], func=AF.Exp), r=["ps_s%d" % b], w=["pT%d" % b])
                    P.G(lambda: nc.gpsimd.tensor_tensor(out=pTm[b][:], in0=pT[b][:], in1=mask[:], op=ALU.mult), r=["pT%d" % b, "mask"], w=["pTm%d" % b])
                    for ab in range(2):
                        P.T(lambda: nc.tensor.matmul(po[b][0:65, 0:128], lhsT=Vx[:, r * NKT + blk + ab, :], rhs=pTm[b][:, ab * 128:(ab + 1) * 128], start=(ab == 0), stop=(ab == 1)),
                            r=["Vx%d" % d, "pTm%d" % b], w=["po%d" % b] if ab == 0 else [], wj=[] if ab == 0 else ["po%d" % b])
                    t0 = r + d * bl * 128
                    dst = accT[:, t0:t0 + 127 * d + 1:d]
                    if di == 0:
                        P.V(lambda: nc.vector.tensor_copy(out=dst, in_=po[b][0:65, 0:128]), r=["po%d" % b], w=["accT"] if (bl == 0) else [], wj=[] if (bl == 0) else ["accT"])
                    else:
                        P.V(lambda: nc.vector.tensor_tensor(out=dst, in0=dst, in1=po[b][0:65, 0:128], op=ALU.add), r=["po%d" % b, "accT"], w=["accT"])
        for c0 in range(0, SB, 512):
            o = oi % 2; oi += 1
            normalize_blk(P, nc, accT[:, c0:c0 + 512], "accT", 512, yT, T0 + c0, ones, ps_bc, rl, ob[o], "ob%d" % o)
    P.finish(); P.close()
    return nc


def dil_mask():
    kk = np.arange(128)[:, None]; qq = np.arange(128)[None, :]
    return np.concatenate([(kk >= qq), (kk <= qq)], axis=1).astype(np.float32)


def vext_dilated(v, d):
    S = v.shape[0]; L = S // d
    out = np.zeros((d, L + 128, 65), np.float32)
    vr = v.reshape(L, d, 64).transpose(1, 0, 2)
    out[:, 64:64 + L, :64] = vr
    out[:, 64:64 + L, 64] = 1.0
    return out.reshape(d * (L + 128), 65)


def run_MA(za, prm, l):
    B, S, _ = za.shape
    nc = build_MA(S)
    cos, sin = rope_tables(S)
    com = {"cos": cos, "sin": sin, "ident": np.eye(128, dtype=np.float32), "mask": dil_mask()}
    maps = []
    for c in range(B * 4):
        b, h = c // 4, c % 4
        v = za[b, :, 512 + h * 64:512 + (h + 1) * 64]
        m = dict(com, q=np.ascontiguousarray(za[b, :, h * 64:(h + 1) * 64]), k=np.ascontiguousarray(za[b, :, 256 + h * 64:256 + (h + 1) * 64]))
        for d in (1, 4, 16):
            m["vx%d" % d] = vext_dilated(v, d)
        maps.append(m)
    res = run_bass_kernel_spmd(nc, maps, core_ids=list(range(B * 4)))
    y = np.zeros((B, S, 256), np.float32)
    for c in range(B * 4):
        b, h = c // 4, c % 4
        y[b, :, h * 64:(h + 1) * 64] = res.results[c]["yT"].T
    return y


TWO_PI = 6.283185307179586
PI = 3.141592653589793


def range_reduce(P, nc, r, ki, tok, shape):
    kf = P.sb(tok + "_kf", shape)
    P.V(lambda: nc.vector.tensor_scalar(out=kf[:], in0=r[:], scalar1=1.0 / TWO_PI, scalar2=None, op0=ALU.mult), r=[tok], w=[tok + "kf"])
    P.V(lambda: nc.vector.tensor_copy(out=ki[:], in_=kf[:]), r=[tok + "kf"], w=[tok + "ki"])
    P.V(lambda: nc.vector.tensor_copy(out=kf[:], in_=ki[:]), r=[tok + "ki"], w=[tok + "kf"])
    P.V(lambda: nc.vector.scalar_tensor_tensor(out=r[:], in0=kf[:], scalar=-TWO_PI, in1=r[:], op0=ALU.mult, op1=ALU.add), r=[tok + "kf", tok], w=[tok])
    for (cmp, thr, add) in ((ALU.is_gt, PI, -TWO_PI), (ALU.is_lt, -PI, TWO_PI), (ALU.is_gt, PI, -TWO_PI)):
        P.V(lambda: nc.vector.tensor_scalar(out=kf[:], in0=r[:], scalar1=thr, scalar2=add, op0=cmp, op1=ALU.mult), r=[tok], w=[tok + "kf"])
        P.V(lambda: nc.vector.tensor_tensor(out=r[:], in0=r[:], in1=kf[:], op=ALU.add), r=[tok, tok + "kf"], w=[tok])
    P.V(lambda: nc.vector.tensor_scalar(out=r[:], in0=r[:], scalar1=3.1415925, scalar2=-3.1415925, op0=ALU.min, op1=ALU.max), r=[tok], w=[tok])


def build_MC(S):
    P = Prog(); nc = P.nc
    T = min(512, S); NCH = S // T
    uT_d = [P.dram("uT%d" % d, [64, S]) for d in range(2)]
    a_re = P.dram("a_re", [2, 4, 64]); a_im = P.dram("a_im", [2, 4, 64]); ldt = P.dram("ldt", [2, 4])
    b_re = P.dram("b_re", [4, 64, 16]); b_im = P.dram("b_im", [4, 64, 16])
    c_re = P.dram("c_re", [2, 4, 16, 64]); c_im = P.dram("c_im", [2, 4, 16, 64])
    ident_d = P.dram("ident", [128, 128]); iota_d = P.dram("iota", [1, T + 1])
    yT_d = [P.dram("yT%d" % d, [64, S], kind="ExternalOutput") for d in range(2)]
    ident = load_ident32(P, nc, ident_d)
    iota = P.sb("iota", [128, T + 1]); P.dma(iota[:], iota_d[0:1, :].partition_broadcast(128), w=["iota"])
    uT = []
    UC = min(2048, S)
    ust = [P.sb("ust%d" % i, [64, UC]) for i in range(2)]
    ui = 0
    for d in range(2):
        u = P.sb("uTb%d" % d, [64, S], BF16)
        for c0 in range(0, S, UC):
            ub = ui % 2; ui += 1
            P.dma(ust[ub][:], uT_d[d][:, c0:c0 + UC], w=["ust%d" % ub])
            P.V(lambda: nc.vector.tensor_copy(out=u[:, c0:c0 + UC], in_=ust[ub][:]), r=["ust%d" % ub], wj=["uT%d" % d])
        uT.append(u)
    ps_t = P.ps("ps_t", [128, 512])
    tiles = {}
    for d in range(2):
        for gp in range(2):
            n = "t%d%d" % (d, gp)
            prm = P.sb(n + "prm", [128, 16])
            pk = n + "prm"
            P.dma(prm[:, 0:1], a_re[d, 2 * gp:2 * gp + 2, :].rearrange("g (p o) -> (g p) o", o=1), w=[pk])
            P.dma(prm[:, 1:2], a_im[d, 2 * gp:2 * gp + 2, :].rearrange("g (p o) -> (g p) o", o=1), wj=[pk])
            for g in range(2):
                P.dma(prm[g * 64:(g + 1) * 64, 2:3], ldt[d:d + 1, 2 * gp + g:2 * gp + g + 1].partition_broadcast(64), wj=[pk])
            c = lambda i: prm[:, i:i + 1]
            P.A(lambda: nc.scalar.activation(out=c(2), in_=c(2), func=AF.Exp), r=[pk], w=[pk])
            P.V(lambda: nc.vector.tensor_tensor(out=c(4), in0=c(1), in1=c(2), op=ALU.mult), r=[pk], w=[pk])
            P.V(lambda: nc.vector.tensor_tensor(out=c(3), in0=c(0), in1=c(2), op=ALU.mult), r=[pk], w=[pk])
            P.A(lambda: nc.scalar.activation(out=c(3), in_=c(3), func=AF.Exp), r=[pk], w=[pk])
            cosT = P.sb(n + "cos", [128, T + 1]); sinT = P.sb(n + "sin", [128, T + 1]); ki = P.sb(n + "ki", [128, T + 1], I32)
            P.V(lambda: nc.vector.tensor_scalar(out=sinT[:], in0=iota[:], scalar1=c(4), scalar2=None, op0=ALU.mult), r=["iota", pk], w=[n + "sin"])
            P.V(lambda: nc.vector.tensor_scalar(out=cosT[:], in0=sinT[:], scalar1=PI / 2, scalar2=None, op0=ALU.add), r=[n + "sin"], w=[n + "cos"])
            range_reduce(P, nc, sinT, ki, n + "sin", [128, T + 1])
            range_reduce(P, nc, cosT, ki, n + "cos", [128, T + 1])
            P.A(lambda: nc.scalar.activation(out=sinT[:], in_=sinT[:], func=AF.Sin), r=[n + "sin"], w=[n + "sin"])
            P.A(lambda: nc.scalar.activation(out=cosT[:], in_=cosT[:], func=AF.Sin), r=[n + "cos"], w=[n + "cos"])
            P.V(lambda: nc.vector.tensor_copy(out=c(5), in_=cosT[:, 1:2]), r=[n + "cos", pk], w=[pk])
            P.V(lambda: nc.vector.tensor_copy(out=c(6), in_=sinT[:, 1:2]), r=[n + "sin", pk], w=[pk])
            P.V(lambda: nc.vector.tensor_copy(out=c(13), in_=cosT[:, T:T + 1]), r=[n + "cos", pk], w=[pk])
            P.V(lambda: nc.vector.tensor_copy(out=c(14), in_=sinT[:, T:T + 1]), r=[n + "sin", pk], w=[pk])
            P.V(lambda: nc.vector.tensor_scalar(out=c(15), in0=c(14), scalar1=-1.0, scalar2=None, op0=ALU.mult), r=[pk], w=[pk])
            P.V(lambda: nc.vector.tensor_tensor(out=c(5), in0=c(5), in1=c(3), op=ALU.mult), r=[pk], w=[pk])
            P.V(lambda: nc.vector.tensor_tensor(out=c(6), in0=c(6), in1=c(3), op=ALU.mult), r=[pk], w=[pk])
            P.V(lambda: nc.vector.tensor_scalar(out=c(7), in0=c(5), scalar1=-1.0, scalar2=None, op0=ALU.add), r=[pk], w=[pk])
            P.V(lambda: nc.vector.tensor_tensor(out=c(8), in0=c(0), in1=c(0), op=ALU.mult), r=[pk], w=[pk])
            P.V(lambda: nc.vector.tensor_tensor(out=c(11), in0=c(1), in1=c(1), op=ALU.mult), r=[pk], w=[pk])
            P.V(lambda: nc.vector.tensor_tensor(out=c(8), in0=c(8), in1=c(11), op=ALU.add), r=[pk], w=[pk])
            P.V(lambda: nc.vector.reciprocal(out=c(8), in_=c(8)), r=[pk], w=[pk])
            P.V(lambda: nc.vector.tensor_tensor(out=c(9), in0=c(7), in1=c(0), op=ALU.mult), r=[pk], w=[pk])
            P.V(lambda: nc.vector.tensor_tensor(out=c(11), in0=c(6), in1=c(1), op=ALU.mult), r=[pk], w=[pk])
            P.V(lambda: nc.vector.tensor_tensor(out=c(9), in0=c(9), in1=c(11), op=ALU.add), r=[pk], w=[pk])
            P.V(lambda: nc.vector.tensor_tensor(out=c(9), in0=c(9), in1=c(8), op=ALU.mult), r=[pk], w=[pk])
            P.V(lambda: nc.vector.tensor_tensor(out=c(10), in0=c(6), in1=c(0), op=ALU.mult), r=[pk], w=[pk])
            P.V(lambda: nc.vector.tensor_tensor(out=c(11), in0=c(7), in1=c(1), op=ALU.mult), r=[pk], w=[pk])
            P.V(lambda: nc.vector.tensor_tensor(out=c(10), in0=c(10), in1=c(11), op=ALU.subtract), r=[pk], w=[pk])
            P.V(lambda: nc.vector.tensor_tensor(out=c(10), in0=c(10), in1=c(8), op=ALU.mult), r=[pk], w=[pk])
            P.V(lambda: nc.vector.tensor_scalar(out=c(12), in0=c(10), scalar1=-1.0, scalar2=None, op0=ALU.mult), r=[pk], w=[pk])
            braw = P.sb(n + "braw", [128, 2, 16]); bk = n + "braw"
            P.dma(braw[:, 0, :], b_re[2 * gp:2 * gp + 2].rearrange("g p c -> (g p) c"), w=[bk])
            P.dma(braw[:, 1, :], b_im[2 * gp:2 * gp + 2].rearrange("g p c -> (g p) c"), wj=[bk])
            BD = P.sb(n + "BD", [128, 2, 64]); tb = P.sb(n + "tb", [128, 16])
            P.V(lambda: nc.vector.memset(BD[:], 0.0), w=[n + "BD"])
            for g in range(2):
                rows = slice(g * 64, (g + 1) * 64); cols = slice(32 * gp + 16 * g, 32 * gp + 16 * g + 16)
                P.V(lambda: nc.vector.tensor_scalar(out=tb[rows, :], in0=braw[rows, 1, :], scalar1=prm[rows, 12:13], scalar2=None, op0=ALU.mult), r=[bk, pk], w=[n + "tb"])
                P.V(lambda: nc.vector.scalar_tensor_tensor(out=BD[rows, 0, cols], in0=braw[rows, 0, :], scalar=prm[rows, 9:10], in1=tb[rows, :], op0=ALU.mult, op1=ALU.add),
                    r=[bk, pk, n + "tb"], wj=[n + "BD"])
                P.V(lambda: nc.vector.tensor_scalar(out=tb[rows, :], in0=braw[rows, 0, :], scalar1=prm[rows, 10:11], scalar2=None, op0=ALU.mult), r=[bk, pk], w=[n + "tb"])
                P.V(lambda: nc.vector.scalar_tensor_tensor(out=BD[rows, 1, cols], in0=braw[rows, 1, :], scalar=prm[rows, 9:10], in1=tb[rows, :], op0=ALU.mult, op1=ALU.add),
                    r=[bk, pk, n + "tb"], wj=[n + "BD"])
            BT = P.sb(n + "BT", [64, 2, 128], BF16)
            for ri in range(2):
                P.T(lambda: nc.tensor.transpose(ps_t[0:64, 0:128], BD[:, ri, :], ident[:]), r=[n + "BD", "ident32"], w=["ps_t"])
                P.V(lambda: nc.vector.tensor_copy(out=BT[:, ri, :], in_=ps_t[0:64, 0:128]), r=["ps_t"], wj=[n + "BT"])
            craw = P.sb(n + "craw", [128, 2, 16]); ck = n + "craw"
            for g in range(2):
                P.dma(craw[g * 64:(g + 1) * 64, 0, :], c_re[d, 2 * gp + g].rearrange("c p -> p c"), wj=[ck], allow_slow_non_contiguous=True)
                P.dma(craw[g * 64:(g + 1) * 64, 1, :], c_im[d, 2 * gp + g].rearrange("c p -> p c"), wj=[ck], allow_slow_non_contiguous=True)
            CT = P.sb(n + "CT", [128, 2, 64], BF16)
            P.V(lambda: nc.vector.memset(CT[:], 0.0), w=[n + "CT"])
            for g in range(2):
                rows = slice(g * 64, (g + 1) * 64); cols = slice(32 * gp + 16 * g, 32 * gp + 16 * g + 16)
                P.V(lambda: nc.vector.tensor_copy(out=CT[rows, 0, cols], in_=craw[rows, 0, :]), r=[ck], wj=[n + "CT"])
                P.V(lambda: nc.vector.tensor_scalar(out=CT[rows, 1, cols], in0=craw[rows, 1, :], scalar1=-1.0, scalar2=None, op0=ALU.mult), r=[ck], wj=[n + "CT"])
            rho_t = P.sb(n + "rho", [128, T])
            P.V(lambda: nc.vector.tensor_scalar(out=rho_t[:], in0=iota[:, 0:T], scalar1=0.0, scalar2=prm[:, 3:4], op0=ALU.mult, op1=ALU.add), r=["iota", pk], w=[n + "rho"])
            init = P.sb(n + "init", [128, 2]); P.V(lambda: nc.vector.memset(init[:], 0.0), w=[n + "init"])
            tiles[(d, gp)] = dict(n=n, prm=prm, pk=pk, cosT=cosT, sinT=sinT, BT=BT, CT=CT, rho=rho_t, init=init)
    ps_b = [P.ps("ps_b%d" % i, [128, 512]) for i in range(2)]
    ps_y = P.ps("ps_y", [128, 512])
    m = [P.sb("m%d" % i, [128, T]) for i in range(4)]
    bp = [P.sb("bp%d" % i, [128, T]) for i in range(2)]
    wv = [P.sb("wv%d" % i, [128, T]) for i in range(2)]
    pp = [P.sb("pp%d" % i, [128, T]) for i in range(4)]
    xb = [[P.sb("xb%d_%d" % (gp, i), [128, T], BF16) for i in range(2)] for gp in range(2)]
    yo = [P.sb("yo%d" % i, [64, T]) for i in range(2)]
    tmpc = P.sb("tmpc", [128, 2])
    it = 0
    for d in range(2):
        for ch in range(NCH):
            cols = slice(ch * T, (ch + 1) * T)
            for gp in range(2):
                t = tiles[(d, gp)]; n = t["n"]; cosT, sinT = t["cosT"], t["sinT"]
                for ri in range(2):
                    P.T(lambda: nc.tensor.matmul(ps_b[ri][:, 0:T], lhsT=t["BT"][:, ri, :], rhs=uT[d][:, cols], start=True, stop=True), r=[n + "BT", "uT%d" % d], w=["ps_b%d" % ri])
                P.V(lambda: nc.vector.tensor_tensor(out=m[0][:], in0=ps_b[0][:, 0:T], in1=cosT[:, 0:T], op=ALU.mult), r=["ps_b0", n + "cos"], w=["m0"])
                P.V(lambda: nc.vector.tensor_tensor(out=m[1][:], in0=ps_b[1][:, 0:T], in1=sinT[:, 0:T], op=ALU.mult), r=["ps_b1", n + "sin"], w=["m1"])
                P.V(lambda: nc.vector.tensor_tensor(out=m[2][:], in0=ps_b[1][:, 0:T], in1=cosT[:, 0:T], op=ALU.mult), r=["ps_b1", n + "cos"], w=["m2"])
                P.V(lambda: nc.vector.tensor_tensor(out=m[3][:], in0=ps_b[0][:, 0:T], in1=sinT[:, 0:T], op=ALU.mult), r=["ps_b0", n + "sin"], w=["m3"])
                P.G(lambda: nc.gpsimd.tensor_tensor(out=bp[0][:], in0=m[0][:], in1=m[1][:], op=ALU.add), r=["m0", "m1"], w=["bp0"])
                P.G(lambda: nc.gpsimd.tensor_tensor(out=bp[1][:], in0=m[2][:], in1=m[3][:], op=ALU.subtract), r=["m2", "m3"], w=["bp1"])
                for ri in range(2):
                    P.V(lambda: nc.vector.tensor_tensor_scan(out=wv[ri][:], data0=t["rho"][:], data1=bp[ri][:], initial=t["init"][:, ri:ri + 1], op0=ALU.mult, op1=ALU.add),
                        r=[n + "rho", "bp%d" % ri, n + "init"], w=["wv%d" % ri])
                prm = t["prm"]
                P.V(lambda: nc.vector.tensor_scalar(out=tmpc[:, 0:1], in0=wv[0][:, T - 1:T], scalar1=prm[:, 13:14], scalar2=None, op0=ALU.mult), r=["wv0", t["pk"]], w=["tmpc"])
                P.V(lambda: nc.vector.tensor_scalar(out=tmpc[:, 1:2], in0=wv[0][:, T - 1:T], scalar1=prm[:, 14:15], scalar2=None, op0=ALU.mult), r=["wv0", t["pk"]], wj=["tmpc"])
                P.V(lambda: nc.vector.scalar_tensor_tensor(out=t["init"][:, 0:1], in0=wv[1][:, T - 1:T], scalar=prm[:, 15:16], in1=tmpc[:, 0:1], op0=ALU.mult, op1=ALU.add),
                    r=["wv1", "tmpc", t["pk"]], w=[n + "init"])
                P.V(lambda: nc.vector.scalar_tensor_tensor(out=t["init"][:, 1:2], in0=wv[1][:, T - 1:T], scalar=prm[:, 13:14], in1=tmpc[:, 1:2], op0=ALU.mult, op1=ALU.add),
                    r=["wv1", "tmpc", t["pk"]], wj=[n + "init"])
                P.G(lambda: nc.gpsimd.tensor_tensor(out=pp[0][:], in0=wv[0][:], in1=cosT[:, 0:T], op=ALU.mult), r=["wv0", n + "cos"], w=["pp0"])
                P.G(lambda: nc.gpsimd.tensor_tensor(out=pp[1][:], in0=wv[1][:], in1=sinT[:, 0:T], op=ALU.mult), r=["wv1", n + "sin"], w=["pp1"])
                P.G(lambda: nc.gpsimd.tensor_tensor(out=xb[gp][0][:], in0=pp[0][:], in1=pp[1][:], op=ALU.subtract), r=["pp0", "pp1"], w=["xb%d_0" % gp])
                P.G(lambda: nc.gpsimd.tensor_tensor(out=pp[2][:], in0=wv[0][:], in1=sinT[:, 0:T], op=ALU.mult), r=["wv0", n + "sin"], w=["pp2"])
                P.G(lambda: nc.gpsimd.tensor_tensor(out=pp[3][:], in0=wv[1][:], in1=cosT[:, 0:T], op=ALU.mult), r=["wv1", n + "cos"], w=["pp3"])
                P.G(lambda: nc.gpsimd.tensor_tensor(out=xb[gp][1][:], in0=pp[2][:], in1=pp[3][:], op=ALU.add), r=["pp2", "pp3"], w=["xb%d_1" % gp])
            k = 0
            for gp in range(2):
                t = tiles[(d, gp)]
                for ri in range(2):
                    P.T(lambda: nc.tensor.matmul(ps_y[0:64, 0:T], lhsT=t["CT"][:, ri, :], rhs=xb[gp][ri][:], start=(k == 0), stop=(k == 3)),
                        r=[t["n"] + "CT", "xb%d_%d" % (gp, ri)], w=["ps_y"] if k == 0 else [], wj=[] if k == 0 else ["ps_y"])
                    k += 1
            b = it % 2; it += 1
            P.V(lambda: nc.vector.tensor_copy(out=yo[b][:], in_=ps_y[0:64, 0:T]), r=["ps_y"], w=["yo%d" % b])
            P.dma(yT_d[d][:, cols], yo[b][:], r=["yo%d" % b], wj=["yT%d" % d], key="yo%d" % b)
    P.finish(); P.close()
    return nc


def mc_inputs(zc_b, prm, l, h, S):
    T = min(512, S)
    u = zc_b[:, h * 64:(h + 1) * 64]
    gs = slice(4 * h, 4 * h + 4)
    return {"uT0": np.ascontiguousarray(u.T), "uT1": np.ascontiguousarray(u[::-1].T),
            "a_re": np.ascontiguousarray(prm["s5_a_re"][l][:, gs]), "a_im": np.ascontiguousarray(prm["s5_a_im"][l][:, gs]),
            "ldt": np.ascontiguousarray(prm["s5_log_dt"][l][:, gs]), "b_re": np.ascontiguousarray(prm["s5_b_re"][l][gs]), "b_im": np.ascontiguousarray(prm["s5_b_im"][l][gs]),
            "c_re": np.ascontiguousarray(prm["s5_c_re"][l][:, gs]), "c_im": np.ascontiguousarray(prm["s5_c_im"][l][:, gs]),
            "ident": np.eye(128, dtype=np.float32), "iota": np.arange(T + 1, dtype=np.float32)[None, :]}


def run_MC(zc, prm, l):
    B, S, _ = zc.shape
    nc = build_MC(S)
    maps = [mc_inputs(zc[c // 4], prm, l, c % 4, S) for c in range(B * 4)]
    res = run_bass_kernel_spmd(nc, maps, core_ids=list(range(B * 4)))
    yf = np.zeros((B, S, 256), np.float32); yb = np.zeros((B, S, 256), np.float32)
    for c in range(B * 4):
        b, h = c // 4, c % 4
        yf[b, :, h * 64:(h + 1) * 64] = res.results[c]["yT0"].T
        yb[b, :, h * 64:(h + 1) * 64] = res.results[c]["yT1"].T[::-1]
    return yf, yb


EM05 = 0.6065306597126334


def build_MB(S):
    P = Prog(); nc = P.nc
    NT = S // 128
    rkv = [P.dram("rkv%d" % d, [S, 192]) for d in range(2)]
    h1 = [[P.dram("h%s1_%d" % (n, d), [64, S]) for d in range(2)] for n in "wa"]
    h2 = [[P.dram("h%s2_%d" % (n, d), [64, S]) for d in range(2)] for n in "wa"]
    w2 = P.dram("w2", [2, 64, 64]); a2 = P.dram("a2", [2, 64, 64]); w0 = P.dram("w0", [2, 64]); a0 = P.dram("a0", [2, 64])
    mu = P.dram("mu", [2, 2, 192]); kka = P.dram("kka", [3, 64])
    ident_d = P.dram("ident", [128, 128]); zsel_d = P.dram("zsel", [64, 32 * 128])
    y_o = P.dram("y", [2, S, 64], kind="ExternalOutput"); bonus_o = P.dram("bonus", [2, S, 64], kind="ExternalOutput")
    pkd = [P.dram("pkd%d" % d, [S, 256], BF16, kind="Internal") for d in range(2)]
    dkd = [P.dram("dkd%d" % d, [S, 64], F32, kind="Internal") for d in range(2)]
    vTs = P.dram("vTs", [128, S], F32, kind="Internal")
    ident = load_ident32(P, nc, ident_d)
    z32 = P.sb("z32", [64, 32 * 128]); zb = P.sb("zb", [64, 32 * 128], BF16)
    P.dma(z32[:], zsel_d[:, :], w=["z32"]); P.V(lambda: nc.vector.tensor_copy(out=zb[:], in_=z32[:]), r=["z32"], w=["zb"])
    def bc(name, src, n):
        t = P.sb(name, [128, n]); P.dma(t[:], src.partition_broadcast(128), w=[name]); return t
    kk_bc = bc("kk_bc", kka[0:1, :], 64); ka_bc = bc("ka_bc", kka[1:2, :], 64); rk_bc = bc("rk_bc", kka[2:3, :], 64)
    ps_t = P.ps("ps_t", [128, 512]); ps_l = P.ps("ps_l", [128, 512])
    thT = P.sb("thT", [64, S], BF16); haT = P.sb("haT", [64, S], BF16)
    CW = min(2048, S)
    ha = P.sb("ha_", [64, CW]); hb = P.sb("hb_", [64, CW])
    vstage = P.sb("vstage", [128, 128]); P.V(lambda: nc.vector.memset(vstage[:], 0.0), w=["vstage"])
    for d in range(2):
        for wi, dst in enumerate((thT, haT)):
            dk_ = "thT" if wi == 0 else "haT"
            for ci, c0 in enumerate(range(0, S, CW)):
                P.dma(ha[:], h1[wi][d][:, c0:c0 + CW], w=["ha"])
                if c0 == 0:
                    P.V(lambda: nc.vector.memset(hb[:, 0:1], 0.0), w=["hb"])
                    P.dma(hb[:, 1:CW], h2[wi][d][:, 0:CW - 1], wj=["hb"])
                else:
                    P.dma(hb[:], h2[wi][d][:, c0 - 1:c0 + CW - 1], w=["hb"])
                P.V(lambda: nc.vector.tensor_tensor(out=ha[:], in0=ha[:], in1=hb[:], op=ALU.add), r=["ha", "hb"], w=["ha"])
                first = (ci == 0)
                if wi == 0:
                    P.A(lambda: nc.scalar.activation(out=dst[:, c0:c0 + CW], in_=ha[:], func=AF.Tanh), r=["ha"], w=[dk_] if first else [], wj=[] if first else [dk_])
                else:
                    P.V(lambda: nc.vector.tensor_copy(out=dst[:, c0:c0 + CW], in_=ha[:]), r=["ha"], w=[dk_] if first else [], wj=[] if first else [dk_])
        w2s = P.sb("w2s%d" % d, [64, 2, 64]); w2b = P.sb("w2b%d" % d, [64, 2, 64], BF16)
        P.dma(w2s[:, 0, :], w2[d], w=["w2s%d" % d]); P.dma(w2s[:, 1, :], a2[d], wj=["w2s%d" % d])
        P.V(lambda: nc.vector.tensor_copy(out=w2b[:], in_=w2s[:]), r=["w2s%d" % d], w=["w2b%d" % d])
        w0_bc = bc("w0_bc%d" % d, w0[d:d + 1, :], 64); a0_bc = bc("a0_bc%d" % d, a0[d:d + 1, :], 64)
        mu0_bc = bc("mu0_bc%d" % d, mu[d, 0:1, :], 192); mu1_bc = bc("mu1_bc%d" % d, mu[d, 1:2, :], 192)
        cur = [P.sb("cur%d_%d" % (d, i), [128, 192]) for i in range(2)]
        prv = [P.sb("prv%d_%d" % (d, i), [128, 192]) for i in range(2)]
        nxt = [P.sb("nxt%d_%d" % (d, i), [128, 192]) for i in range(2)]
        d0 = P.sb("d0_%d" % d, [128, 192]); d1 = P.sb("d1_%d" % d, [128, 192]); mix = P.sb("mix%d" % d, [128, 192])
        dec = [P.sb("dec%d_%d" % (d, i), [128, 64]) for i in range(2)]
        pack = [P.sb("pack%d_%d" % (d, i), [128, 256], BF16) for i in range(2)]
        bon = [P.sb("bon%d_%d" % (d, i), [128, 64]) for i in range(2)]
        vto = [P.sb("vto%d_%d" % (d, i), [128, 128]) for i in range(2)]
        icl = P.sb("icl%d" % d, [128, 64]); kkt = P.sb("kkt%d" % d, [128, 64]); kap = P.sb("kap%d" % d, [128, 64]); kd = P.sb("kd%d" % d, [128, 64])
        t1 = P.sb("t1_%d" % d, [128, 64]); sq = P.sb("sq%d" % d, [128, 64]); ss = P.sb("ss%d" % d, [128, 1]); sb_ = P.sb("sb%d" % d, [128, 1])
        for t in range(NT):
            b = t % 2; r0 = t * 128
            ck, pk_, nk = "cur%d" % b, "prv%d" % b, "nxt%d" % b
            P.dma(cur[b][:], rkv[d][r0:r0 + 128, :], w=[ck])
            if t == 0:
                P.V(lambda: nc.vector.memset(prv[b][:], 0.0), w=[pk_])
                P.dma(prv[b][1:128, :], rkv[d][0:127, :], wj=[pk_])
            else:
                P.dma(prv[b][:], rkv[d][r0 - 1:r0 + 127, :], w=[pk_])
            if t == NT - 1:
                P.V(lambda: nc.vector.memset(nxt[b][:], 0.0), w=[nk])
                P.dma(nxt[b][0:127, :], rkv[d][r0 + 1:r0 + 128, :], wj=[nk])
            else:
                P.dma(nxt[b][:], rkv[d][r0 + 1:r0 + 129, :], w=[nk])
            P.T(lambda: nc.tensor.matmul(ps_l[:, 0:64], lhsT=thT[:, r0:r0 + 128], rhs=w2b[:, 0, :], start=True, stop=True), r=["thT", "w2b%d" % d], w=["ps_l"])
            P.T(lambda: nc.tensor.matmul(ps_l[:, 64:128], lhsT=haT[:, r0:r0 + 128], rhs=w2b[:, 1, :], start=True, stop=True), r=["haT", "w2b%d" % d], wj=["ps_l"])
            P.V(lambda: nc.vector.tensor_tensor(out=d0[:], in0=prv[b][:], in1=cur[b][:], op=ALU.subtract), r=[pk_, ck], w=["d0"])
            P.V(lambda: nc.vector.tensor_tensor(out=d0[:], in0=d0[:], in1=mu0_bc[:], op=ALU.mult), r=["d0", "mu0_bc%d" % d], w=["d0"])
            P.V(lambda: nc.vector.tensor_tensor(out=d1[:], in0=nxt[b][:], in1=cur[b][:], op=ALU.subtract), r=[nk, ck], w=["d1"])
            P.V(lambda: nc.vector.tensor_tensor(out=d1[:], in0=d1[:], in1=mu1_bc[:], op=ALU.mult), r=["d1", "mu1_bc%d" % d], w=["d1"])
            P.V(lambda: nc.vector.tensor_tensor(out=d0[:], in0=d0[:], in1=d1[:], op=ALU.add), r=["d0", "d1"], w=["d0"])
            P.V(lambda: nc.vector.tensor_tensor(out=mix[:], in0=cur[b][:], in1=d0[:], op=ALU.add), r=[ck, "d0"], w=["mix"])
            rr, kp, vp = mix[:, 0:64], mix[:, 64:128], mix[:, 128:192]
            P.V(lambda: nc.vector.tensor_tensor(out=t1[:], in0=ps_l[:, 0:64], in1=w0_bc[:], op=ALU.add), r=["ps_l", "w0_bc%d" % d], w=["t1"])
            P.A(lambda: nc.scalar.activation(out=t1[:], in_=t1[:], func=AF.Sigmoid), r=["t1"], w=["t1"])
            P.A(lambda: nc.scalar.activation(out=dec[b][:], in_=t1[:], func=AF.Exp, scale=-EM05), r=["t1"], w=["dec%d" % b])
            P.V(lambda: nc.vector.tensor_tensor(out=icl[:], in0=ps_l[:, 64:128], in1=a0_bc[:], op=ALU.add), r=["ps_l", "a0_bc%d" % d], w=["icl"])
            P.A(lambda: nc.scalar.activation(out=icl[:], in_=icl[:], func=AF.Sigmoid), r=["icl"], w=["icl"])
            P.V(lambda: nc.vector.tensor_tensor(out=kkt[:], in0=kp, in1=kk_bc[:], op=ALU.mult), r=["mix", "kk_bc"], w=["kkt"])
            P.A(lambda: nc.scalar.activation(out=sq[:], in_=kkt[:], func=AF.Square, accum_out=ss[:]), r=["kkt"], w=["sq", "ss"])
            P.V(lambda: nc.vector.tensor_scalar(out=ss[:], in0=ss[:], scalar1=1e-12, scalar2=None, op0=ALU.add), r=["ss"], w=["ss"])
            P.A(lambda: nc.scalar.sqrt(out=ss[:], in_=ss[:]), r=["ss"], w=["ss"])
            P.V(lambda: nc.vector.reciprocal(out=ss[:], in_=ss[:]), r=["ss"], w=["ss"])
            P.V(lambda: nc.vector.tensor_scalar(out=kap[:], in0=kkt[:], scalar1=ss[:], scalar2=None, op0=ALU.mult), r=["kkt", "ss"], w=["kap"])
            P.V(lambda: nc.vector.tensor_tensor(out=t1[:], in0=icl[:], in1=ka_bc[:], op=ALU.mult), r=["icl", "ka_bc"], w=["t1"])
            P.V(lambda: nc.vector.scalar_tensor_tensor(out=t1[:], in0=t1[:], scalar=1.0, in1=ka_bc[:], op0=ALU.add, op1=ALU.subtract), r=["t1", "ka_bc"], w=["t1"])
            P.V(lambda: nc.vector.tensor_tensor(out=kd[:], in0=kp, in1=t1[:], op=ALU.mult), r=["mix", "t1"], w=["kd"])
            pkk = "pack%d" % b
            P.V(lambda: nc.vector.tensor_copy(out=pack[b][:, 0:64], in_=rr), r=["mix"], w=[pkk])
            P.V(lambda: nc.vector.tensor_copy(out=pack[b][:, 64:128], in_=kap[:]), r=["kap"], wj=[pkk])
            P.V(lambda: nc.vector.scalar_tensor_tensor(out=pack[b][:, 128:192], in0=icl[:], scalar=-1.0, in1=kap[:], op0=ALU.mult, op1=ALU.mult), r=["icl", "kap"], wj=[pkk])
            P.V(lambda: nc.vector.tensor_copy(out=pack[b][:, 192:256], in_=kd[:]), r=["kd"], wj=[pkk])
            P.dma(pkd[d][r0:r0 + 128, :], pack[b][:], r=[pkk], wj=["scr"], key=pkk)
            P.dma(dkd[d][r0:r0 + 128, :], dec[b][:], r=["dec%d" % b], wj=["scr"], key="dec%d" % b)
            P.V(lambda: nc.vector.tensor_tensor(out=t1[:], in0=rr, in1=kd[:], op=ALU.mult), r=["mix", "kd"], w=["t1"])
            P.V(lambda: nc.vector.scalar_tensor_tensor(out=sq[:], in0=t1[:], scalar=1.0, in1=rk_bc[:], op0=ALU.mult, op1=ALU.mult, accum_out=sb_[:]),
                r=["t1", "rk_bc"], w=["sq", "sb_"])
            P.V(lambda: nc.vector.tensor_scalar(out=bon[b][:], in0=vp, scalar1=sb_[:], scalar2=None, op0=ALU.mult), r=["mix", "sb_"], w=["bon%d" % b])
            P.dma(bonus_o[d, r0:r0 + 128, :], bon[b][:], r=["bon%d" % b], wj=["bonus"], key="bon%d" % b)
            P.V(lambda: nc.vector.tensor_copy(out=vstage[:, 64 * d:64 * d + 64], in_=vp), r=["mix"], w=["vstage"])
            P.T(lambda: nc.tensor.transpose(ps_t[:, 0:128], vstage[:], ident[:]), r=["vstage", "ident32"], w=["ps_t"])
            P.V(lambda: nc.vector.tensor_copy(out=vto[b][64 * d:64 * d + 64, :], in_=ps_t[64 * d:64 * d + 64, 0:128]), r=["ps_t"], w=["vto%d" % b])
            P.dma(vTs[64 * d:64 * d + 64, r0:r0 + 128], vto[b][64 * d:64 * d + 64, :], r=["vto%d" % b], wj=["scr"], key="vto%d" % b)
    SEG = min(1024, S); NB = 4
    St = P.sb("St", [128, 64]); P.V(lambda: nc.vector.memset(St[:], 0.0), w=["S"])
    junk = P.sb("junk", [128, 64]); sk = P.sb("sk", [128, 1])
    bcp = [P.ps("bcp%d" % i, [128, 512]) for i in range(NB)]
    pkt = [P.sb("pkt%d" % i, [64, 4, 256], BF16) for i in range(2)]
    dkt = [P.sb("dkt%d" % i, [64, 4, 64]) for i in range(2)]
    vseg = [P.sb("vseg%d" % i, [128, SEG]) for i in range(2)]
    yseg = [P.sb("yseg%d" % i, [128, SEG]) for i in range(2)]
    yo = [P.sb("yo%d" % i, [128, 128]) for i in range(2)]
    step = 0; oi = 0
    for sg in range(S // SEG):
        sb2 = sg % 2; vk = "vseg%d" % sb2; yk = "yseg%d" % sb2
        P.dma(vseg[sb2][:], vTs[:, sg * SEG:(sg + 1) * SEG], r=["scr"], w=[vk])
        for blk in range(SEG // 128):
            s0 = sg * SEG + blk * 128; bb = (s0 // 128) % 2
            pk_, dk_ = "pkt%d" % bb, "dkt%d" % bb
            for d in range(2):
                P.dma(pkt[bb][32 * d:32 * d + 32, :, :], pkd[d][s0:s0 + 128, :].rearrange("(g q) c -> q g c", q=32), r=["scr"], w=[pk_] if d == 0 else [], wj=[] if d == 0 else [pk_])
                P.dma(dkt[bb][32 * d:32 * d + 32, :, :], dkd[d][s0:s0 + 128, :].rearrange("(g q) c -> q g c", q=32), r=["scr"], w=[dk_] if d == 0 else [], wj=[] if d == 0 else [dk_])
            for g in range(4):
                for j in range(32):
                    sl = step % NB; step += 1; bk = "bcp%d" % sl
                    col = blk * 128 + g * 32 + j
                    P.T(lambda: nc.tensor.matmul(bcp[sl][:, 0:256], lhsT=zb[:, j * 128:(j + 1) * 128], rhs=pkt[bb][:, g, :], start=True, stop=True), r=["zb", pk_], w=[bk])
                    P.T(lambda: nc.tensor.matmul(bcp[sl][:, 256:320], lhsT=z32[:, j * 128:(j + 1) * 128], rhs=dkt[bb][:, g, :], start=True, stop=True), r=["z32", dk_], wj=[bk])
                    P.ses = bool(os.environ.get("FORCE_SES"))
                    rb, kapb, nbb, kdb, wb = bcp[sl][:, 0:64], bcp[sl][:, 64:128], bcp[sl][:, 128:192], bcp[sl][:, 192:256], bcp[sl][:, 256:320]
                    P.V(lambda: nc.vector.scalar_tensor_tensor(out=junk[:], in0=St[:], scalar=1.0, in1=kapb, op0=ALU.mult, op1=ALU.mult, accum_out=sk[:]),
                        r=["S", bk], w=["junk", "sk"])
                    P.V(lambda: nc.vector.tensor_tensor(out=St[:], in0=St[:], in1=wb, op=ALU.mult), r=["S", bk, "junk"], w=["S"])
                    P.V(lambda: nc.vector.scalar_tensor_tensor(out=St[:], in0=nbb, scalar=sk[:], in1=St[:], op0=ALU.mult, op1=ALU.add), r=["S", bk, "sk"], w=["S"])
                    P.V(lambda: nc.vector.scalar_tensor_tensor(out=St[:], in0=kdb, scalar=vseg[sb2][:, col:col + 1], in1=St[:], op0=ALU.mult, op1=ALU.add), r=["S", bk, vk], w=["S"])
                    P.V(lambda: nc.vector.scalar_tensor_tensor(out=junk[:], in0=St[:], scalar=1.0, in1=rb, op0=ALU.mult, op1=ALU.mult, accum_out=yseg[sb2][:, col:col + 1]),
                        r=["S", bk], w=["junk"], wj=[yk])
                    P.ses = True
        for blk in range(SEG // 128):
            ob = oi % 2; oi += 1; r0 = sg * SEG + blk * 128
            P.T(lambda: nc.tensor.transpose(ps_t[:, 0:128], yseg[sb2][:, blk * 128:(blk + 1) * 128], ident[:]), r=[yk, "ident32"], w=["ps_t"])
            P.V(lambda: nc.vector.tensor_copy(out=yo[ob][:], in_=ps_t[:, 0:128]), r=["ps_t"], w=["yo%d" % ob])
            for d in range(2):
                P.dma(y_o[d, r0:r0 + 128, :], yo[ob][:, 64 * d:64 * d + 64], r=["yo%d" % ob], wj=["y"], key="yo%d" % ob)
        P.V(lambda: nc.vector.memset(junk[:], 0.0), r=[yk], w=["junk"])
    P.finish(); P.close()
    return nc


def zsel_const():
    z = np.zeros((64, 32, 128), np.float32)
    for j in range(32):
        z[j, j, 0:64] = 1.0
        z[32 + j, j, 64:128] = 1.0
    return z.reshape(64, 32 * 128)


def mb_inputs(zb_b, zl_b, prm, l, h):
    hs = slice(h * 64, (h + 1) * 64)
    rkvh = np.concatenate([zb_b[:, h * 64:(h + 1) * 64], zb_b[:, 256 + h * 64:256 + (h + 1) * 64], zb_b[:, 512 + h * 64:512 + (h + 1) * 64]], axis=1)
    m = {"ident": np.eye(128, dtype=np.float32), "zsel": zsel_const(),
         "w2": np.ascontiguousarray(prm["rwkv_w2"][l][:, :, hs]), "a2": np.ascontiguousarray(prm["rwkv_a2"][l][:, :, hs]),
         "w0": np.ascontiguousarray(prm["rwkv_w0"][l][:, hs]), "a0": np.ascontiguousarray(prm["rwkv_a0"][l][:, hs]),
         "kka": np.stack([prm["rwkv_k_k"][l][hs], prm["rwkv_k_a"][l][hs], prm["rwkv_r_k"][l][hs]])}
    mur = prm["rwkv_mu_rkv"][l]
    muh = np.stack([np.concatenate([mur[i, j, hs] for j in range(3)]) for i in range(2)])
    m["mu"] = np.stack([muh, muh[::-1]])
    for d in range(2):
        o = lambda a: np.ascontiguousarray(a if d == 0 else a[::-1])
        m["rkv%d" % d] = o(rkvh)
        base = d * 256
        for wi, nm in enumerate("wa"):
            m["h%s1_%d" % (nm, d)] = np.ascontiguousarray(o(zl_b[:, base + wi * 128:base + wi * 128 + 64]).T)
            m["h%s2_%d" % (nm, d)] = np.ascontiguousarray(o(zl_b[:, base + wi * 128 + 64:base + wi * 128 + 128]).T)
    return m


def run_MB(zb, zl, prm, l):
    B, S, _ = zb.shape
    nc = build_MB(S)
    maps = [mb_inputs(zb[c // 4], zl[c // 4], prm, l, c % 4) for c in range(B * 4)]
    res = run_bass_kernel_spmd(nc, maps, core_ids=list(range(B * 4)))
    outs = [np.zeros((B, S, 256), np.float32) for _ in range(4)]
    for c in range(B * 4):
        b, h = c // 4, c % 4
        r = res.results[c]
        outs[0][b, :, h * 64:(h + 1) * 64] = r["y"][0]
        outs[1][b, :, h * 64:(h + 1) * 64] = r["y"][1][::-1]
        outs[2][b, :, h * 64:(h + 1) * 64] = r["bonus"][0]
        outs[3][b, :, h * 64:(h + 1) * 64] = r["bonus"][1][::-1]
    return outs


def build_F1(TC):
    P = Prog(); nc = P.nc
    x = P.dram("x", [TC, 1024]); g = P.dram("g", [1, 1024]); ident_d = P.dram("ident", [128, 128])
    br = {n: P.dram(n, [TC, 256]) for n in ("ya", "yd", "rwf", "rwb", "bnf", "bnb", "cf", "cb", "u")}
    hg = P.dram("hg", [TC, 128])
    wgate = P.dram("wgate", [1024, 4096]); gate_b = P.dram("gate_b", [1, 4096]); w_branch = P.dram("w_branch", [4, 256, 1024]); w_out = P.dram("w_out", [1024, 1024])
    glu_w = P.dram("glu_w", [256, 512]); glu_b = P.dram("glu_b", [1, 512]); g2 = P.dram("g2", [128, 256])
    vecs = P.dram("vecs", [3, 256])
    y = P.dram("y", [TC, 1024], kind="ExternalOutput")
    ident = load_ident32(P, nc, ident_d)
    def bc(name, src, n):
        t = P.sb(name, [128, n]); P.dma(t[:], src.partition_broadcast(128), w=[name]); return t
    gbc = bc("gbc", g[0:1, :], 1024); gb_bc = bc("gb_bc", gate_b[0:1, :], 4096); glub_bc = bc("glub_bc", glu_b[0:1, :], 512)
    lnw_bc = bc("lnw_bc", vecs[0:1, :], 256); lnb_bc = bc("lnb_bc", vecs[1:2, :], 256); d_bc = bc("d_bc", vecs[2:3, :], 256)
    Wg = P.sb("Wg", [128, 8, 4096], BF16); Wbr = P.sb("Wbr", [128, 8, 1024], BF16); Wo = P.sb("Wo", [128, 8, 1024], BF16)
    glub = P.sb("glub", [128, 2, 512], BF16); g2b = P.sb("g2b", [128, 256], BF16)
    stg = [P.sb("stg%d" % i, [128, 1024]) for i in range(2)]
    si = 0
    def load_cast(dst, src, n, tok):
        nonlocal si
        b = si % 2; si += 1
        P.dma(stg[b][:, 0:n], src, w=["stg%d" % b])
        if b == 0:
            P.V(lambda: nc.vector.tensor_copy(out=dst, in_=stg[b][:, 0:n]), r=["stg%d" % b], wj=[tok])
        else:
            P.G(lambda: nc.gpsimd.tensor_copy(out=dst, in_=stg[b][:, 0:n]), r=["stg%d" % b], wj=[tok])
    for kc in range(8):
        rows = slice(kc * 128, (kc + 1) * 128)
        for q in range(4):
            load_cast(Wg[:, kc, q * 1024:(q + 1) * 1024], wgate[rows, q * 1024:(q + 1) * 1024], 1024, "Wg")
        load_cast(Wo[:, kc, :], w_out[rows, :], 1024, "Wo")
        load_cast(Wbr[:, kc, :], w_branch[kc // 2, (kc % 2) * 128:(kc % 2 + 1) * 128, :], 1024, "Wbr")
    for kc in range(2):
        load_cast(glub[:, kc, :], glu_w[kc * 128:(kc + 1) * 128, :], 512, "glub")
    load_cast(g2b[:], g2[:, :], 256, "g2b")
    xt = [P.sb("xt%d" % i, [128, 1024]) for i in range(2)]
    xn32 = P.sb("xn32", [128, 1024]); ss = P.sb("ss", [128, 1])
    xnT = P.sb("xnT", [128, 8, 128], BF16); mT = P.sb("mT", [128, 8, 128], BF16)
    gate = P.sb("gate", [128, 1024]); merged = P.sb("merged", [128, 1024]); tmpm = P.sb("tmpm", [128, 1024])
    inp = {n: P.sb("i_" + n, [128, 256]) for n in br}
    hgt = P.sb("hgt", [128, 128]); hgT = P.sb("hgT", [128, 128], BF16)
    ys = P.sb("ys", [128, 256]); sqh = P.sb("sqh", [128, 64]); st = P.sb("st", [128, 12])
    ybf = P.sb("ybf", [128, 256]); ycf = P.sb("ycf", [128, 256]); t256 = P.sb("t256", [128, 256]); h512 = P.sb("h512", [128, 512])
    yT = P.sb("yT", [128, 2, 128], BF16)
    pst = P.ps("pst", [128, 8, 128]); pbr = P.ps("pbr", [128, 1024]); pg = P.ps("pg", [128, 1024]); po = P.ps("po", [128, 1024])
    NT = TC // 128
    for t in range(NT):
        b = t % 2; r0 = t * 128; xk = "xt%d" % b
        P.dma(xt[b][:], x[r0:r0 + 128, :], w=[xk])
        for n in br:
            P.dma(inp[n][:], br[n][r0:r0 + 128, :], w=["i_" + n])
        P.dma(hgt[:], hg[r0:r0 + 128, :], w=["hgt"])
        P.A(lambda: nc.scalar.activation(out=xn32[:], in_=xt[b][:], func=AF.Square, accum_out=ss[:]), r=[xk], w=["xn32", "ss"])
        P.V(lambda: nc.vector.tensor_scalar(out=ss[:], in0=ss[:], scalar1=1.0 / 1024, scalar2=1e-6, op0=ALU.mult, op1=ALU.add), r=["ss"], w=["ss"])
        P.A(lambda: nc.scalar.sqrt(out=ss[:], in_=ss[:]), r=["ss"], w=["ss"])
        P.V(lambda: nc.vector.reciprocal(out=ss[:], in_=ss[:]), r=["ss"], w=["ss"])
        P.V(lambda: nc.vector.scalar_tensor_tensor(out=xn32[:], in0=xt[b][:], scalar=ss[:], in1=gbc[:], op0=ALU.mult, op1=ALU.mult), r=[xk, "ss", "gbc"], w=["xn32"])
        for kc in range(8):
            P.T(lambda: nc.tensor.transpose(pst[:, kc, :], xn32[:, kc * 128:(kc + 1) * 128], ident[:]), r=["xn32", "ident32"], w=["pst"] if kc == 0 else [], wj=[] if kc == 0 else ["pst"])
        P.V(lambda: nc.vector.tensor_copy(out=xnT[:], in_=pst[:]), r=["pst"], w=["xnT"])
        P.V(lambda: nc.vector.tensor_tensor(out=ys[:], in0=inp["rwf"][:], in1=inp["rwb"][:], op=ALU.add), r=["i_rwf", "i_rwb"], w=["ys"])
        P.V(lambda: nc.vector.tensor_reduce(out=st[:, 0:4], in_=ys[:].rearrange("p (h n) -> p h n", h=4), axis=AX.X, op=ALU.add), r=["ys"], w=["st"])
        for h in range(4):
            P.A(lambda: nc.scalar.activation(out=sqh[:], in_=ys[:, h * 64:(h + 1) * 64], func=AF.Square, accum_out=st[:, 4 + h:5 + h]), r=["ys", "st"], w=["sqh", "st"])
        P.V(lambda: nc.vector.tensor_scalar(out=st[:, 0:8], in0=st[:, 0:8], scalar1=1.0 / 64, scalar2=None, op0=ALU.mult), r=["st"], w=["st"])
        P.V(lambda: nc.vector.tensor_tensor(out=st[:, 8:12], in0=st[:, 0:4], in1=st[:, 0:4], op=ALU.mult), r=["st"], w=["st"])
        P.V(lambda: nc.vector.tensor_tensor(out=st[:, 4:8], in0=st[:, 4:8], in1=st[:, 8:12], op=ALU.subtract), r=["st"], w=["st"])
        P.V(lambda: nc.vector.tensor_scalar(out=st[:, 4:8], in0=st[:, 4:8], scalar1=64e-5, scalar2=None, op0=ALU.add), r=["st"], w=["st"])
        P.A(lambda: nc.scalar.sqrt(out=st[:, 4:8], in_=st[:, 4:8]), r=["st"], w=["st"])
        P.V(lambda: nc.vector.reciprocal(out=st[:, 4:8], in_=st[:, 4:8]), r=["st"], w=["st"])
        for h in range(4):
            P.V(lambda: nc.vector.tensor_scalar(out=ybf[:, h * 64:(h + 1) * 64], in0=ys[:, h * 64:(h + 1) * 64], scalar1=st[:, h:h + 1], scalar2=st[:, 4 + h:5 + h], op0=ALU.subtract, op1=ALU.mult),
                r=["ys", "st"], w=["ybf"] if h == 0 else [], wj=[] if h == 0 else ["ybf"])
        P.V(lambda: nc.vector.tensor_tensor(out=ybf[:], in0=ybf[:], in1=lnw_bc[:], op=ALU.mult), r=["ybf", "lnw_bc"], w=["ybf"])
        P.V(lambda: nc.vector.tensor_tensor(out=ybf[:], in0=ybf[:], in1=lnb_bc[:], op=ALU.add), r=["ybf", "lnb_bc"], w=["ybf"])
        P.V(lambda: nc.vector.tensor_tensor(out=ybf[:], in0=ybf[:], in1=inp["bnf"][:], op=ALU.add), r=["ybf", "i_bnf"], w=["ybf"])
        P.V(lambda: nc.vector.tensor_tensor(out=ybf[:], in0=ybf[:], in1=inp["bnb"][:], op=ALU.add), r=["ybf", "i_bnb"], w=["ybf"])
        P.A(lambda: nc.scalar.activation(out=hgt[:], in_=hgt[:], func=AF.Sigmoid), r=["hgt"], w=["hgt"])
        P.T(lambda: nc.tensor.transpose(po[:, 0:128], hgt[:], ident[:]), r=["hgt", "ident32"], w=["po"])
        P.V(lambda: nc.vector.tensor_copy(out=hgT[:], in_=po[:, 0:128]), r=["po"], w=["hgT"])
        P.T(lambda: nc.tensor.matmul(po[:, 0:256], lhsT=hgT[:], rhs=g2b[:], start=True, stop=True), r=["hgT", "g2b"], w=["po"])
        P.V(lambda: nc.vector.tensor_tensor(out=ybf[:], in0=ybf[:], in1=po[:, 0:256], op=ALU.mult), r=["ybf", "po"], w=["ybf"])
        P.V(lambda: nc.vector.tensor_tensor(out=ycf[:], in0=inp["u"][:], in1=d_bc[:], op=ALU.mult), r=["i_u", "d_bc"], w=["ycf"])
        P.V(lambda: nc.vector.tensor_tensor(out=ycf[:], in0=ycf[:], in1=inp["cf"][:], op=ALU.add), r=["ycf", "i_cf"], w=["ycf"])
        P.V(lambda: nc.vector.tensor_tensor(out=ycf[:], in0=ycf[:], in1=inp["cb"][:], op=ALU.add), r=["ycf", "i_cb"], w=["ycf"])
        P.V(lambda: nc.vector.tensor_tensor(out=t256[:], in0=ycf[:], in1=ycf[:], op=ALU.mult), r=["ycf"], w=["t256"])
        P.V(lambda: nc.vector.tensor_scalar(out=t256[:], in0=t256[:], scalar1=0.044715, scalar2=1.0, op0=ALU.mult, op1=ALU.add), r=["t256"], w=["t256"])
        P.V(lambda: nc.vector.tensor_tensor(out=t256[:], in0=t256[:], in1=ycf[:], op=ALU.mult), r=["t256", "ycf"], w=["t256"])
        P.A(lambda: nc.scalar.activation(out=t256[:], in_=t256[:], func=AF.Sigmoid, scale=1.5957691216057308), r=["t256"], w=["t256"])
        P.V(lambda: nc.vector.tensor_tensor(out=ycf[:], in0=ycf[:], in1=t256[:], op=ALU.mult), r=["ycf", "t256"], w=["ycf"])
        for kc in range(2):
            P.T(lambda: nc.tensor.transpose(pst[:, kc, :], ycf[:, kc * 128:(kc + 1) * 128], ident[:]), r=["ycf", "ident32"], w=["pst"] if kc == 0 else [], wj=[] if kc == 0 else ["pst"])
        P.V(lambda: nc.vector.tensor_copy(out=yT[:], in_=pst[:, 0:2, :]), r=["pst"], w=["yT"])
        for kc in range(2):
            P.T(lambda: nc.tensor.matmul(po[:, 0:512], lhsT=yT[:, kc, :], rhs=glub[:, kc, :], start=(kc == 0), stop=(kc == 1)), r=["yT", "glub"], w=["po"] if kc == 0 else [], wj=[] if kc == 0 else ["po"])
        P.V(lambda: nc.vector.tensor_tensor(out=h512[:], in0=po[:, 0:512], in1=glub_bc[:], op=ALU.add), r=["po", "glub_bc"], w=["h512"])
        P.A(lambda: nc.scalar.activation(out=t256[:], in_=h512[:, 256:512], func=AF.Sigmoid), r=["h512"], w=["t256"])
        P.V(lambda: nc.vector.tensor_tensor(out=ycf[:], in0=h512[:, 0:256], in1=t256[:], op=ALU.mult), r=["h512", "t256"], w=["ycf"])
        srcs = [(inp["ya"], "i_ya"), (ybf, "ybf"), (ycf, "ycf"), (inp["yd"], "i_yd")]
        for i, (src, stok) in enumerate(srcs):
            for kc in range(2):
                P.T(lambda: nc.tensor.transpose(pst[:, kc, :], src[:, kc * 128:(kc + 1) * 128], ident[:]), r=[stok, "ident32"], w=["pst"] if kc == 0 else [], wj=[] if kc == 0 else ["pst"])
            P.V(lambda: nc.vector.tensor_copy(out=yT[:], in_=pst[:, 0:2, :]), r=["pst"], w=["yT"])
            for half in range(2):
                cs = slice(half * 512, (half + 1) * 512)
                for kc in range(2):
                    P.T(lambda: nc.tensor.matmul(pbr[:, cs], lhsT=yT[:, kc, :], rhs=Wbr[:, i * 2 + kc, cs], start=(kc == 0), stop=(kc == 1)),
                        r=["yT", "Wbr"], w=["pbr"] if (kc == 0 and half == 0) else [], wj=[] if (kc == 0 and half == 0) else ["pbr"])
                for kc in range(8):
                    P.T(lambda: nc.tensor.matmul(pg[:, cs], lhsT=xnT[:, kc, :], rhs=Wg[:, kc, i * 1024 + half * 512:i * 1024 + (half + 1) * 512], start=(kc == 0), stop=(kc == 7)),
                        r=["xnT", "Wg"], w=["pg"] if (kc == 0 and half == 0) else [], wj=[] if (kc == 0 and half == 0) else ["pg"])
            P.V(lambda: nc.vector.tensor_tensor(out=gate[:], in0=pg[:], in1=gb_bc[:, i * 1024:(i + 1) * 1024], op=ALU.add), r=["pg", "gb_bc"], w=["gate"])
            P.A(lambda: nc.scalar.activation(out=gate[:], in_=gate[:], func=AF.Sigmoid), r=["gate"], w=["gate"])
            if i == 0:
                P.V(lambda: nc.vector.tensor_tensor(out=merged[:], in0=pbr[:], in1=gate[:], op=ALU.mult), r=["pbr", "gate"], w=["merged"])
            else:
                P.V(lambda: nc.vector.tensor_tensor(out=tmpm[:], in0=pbr[:], in1=gate[:], op=ALU.mult), r=["pbr", "gate"], w=["tmpm"])
                P.G(lambda: nc.gpsimd.tensor_tensor(out=merged[:], in0=merged[:], in1=tmpm[:], op=ALU.add), r=["merged", "tmpm"], w=["merged"])
        for kc in range(8):
            P.T(lambda: nc.tensor.transpose(pst[:, kc, :], merged[:, kc * 128:(kc + 1) * 128], ident[:]), r=["merged", "ident32"], w=["pst"] if kc == 0 else [], wj=[] if kc == 0 else ["pst"])
        P.V(lambda: nc.vector.tensor_copy(out=mT[:], in_=pst[:]), r=["pst"], w=["mT"])
        for half in range(2):
            cs = slice(half * 512, (half + 1) * 512)
            for kc in range(8):
                P.T(lambda: nc.tensor.matmul(po[:, cs], lhsT=mT[:, kc, :], rhs=Wo[:, kc, cs], start=(kc == 0), stop=(kc == 7)),
                    r=["mT", "Wo"], w=["po"] if (kc == 0 and half == 0) else [], wj=[] if (kc == 0 and half == 0) else ["po"])
        P.V(lambda: nc.vector.tensor_tensor(out=xt[b][:], in0=xt[b][:], in1=po[:], op=ALU.add), r=[xk, "po"], w=[xk])
        P.dma(y[r0:r0 + 128, :], xt[b][:], r=[xk], wj=["y"], key=xk)
    P.finish(); P.close()
    return nc


def run_F1(xf, brs, hg, prm, l, ncores=8):
    T = xf.shape[0]; TC = T // ncores
    nc = build_F1(TC)
    com = {"g": prm["norm_mix_g"][l][None, :], "ident": np.eye(128, dtype=np.float32),
           "wgate": np.ascontiguousarray(prm["w_in"][l][:, 2304:]), "gate_b": prm["gate_b"][l].reshape(1, 4096), "w_branch": prm["w_branch"][l], "w_out": prm["w_out"][l],
           "glu_w": prm["s5_glu_w"][l], "glu_b": prm["s5_glu_b"][l][None, :], "g2": prm["rwkv_g2"][l],
           "vecs": np.stack([prm["rwkv_ln_w"][l], prm["rwkv_ln_b"][l], prm["s5_d"][l]])}
    maps = []
    for c in range(ncores):
        sl = slice(c * TC, (c + 1) * TC)
        m = dict(com, x=xf[sl], hg=np.ascontiguousarray(hg[sl]))
        for n, a in brs.items():
            m[n] = np.ascontiguousarray(a[sl])
        maps.append(m)
    res = run_bass_kernel_spmd(nc, maps, core_ids=list(range(ncores)))
    return np.concatenate([r["y"] for r in res.results], axis=0)


def kernel(**inp):
    prm = {k: np.ascontiguousarray(np.asarray(v, dtype=np.float32)) for k, v in inp.items()}
    x = prm["x"]
    B, S, D = x.shape
    xf = np.ascontiguousarray(x.reshape(B * S, D))
    f2 = lambda a: np.ascontiguousarray(a.reshape(B * S, a.shape[-1]))
    for l in range(2):
        zP = run_P(xf, prm, l)
        z = zP.reshape(B, S, NZ)
        ya = run_MA(z[:, :, 0:768], prm, l)
        rwf, rwb, bnf, bnb = run_MB(z[:, :, 768:1536], z[:, :, 2304:2944], prm, l)
        cf, cb = run_MC(z[:, :, 1536:1792], prm, l)
        yd = run_MD(z[:, :, 1792:2048], z[:, :, 2048:2176], z[:, :, 2176:2304], prm, l)
        brs = {"ya": f2(ya), "yd": f2(yd), "rwf": f2(rwf), "rwb": f2(rwb), "bnf": f2(bnf), "bnb": f2(bnb),
               "cf": f2(cf), "cb": f2(cb), "u": f2(z[:, :, 1536:1792])}
        xmid = run_F1(xf, brs, np.ascontiguousarray(zP[:, 2816:2944]), prm, l)
        del zP, z, brs
        xf = run_F2(xmid, prm, l)
    return xf.reshape(B, S, D).astype(np.float32)
```

```python
import os
from concourse.bass_utils import run_bass_kernel_spmd

from contextlib import ExitStack
import numpy as np
import concourse.bass as bass
import concourse.mybir as mybir

F32 = mybir.dt.float32
BF16 = mybir.dt.bfloat16
I32 = mybir.dt.int32
ALU = mybir.AluOpType
AF = mybir.ActivationFunctionType
AX = mybir.AxisListType

COMPUTE = ("tensor", "vector", "scalar", "gpsimd")


class Prog:
    def __init__(self, same_engine_sync=True):
        self.nc = bass.Bass("TRN2", target_bir_lowering=False)
        self.es = ExitStack()
        self.eng = {"tensor": self.nc.tensor, "vector": self.nc.vector, "scalar": self.nc.scalar,
                    "gpsimd": self.nc.gpsimd, "sync": self.nc.sync}
        self.esem = {e: self.es.enter_context(self.nc.semaphore("s_" + e)) for e in COMPUTE}
        self.ecount = {e: 0 for e in COMPUTE}
        self.waited = {}
        self.tsem = {}
        self.writers = {}
        self.readers = {}
        self.gen = {}
        self.ses = True
        self.excl = {}
        self.same_engine_sync = same_engine_sync
        self.n_inst = 0

    def dram(self, name, shape, dtype=F32, kind="ExternalInput"):
        return self.nc.dram_tensor(name, list(shape), dtype, kind=kind).ap()

    def sb(self, name, shape, dtype=F32):
        return self.es.enter_context(self.nc.sbuf_tensor("sb_" + name, list(shape), dtype))

    def ps(self, name, shape, dtype=F32):
        return self.es.enter_context(self.nc.psum_tensor("ps_" + name, list(shape), dtype))

    def _wait(self, eng, ev):
        kind, a, b = ev
        if kind == "e":
            if a == eng and not (self.same_engine_sync and self.ses and eng != "tensor"):
                return
            sem, val, key = self.esem[a], b, (eng, "e" + a)
        else:
            sem, val, key = self.tsem[a][0], b, (eng, "d", a)
        if self.waited.get(key, -1) >= val:
            return
        self.waited[key] = val
        self.eng[eng].wait_ge(sem, val)

    def _deps(self, eng, r, w, wj=()):
        evs = []
        for t in r:
            evs += self.writers.get(t, [])
        for t in w:
            prior = list(self.writers.get(t, ())) + list(self.readers.get(t, ()))
            evs += prior
            self.gen[t] = prior
        for t in wj:
            evs += self.gen.get(t, [])
            evs += self.readers.get(t, [])
            if t in self.excl:
                evs.append(self.excl[t])
        mx = {}
        for (k, a, b) in evs:
            if mx.get((k, a), -1) < b:
                mx[(k, a)] = b
        for (k, a), b in mx.items():
            self._wait(eng, (k, a, b))

    def _commit(self, ev, r, w, wj=()):
        for t in r:
            self.readers.setdefault(t, []).append(ev)
        for t in w:
            self.writers[t] = [ev]
            self.readers[t] = []
            self.excl[t] = ev
        for t in wj:
            self.writers.setdefault(t, []).append(ev)

    def op(self, eng, fn, r=(), w=(), wj=()):
        self._deps(eng, r, w, wj)
        ins = fn()
        self.ecount[eng] += 1
        ins.then_inc(self.esem[eng], 1)
        self._commit(("e", eng, self.ecount[eng]), r, w, wj)
        self.n_inst += 1
        return ins

    def T(self, fn, r=(), w=(), wj=()): return self.op("tensor", fn, r, w, wj)
    def V(self, fn, r=(), w=(), wj=()): return self.op("vector", fn, r, w, wj)
    def A(self, fn, r=(), w=(), wj=()): return self.op("scalar", fn, r, w, wj)
    def G(self, fn, r=(), w=(), wj=()): return self.op("gpsimd", fn, r, w, wj)

    def dma(self, out, in_, r=(), w=(), wj=(), q="sync", key=None, **kw):
        if key is None:
            key = (list(w) + list(wj) + list(r))[0]
        self._deps(q, r, w, wj)
        if key not in self.tsem:
            self.tsem[key] = [self.es.enter_context(self.nc.semaphore("d%d" % len(self.tsem))), 0]
        ent = self.tsem[key]
        ent[1] += 16
        self.eng[q].dma_start(out=out, in_=in_, **kw).then_inc(ent[0], 16)
        self._commit(("d", key, ent[1]), r, w, wj)
        self.n_inst += 1

    def finish(self, q="sync"):
        for key, ent in self.tsem.items():
            self._wait(q, ("d", key, ent[1]))
        return self.nc

    def close(self):
        self.es.close()


NZ = 2944


def rmsnorm_tile(P, nc, xt, xtok, gbc, gtok, out_bf, otok, scr, eps=1e-6, D=1024):
    sq, ss = scr["sq"], scr["ss"]
    P.A(lambda: nc.scalar.activation(out=sq[:], in_=xt, func=AF.Square, accum_out=ss[:]), r=[xtok], w=["sq", "ss"])
    P.V(lambda: nc.vector.tensor_scalar(out=ss[:], in0=ss[:], scalar1=1.0 / D, scalar2=eps, op0=ALU.mult, op1=ALU.add), r=["ss"], w=["ss"])
    P.A(lambda: nc.scalar.sqrt(out=ss[:], in_=ss[:]), r=["ss"], w=["ss"])
    P.V(lambda: nc.vector.reciprocal(out=ss[:], in_=ss[:]), r=["ss"], w=["ss"])
    P.V(lambda: nc.vector.scalar_tensor_tensor(out=out_bf, in0=xt, scalar=ss[:], in1=gbc, op0=ALU.mult, op1=ALU.mult),
        r=[xtok, "ss", gtok], w=[otok])


def load_ident(P, nc, ident_d):
    i32 = P.sb("ident32", [128, 128]); ib = P.sb("identb", [128, 128], BF16)
    P.dma(i32[:], ident_d[:, :], w=["ident32"])
    P.V(lambda: nc.vector.tensor_copy(out=ib[:], in_=i32[:]), r=["ident32"], w=["ident"])
    return ib


def build_P(TC):
    P = Prog(); nc = P.nc
    x = P.dram("x", [TC, 1024]); g = P.dram("g", [1, 1024]); ident_d = P.dram("ident", [128, 128])
    w_mix = P.dram("w_mix", [1024, 2304]); w1 = P.dram("w1", [2, 1024, 64]); a1 = P.dram("a1", [2, 1024, 64])
    g1 = P.dram("g1", [1024, 128]); mu = P.dram("mu", [2, 1024])
    z = P.dram("z", [TC, NZ], kind="ExternalOutput")
    Wsb = P.sb("Wsb", [128, 8, NZ], BF16)
    stg = [P.sb("stg%d" % i, [128, NZ]) for i in range(2)]
    gbc = P.sb("gbc", [128, 1024]); mut = P.sb("mut", [128, 2, 8]); omu = P.sb("omu", [128, 2, 8])
    ident = load_ident(P, nc, ident_d)
    P.dma(gbc[:], g[0:1, :].partition_broadcast(128), w=["gbc"])
    for d in range(2):
        P.dma(mut[:, d, :], mu[d].rearrange("(c p) -> p c", p=128), wj=["mut"], allow_slow_non_contiguous=True)
    P.V(lambda: nc.vector.tensor_scalar(out=omu[:], in0=mut[:], scalar1=-1.0, scalar2=1.0, op0=ALU.mult, op1=ALU.add), r=["mut"], w=["omu"])
    for kc in range(8):
        s = stg[kc % 2]; tk = "stg%d" % (kc % 2)
        rows = slice(kc * 128, (kc + 1) * 128)
        P.dma(s[:, 0:2304], w_mix[rows, :], w=[tk])
        for d in range(2):
            P.dma(s[:, 2304 + d * 256:2304 + d * 256 + 64], w1[d, rows, :], wj=[tk])
            P.dma(s[:, 2304 + d * 256 + 128:2304 + d * 256 + 192], a1[d, rows, :], wj=[tk])
        P.dma(s[:, 2816:2944], g1[rows, :], wj=[tk])
        P.A(lambda: nc.scalar.copy(out=Wsb[:, kc, 0:1152], in_=s[:, 0:1152]), r=[tk], wj=["Wsb"])
        P.V(lambda: nc.vector.tensor_copy(out=Wsb[:, kc, 1152:2304], in_=s[:, 1152:2304]), r=[tk], wj=["Wsb"])
        for d in range(2):
            for wa in range(2):
                b0 = 2304 + d * 256 + wa * 128
                P.V(lambda: nc.vector.tensor_scalar(out=Wsb[:, kc, b0 + 64:b0 + 128], in0=s[:, b0:b0 + 64], scalar1=mut[:, d, kc:kc + 1], scalar2=None, op0=ALU.mult),
                    r=[tk, "mut"], wj=["Wsb"])
                P.V(lambda: nc.vector.tensor_scalar(out=Wsb[:, kc, b0:b0 + 64], in0=s[:, b0:b0 + 64], scalar1=omu[:, d, kc:kc + 1], scalar2=None, op0=ALU.mult),
                    r=[tk, "omu"], wj=["Wsb"])
        P.V(lambda: nc.vector.tensor_copy(out=Wsb[:, kc, 2816:2944], in_=s[:, 2816:2944]), r=[tk], wj=["Wsb"])
    xt = [P.sb("xt%d" % i, [128, 1024]) for i in range(2)]
    xnb = P.sb("xnb", [128, 1024], BF16)
    xnT = P.sb("xnT", [128, 8, 128], BF16)
    zt = [P.sb("zt%d" % i, [128, NZ]) for i in range(2)]
    scr = {"sq": P.sb("sq", [128, 1024]), "ss": P.sb("ss", [128, 1])}
    pst = P.ps("pst", [128, 8, 128], BF16)
    psz = [P.ps("psz%d" % i, [128, 512]) for i in range(4)]
    NT = TC // 128
    P.dma(xt[0][:], x[0:128, :], w=["xt0"])
    ci = 0
    for t in range(NT):
        b = t % 2
        if t + 1 < NT:
            P.dma(xt[1 - b][:], x[(t + 1) * 128:(t + 2) * 128, :], w=["xt%d" % (1 - b)])
        rmsnorm_tile(P, nc, xt[b][:], "xt%d" % b, gbc[:], "gbc", xnb[:], "xnb", scr)
        for kc in range(8):
            P.T(lambda: nc.tensor.transpose(pst[:, kc, :], xnb[:, kc * 128:(kc + 1) * 128], ident[:]), r=["xnb", "ident"],
                w=["pst"] if kc == 0 else [], wj=[] if kc == 0 else ["pst"])
        P.V(lambda: nc.vector.tensor_copy(out=xnT[:], in_=pst[:]), r=["pst"], w=["xnT"])
        for n0 in range(0, NZ, 512):
            n1 = min(NZ, n0 + 512); pz = psz[ci % 4]; pk = "psz%d" % (ci % 4)
            for kc in range(8):
                P.T(lambda: nc.tensor.matmul(pz[:, 0:n1 - n0], lhsT=xnT[:, kc, :], rhs=Wsb[:, kc, n0:n1], start=(kc == 0), stop=(kc == 7)),
                    r=["xnT", "Wsb"], w=[pk] if kc == 0 else [], wj=[] if kc == 0 else [pk])
            first = (n0 == 0)
            if ci % 2 == 0:
                P.A(lambda: nc.scalar.copy(out=zt[b][:, n0:n1], in_=pz[:, 0:n1 - n0]), r=[pk], w=["zt%d" % b] if first else [], wj=[] if first else ["zt%d" % b])
            else:
                P.V(lambda: nc.vector.tensor_copy(out=zt[b][:, n0:n1], in_=pz[:, 0:n1 - n0]), r=[pk], w=["zt%d" % b] if first else [], wj=[] if first else ["zt%d" % b])
            ci += 1
        P.dma(z[t * 128:(t + 1) * 128, :], zt[b][:], r=["zt%d" % b], wj=["z"], key="zt%d" % b)
    P.finish(); P.close()
    return nc


def run_P(xf, prm, l, ncores=8):
    T = xf.shape[0]; TC = T // ncores
    nc = build_P(TC)
    com = {"g": prm["norm_mix_g"][l][None, :], "ident": np.eye(128, dtype=np.float32),
           "w_mix": np.ascontiguousarray(prm["w_in"][l][:, :2304]), "w1": prm["rwkv_w1"][l], "a1": prm["rwkv_a1"][l],
           "g1": prm["rwkv_g1"][l], "mu": prm["rwkv_mu_x"][l]}
    maps = [dict(com, x=xf[c * TC:(c + 1) * TC]) for c in range(ncores)]
    res = run_bass_kernel_spmd(nc, maps, core_ids=list(range(ncores)))
    return np.concatenate([r["z"] for r in res.results], axis=0)


def load_ident32(P, nc, ident_d):
    i32 = P.sb("ident32", [128, 128])
    P.dma(i32[:], ident_d[:, :], w=["ident32"])
    return i32


import os
DBG = int(os.environ.get('DBG', '0'))


def build_F2(TC, F, E, moe, final):
    P = Prog(); nc = P.nc
    x = P.dram("x", [TC, 1024]); g = P.dram("g", [1, 1024]); ident_d = P.dram("ident", [128, 128])
    wg = P.dram("wg", [E, 1024, F]); wu = P.dram("wu", [E, 1024, F]); wd = P.dram("wd", [E, F, 1024])
    if moe:
        router = P.dram("router", [1024, 8])
    if final:
        gf = P.dram("gf", [1, 1024])
    y = P.dram("y", [TC, 1024], kind="ExternalOutput")
    ST = min(TC, 1024); NTT = ST // 128; NST = TC // ST; NTH = max(1, ST // 512); TH = min(512, ST)
    NFC = F // 128
    GC = 11 if NFC % 11 == 0 else 7
    NG = NFC // GC
    ident = load_ident32(P, nc, ident_d)
    gbc = P.sb("gbc", [128, 1024]); P.dma(gbc[:], g[0:1, :].partition_broadcast(128), w=["gbc"])
    if final:
        gfbc = P.sb("gfbc", [128, 1024]); P.dma(gfbc[:], gf[0:1, :].partition_broadcast(128), w=["gfbc"])
    if moe:
        rsb = P.sb("rsb", [128, 8, 8])
        if not (DBG & 8):
            P.dma(rsb[:], router.rearrange("(kc p) e -> p kc e", p=128), w=["rsb"])
        xnT32 = P.sb("xnT32", [128, 8, 128])
        wt = P.sb("wt", [128, NTT, 8]); lg = P.sb("lg", [128, 8]); eq1 = P.sb("eq1", [128, 8]); eq2 = P.sb("eq2", [128, 8])
        m1 = P.sb("m1", [128, 1]); m2 = P.sb("m2", [128, 1])
    acc = P.sb("acc", [128, NTT, 1024])
    xnT = P.sb("xnT", [128, 8, ST], BF16)
    xn32 = P.sb("xn32", [128, 1024]); sq = P.sb("sq", [128, 1024]); ss = P.sb("ss", [128, 1])
    hT = P.sb("hT", [128, GC, ST], BF16)
    wdb = P.sb("wdb", [128, GC, 1024], BF16)
    wds = [P.sb("wds%d" % i, [128, 1024]) for i in range(2)]
    wgs = [P.sb("wgs%d" % i, [128, 8, 128]) for i in range(2)]
    wus = [P.sb("wus%d" % i, [128, 8, 128]) for i in range(2)]
    wgb = [P.sb("wgb%d" % i, [128, 8, 128], BF16) for i in range(2)]
    wub = [P.sb("wub%d" % i, [128, 8, 128], BF16) for i in range(2)]
    sg = [P.sb("sg%d" % i, [128, TH]) for i in range(2)]
    pst = P.ps("pst", [128, 8, 128])
    psg = [P.ps("psg%d" % i, [128, 512]) for i in range(2)]
    psu = [P.ps("psu%d" % i, [128, 512]) for i in range(2)]
    pso = P.ps("pso", [128, 1024])
    wi = 0; di = 0; gi = 0
    for st in range(NST):
        t0 = st * ST
        for tt in range(NTT):
            atok = "acc%d" % tt
            P.dma(acc[:, tt, :], x[t0 + tt * 128:t0 + (tt + 1) * 128, :], w=[atok])
            P.A(lambda: nc.scalar.activation(out=sq[:], in_=acc[:, tt, :], func=AF.Square, accum_out=ss[:]), r=[atok], w=["sq", "ss"])
            P.V(lambda: nc.vector.tensor_scalar(out=ss[:], in0=ss[:], scalar1=1.0 / 1024, scalar2=1e-6, op0=ALU.mult, op1=ALU.add), r=["ss"], w=["ss"])
            P.A(lambda: nc.scalar.sqrt(out=ss[:], in_=ss[:]), r=["ss"], w=["ss"])
            P.V(lambda: nc.vector.reciprocal(out=ss[:], in_=ss[:]), r=["ss"], w=["ss"])
            P.V(lambda: nc.vector.scalar_tensor_tensor(out=xn32[:], in0=acc[:, tt, :], scalar=ss[:], in1=gbc[:], op0=ALU.mult, op1=ALU.mult),
                r=[atok, "ss", "gbc"], w=["xn32"])
            for kc in range(8):
                P.T(lambda: nc.tensor.transpose(pst[:, kc, :], xn32[:, kc * 128:(kc + 1) * 128], ident[:]), r=["xn32", "ident32"],
                    w=["pst"] if kc == 0 else [], wj=[] if kc == 0 else ["pst"])
            P.V(lambda: nc.vector.tensor_copy(out=xnT[:, :, tt * 128:(tt + 1) * 128], in_=pst[:]), r=["pst"], w=["xnT%d" % tt])
            if moe:
                if not (DBG & 16):
                    P.V(lambda: nc.vector.tensor_copy(out=xnT32[:], in_=pst[:]), r=["pst"], w=["xnT32"])
                for kc in range(8 if not (DBG & 1) else 0):
                    P.T(lambda: nc.tensor.matmul(pso[:, 0:8], lhsT=xnT32[:, kc, :], rhs=rsb[:, kc, :], start=(kc == 0), stop=(kc == 7)),
                        r=["xnT32", "rsb"], w=["pso"] if kc == 0 else [], wj=[] if kc == 0 else ["pso"])
                if DBG & 2:
                    P.V(lambda: nc.vector.memset(wt[:, tt, :], 0.5), w=["wt%d" % tt])
                    continue
                if DBG & 1:
                    P.V(lambda: nc.vector.memset(lg[:], 0.5), w=["lg"])
                else:
                    P.V(lambda: nc.vector.tensor_copy(out=lg[:], in_=pso[:, 0:8]), r=["pso"], w=["lg"])
                P.V(lambda: nc.vector.tensor_reduce(out=m1[:], in_=lg[:], axis=AX.X, op=ALU.max), r=["lg"], w=["m1"])
                P.V(lambda: nc.vector.tensor_scalar(out=eq1[:], in0=lg[:], scalar1=m1[:], scalar2=None, op0=ALU.is_equal), r=["lg", "m1"], w=["eq1"])
                P.V(lambda: nc.vector.scalar_tensor_tensor(out=lg[:], in0=eq1[:], scalar=-1e30, in1=lg[:], op0=ALU.mult, op1=ALU.add), r=["eq1", "lg"], w=["lg"])
                P.V(lambda: nc.vector.tensor_reduce(out=m2[:], in_=lg[:], axis=AX.X, op=ALU.max), r=["lg"], w=["m2"])
                P.V(lambda: nc.vector.tensor_scalar(out=eq2[:], in0=lg[:], scalar1=m2[:], scalar2=None, op0=ALU.is_equal), r=["lg", "m2"], w=["eq2"])
                P.V(lambda: nc.vector.tensor_tensor(out=m1[:], in0=m1[:], in1=m2[:], op=ALU.subtract), r=["m1", "m2"], w=["m1"])
                P.A(lambda: nc.scalar.activation(out=m1[:], in_=m1[:], func=AF.Sigmoid), r=["m1"], w=["m1"])
                P.V(lambda: nc.vector.tensor_tensor(out=eq1[:], in0=eq1[:], in1=eq2[:], op=ALU.subtract), r=["eq1", "eq2"], w=["eq1"])
                P.V(lambda: nc.vector.scalar_tensor_tensor(out=wt[:, tt, :], in0=eq1[:], scalar=m1[:], in1=eq2[:], op0=ALU.mult, op1=ALU.add),
                    r=["eq1", "eq2", "m1"], w=["wt%d" % tt])
        xtoks = ["xnT%d" % tt for tt in range(NTT)]
        for e in range(E):
            for grp in range(NG):
                for fci in range(GC):
                    fc = grp * GC + fci; b = wi % 2; wi += 1
                    P.dma(wgs[b][:], wg[e, :, fc * 128:(fc + 1) * 128].rearrange("(kc p) f -> p kc f", p=128), w=["wgs%d" % b])
                    P.dma(wus[b][:], wu[e, :, fc * 128:(fc + 1) * 128].rearrange("(kc p) f -> p kc f", p=128), w=["wus%d" % b])
                    P.G(lambda: nc.gpsimd.tensor_copy(out=wgb[b][:], in_=wgs[b][:]), r=["wgs%d" % b], w=["wgb%d" % b])
                    P.G(lambda: nc.gpsimd.tensor_copy(out=wub[b][:], in_=wus[b][:]), r=["wus%d" % b], w=["wub%d" % b])
                    for th in range(NTH):
                        pb = gi % 2; gi += 1
                        cols = slice(th * TH, (th + 1) * TH)
                        ttk = xtoks[th * (TH // 128):(th + 1) * (TH // 128)]
                        for kc in range(8):
                            P.T(lambda: nc.tensor.matmul(psg[pb][:, 0:TH], lhsT=wgb[b][:, kc, :], rhs=xnT[:, kc, cols], start=(kc == 0), stop=(kc == 7)),
                                r=["wgb%d" % b] + ttk, w=["psg%d" % pb] if kc == 0 else [], wj=[] if kc == 0 else ["psg%d" % pb])
                        for kc in range(8):
                            P.T(lambda: nc.tensor.matmul(psu[pb][:, 0:TH], lhsT=wub[b][:, kc, :], rhs=xnT[:, kc, cols], start=(kc == 0), stop=(kc == 7)),
                                r=["wub%d" % b] + ttk, w=["psu%d" % pb] if kc == 0 else [], wj=[] if kc == 0 else ["psu%d" % pb])
                        P.A(lambda: nc.scalar.activation(out=sg[pb][:], in_=psg[pb][:, 0:TH], func=AF.Silu), r=["psg%d" % pb], w=["sg%d" % pb])
                        P.V(lambda: nc.vector.tensor_tensor(out=hT[:, fci, cols], in0=psu[pb][:, 0:TH], in1=sg[pb][:], op=ALU.mult),
                            r=["psu%d" % pb, "sg%d" % pb], w=["hT%d_%d" % (fci, th)])
                for fci in range(GC):
                    fc = grp * GC + fci; b = di % 2; di += 1
                    P.dma(wds[b][:], wd[e, fc * 128:(fc + 1) * 128, :], w=["wds%d" % b])
                    P.G(lambda: nc.gpsimd.tensor_copy(out=wdb[:, fci, :], in_=wds[b][:]), r=["wds%d" % b], w=["wdb%d" % fci])
                for tt in range(NTT):
                    th = (tt * 128) // TH
                    pso_, ptk = (pso[:], "pso") if tt % 2 == 0 else (pst[:].rearrange("p a b -> p (a b)"), "pst")
                    for half in range(2):
                        for fci in range(GC):
                            P.T(lambda: nc.tensor.matmul(pso_[:, half * 512:(half + 1) * 512], lhsT=hT[:, fci, tt * 128:(tt + 1) * 128],
                                                         rhs=wdb[:, fci, half * 512:(half + 1) * 512], start=(fci == 0), stop=(fci == GC - 1)),
                                r=["hT%d_%d" % (fci, th), "wdb%d" % fci], w=[ptk] if (fci == 0 and half == 0) else [], wj=[] if (fci == 0 and half == 0) else [ptk])
                    atok = "acc%d" % tt
                    if moe and not (DBG & 4):
                        P.V(lambda: nc.vector.scalar_tensor_tensor(out=acc[:, tt, :], in0=pso_, scalar=wt[:, tt, e:e + 1], in1=acc[:, tt, :], op0=ALU.mult, op1=ALU.add),
                            r=[ptk, "wt%d" % tt, atok], w=[atok])
                    else:
                        P.V(lambda: nc.vector.tensor_tensor(out=acc[:, tt, :], in0=pso_, in1=acc[:, tt, :], op=ALU.add), r=[ptk, atok], w=[atok])
        for tt in range(NTT):
            atok = "acc%d" % tt
            if final:
                P.A(lambda: nc.scalar.activation(out=sq[:], in_=acc[:, tt, :], func=AF.Square, accum_out=ss[:]), r=[atok], w=["sq", "ss"])
                P.V(lambda: nc.vector.tensor_scalar(out=ss[:], in0=ss[:], scalar1=1.0 / 1024, scalar2=1e-6, op0=ALU.mult, op1=ALU.add), r=["ss"], w=["ss"])
                P.A(lambda: nc.scalar.sqrt(out=ss[:], in_=ss[:]), r=["ss"], w=["ss"])
                P.V(lambda: nc.vector.reciprocal(out=ss[:], in_=ss[:]), r=["ss"], w=["ss"])
                P.V(lambda: nc.vector.scalar_tensor_tensor(out=acc[:, tt, :], in0=acc[:, tt, :], scalar=ss[:], in1=gfbc[:], op0=ALU.mult, op1=ALU.mult),
                    r=[atok, "ss", "gfbc"], w=[atok])
            P.dma(y[t0 + tt * 128:t0 + (tt + 1) * 128, :], acc[:, tt, :], r=[atok], wj=["y"], key=atok)
    P.finish(); P.close()
    return nc


def run_F2(xf, prm, l, ncores=8):
    T = xf.shape[0]; TC = T // ncores
    moe = (l % 2 == 1); final = (l == 1); i = l // 2
    com = {"g": prm["norm_ffn_g"][l][None, :], "ident": np.eye(128, dtype=np.float32)}
    if moe:
        com.update(wg=prm["moe_w_gate"][i], wu=prm["moe_w_up"][i], wd=prm["moe_w_down"][i], router=prm["moe_router"][i])
        E, F = 8, 3584
    else:
        com.update(wg=prm["dense_w_gate"][i][None], wu=prm["dense_w_up"][i][None], wd=prm["dense_w_down"][i][None])
        E, F = 1, 2816
    if final:
        com["gf"] = prm["final_norm_g"][None, :]
    nc = build_F2(TC, F, E, moe, final)
    maps = [dict(com, x=xf[c * TC:(c + 1) * TC]) for c in range(ncores)]
    res = run_bass_kernel_spmd(nc, maps, core_ids=list(range(ncores)))
    return np.concatenate([r["y"] for r in res.results], axis=0)


def prep_qk(P, nc, src, dstT, PADC, S, nrot, cos_d, sin_d, ident, scale, norm_g, pfx, ps_t):
    NT = S // 128
    xt = [P.sb(pfx + "x%d" % i, [128, 64]) for i in range(2)]
    cs = [P.sb(pfx + "c%d" % i, [128, 2, nrot]) for i in range(2)]
    tmp = P.sb(pfx + "tmp", [128, 4, nrot]); sq = P.sb(pfx + "sq", [128, 64]); ss = P.sb(pfx + "ss", [128, 1])
    if norm_g is not None:
        gbc = P.sb(pfx + "gbc", [128, 64]); P.dma(gbc[:], norm_g[0:1, :].partition_broadcast(128), w=[pfx + "gbc"])
    for t in range(NT):
        b = t % 2; xk = pfx + "x%d" % b; ck = pfx + "c%d" % b
        x = xt[b]; c = cs[b]
        P.dma(x[:], src[t * 128:(t + 1) * 128, :], w=[xk])
        P.dma(c[:, 0, :], cos_d[t * 128:(t + 1) * 128, :], w=[ck])
        P.dma(c[:, 1, :], sin_d[t * 128:(t + 1) * 128, :], wj=[ck])
        if norm_g is not None:
            P.A(lambda: nc.scalar.activation(out=sq[:], in_=x[:], func=AF.Square, accum_out=ss[:]), r=[xk], w=[pfx + "sq", pfx + "ss"])
            P.V(lambda: nc.vector.tensor_scalar(out=ss[:], in0=ss[:], scalar1=1.0 / 64, scalar2=1e-6, op0=ALU.mult, op1=ALU.add), r=[pfx + "ss"], w=[pfx + "ss"])
            P.A(lambda: nc.scalar.sqrt(out=ss[:], in_=ss[:]), r=[pfx + "ss"], w=[pfx + "ss"])
            P.V(lambda: nc.vector.reciprocal(out=ss[:], in_=ss[:]), r=[pfx + "ss"], w=[pfx + "ss"])
            P.V(lambda: nc.vector.scalar_tensor_tensor(out=x[:], in0=x[:], scalar=ss[:], in1=gbc[:], op0=ALU.mult, op1=ALU.mult), r=[xk, pfx + "ss", pfx + "gbc"], w=[xk])
        n = nrot
        x1, x2 = x[:, 0:n], x[:, n:2 * n]
        tk = pfx + "tmp"
        P.V(lambda: nc.vector.tensor_tensor(out=tmp[:, 0, :], in0=x1, in1=c[:, 0, :], op=ALU.mult), r=[xk, ck], w=[tk])
        P.V(lambda: nc.vector.tensor_tensor(out=tmp[:, 1, :], in0=x2, in1=c[:, 1, :], op=ALU.mult), r=[xk, ck], wj=[tk])
        P.V(lambda: nc.vector.tensor_tensor(out=tmp[:, 2, :], in0=x2, in1=c[:, 0, :], op=ALU.mult), r=[xk, ck], wj=[tk])
        P.V(lambda: nc.vector.tensor_tensor(out=tmp[:, 3, :], in0=x1, in1=c[:, 1, :], op=ALU.mult), r=[xk, ck], wj=[tk])
        P.V(lambda: nc.vector.tensor_tensor(out=x1, in0=tmp[:, 0, :], in1=tmp[:, 1, :], op=ALU.subtract), r=[tk], w=[xk])
        P.V(lambda: nc.vector.tensor_tensor(out=x2, in0=tmp[:, 2, :], in1=tmp[:, 3, :], op=ALU.add), r=[tk], wj=[xk])
        P.T(lambda: nc.tensor.transpose(ps_t[0:64, 0:128], x[:], ident[:]), r=[xk, "ident32"], w=["ps_t"])
        P.V(lambda: nc.vector.tensor_scalar(out=dstT[:, PADC + t * 128:PADC + (t + 1) * 128], in0=ps_t[0:64, 0:128], scalar1=scale, scalar2=None, op0=ALU.mult),
            r=["ps_t"], wj=[pfx + "T"])


def normalize_blk(P, nc, acc, acctok, W, yT, c0, ones, ps_bc, rl, ob, obtok):
    P.V(lambda: nc.vector.reciprocal(out=rl[64:65, 0:W], in_=acc[64:65, 0:W]), r=[acctok], w=["rl"])
    P.T(lambda: nc.tensor.matmul(ps_bc[0:64, 0:W], lhsT=ones[64:65, 0:64], rhs=rl[64:65, 0:W], start=True, stop=True), r=["rl", "ones"], w=["ps_bc"])
    P.V(lambda: nc.vector.tensor_tensor(out=ob[:, 0:W], in0=acc[0:64, 0:W], in1=ps_bc[0:64, 0:W], op=ALU.mult), r=[acctok, "ps_bc"], w=[obtok])
    P.dma(yT[:, c0:c0 + W], ob[:, 0:W], r=[obtok], wj=["yT"], key=obtok)


def build_MD(S):
    P = Prog(); nc = P.nc
    q = P.dram("q", [S, 64]); k = P.dram("k", [S, 64]); v = P.dram("v", [S, 64])
    qg = P.dram("qg", [1, 64]); kg = P.dram("kg", [1, 64]); cos_d = P.dram("cos", [S, 32]); sin_d = P.dram("sin", [S, 32])
    ident_d = P.dram("ident", [128, 128])
    yT = P.dram("yT", [64, S], kind="ExternalOutput")
    NT = S // 128
    ident = load_ident32(P, nc, ident_d)
    ones = P.sb("ones", [128, 64]); P.V(lambda: nc.vector.memset(ones[:], 1.0), w=["ones"])
    qT = P.sb("qT", [64, S], BF16); kT = P.sb("kT", [64, S], BF16)
    ps_t = P.ps("ps_t", [128, 512])
    prep_qk(P, nc, q, qT, 0, S, 32, cos_d, sin_d, ident, 0.125, qg, "q", ps_t)
    prep_qk(P, nc, k, kT, 0, S, 32, cos_d, sin_d, ident, 1.0, kg, "k", ps_t)
    Vx = P.sb("Vx", [128, NT, 65], BF16)
    P.V(lambda: nc.vector.memset(Vx[:, :, 64:65], 1.0), w=["Vx"])
    VC = min(16, NT)
    vst = [P.sb("vst%d" % i, [128, VC, 64]) for i in range(2)]
    for ci, m0 in enumerate(range(0, NT, VC)):
        vb = ci % 2
        P.dma(vst[vb][:], v[m0 * 128:(m0 + VC) * 128, :].rearrange("(m p) c -> p m c", p=128), w=["vst%d" % vb])
        P.V(lambda: nc.vector.tensor_copy(out=Vx[:, m0:m0 + VC, 0:64], in_=vst[vb][:]), r=["vst%d" % vb], wj=["Vx"])
    acc = [P.sb("acc%d" % i, [65, 512]) for i in range(2)]
    rl = P.sb("rl", [65, 512]); ob = [P.sb("ob%d" % i, [64, 512]) for i in range(2)]
    NB = 3
    ps_s = [P.ps("ps_s%d" % i, [128, 512]) for i in range(NB)]
    pT = [P.sb("pT%d" % i, [128, 512], BF16) for i in range(NB)]
    po = [P.ps("po%d" % i, [128, 512]) for i in range(2)]
    ps_bc = P.ps("ps_bc", [128, 512])
    W = min(512, S)
    its = [(qb, kt) for qb in range(S // W) for kt in range(NT)]
    LA = 2

    def emit_S(i):
        qb, kt = its[i]; b = i % NB
        P.T(lambda: nc.tensor.matmul(ps_s[b][:, 0:W], lhsT=kT[:, kt * 128:(kt + 1) * 128], rhs=qT[:, qb * W:(qb + 1) * W], start=True, stop=True),
            r=["qT", "kT"], w=["ps_s%d" % b])

    for i in range(min(LA, len(its))):
        emit_S(i)
    for i, (qb, kt) in enumerate(its):
        b = i % NB; pb = qb % 2
        if i + LA < len(its):
            emit_S(i + LA)
        P.A(lambda: nc.scalar.activation(out=pT[b][:, 0:W], in_=ps_s[b][:, 0:W], func=AF.Exp), r=["ps_s%d" % b], w=["pT%d" % b])
        P.T(lambda: nc.tensor.matmul(po[pb][0:65, 0:W], lhsT=Vx[:, kt, :], rhs=pT[b][:, 0:W], start=(kt == 0), stop=(kt == NT - 1)),
            r=["Vx", "pT%d" % b], w=["po%d" % pb] if kt == 0 else [], wj=[] if kt == 0 else ["po%d" % pb])
        if kt == NT - 1:
            P.V(lambda: nc.vector.tensor_copy(out=acc[pb][:, 0:W], in_=po[pb][0:65, 0:W]), r=["po%d" % pb], w=["acc%d" % pb])
            normalize_blk(P, nc, acc[pb], "acc%d" % pb, W, yT, qb * W, ones, ps_bc, rl, ob[pb], "ob%d" % pb)
    P.finish(); P.close()
    return nc


def axial_tables(S):
    def ang(pos, n, theta):
        inv = (np.float32(theta) ** (-np.arange(n, dtype=np.float32) / np.float32(n))).astype(np.float32)
        return pos.astype(np.float32)[:, None] * inv[None, :]
    t = np.arange(S)
    a = np.concatenate([ang(t // 64, 16, 10000.0), ang(t % 64, 16, 10000.0)], axis=-1).astype(np.float32)
    return np.cos(a).astype(np.float32), np.sin(a).astype(np.float32)


def rope_tables(S):
    inv = (np.float32(500000.0) ** (-np.arange(8, dtype=np.float32) / np.float32(8))).astype(np.float32)
    a = (np.arange(S).astype(np.float32)[:, None] * inv[None, :]).astype(np.float32)
    return np.cos(a).astype(np.float32), np.sin(a).astype(np.float32)


def run_MD(zq, zk, zv, prm, l):
    B, S, _ = zq.shape
    nc = build_MD(S)
    cos, sin = axial_tables(S)
    com = {"qg": prm["gqa_q_norm"][l][None, :], "kg": prm["gqa_k_norm"][l][None, :], "cos": cos, "sin": sin, "ident": np.eye(128, dtype=np.float32)}
    maps = []
    for c in range(B * 4):
        b, h = c // 4, c % 4
        maps.append(dict(com, q=np.ascontiguousarray(zq[b, :, h * 64:(h + 1) * 64]), k=np.ascontiguousarray(zk[b, :, (h // 2) * 64:(h // 2 + 1) * 64]),
                         v=np.ascontiguousarray(zv[b, :, (h // 2) * 64:(h // 2 + 1) * 64])))
    res = run_bass_kernel_spmd(nc, maps, core_ids=list(range(B * 4)))
    y = np.zeros((B, S, 256), np.float32)
    for c in range(B * 4):
        b, h = c // 4, c % 4
        y[b, :, h * 64:(h + 1) * 64] = res.results[c]["yT"].T
    return y


def build_MA(S):
    P = Prog(); nc = P.nc
    PADC = 1024
    DIL = (1, 4, 16)
    q = P.dram("q", [S, 64]); k = P.dram("k", [S, 64])
    vx = [P.dram("vx%d" % d, [d * (S // d + 128), 65]) for d in DIL]
    cos_d = P.dram("cos", [S, 8]); sin_d = P.dram("sin", [S, 8])
    ident_d = P.dram("ident", [128, 128]); mask_d = P.dram("mask", [128, 256])
    yT = P.dram("yT", [64, S], kind="ExternalOutput")
    ident = load_ident32(P, nc, ident_d)
    ones = P.sb("ones", [128, 64]); P.V(lambda: nc.vector.memset(ones[:], 1.0), w=["ones"])
    m32 = P.sb("m32", [128, 256]); mask = P.sb("maskb", [128, 256], BF16)
    P.dma(m32[:], mask_d[:, :], w=["m32"]); P.V(lambda: nc.vector.tensor_copy(out=mask[:], in_=m32[:]), r=["m32"], w=["mask"])
    qT = P.sb("qT", [64, S + 2 * PADC], BF16); kT = P.sb("kT", [64, S + 2 * PADC], BF16)
    P.V(lambda: nc.vector.memset(kT[:, 0:PADC], 0.0), w=["kT"]); P.V(lambda: nc.vector.memset(kT[:, PADC + S:], 0.0), wj=["kT"])
    P.V(lambda: nc.vector.memset(qT[:, 0:PADC], 0.0), w=["qT"]); P.V(lambda: nc.vector.memset(qT[:, PADC + S:], 0.0), wj=["qT"])
    ps_t = P.ps("ps_t", [128, 512])
    prep_qk(P, nc, q, qT, PADC, S, 8, cos_d, sin_d, ident, 0.125, None, "q", ps_t)
    prep_qk(P, nc, k, kT, PADC, S, 8, cos_d, sin_d, ident, 1.0, None, "k", ps_t)
    SB = 2048
    accT = P.sb("accT", [65, SB])
    NB = 3
    ps_s = [P.ps("ps_s%d" % i, [128, 512]) for i in range(NB)]
    pT = [P.sb("pT%d" % i, [128, 256], BF16) for i in range(NB)]
    pTm = [P.sb("pTm%d" % i, [128, 256], BF16) for i in range(NB)]
    po = [P.ps("po%d" % i, [128, 512]) for i in range(NB)]
    ps_bc = ps_t
    rl = P.sb("rl", [65, 512]); ob = [P.sb("ob%d" % i, [64, 512]) for i in range(2)]
    Vxs = {}
    vst = [P.sb("vst%d" % i, [128, 16, 65]) for i in range(2)]
    ci = 0
    for di, d in enumerate(DIL):
        L = S // d; NKT = L // 128 + 1; NTL = d * NKT
        Vx = P.sb("Vx%d" % d, [128, NTL, 65], BF16)
        for m0 in range(0, NTL, 16):
            m1 = min(NTL, m0 + 16); vb = ci % 2; ci += 1
            P.dma(vst[vb][:, 0:m1 - m0, :], vx[di][m0 * 128:m1 * 128, :].rearrange("(m p) c -> p m c", p=128), w=["vst%d" % vb])
            P.V(lambda: nc.vector.tensor_copy(out=Vx[:, m0:m1, :], in_=vst[vb][:, 0:m1 - m0, :]), r=["vst%d" % vb], wj=["Vx%d" % d])
        Vxs[d] = (Vx, NKT)
    oi = 0
    its = []
    for sbi in range(S // SB):
        for di, d in enumerate(DIL):
            for r in range(d):
                for bl in range(SB // (128 * d)):
                    its.append((sbi, di, d, r, bl))
    LA = 2

    def emit_S(i):
        sbi, di, d, r, bl = its[i]; b = i % NB
        i0 = (sbi * SB // d) + bl * 128
        qs = PADC + r + d * i0
        rhs = qT[:, qs:qs + 127 * d + 1:d]
        for ab in range(2):
            ks = PADC + r + d * (i0 - 64 + 128 * ab)
            P.T(lambda: nc.tensor.matmul(ps_s[b][:, ab * 128:(ab + 1) * 128], lhsT=kT[:, ks:ks + 127 * d + 1:d], rhs=rhs, start=True, stop=True),
                r=["qT", "kT"], w=["ps_s%d" % b] if ab == 0 else [], wj=[] if ab == 0 else ["ps_s%d" % b])

    for i in range(min(LA, len(its))):
        emit_S(i)
    for i, (sbi, di, d, r, bl) in enumerate(its):
        b = i % NB
        Vx, NKT = Vxs[d]
        i0 = (sbi * SB // d) + bl * 128; blk = i0 // 128
        if i + LA < len(its):
            emit_S(i + LA)
        P.A(lambda: nc.scalar.activation(out=pT[b][:], in_=ps_s[b][:, 0:256], func=AF.Exp), r=["ps_s%d" % b], w=["pT%d" % b])
        P.G(lambda: nc.gpsimd.tensor_tensor(out=pTm[b][:], in0=pT[b][:], in1=mask[:], op=ALU.mult), r=["pT%d" % b, "mask"], w=["pTm%d" % b])
        for ab in range(2):
            P.T(lambda: nc.tensor.matmul(po[b][0:65, 0:128], lhsT=Vx[:, r * NKT + blk + ab, :], rhs=pTm[b][:, ab * 128:(ab + 1) * 128], start=(ab == 0), stop=(ab == 1)),
                r=["Vx%d" % d, "pTm%d" % b], w=["po%d" % b] if ab == 0 else [], wj=[] if ab == 0 else ["po%d" % b])
        t0 = r + d * bl * 128
        dst = accT[:, t0:t0 + 127 * d + 1:d]
        if di == 0:
            P.V(lambda: nc.vector.tensor_copy(out=dst, in_=po[b][0:65, 0:128]), r=["po%d" % b], w=["accT"] if (bl == 0) else [], wj=[] if (bl == 0) else ["accT"])
        else:
            P.V(lambda: nc.vector.tensor_tensor(out=dst, in0=dst, in1=po[b][0:65, 0:128], op=ALU.add), r=["po%d" % b, "accT"], w=["accT"])
        last = (i + 1 == len(its)) or (its[i + 1][0] != sbi)
        if last:
            for c0 in range(0, SB, 512):
                o = oi % 2; oi += 1
                normalize_blk(P, nc, accT[:, c0:c0 + 512], "accT", 512, yT, sbi * SB + c0, ones, ps_bc, rl, ob[o], "ob%d" % o)
    P.finish(); P.close()
    return nc


def dil_mask():
    kk = np.arange(128)[:, None]; qq = np.arange(128)[None, :]
    return np.concatenate([(kk >= qq), (kk <= qq)], axis=1).astype(np.float32)


def vext_dilated(v, d):
    S = v.shape[0]; L = S // d
    out = np.zeros((d, L + 128, 65), np.float32)
    vr = v.reshape(L, d, 64).transpose(1, 0, 2)
    out[:, 64:64 + L, :64] = vr
    out[:, 64:64 + L, 64] = 1.0
    return out.reshape(d * (L + 128), 65)


def run_MA(za, prm, l):
    B, S, _ = za.shape
    nc = build_MA(S)
    cos, sin = rope_tables(S)
    com = {"cos": cos, "sin": sin, "ident": np.eye(128, dtype=np.float32), "mask": dil_mask()}
    maps = []
    for c in range(B * 4):
        b, h = c // 4, c % 4
        v = za[b, :, 512 + h * 64:512 + (h + 1) * 64]
        m = dict(com, q=np.ascontiguousarray(za[b, :, h * 64:(h + 1) * 64]), k=np.ascontiguousarray(za[b, :, 256 + h * 64:256 + (h + 1) * 64]))
        for d in (1, 4, 16):
            m["vx%d" % d] = vext_dilated(v, d)
        maps.append(m)
    res = run_bass_kernel_spmd(nc, maps, core_ids=list(range(B * 4)))
    y = np.zeros((B, S, 256), np.float32)
    for c in range(B * 4):
        b, h = c // 4, c % 4
        y[b, :, h * 64:(h + 1) * 64] = res.results[c]["yT"].T
    return y


TWO_PI = 6.283185307179586
PI = 3.141592653589793


def range_reduce(P, nc, r, ki, tok, shape):
    kf = P.sb(tok + "_kf", shape)
    P.V(lambda: nc.vector.tensor_scalar(out=kf[:], in0=r[:], scalar1=1.0 / TWO_PI, scalar2=None, op0=ALU.mult), r=[tok], w=[tok + "kf"])
    P.V(lambda: nc.vector.tensor_copy(out=ki[:], in_=kf[:]), r=[tok + "kf"], w=[tok + "ki"])
    P.V(lambda: nc.vector.tensor_copy(out=kf[:], in_=ki[:]), r=[tok + "ki"], w=[tok + "kf"])
    P.V(lambda: nc.vector.scalar_tensor_tensor(out=r[:], in0=kf[:], scalar=-TWO_PI, in1=r[:], op0=ALU.mult, op1=ALU.add), r=[tok + "kf", tok], w=[tok])
    for (cmp, thr, add) in ((ALU.is_gt, PI, -TWO_PI), (ALU.is_lt, -PI, TWO_PI), (ALU.is_gt, PI, -TWO_PI)):
        P.V(lambda: nc.vector.tensor_scalar(out=kf[:], in0=r[:], scalar1=thr, scalar2=add, op0=cmp, op1=ALU.mult), r=[tok], w=[tok + "kf"])
        P.V(lambda: nc.vector.tensor_tensor(out=r[:], in0=r[:], in1=kf[:], op=ALU.add), r=[tok, tok + "kf"], w=[tok])
    P.V(lambda: nc.vector.tensor_scalar(out=r[:], in0=r[:], scalar1=3.1415925, scalar2=-3.1415925, op0=ALU.min, op1=ALU.max), r=[tok], w=[tok])


def build_MC(S):
    P = Prog(); nc = P.nc
    T = min(512, S); NCH = S // T
    uT_d = [P.dram("uT%d" % d, [64, S]) for d in range(2)]
    a_re = P.dram("a_re", [2, 4, 64]); a_im = P.dram("a_im", [2, 4, 64]); ldt = P.dram("ldt", [2, 4])
    b_re = P.dram("b_re", [4, 64, 16]); b_im = P.dram("b_im", [4, 64, 16])
    c_re = P.dram("c_re", [2, 4, 16, 64]); c_im = P.dram("c_im", [2, 4, 16, 64])
    ident_d = P.dram("ident", [128, 128]); iota_d = P.dram("iota", [1, T + 1])
    yT_d = [P.dram("yT%d" % d, [64, S], kind="ExternalOutput") for d in range(2)]
    ident = load_ident32(P, nc, ident_d)
    iota = P.sb("iota", [128, T + 1]); P.dma(iota[:], iota_d[0:1, :].partition_broadcast(128), w=["iota"])
    uT = []
    UC = min(2048, S)
    ust = [P.sb("ust%d" % i, [64, UC]) for i in range(2)]
    ui = 0
    for d in range(2):
        u = P.sb("uTb%d" % d, [64, S], BF16)
        for c0 in range(0, S, UC):
            ub = ui % 2; ui += 1
            P.dma(ust[ub][:], uT_d[d][:, c0:c0 + UC], w=["ust%d" % ub])
            P.V(lambda: nc.vector.tensor_copy(out=u[:, c0:c0 + UC], in_=ust[ub][:]), r=["ust%d" % ub], wj=["uT%d" % d])
        uT.append(u)
    ps_t = P.ps("ps_t", [128, 512])
    tiles = {}
    for d in range(2):
        for gp in range(2):
            n = "t%d%d" % (d, gp)
            prm = P.sb(n + "prm", [128, 16])
            pk = n + "prm"
            P.dma(prm[:, 0:1], a_re[d, 2 * gp:2 * gp + 2, :].rearrange("g (p o) -> (g p) o", o=1), w=[pk])
            P.dma(prm[:, 1:2], a_im[d, 2 * gp:2 * gp + 2, :].rearrange("g (p o) -> (g p) o", o=1), wj=[pk])
            for g in range(2):
                P.dma(prm[g * 64:(g + 1) * 64, 2:3], ldt[d:d + 1, 2 * gp + g:2 * gp + g + 1].partition_broadcast(64), wj=[pk])
            c = lambda i: prm[:, i:i + 1]
            P.A(lambda: nc.scalar.activation(out=c(2), in_=c(2), func=AF.Exp), r=[pk], w=[pk])
            P.V(lambda: nc.vector.tensor_tensor(out=c(4), in0=c(1), in1=c(2), op=ALU.mult), r=[pk], w=[pk])
            P.V(lambda: nc.vector.tensor_tensor(out=c(3), in0=c(0), in1=c(2), op=ALU.mult), r=[pk], w=[pk])
            P.A(lambda: nc.scalar.activation(out=c(3), in_=c(3), func=AF.Exp), r=[pk], w=[pk])
            cosT = P.sb(n + "cos", [128, T + 1]); sinT = P.sb(n + "sin", [128, T + 1]); ki = P.sb(n + "ki", [128, T + 1], I32)
            P.V(lambda: nc.vector.tensor_scalar(out=sinT[:], in0=iota[:], scalar1=c(4), scalar2=None, op0=ALU.mult), r=["iota", pk], w=[n + "sin"])
            P.V(lambda: nc.vector.tensor_scalar(out=cosT[:], in0=sinT[:], scalar1=PI / 2, scalar2=None, op0=ALU.add), r=[n + "sin"], w=[n + "cos"])
            range_reduce(P, nc, sinT, ki, n + "sin", [128, T + 1])
            range_reduce(P, nc, cosT, ki, n + "cos", [128, T + 1])
            P.A(lambda: nc.scalar.activation(out=sinT[:], in_=sinT[:], func=AF.Sin), r=[n + "sin"], w=[n + "sin"])
            P.A(lambda: nc.scalar.activation(out=cosT[:], in_=cosT[:], func=AF.Sin), r=[n + "cos"], w=[n + "cos"])
            P.V(lambda: nc.vector.tensor_copy(out=c(5), in_=cosT[:, 1:2]), r=[n + "cos", pk], w=[pk])
            P.V(lambda: nc.vector.tensor_copy(out=c(6), in_=sinT[:, 1:2]), r=[n + "sin", pk], w=[pk])
            P.V(lambda: nc.vector.tensor_copy(out=c(13), in_=cosT[:, T:T + 1]), r=[n + "cos", pk], w=[pk])
            P.V(lambda: nc.vector.tensor_copy(out=c(14), in_=sinT[:, T:T + 1]), r=[n + "sin", pk], w=[pk])
            P.V(lambda: nc.vector.tensor_scalar(out=c(15), in0=c(14), scalar1=-1.0, scalar2=None, op0=ALU.mult), r=[pk], w=[pk])
            P.V(lambda: nc.vector.tensor_tensor(out=c(5), in0=c(5), in1=c(3), op=ALU.mult), r=[pk], w=[pk])
            P.V(lambda: nc.vector.tensor_tensor(out=c(6), in0=c(6), in1=c(3), op=ALU.mult), r=[pk], w=[pk])
            P.V(lambda: nc.vector.tensor_scalar(out=c(7), in0=c(5), scalar1=-1.0, scalar2=None, op0=ALU.add), r=[pk], w=[pk])
            P.V(lambda: nc.vector.tensor_tensor(out=c(8), in0=c(0), in1=c(0), op=ALU.mult), r=[pk], w=[pk])
            P.V(lambda: nc.vector.tensor_tensor(out=c(11), in0=c(1), in1=c(1), op=ALU.mult), r=[pk], w=[pk])
            P.V(lambda: nc.vector.tensor_tensor(out=c(8), in0=c(8), in1=c(11), op=ALU.add), r=[pk], w=[pk])
            P.V(lambda: nc.vector.reciprocal(out=c(8), in_=c(8)), r=[pk], w=[pk])
            P.V(lambda: nc.vector.tensor_tensor(out=c(9), in0=c(7), in1=c(0), op=ALU.mult), r=[pk], w=[pk])
            P.V(lambda: nc.vector.tensor_tensor(out=c(11), in0=c(6), in1=c(1), op=ALU.mult), r=[pk], w=[pk])
            P.V(lambda: nc.vector.tensor_tensor(out=c(9), in0=c(9), in1=c(11), op=ALU.add), r=[pk], w=[pk])
            P.V(lambda: nc.vector.tensor_tensor(out=c(9), in0=c(9), in1=c(8), op=ALU.mult), r=[pk], w=[pk])
            P.V(lambda: nc.vector.tensor_tensor(out=c(10), in0=c(6), in1=c(0), op=ALU.mult), r=[pk], w=[pk])
            P.V(lambda: nc.vector.tensor_tensor(out=c(11), in0=c(7), in1=c(1), op=ALU.mult), r=[pk], w=[pk])
            P.V(lambda: nc.vector.tensor_tensor(out=c(10), in0=c(10), in1=c(11), op=ALU.subtract), r=[pk], w=[pk])
            P.V(lambda: nc.vector.tensor_tensor(out=c(10), in0=c(10), in1=c(8), op=ALU.mult), r=[pk], w=[pk])
            P.V(lambda: nc.vector.tensor_scalar(out=c(12), in0=c(10), scalar1=-1.0, scalar2=None, op0=ALU.mult), r=[pk], w=[pk])
            braw = P.sb(n + "braw", [128, 2, 16]); bk = n + "braw"
            P.dma(braw[:, 0, :], b_re[2 * gp:2 * gp + 2].rearrange("g p c -> (g p) c"), w=[bk])
            P.dma(braw[:, 1, :], b_im[2 * gp:2 * gp + 2].rearrange("g p c -> (g p) c"), wj=[bk])
            BD = P.sb(n + "BD", [128, 2, 64]); tb = P.sb(n + "tb", [128, 16])
            P.V(lambda: nc.vector.memset(BD[:], 0.0), w=[n + "BD"])
            for g in range(2):
                rows = slice(g * 64, (g + 1) * 64); cols = slice(32 * gp + 16 * g, 32 * gp + 16 * g + 16)
                P.V(lambda: nc.vector.tensor_scalar(out=tb[rows, :], in0=braw[rows, 1, :], scalar1=prm[rows, 12:13], scalar2=None, op0=ALU.mult), r=[bk, pk], w=[n + "tb"])
                P.V(lambda: nc.vector.scalar_tensor_tensor(out=BD[rows, 0, cols], in0=braw[rows, 0, :], scalar=prm[rows, 9:10], in1=tb[rows, :], op0=ALU.mult, op1=ALU.add),
                    r=[bk, pk, n + "tb"], wj=[n + "BD"])
                P.V(lambda: nc.vector.tensor_scalar(out=tb[rows, :], in0=braw[rows, 0, :], scalar1=prm[rows, 10:11], scalar2=None, op0=ALU.mult), r=[bk, pk], w=[n + "tb"])
                P.V(lambda: nc.vector.scalar_tensor_tensor(out=BD[rows, 1, cols], in0=braw[rows, 1, :], scalar=prm[rows, 9:10], in1=tb[rows, :], op0=ALU.mult, op1=ALU.add),
                    r=[bk, pk, n + "tb"], wj=[n + "BD"])
            BT = P.sb(n + "BT", [64, 2, 128], BF16)
            for ri in range(2):
                P.T(lambda: nc.tensor.transpose(ps_t[0:64, 0:128], BD[:, ri, :], ident[:]), r=[n + "BD", "ident32"], w=["ps_t"])
                P.V(lambda: nc.vector.tensor_copy(out=BT[:, ri, :], in_=ps_t[0:64, 0:128]), r=["ps_t"], wj=[n + "BT"])
            craw = P.sb(n + "craw", [128, 2, 16]); ck = n + "craw"
            for g in range(2):
                P.dma(craw[g * 64:(g + 1) * 64, 0, :], c_re[d, 2 * gp + g].rearrange("c p -> p c"), wj=[ck], allow_slow_non_contiguous=True)
                P.dma(craw[g * 64:(g + 1) * 64, 1, :], c_im[d, 2 * gp + g].rearrange("c p -> p c"), wj=[ck], allow_slow_non_contiguous=True)
            CT = P.sb(n + "CT", [128, 2, 64], BF16)
            P.V(lambda: nc.vector.memset(CT[:], 0.0), w=[n + "CT"])
            for g in range(2):
                rows = slice(g * 64, (g + 1) * 64); cols = slice(32 * gp + 16 * g, 32 * gp + 16 * g + 16)
                P.V(lambda: nc.vector.tensor_copy(out=CT[rows, 0, cols], in_=craw[rows, 0, :]), r=[ck], wj=[n + "CT"])
                P.V(lambda: nc.vector.tensor_scalar(out=CT[rows, 1, cols], in0=craw[rows, 1, :], scalar1=-1.0, scalar2=None, op0=ALU.mult), r=[ck], wj=[n + "CT"])
            rho_t = P.sb(n + "rho", [128, T])
            P.V(lambda: nc.vector.tensor_scalar(out=rho_t[:], in0=iota[:, 0:T], scalar1=0.0, scalar2=prm[:, 3:4], op0=ALU.mult, op1=ALU.add), r=["iota", pk], w=[n + "rho"])
            init = P.sb(n + "init", [128, 2]); P.V(lambda: nc.vector.memset(init[:], 0.0), w=[n + "init"])
            tiles[(d, gp)] = dict(n=n, prm=prm, pk=pk, cosT=cosT, sinT=sinT, BT=BT, CT=CT, rho=rho_t, init=init)
    ps_b = [P.ps("ps_b%d" % i, [128, 512]) for i in range(2)]
    ps_y = P.ps("ps_y", [128, 512])
    m = [P.sb("m%d" % i, [128, T]) for i in range(4)]
    bp = [P.sb("bp%d" % i, [128, T]) for i in range(2)]
    wv = [P.sb("wv%d" % i, [128, T]) for i in range(2)]
    pp = [P.sb("pp%d" % i, [128, T]) for i in range(4)]
    xb = [[P.sb("xb%d_%d" % (gp, i), [128, T], BF16) for i in range(2)] for gp in range(2)]
    yo = [P.sb("yo%d" % i, [64, T]) for i in range(2)]
    tmpc = P.sb("tmpc", [128, 2])
    it = 0
    for d in range(2):
        for ch in range(NCH):
            cols = slice(ch * T, (ch + 1) * T)
            for gp in range(2):
                t = tiles[(d, gp)]; n = t["n"]; cosT, sinT = t["cosT"], t["sinT"]
                for ri in range(2):
                    P.T(lambda: nc.tensor.matmul(ps_b[ri][:, 0:T], lhsT=t["BT"][:, ri, :], rhs=uT[d][:, cols], start=True, stop=True), r=[n + "BT", "uT%d" % d], w=["ps_b%d" % ri])
                P.V(lambda: nc.vector.tensor_tensor(out=m[0][:], in0=ps_b[0][:, 0:T], in1=cosT[:, 0:T], op=ALU.mult), r=["ps_b0", n + "cos"], w=["m0"])
                P.V(lambda: nc.vector.tensor_tensor(out=m[1][:], in0=ps_b[1][:, 0:T], in1=sinT[:, 0:T], op=ALU.mult), r=["ps_b1", n + "sin"], w=["m1"])
                P.V(lambda: nc.vector.tensor_tensor(out=m[2][:], in0=ps_b[1][:, 0:T], in1=cosT[:, 0:T], op=ALU.mult), r=["ps_b1", n + "cos"], w=["m2"])
                P.V(lambda: nc.vector.tensor_tensor(out=m[3][:], in0=ps_b[0][:, 0:T], in1=sinT[:, 0:T], op=ALU.mult), r=["ps_b0", n + "sin"], w=["m3"])
                P.G(lambda: nc.gpsimd.tensor_tensor(out=bp[0][:], in0=m[0][:], in1=m[1][:], op=ALU.add), r=["m0", "m1"], w=["bp0"])
                P.G(lambda: nc.gpsimd.tensor_tensor(out=bp[1][:], in0=m[2][:], in1=m[3][:], op=ALU.subtract), r=["m2", "m3"], w=["bp1"])
                for ri in range(2):
                    P.V(lambda: nc.vector.tensor_tensor_scan(out=wv[ri][:], data0=t["rho"][:], data1=bp[ri][:], initial=t["init"][:, ri:ri + 1], op0=ALU.mult, op1=ALU.add),
                        r=[n + "rho", "bp%d" % ri, n + "init"], w=["wv%d" % ri])
                prm = t["prm"]
                P.V(lambda: nc.vector.tensor_scalar(out=tmpc[:, 0:1], in0=wv[0][:, T - 1:T], scalar1=prm[:, 13:14], scalar2=None, op0=ALU.mult), r=["wv0", t["pk"]], w=["tmpc"])
                P.V(lambda: nc.vector.tensor_scalar(out=tmpc[:, 1:2], in0=wv[0][:, T - 1:T], scalar1=prm[:, 14:15], scalar2=None, op0=ALU.mult), r=["wv0", t["pk"]], wj=["tmpc"])
                P.V(lambda: nc.vector.scalar_tensor_tensor(out=t["init"][:, 0:1], in0=wv[1][:, T - 1:T], scalar=prm[:, 15:16], in1=tmpc[:, 0:1], op0=ALU.mult, op1=ALU.add),
                    r=["wv1", "tmpc", t["pk"]], w=[n + "init"])
                P.V(lambda: nc.vector.scalar_tensor_tensor(out=t["init"][:, 1:2], in0=wv[1][:, T - 1:T], scalar=prm[:, 13:14], in1=tmpc[:, 1:2], op0=ALU.mult, op1=ALU.add),
                    r=["wv1", "tmpc", t["pk"]], wj=[n + "init"])
                P.G(lambda: nc.gpsimd.tensor_tensor(out=pp[0][:], in0=wv[0][:], in1=cosT[:, 0:T], op=ALU.mult), r=["wv0", n + "cos"], w=["pp0"])
                P.G(lambda: nc.gpsimd.tensor_tensor(out=pp[1][:], in0=wv[1][:], in1=sinT[:, 0:T], op=ALU.mult), r=["wv1", n + "sin"], w=["pp1"])
                P.G(lambda: nc.gpsimd.tensor_tensor(out=xb[gp][0][:], in0=pp[0][:], in1=pp[1][:], op=ALU.subtract), r=["pp0", "pp1"], w=["xb%d_0" % gp])
                P.G(lambda: nc.gpsimd.tensor_tensor(out=pp[2][:], in0=wv[0][:], in1=sinT[:, 0:T], op=ALU.mult), r=["wv0", n + "sin"], w=["pp2"])
                P.G(lambda: nc.gpsimd.tensor_tensor(out=pp[3][:], in0=wv[1][:], in1=cosT[:, 0:T], op=ALU.mult), r=["wv1", n + "cos"], w=["pp3"])
                P.G(lambda: nc.gpsimd.tensor_tensor(out=xb[gp][1][:], in0=pp[2][:], in1=pp[3][:], op=ALU.add), r=["pp2", "pp3"], w=["xb%d_1" % gp])
            k = 0
            for gp in range(2):
                t = tiles[(d, gp)]
                for ri in range(2):
                    P.T(lambda: nc.tensor.matmul(ps_y[0:64, 0:T], lhsT=t["CT"][:, ri, :], rhs=xb[gp][ri][:], start=(k == 0), stop=(k == 3)),
                        r=[t["n"] + "CT", "xb%d_%d" % (gp, ri)], w=["ps_y"] if k == 0 else [], wj=[] if k == 0 else ["ps_y"])
                    k += 1
            b = it % 2; it += 1
            P.V(lambda: nc.vector.tensor_copy(out=yo[b][:], in_=ps_y[0:64, 0:T]), r=["ps_y"], w=["yo%d" % b])
            P.dma(yT_d[d][:, cols], yo[b][:], r=["yo%d" % b], wj=["yT%d" % d], key="yo%d" % b)
    P.finish(); P.close()
    return nc


def mc_inputs(zc_b, prm, l, h, S):
    T = min(512, S)
    u = zc_b[:, h * 64:(h + 1) * 64]
    gs = slice(4 * h, 4 * h + 4)
    return {"uT0": np.ascontiguousarray(u.T), "uT1": np.ascontiguousarray(u[::-1].T),
            "a_re": np.ascontiguousarray(prm["s5_a_re"][l][:, gs]), "a_im": np.ascontiguousarray(prm["s5_a_im"][l][:, gs]),
            "ldt": np.ascontiguousarray(prm["s5_log_dt"][l][:, gs]), "b_re": np.ascontiguousarray(prm["s5_b_re"][l][gs]), "b_im": np.ascontiguousarray(prm["s5_b_im"][l][gs]),
            "c_re": np.ascontiguousarray(prm["s5_c_re"][l][:, gs]), "c_im": np.ascontiguousarray(prm["s5_c_im"][l][:, gs]),
            "ident": np.eye(128, dtype=np.float32), "iota": np.arange(T + 1, dtype=np.float32)[None, :]}


def run_MC(zc, prm, l):
    B, S, _ = zc.shape
    nc = build_MC(S)
    maps = [mc_inputs(zc[c // 4], prm, l, c % 4, S) for c in range(B * 4)]
    res = run_bass_kernel_spmd(nc, maps, core_ids=list(range(B * 4)))
    yf = np.zeros((B, S, 256), np.float32); yb = np.zeros((B, S, 256), np.float32)
    for c in range(B * 4):
        b, h = c // 4, c % 4
        yf[b, :, h * 64:(h + 1) * 64] = res.results[c]["yT0"].T
        yb[b, :, h * 64:(h + 1) * 64] = res.results[c]["yT1"].T[::-1]
    return yf, yb


EM05 = 0.6065306597126334


def build_MB(S):
    P = Prog(); nc = P.nc
    NT = S // 128
    rkv = [P.dram("rkv%d" % d, [S, 192]) for d in range(2)]
    h1 = [[P.dram("h%s1_%d" % (n, d), [64, S]) for d in range(2)] for n in "wa"]
    h2 = [[P.dram("h%s2_%d" % (n, d), [64, S]) for d in range(2)] for n in "wa"]
    w2 = P.dram("w2", [2, 64, 64]); a2 = P.dram("a2", [2, 64, 64]); w0 = P.dram("w0", [2, 64]); a0 = P.dram("a0", [2, 64])
    mu = P.dram("mu", [2, 2, 192]); kka = P.dram("kka", [3, 64])
    ident_d = P.dram("ident", [128, 128]); zsel_d = P.dram("zsel", [64, 32 * 128])
    y_o = P.dram("y", [2, S, 64], kind="ExternalOutput"); bonus_o = P.dram("bonus", [2, S, 64], kind="ExternalOutput")
    pkd = [P.dram("pkd%d" % d, [S, 256], BF16, kind="Internal") for d in range(2)]
    dkd = [P.dram("dkd%d" % d, [S, 64], F32, kind="Internal") for d in range(2)]
    vTs = P.dram("vTs", [128, S], F32, kind="Internal")
    ident = load_ident32(P, nc, ident_d)
    z32 = P.sb("z32", [64, 32 * 128]); zb = P.sb("zb", [64, 32 * 128], BF16)
    P.dma(z32[:], zsel_d[:, :], w=["z32"]); P.V(lambda: nc.vector.tensor_copy(out=zb[:], in_=z32[:]), r=["z32"], w=["zb"])
    def bc(name, src, n):
        t = P.sb(name, [128, n]); P.dma(t[:], src.partition_broadcast(128), w=[name]); return t
    kk_bc = bc("kk_bc", kka[0:1, :], 64); ka_bc = bc("ka_bc", kka[1:2, :], 64); rk_bc = bc("rk_bc", kka[2:3, :], 64)
    ps_t = P.ps("ps_t", [128, 512]); ps_l = P.ps("ps_l", [128, 512])
    thT = P.sb("thT", [64, S], BF16); haT = P.sb("haT", [64, S], BF16)
    CW = min(2048, S)
    ha = P.sb("ha_", [64, CW]); hb = P.sb("hb_", [64, CW])
    vstage = P.sb("vstage", [128, 128]); P.V(lambda: nc.vector.memset(vstage[:], 0.0), w=["vstage"])
    zrow = P.sb("zrow", [1, 64], BF16); P.V(lambda: nc.vector.memset(zrow[:], 0.0), w=["zrow"])
    for d in range(2):
        for wi, dst in enumerate((thT, haT)):
            dk_ = "thT" if wi == 0 else "haT"
            for ci, c0 in enumerate(range(0, S, CW)):
                P.dma(ha[:], h1[wi][d][:, c0:c0 + CW], w=["ha"])
                if c0 == 0:
                    P.V(lambda: nc.vector.memset(hb[:, 0:1], 0.0), w=["hb"])
                    P.dma(hb[:, 1:CW], h2[wi][d][:, 0:CW - 1], wj=["hb"])
                else:
                    P.dma(hb[:], h2[wi][d][:, c0 - 1:c0 + CW - 1], w=["hb"])
                P.V(lambda: nc.vector.tensor_tensor(out=ha[:], in0=ha[:], in1=hb[:], op=ALU.add), r=["ha", "hb"], w=["ha"])
                first = (ci == 0)
                if wi == 0:
                    P.A(lambda: nc.scalar.activation(out=dst[:, c0:c0 + CW], in_=ha[:], func=AF.Tanh), r=["ha"], w=[dk_] if first else [], wj=[] if first else [dk_])
                else:
                    P.V(lambda: nc.vector.tensor_copy(out=dst[:, c0:c0 + CW], in_=ha[:]), r=["ha"], w=[dk_] if first else [], wj=[] if first else [dk_])
        w2s = P.sb("w2s%d" % d, [64, 2, 64]); w2b = P.sb("w2b%d" % d, [64, 2, 64], BF16)
        P.dma(w2s[:, 0, :], w2[d], w=["w2s%d" % d]); P.dma(w2s[:, 1, :], a2[d], wj=["w2s%d" % d])
        P.V(lambda: nc.vector.tensor_copy(out=w2b[:], in_=w2s[:]), r=["w2s%d" % d], w=["w2b%d" % d])
        w0_bc = bc("w0_bc%d" % d, w0[d:d + 1, :], 64); a0_bc = bc("a0_bc%d" % d, a0[d:d + 1, :], 64)
        mu0_bc = bc("mu0_bc%d" % d, mu[d, 0:1, :], 192); mu1_bc = bc("mu1_bc%d" % d, mu[d, 1:2, :], 192)
        cur = [P.sb("cur%d_%d" % (d, i), [128, 192]) for i in range(2)]
        prv = [P.sb("prv%d_%d" % (d, i), [128, 192]) for i in range(2)]
        nxt = [P.sb("nxt%d_%d" % (d, i), [128, 192]) for i in range(2)]
        d0 = P.sb("d0_%d" % d, [128, 192]); d1 = P.sb("d1_%d" % d, [128, 192]); mix = P.sb("mix%d" % d, [128, 192])
        dec = [P.sb("dec%d_%d" % (d, i), [128, 64]) for i in range(2)]
        pack = [P.sb("pack%d_%d" % (d, i), [128, 256], BF16) for i in range(2)]
        bon = [P.sb("bon%d_%d" % (d, i), [128, 64]) for i in range(2)]
        vto = [P.sb("vto%d_%d" % (d, i), [128, 128]) for i in range(2)]
        icl = P.sb("icl%d" % d, [128, 64]); kkt = P.sb("kkt%d" % d, [128, 64]); kap = P.sb("kap%d" % d, [128, 64]); kd = P.sb("kd%d" % d, [128, 64])
        t1 = P.sb("t1_%d" % d, [128, 64]); sq = P.sb("sq%d" % d, [128, 64]); ss = P.sb("ss%d" % d, [128, 1]); sb_ = P.sb("sb%d" % d, [128, 1])
        for t in range(NT):
            b = t % 2; r0 = t * 128
            ck, pk_, nk = "cur%d" % b, "prv%d" % b, "nxt%d" % b
            P.dma(cur[b][:], rkv[d][r0:r0 + 128, :], w=[ck])
            if t == 0:
                P.V(lambda: nc.vector.memset(prv[b][:], 0.0), w=[pk_])
                P.dma(prv[b][1:128, :], rkv[d][0:127, :], wj=[pk_])
            else:
                P.dma(prv[b][:], rkv[d][r0 - 1:r0 + 127, :], w=[pk_])
            if t == NT - 1:
                P.V(lambda: nc.vector.memset(nxt[b][:], 0.0), w=[nk])
                P.dma(nxt[b][0:127, :], rkv[d][r0 + 1:r0 + 128, :], wj=[nk])
            else:
                P.dma(nxt[b][:], rkv[d][r0 + 1:r0 + 129, :], w=[nk])
            P.T(lambda: nc.tensor.matmul(ps_l[:, 0:64], lhsT=thT[:, r0:r0 + 128], rhs=w2b[:, 0, :], start=True, stop=True), r=["thT", "w2b%d" % d], w=["ps_l"])
            P.T(lambda: nc.tensor.matmul(ps_l[:, 64:128], lhsT=haT[:, r0:r0 + 128], rhs=w2b[:, 1, :], start=True, stop=True), r=["haT", "w2b%d" % d], wj=["ps_l"])
            P.V(lambda: nc.vector.tensor_tensor(out=d0[:], in0=prv[b][:], in1=cur[b][:], op=ALU.subtract), r=[pk_, ck], w=["d0"])
            P.V(lambda: nc.vector.tensor_tensor(out=d0[:], in0=d0[:], in1=mu0_bc[:], op=ALU.mult), r=["d0", "mu0_bc%d" % d], w=["d0"])
            P.V(lambda: nc.vector.tensor_tensor(out=d1[:], in0=nxt[b][:], in1=cur[b][:], op=ALU.subtract), r=[nk, ck], w=["d1"])
            P.V(lambda: nc.vector.tensor_tensor(out=d1[:], in0=d1[:], in1=mu1_bc[:], op=ALU.mult), r=["d1", "mu1_bc%d" % d], w=["d1"])
            P.V(lambda: nc.vector.tensor_tensor(out=d0[:], in0=d0[:], in1=d1[:], op=ALU.add), r=["d0", "d1"], w=["d0"])
            P.V(lambda: nc.vector.tensor_tensor(out=mix[:], in0=cur[b][:], in1=d0[:], op=ALU.add), r=[ck, "d0"], w=["mix"])
            rr, kp, vp = mix[:, 0:64], mix[:, 64:128], mix[:, 128:192]
            P.V(lambda: nc.vector.tensor_tensor(out=t1[:], in0=ps_l[:, 0:64], in1=w0_bc[:], op=ALU.add), r=["ps_l", "w0_bc%d" % d], w=["t1"])
            P.A(lambda: nc.scalar.activation(out=t1[:], in_=t1[:], func=AF.Sigmoid), r=["t1"], w=["t1"])
            P.A(lambda: nc.scalar.activation(out=dec[b][:], in_=t1[:], func=AF.Exp, scale=-EM05), r=["t1"], w=["dec%d" % b])
            P.V(lambda: nc.vector.tensor_tensor(out=icl[:], in0=ps_l[:, 64:128], in1=a0_bc[:], op=ALU.add), r=["ps_l", "a0_bc%d" % d], w=["icl"])
            P.A(lambda: nc.scalar.activation(out=icl[:], in_=icl[:], func=AF.Sigmoid), r=["icl"], w=["icl"])
            P.V(lambda: nc.vector.tensor_tensor(out=kkt[:], in0=kp, in1=kk_bc[:], op=ALU.mult), r=["mix", "kk_bc"], w=["kkt"])
            P.A(lambda: nc.scalar.activation(out=sq[:], in_=kkt[:], func=AF.Square, accum_out=ss[:]), r=["kkt"], w=["sq", "ss"])
            P.V(lambda: nc.vector.tensor_scalar(out=ss[:], in0=ss[:], scalar1=1e-12, scalar2=None, op0=ALU.add), r=["ss"], w=["ss"])
            P.A(lambda: nc.scalar.sqrt(out=ss[:], in_=ss[:]), r=["ss"], w=["ss"])
            P.V(lambda: nc.vector.reciprocal(out=ss[:], in_=ss[:]), r=["ss"], w=["ss"])
            P.V(lambda: nc.vector.tensor_scalar(out=kap[:], in0=kkt[:], scalar1=ss[:], scalar2=None, op0=ALU.mult), r=["kkt", "ss"], w=["kap"])
            P.V(lambda: nc.vector.tensor_tensor(out=t1[:], in0=icl[:], in1=ka_bc[:], op=ALU.mult), r=["icl", "ka_bc"], w=["t1"])
            P.V(lambda: nc.vector.scalar_tensor_tensor(out=t1[:], in0=t1[:], scalar=1.0, in1=ka_bc[:], op0=ALU.add, op1=ALU.subtract), r=["t1", "ka_bc"], w=["t1"])
            P.V(lambda: nc.vector.tensor_tensor(out=kd[:], in0=kp, in1=t1[:], op=ALU.mult), r=["mix", "t1"], w=["kd"])
            pkk = "pack%d" % b
            P.V(lambda: nc.vector.tensor_copy(out=pack[b][:, 0:64], in_=rr), r=["mix"], w=[pkk])
            P.V(lambda: nc.vector.tensor_copy(out=pack[b][:, 64:128], in_=kap[:]), r=["kap"], wj=[pkk])
            P.V(lambda: nc.vector.scalar_tensor_tensor(out=pack[b][:, 128:192], in0=icl[:], scalar=-1.0, in1=kap[:], op0=ALU.mult, op1=ALU.mult), r=["icl", "kap"], wj=[pkk])
            P.V(lambda: nc.vector.tensor_copy(out=pack[b][:, 192:256], in_=kd[:]), r=["kd"], wj=[pkk])
            P.dma(pkd[d][r0:r0 + 128, 0:64], pack[b][:, 0:64], r=[pkk], wj=["scr"], key=pkk)
            P.dma(pkd[d][r0:r0 + 128, 128:256], pack[b][:, 128:256], r=[pkk], wj=["scr"], key=pkk)
            if t == 0:
                P.dma(pkd[d][0:127, 64:128], pack[b][1:128, 64:128], r=[pkk], wj=["scr"], key=pkk)
            else:
                P.dma(pkd[d][r0 - 1:r0 + 127, 64:128], pack[b][:, 64:128], r=[pkk], wj=["scr"], key=pkk)
            if t == NT - 1:
                P.dma(pkd[d][S - 1:S, 64:128], zrow[0:1, :], r=["zrow"], wj=["scr"], key="zrow")
            P.dma(dkd[d][r0:r0 + 128, :], dec[b][:], r=["dec%d" % b], wj=["scr"], key="dec%d" % b)
            P.V(lambda: nc.vector.tensor_tensor(out=t1[:], in0=rr, in1=kd[:], op=ALU.mult), r=["mix", "kd"], w=["t1"])
            P.V(lambda: nc.vector.scalar_tensor_tensor(out=sq[:], in0=t1[:], scalar=1.0, in1=rk_bc[:], op0=ALU.mult, op1=ALU.mult, accum_out=sb_[:]),
                r=["t1", "rk_bc"], w=["sq", "sb_"])
            P.V(lambda: nc.vector.tensor_scalar(out=bon[b][:], in0=vp, scalar1=sb_[:], scalar2=None, op0=ALU.mult), r=["mix", "sb_"], w=["bon%d" % b])
            P.dma(bonus_o[d, r0:r0 + 128, :], bon[b][:], r=["bon%d" % b], wj=["bonus"], key="bon%d" % b)
            P.V(lambda: nc.vector.tensor_copy(out=vstage[:, 64 * d:64 * d + 64], in_=vp), r=["mix"], w=["vstage"])
            P.T(lambda: nc.tensor.transpose(ps_t[:, 0:128], vstage[:], ident[:]), r=["vstage", "ident32"], w=["ps_t"])
            P.V(lambda: nc.vector.tensor_copy(out=vto[b][64 * d:64 * d + 64, :], in_=ps_t[64 * d:64 * d + 64, 0:128]), r=["ps_t"], w=["vto%d" % b])
            P.dma(vTs[64 * d:64 * d + 64, r0:r0 + 128], vto[b][64 * d:64 * d + 64, :], r=["vto%d" % b], wj=["scr"], key="vto%d" % b)
    SEG = min(1024, S); NB = 4
    St = P.sb("St", [128, 64]); P.V(lambda: nc.vector.memset(St[:], 0.0), w=["S"])
    prod = P.sb("prod", [128, 2, 64])
    zcol = P.sb("zcol", [128, 1]); P.V(lambda: nc.vector.memset(zcol[:], 0.0), w=["zcol"])
    bcp = [P.ps("bcp%d" % i, [128, 512]) for i in range(NB)]
    pkt = [P.sb("pkt%d" % i, [64, 4, 256], BF16) for i in range(2)]
    dkt = [P.sb("dkt%d" % i, [64, 4, 64]) for i in range(2)]
    vseg = [P.sb("vseg%d" % i, [128, SEG]) for i in range(2)]
    yseg = [P.sb("yseg%d" % i, [128, SEG, 2]) for i in range(2)]
    yo = [P.sb("yo%d" % i, [128, 128]) for i in range(2)]
    St_b = St[:].unsqueeze(1).to_broadcast([128, 2, 64])
    step = 0; oi = 0
    sk_ap, sk_tok = zcol[:], "zcol"
    for sg in range(S // SEG):
        sb2 = sg % 2; vk = "vseg%d" % sb2; yk = "yseg%d" % sb2
        P.dma(vseg[sb2][:], vTs[:, sg * SEG:(sg + 1) * SEG], r=["scr"], w=[vk])
        for blk in range(SEG // 128):
            s0 = sg * SEG + blk * 128; bb = (s0 // 128) % 2
            pk_, dk_ = "pkt%d" % bb, "dkt%d" % bb
            for d in range(2):
                P.dma(pkt[bb][32 * d:32 * d + 32, :, :], pkd[d][s0:s0 + 128, :].rearrange("(g q) c -> q g c", q=32), r=["scr"], w=[pk_] if d == 0 else [], wj=[] if d == 0 else [pk_])
                P.dma(dkt[bb][32 * d:32 * d + 32, :, :], dkd[d][s0:s0 + 128, :].rearrange("(g q) c -> q g c", q=32), r=["scr"], w=[dk_] if d == 0 else [], wj=[] if d == 0 else [dk_])
            for g in range(4):
                for j in range(32):
                    sl = step % NB; step += 1; bk = "bcp%d" % sl
                    col = blk * 128 + g * 32 + j
                    P.T(lambda: nc.tensor.matmul(bcp[sl][:, 0:256], lhsT=zb[:, j * 128:(j + 1) * 128], rhs=pkt[bb][:, g, :], start=True, stop=True), r=["zb", pk_], w=[bk])
                    P.T(lambda: nc.tensor.matmul(bcp[sl][:, 256:320], lhsT=z32[:, j * 128:(j + 1) * 128], rhs=dkt[bb][:, g, :], start=True, stop=True), r=["z32", dk_], wj=[bk])
                    P.ses = bool(os.environ.get("FORCE_SES"))
                    nbb, kdb, wb = bcp[sl][:, 128:192], bcp[sl][:, 192:256], bcp[sl][:, 256:320]
                    rk2 = bcp[sl][:, 0:128].rearrange("p (a n) -> p a n", a=2)
                    P.V(lambda: nc.vector.tensor_tensor(out=St[:], in0=St[:], in1=wb, op=ALU.mult), r=["S", bk], w=["S"])
                    P.V(lambda: nc.vector.scalar_tensor_tensor(out=St[:], in0=nbb, scalar=sk_ap, in1=St[:], op0=ALU.mult, op1=ALU.add), r=["S", bk, sk_tok], w=["S"])
                    P.V(lambda: nc.vector.scalar_tensor_tensor(out=St[:], in0=kdb, scalar=vseg[sb2][:, col:col + 1], in1=St[:], op0=ALU.mult, op1=ALU.add), r=["S", bk, vk], w=["S"])
                    P.V(lambda: nc.vector.tensor_tensor(out=prod[:], in0=St_b, in1=rk2, op=ALU.mult), r=["S", bk], w=["prod"])
                    P.V(lambda: nc.vector.tensor_reduce(out=yseg[sb2][:, col, :], in_=prod[:], axis=AX.X, op=ALU.add), r=["prod"], wj=[yk])
                    P.ses = True
                    sk_ap, sk_tok = yseg[sb2][:, col, 1:2], yk
        for blk in range(SEG // 128):
            ob = oi % 2; oi += 1; r0 = sg * SEG + blk * 128
            P.T(lambda: nc.tensor.transpose(ps_t[:, 0:128], yseg[sb2][:, blk * 128:(blk + 1) * 128, 0], ident[:]), r=[yk, "ident32"], w=["ps_t"])
            P.V(lambda: nc.vector.tensor_copy(out=yo[ob][:], in_=ps_t[:, 0:128]), r=["ps_t"], w=["yo%d" % ob])
            for d in range(2):
                P.dma(y_o[d, r0:r0 + 128, :], yo[ob][:, 64 * d:64 * d + 64], r=["yo%d" % ob], wj=["y"], key="yo%d" % ob)
        P.V(lambda: nc.vector.memset(zcol[:], 0.0), r=[yk], w=["zcol2"])
    P.finish(); P.close()
    return nc


def zsel_const():
    z = np.zeros((64, 32, 128), np.float32)
    for j in range(32):
        z[j, j, 0:64] = 1.0
        z[32 + j, j, 64:128] = 1.0
    return z.reshape(64, 32 * 128)


def mb_inputs(zb_b, zl_b, prm, l, h):
    hs = slice(h * 64, (h + 1) * 64)
    rkvh = np.concatenate([zb_b[:, h * 64:(h + 1) * 64], zb_b[:, 256 + h * 64:256 + (h + 1) * 64], zb_b[:, 512 + h * 64:512 + (h + 1) * 64]], axis=1)
    m = {"ident": np.eye(128, dtype=np.float32), "zsel": zsel_const(),
         "w2": np.ascontiguousarray(prm["rwkv_w2"][l][:, :, hs]), "a2": np.ascontiguousarray(prm["rwkv_a2"][l][:, :, hs]),
         "w0": np.ascontiguousarray(prm["rwkv_w0"][l][:, hs]), "a0": np.ascontiguousarray(prm["rwkv_a0"][l][:, hs]),
         "kka": np.stack([prm["rwkv_k_k"][l][hs], prm["rwkv_k_a"][l][hs], prm["rwkv_r_k"][l][hs]])}
    mur = prm["rwkv_mu_rkv"][l]
    muh = np.stack([np.concatenate([mur[i, j, hs] for j in range(3)]) for i in range(2)])
    m["mu"] = np.stack([muh, muh[::-1]])
    for d in range(2):
        o = lambda a: np.ascontiguousarray(a if d == 0 else a[::-1])
        m["rkv%d" % d] = o(rkvh)
        base = d * 256
        for wi, nm in enumerate("wa"):
            m["h%s1_%d" % (nm, d)] = np.ascontiguousarray(o(zl_b[:, base + wi * 128:base + wi * 128 + 64]).T)
            m["h%s2_%d" % (nm, d)] = np.ascontiguousarray(o(zl_b[:, base + wi * 128 + 64:base + wi * 128 + 128]).T)
    return m


def run_MB(zb, zl, prm, l):
    B, S, _ = zb.shape
    nc = build_MB(S)
    maps = [mb_inputs(zb[c // 4], zl[c // 4], prm, l, c % 4) for c in range(B * 4)]
    res = run_bass_kernel_spmd(nc, maps, core_ids=list(range(B * 4)))
    outs = [np.zeros((B, S, 256), np.float32) for _ in range(4)]
    for c in range(B * 4):
        b, h = c // 4, c % 4
        r = res.results[c]
        outs[0][b, :, h * 64:(h + 1) * 64] = r["y"][0]
        outs[1][b, :, h * 64:(h + 1) * 64] = r["y"][1][::-1]
        outs[2][b, :, h * 64:(h + 1) * 64] = r["bonus"][0]
        outs[3][b, :, h * 64:(h + 1) * 64] = r["bonus"][1][::-1]
    return outs


def build_F1(TC):
    P = Prog(); nc = P.nc
    x = P.dram("x", [TC, 1024]); g = P.dram("g", [1, 1024]); ident_d = P.dram("ident", [128, 128])
    br = {n: P.dram(n, [TC, 256]) for n in ("ya", "yd", "rwf", "rwb", "bnf", "bnb", "cf", "cb", "u")}
    hg = P.dram("hg", [TC, 128])
    wgate = P.dram("wgate", [1024, 4096]); gate_b = P.dram("gate_b", [1, 4096]); w_branch = P.dram("w_branch", [4, 256, 1024]); w_out = P.dram("w_out", [1024, 1024])
    glu_w = P.dram("glu_w", [256, 512]); glu_b = P.dram("glu_b", [1, 512]); g2 = P.dram("g2", [128, 256])
    vecs = P.dram("vecs", [3, 256])
    y = P.dram("y", [TC, 1024], kind="ExternalOutput")
    ident = load_ident32(P, nc, ident_d)
    def bc(name, src, n):
        t = P.sb(name, [128, n]); P.dma(t[:], src.partition_broadcast(128), w=[name]); return t
    gbc = bc("gbc", g[0:1, :], 1024); gb_bc = bc("gb_bc", gate_b[0:1, :], 4096); glub_bc = bc("glub_bc", glu_b[0:1, :], 512)
    lnw_bc = bc("lnw_bc", vecs[0:1, :], 256); lnb_bc = bc("lnb_bc", vecs[1:2, :], 256); d_bc = bc("d_bc", vecs[2:3, :], 256)
    Wg = P.sb("Wg", [128, 8, 4096], BF16); Wbr = P.sb("Wbr", [128, 8, 1024], BF16); Wo = P.sb("Wo", [128, 8, 1024], BF16)
    glub = P.sb("glub", [128, 2, 512], BF16); g2b = P.sb("g2b", [128, 256], BF16)
    stg = [P.sb("stg%d" % i, [128, 1024]) for i in range(2)]
    si = 0
    def load_cast(dst, src, n, tok):
        nonlocal si
        b = si % 2; si += 1
        P.dma(stg[b][:, 0:n], src, w=["stg%d" % b])
        if b == 0:
            P.V(lambda: nc.vector.tensor_copy(out=dst, in_=stg[b][:, 0:n]), r=["stg%d" % b], wj=[tok])
        else:
            P.G(lambda: nc.gpsimd.tensor_copy(out=dst, in_=stg[b][:, 0:n]), r=["stg%d" % b], wj=[tok])
    for kc in range(8):
        rows = slice(kc * 128, (kc + 1) * 128)
        for q in range(4):
            load_cast(Wg[:, kc, q * 1024:(q + 1) * 1024], wgate[rows, q * 1024:(q + 1) * 1024], 1024, "Wg")
        load_cast(Wo[:, kc, :], w_out[rows, :], 1024, "Wo")
        load_cast(Wbr[:, kc, :], w_branch[kc // 2, (kc % 2) * 128:(kc % 2 + 1) * 128, :], 1024, "Wbr")
    for kc in range(2):
        load_cast(glub[:, kc, :], glu_w[kc * 128:(kc + 1) * 128, :], 512, "glub")
    load_cast(g2b[:], g2[:, :], 256, "g2b")
    xt = [P.sb("xt%d" % i, [128, 1024]) for i in range(2)]
    xn32 = P.sb("xn32", [128, 1024]); ss = P.sb("ss", [128, 1])
    xnT = P.sb("xnT", [128, 8, 128], BF16); mT = P.sb("mT", [128, 8, 128], BF16)
    gate = P.sb("gate", [128, 1024]); merged = P.sb("merged", [128, 1024]); tmpm = P.sb("tmpm", [128, 1024])
    inp = {n: P.sb("i_" + n, [128, 256]) for n in br}
    hgt = P.sb("hgt", [128, 128]); hgT = P.sb("hgT", [128, 128], BF16)
    ys = P.sb("ys", [128, 256]); sqh = P.sb("sqh", [128, 64]); st = P.sb("st", [128, 12])
    ybf = P.sb("ybf", [128, 256]); ycf = P.sb("ycf", [128, 256]); t256 = P.sb("t256", [128, 256]); h512 = P.sb("h512", [128, 512])
    yT = P.sb("yT", [128, 2, 128], BF16)
    pst = P.ps("pst", [128, 8, 128]); pbr = P.ps("pbr", [128, 1024]); pg = P.ps("pg", [128, 1024]); po = P.ps("po", [128, 1024])
    NT = TC // 128
    for t in range(NT):
        b = t % 2; r0 = t * 128; xk = "xt%d" % b
        P.dma(xt[b][:], x[r0:r0 + 128, :], w=[xk])
        for n in br:
            P.dma(inp[n][:], br[n][r0:r0 + 128, :], w=["i_" + n])
        P.dma(hgt[:], hg[r0:r0 + 128, :], w=["hgt"])
        P.A(lambda: nc.scalar.activation(out=xn32[:], in_=xt[b][:], func=AF.Square, accum_out=ss[:]), r=[xk], w=["xn32", "ss"])
        P.V(lambda: nc.vector.tensor_scalar(out=ss[:], in0=ss[:], scalar1=1.0 / 1024, scalar2=1e-6, op0=ALU.mult, op1=ALU.add), r=["ss"], w=["ss"])
        P.A(lambda: nc.scalar.sqrt(out=ss[:], in_=ss[:]), r=["ss"], w=["ss"])
        P.V(lambda: nc.vector.reciprocal(out=ss[:], in_=ss[:]), r=["ss"], w=["ss"])
        P.V(lambda: nc.vector.scalar_tensor_tensor(out=xn32[:], in0=xt[b][:], scalar=ss[:], in1=gbc[:], op0=ALU.mult, op1=ALU.mult), r=[xk, "ss", "gbc"], w=["xn32"])
        for kc in range(8):
            P.T(lambda: nc.tensor.transpose(pst[:, kc, :], xn32[:, kc * 128:(kc + 1) * 128], ident[:]), r=["xn32", "ident32"], w=["pst"] if kc == 0 else [], wj=[] if kc == 0 else ["pst"])
        P.V(lambda: nc.vector.tensor_copy(out=xnT[:], in_=pst[:]), r=["pst"], w=["xnT"])
        P.V(lambda: nc.vector.tensor_tensor(out=ys[:], in0=inp["rwf"][:], in1=inp["rwb"][:], op=ALU.add), r=["i_rwf", "i_rwb"], w=["ys"])
        P.V(lambda: nc.vector.tensor_reduce(out=st[:, 0:4], in_=ys[:].rearrange("p (h n) -> p h n", h=4), axis=AX.X, op=ALU.add), r=["ys"], w=["st"])
        for h in range(4):
            P.A(lambda: nc.scalar.activation(out=sqh[:], in_=ys[:, h * 64:(h + 1) * 64], func=AF.Square, accum_out=st[:, 4 + h:5 + h]), r=["ys", "st"], w=["sqh", "st"])
        P.V(lambda: nc.vector.tensor_scalar(out=st[:, 0:8], in0=st[:, 0:8], scalar1=1.0 / 64, scalar2=None, op0=ALU.mult), r=["st"], w=["st"])
        P.V(lambda: nc.vector.tensor_tensor(out=st[:, 8:12], in0=st[:, 0:4], in1=st[:, 0:4], op=ALU.mult), r=["st"], w=["st"])
        P.V(lambda: nc.vector.tensor_tensor(out=st[:, 4:8], in0=st[:, 4:8], in1=st[:, 8:12], op=ALU.subtract), r=["st"], w=["st"])
        P.V(lambda: nc.vector.tensor_scalar(out=st[:, 4:8], in0=st[:, 4:8], scalar1=64e-5, scalar2=None, op0=ALU.add), r=["st"], w=["st"])
        P.A(lambda: nc.scalar.sqrt(out=st[:, 4:8], in_=st[:, 4:8]), r=["st"], w=["st"])
        P.V(lambda: nc.vector.reciprocal(out=st[:, 4:8], in_=st[:, 4:8]), r=["st"], w=["st"])
        for h in range(4):
            P.V(lambda: nc.vector.tensor_scalar(out=ybf[:, h * 64:(h + 1) * 64], in0=ys[:, h * 64:(h + 1) * 64], scalar1=st[:, h:h + 1], scalar2=st[:, 4 + h:5 + h], op0=ALU.subtract, op1=ALU.mult),
                r=["ys", "st"], w=["ybf"] if h == 0 else [], wj=[] if h == 0 else ["ybf"])
        P.V(lambda: nc.vector.tensor_tensor(out=ybf[:], in0=ybf[:], in1=lnw_bc[:], op=ALU.mult), r=["ybf", "lnw_bc"], w=["ybf"])
        P.V(lambda: nc.vector.tensor_tensor(out=ybf[:], in0=ybf[:], in1=lnb_bc[:], op=ALU.add), r=["ybf", "lnb_bc"], w=["ybf"])
        P.V(lambda: nc.vector.tensor_tensor(out=ybf[:], in0=ybf[:], in1=inp["bnf"][:], op=ALU.add), r=["ybf", "i_bnf"], w=["ybf"])
        P.V(lambda: nc.vector.tensor_tensor(out=ybf[:], in0=ybf[:], in1=inp["bnb"][:], op=ALU.add), r=["ybf", "i_bnb"], w=["ybf"])
        P.A(lambda: nc.scalar.activation(out=hgt[:], in_=hgt[:], func=AF.Sigmoid), r=["hgt"], w=["hgt"])
        P.T(lambda: nc.tensor.transpose(po[:, 0:128], hgt[:], ident[:]), r=["hgt", "ident32"], w=["po"])
        P.V(lambda: nc.vector.tensor_copy(out=hgT[:], in_=po[:, 0:128]), r=["po"], w=["hgT"])
        P.T(lambda: nc.tensor.matmul(po[:, 0:256], lhsT=hgT[:], rhs=g2b[:], start=True, stop=True), r=["hgT", "g2b"], w=["po"])
        P.V(lambda: nc.vector.tensor_tensor(out=ybf[:], in0=ybf[:], in1=po[:, 0:256], op=ALU.mult), r=["ybf", "po"], w=["ybf"])
        P.V(lambda: nc.vector.tensor_tensor(out=ycf[:], in0=inp["u"][:], in1=d_bc[:], op=ALU.mult), r=["i_u", "d_bc"], w=["ycf"])
        P.V(lambda: nc.vector.tensor_tensor(out=ycf[:], in0=ycf[:], in1=inp["cf"][:], op=ALU.add), r=["ycf", "i_cf"], w=["ycf"])
        P.V(lambda: nc.vector.tensor_tensor(out=ycf[:], in0=ycf[:], in1=inp["cb"][:], op=ALU.add), r=["ycf", "i_cb"], w=["ycf"])
        P.V(lambda: nc.vector.tensor_tensor(out=t256[:], in0=ycf[:], in1=ycf[:], op=ALU.mult), r=["ycf"], w=["t256"])
        P.V(lambda: nc.vector.tensor_scalar(out=t256[:], in0=t256[:], scalar1=0.044715, scalar2=1.0, op0=ALU.mult, op1=ALU.add), r=["t256"], w=["t256"])
        P.V(lambda: nc.vector.tensor_tensor(out=t256[:], in0=t256[:], in1=ycf[:], op=ALU.mult), r=["t256", "ycf"], w=["t256"])
        P.A(lambda: nc.scalar.activation(out=t256[:], in_=t256[:], func=AF.Sigmoid, scale=1.5957691216057308), r=["t256"], w=["t256"])
        P.V(lambda: nc.vector.tensor_tensor(out=ycf[:], in0=ycf[:], in1=t256[:], op=ALU.mult), r=["ycf", "t256"], w=["ycf"])
        for kc in range(2):
            P.T(lambda: nc.tensor.transpose(pst[:, kc, :], ycf[:, kc * 128:(kc + 1) * 128], ident[:]), r=["ycf", "ident32"], w=["pst"] if kc == 0 else [], wj=[] if kc == 0 else ["pst"])
        P.V(lambda: nc.vector.tensor_copy(out=yT[:], in_=pst[:, 0:2, :]), r=["pst"], w=["yT"])
        for kc in range(2):
            P.T(lambda: nc.tensor.matmul(po[:, 0:512], lhsT=yT[:, kc, :], rhs=glub[:, kc, :], start=(kc == 0), stop=(kc == 1)), r=["yT", "glub"], w=["po"] if kc == 0 else [], wj=[] if kc == 0 else ["po"])
        P.V(lambda: nc.vector.tensor_tensor(out=h512[:], in0=po[:, 0:512], in1=glub_bc[:], op=ALU.add), r=["po", "glub_bc"], w=["h512"])
        P.A(lambda: nc.scalar.activation(out=t256[:], in_=h512[:, 256:512], func=AF.Sigmoid), r=["h512"], w=["t256"])
        P.V(lambda: nc.vector.tensor_tensor(out=ycf[:], in0=h512[:, 0:256], in1=t256[:], op=ALU.mult), r=["h512", "t256"], w=["ycf"])
        srcs = [(inp["ya"], "i_ya"), (ybf, "ybf"), (ycf, "ycf"), (inp["yd"], "i_yd")]
        for i, (src, stok) in enumerate(srcs):
            for kc in range(2):
                P.T(lambda: nc.tensor.transpose(pst[:, kc, :], src[:, kc * 128:(kc + 1) * 128], ident[:]), r=[stok, "ident32"], w=["pst"] if kc == 0 else [], wj=[] if kc == 0 else ["pst"])
            P.V(lambda: nc.vector.tensor_copy(out=yT[:], in_=pst[:, 0:2, :]), r=["pst"], w=["yT"])
            for half in range(2):
                cs = slice(half * 512, (half + 1) * 512)
                for kc in range(2):
                    P.T(lambda: nc.tensor.matmul(pbr[:, cs], lhsT=yT[:, kc, :], rhs=Wbr[:, i * 2 + kc, cs], start=(kc == 0), stop=(kc == 1)),
                        r=["yT", "Wbr"], w=["pbr"] if (kc == 0 and half == 0) else [], wj=[] if (kc == 0 and half == 0) else ["pbr"])
                for kc in range(8):
                    P.T(lambda: nc.tensor.matmul(pg[:, cs], lhsT=xnT[:, kc, :], rhs=Wg[:, kc, i * 1024 + half * 512:i * 1024 + (half + 1) * 512], start=(kc == 0), stop=(kc == 7)),
                        r=["xnT", "Wg"], w=["pg"] if (kc == 0 and half == 0) else [], wj=[] if (kc == 0 and half == 0) else ["pg"])
            P.V(lambda: nc.vector.tensor_tensor(out=gate[:], in0=pg[:], in1=gb_bc[:, i * 1024:(i + 1) * 1024], op=ALU.add), r=["pg", "gb_bc"], w=["gate"])
            P.A(lambda: nc.scalar.activation(out=gate[:], in_=gate[:], func=AF.Sigmoid), r=["gate"], w=["gate"])
            if i == 0:
                P.V(lambda: nc.vector.tensor_tensor(out=merged[:], in0=pbr[:], in1=gate[:], op=ALU.mult), r=["pbr", "gate"], w=["merged"])
            else:
                P.V(lambda: nc.vector.tensor_tensor(out=tmpm[:], in0=pbr[:], in1=gate[:], op=ALU.mult), r=["pbr", "gate"], w=["tmpm"])
                P.G(lambda: nc.gpsimd.tensor_tensor(out=merged[:], in0=merged[:], in1=tmpm[:], op=ALU.add), r=["merged", "tmpm"], w=["merged"])
        for kc in range(8):
            P.T(lambda: nc.tensor.transpose(pst[:, kc, :], merged[:, kc * 128:(kc + 1) * 128], ident[:]), r=["merged", "ident32"], w=["pst"] if kc == 0 else [], wj=[] if kc == 0 else ["pst"])
        P.V(lambda: nc.vector.tensor_copy(out=mT[:], in_=pst[:]), r=["pst"], w=["mT"])
        for half in range(2):
            cs = slice(half * 512, (half + 1) * 512)
            for kc in range(8):
                P.T(lambda: nc.tensor.matmul(po[:, cs], lhsT=mT[:, kc, :], rhs=Wo[:, kc, cs], start=(kc == 0), stop=(kc == 7)),
                    r=["mT", "Wo"], w=["po"] if (kc == 0 and half == 0) else [], wj=[] if (kc == 0 and half == 0) else ["po"])
        P.V(lambda: nc.vector.tensor_tensor(out=xt[b][:], in0=xt[b][:], in1=po[:], op=ALU.add), r=[xk, "po"], w=[xk])
        P.dma(y[r0:r0 + 128, :], xt[b][:], r=[xk], wj=["y"], key=xk)
    P.finish(); P.close()
    return nc


def run_F1(xf, brs, hg, prm, l, ncores=8):
    T = xf.shape[0]; TC = T // ncores
    nc = build_F1(TC)
    com = {"g": prm["norm_mix_g"][l][None, :], "ident": np.eye(128, dtype=np.float32),
           "wgate": np.ascontiguousarray(prm["w_in"][l][:, 2304:]), "gate_b": prm["gate_b"][l].reshape(1, 4096), "w_branch": prm["w_branch"][l], "w_out": prm["w_out"][l],
           "glu_w": prm["s5_glu_w"][l], "glu_b": prm["s5_glu_b"][l][None, :], "g2": prm["rwkv_g2"][l],
           "vecs": np.stack([prm["rwkv_ln_w"][l], prm["rwkv_ln_b"][l], prm["s5_d"][l]])}
    maps = []
    for c in range(ncores):
        sl = slice(c * TC, (c + 1) * TC)
        m = dict(com, x=xf[sl], hg=np.ascontiguousarray(hg[sl]))
        for n, a in brs.items():
            m[n] = np.ascontiguousarray(a[sl])
        maps.append(m)
    res = run_bass_kernel_spmd(nc, maps, core_ids=list(range(ncores)))
    return np.concatenate([r["y"] for r in res.results], axis=0)


def kernel(**inp):
    prm = {k: np.ascontiguousarray(np.asarray(v, dtype=np.float32)) for k, v in inp.items()}
    x = prm["x"]
    B, S, D = x.shape
    xf = np.ascontiguousarray(x.reshape(B * S, D))
    f2 = lambda a: np.ascontiguousarray(a.reshape(B * S, a.shape[-1]))
    for l in range(2):
        zP = run_P(xf, prm, l)
        z = zP.reshape(B, S, NZ)
        ya = run_MA(z[:, :, 0:768], prm, l)
        rwf, rwb, bnf, bnb = run_MB(z[:, :, 768:1536], z[:, :, 2304:2944], prm, l)
        cf, cb = run_MC(z[:, :, 1536:1792], prm, l)
        yd = run_MD(z[:, :, 1792:2048], z[:, :, 2048:2176], z[:, :, 2176:2304], prm, l)
        brs = {"ya": f2(ya), "yd": f2(yd), "rwf": f2(rwf), "rwb": f2(rwb), "bnf": f2(bnf), "bnb": f2(bnb),
               "cf": f2(cf), "cb": f2(cb), "u": f2(z[:, :, 1536:1792])}
        xmid = run_F1(xf, brs, np.ascontiguousarray(zP[:, 2816:2944]), prm, l)
        del zP, z, brs
        xf = run_F2(xmid, prm, l)
    return xf.reshape(B, S, D).astype(np.float32)
```

```python
import os
from concourse.bass_utils import run_bass_kernel_spmd

from contextlib import ExitStack
import numpy as np
import concourse.bass as bass
import concourse.mybir as mybir

F32 = mybir.dt.float32
BF16 = mybir.dt.bfloat16
I32 = mybir.dt.int32
ALU = mybir.AluOpType
AF = mybir.ActivationFunctionType
AX = mybir.AxisListType

COMPUTE = ("tensor", "vector", "scalar", "gpsimd")


class Prog:
    def __init__(self, same_engine_sync=True):
        self.nc = bass.Bass("TRN2", target_bir_lowering=False)
        self.es = ExitStack()
        self.eng = {"tensor": self.nc.tensor, "vector": self.nc.vector, "scalar": self.nc.scalar,
                    "gpsimd": self.nc.gpsimd, "sync": self.nc.sync}
        self.esem = {e: self.es.enter_context(self.nc.semaphore("s_" + e)) for e in COMPUTE}
        self.ecount = {e: 0 for e in COMPUTE}
        self.waited = {}
        self.tsem = {}
        self.writers = {}
        self.readers = {}
        self.gen = {}
        self.ses = True
        self.excl = {}
        self.same_engine_sync = same_engine_sync
        self.n_inst = 0

    def dram(self, name, shape, dtype=F32, kind="ExternalInput"):
        return self.nc.dram_tensor(name, list(shape), dtype, kind=kind).ap()

    def sb(self, name, shape, dtype=F32):
        return self.es.enter_context(self.nc.sbuf_tensor("sb_" + name, list(shape), dtype))

    def ps(self, name, shape, dtype=F32):
        return self.es.enter_context(self.nc.psum_tensor("ps_" + name, list(shape), dtype))

    def _wait(self, eng, ev):
        kind, a, b = ev
        if kind == "e":
            if a == eng and not (self.same_engine_sync and self.ses and eng != "tensor"):
                return
            sem, val, key = self.esem[a], b, (eng, "e" + a)
        else:
            sem, val, key = self.tsem[a][0], b, (eng, "d", a)
        if self.waited.get(key, -1) >= val:
            return
        self.waited[key] = val
        self.eng[eng].wait_ge(sem, val)

    def _deps(self, eng, r, w, wj=()):
        evs = []
        for t in r:
            evs += self.writers.get(t, [])
        for t in w:
            prior = list(self.writers.get(t, ())) + list(self.readers.get(t, ()))
            evs += prior
            self.gen[t] = prior
        for t in wj:
            evs += self.gen.get(t, [])
            evs += self.readers.get(t, [])
            if t in self.excl:
                evs.append(self.excl[t])
        mx = {}
        for (k, a, b) in evs:
            if mx.get((k, a), -1) < b:
                mx[(k, a)] = b
        for (k, a), b in mx.items():
            self._wait(eng, (k, a, b))

    def _commit(self, ev, r, w, wj=()):
        for t in r:
            self.readers.setdefault(t, []).append(ev)
        for t in w:
            self.writers[t] = [ev]
            self.readers[t] = []
            self.excl[t] = ev
        for t in wj:
            self.writers.setdefault(t, []).append(ev)

    def op(self, eng, fn, r=(), w=(), wj=()):
        self._deps(eng, r, w, wj)
        ins = fn()
        self.ecount[eng] += 1
        ins.then_inc(self.esem[eng], 1)
        self._commit(("e", eng, self.ecount[eng]), r, w, wj)
        self.n_inst += 1
        return ins

    def T(self, fn, r=(), w=(), wj=()): return self.op("tensor", fn, r, w, wj)
    def V(self, fn, r=(), w=(), wj=()): return self.op("vector", fn, r, w, wj)
    def A(self, fn, r=(), w=(), wj=()): return self.op("scalar", fn, r, w, wj)
    def G(self, fn, r=(), w=(), wj=()): return self.op("gpsimd", fn, r, w, wj)

    def dma(self, out, in_, r=(), w=(), wj=(), q="sync", key=None, **kw):
        if key is None:
            key = (list(w) + list(wj) + list(r))[0]
        self._deps(q, r, w, wj)
        if key not in self.tsem:
            self.tsem[key] = [self.es.enter_context(self.nc.semaphore("d%d" % len(self.tsem))), 0]
        ent = self.tsem[key]
        ent[1] += 16
        self.eng[q].dma_start(out=out, in_=in_, **kw).then_inc(ent[0], 16)
        self._commit(("d", key, ent[1]), r, w, wj)
        self.n_inst += 1

    def finish(self, q="sync"):
        for key, ent in self.tsem.items():
            self._wait(q, ("d", key, ent[1]))
        return self.nc

    def close(self):
        self.es.close()


class NS:
    def __init__(self, P, pfx):
        self.P = P; self.pfx = pfx; self.nc = P.nc; self.es = ExitStack()

    @property
    def ses(self): return self.P.ses

    @ses.setter
    def ses(self, v): self.P.ses = v

    def _t(self, toks): return [self.pfx + x for x in toks]

    def dram(self, name, shape, dtype=F32, kind="ExternalInput"):
        return self.P.dram(self.pfx + name, shape, dtype, kind)

    def sb(self, name, shape, dtype=F32):
        return self.es.enter_context(self.nc.sbuf_tensor("sb_" + self.pfx + name, list(shape), dtype))

    def ps(self, name, shape, dtype=F32):
        return self.es.enter_context(self.nc.psum_tensor("ps_" + self.pfx + name, list(shape), dtype))

    def op(self, eng, fn, r=(), w=(), wj=()): return self.P.op(eng, fn, self._t(r), self._t(w), self._t(wj))
    def T(self, fn, r=(), w=(), wj=()): return self.op("tensor", fn, r, w, wj)
    def V(self, fn, r=(), w=(), wj=()): return self.op("vector", fn, r, w, wj)
    def A(self, fn, r=(), w=(), wj=()): return self.op("scalar", fn, r, w, wj)
    def G(self, fn, r=(), w=(), wj=()): return self.op("gpsimd", fn, r, w, wj)

    def dma(self, out, in_, r=(), w=(), wj=(), q="sync", key=None, **kw):
        return self.P.dma(out, in_, self._t(r), self._t(w), self._t(wj), q, None if key is None else self.pfx + key, **kw)

    def close_ns(self, barrier=True):
        if barrier:
            P = self.P
            evs = []
            for d_ in (P.writers, P.readers):
                for t, lst in d_.items():
                    if isinstance(t, str) and t.startswith(self.pfx):
                        evs += lst
            mx = {}
            for (k, a, b) in evs:
                if mx.get((k, a), -1) < b:
                    mx[(k, a)] = b
            for eng in list(COMPUTE) + ["sync"]:
                for (k, a), b in mx.items():
                    P._wait(eng, (k, a, b))
        self.es.close()


NZ = 2944


def rmsnorm_tile(P, nc, xt, xtok, gbc, gtok, out_bf, otok, scr, eps=1e-6, D=1024):
    sq, ss = scr["sq"], scr["ss"]
    P.A(lambda: nc.scalar.activation(out=sq[:], in_=xt, func=AF.Square, accum_out=ss[:]), r=[xtok], w=["sq", "ss"])
    P.V(lambda: nc.vector.tensor_scalar(out=ss[:], in0=ss[:], scalar1=1.0 / D, scalar2=eps, op0=ALU.mult, op1=ALU.add), r=["ss"], w=["ss"])
    P.A(lambda: nc.scalar.sqrt(out=ss[:], in_=ss[:]), r=["ss"], w=["ss"])
    P.V(lambda: nc.vector.reciprocal(out=ss[:], in_=ss[:]), r=["ss"], w=["ss"])
    P.V(lambda: nc.vector.scalar_tensor_tensor(out=out_bf, in0=xt, scalar=ss[:], in1=gbc, op0=ALU.mult, op1=ALU.mult),
        r=[xtok, "ss", gtok], w=[otok])


def load_ident(P, nc, ident_d):
    i32 = P.sb("ident32", [128, 128]); ib = P.sb("identb", [128, 128], BF16)
    P.dma(i32[:], ident_d[:, :], w=["ident32"])
    P.V(lambda: nc.vector.tensor_copy(out=ib[:], in_=i32[:]), r=["ident32"], w=["ident"])
    return ib


def build_P(TC):
    P = Prog(); nc = P.nc
    x = P.dram("x", [TC, 1024]); g = P.dram("g", [1, 1024]); ident_d = P.dram("ident", [128, 128])
    w_mix = P.dram("w_mix", [1024, 2304]); w1 = P.dram("w1", [2, 1024, 64]); a1 = P.dram("a1", [2, 1024, 64])
    g1 = P.dram("g1", [1024, 128]); mu = P.dram("mu", [2, 1024])
    z = P.dram("z", [TC, NZ], kind="ExternalOutput")
    Wsb = P.sb("Wsb", [128, 8, NZ], BF16)
    stg = [P.sb("stg%d" % i, [128, NZ]) for i in range(2)]
    gbc = P.sb("gbc", [128, 1024]); mut = P.sb("mut", [128, 2, 8]); omu = P.sb("omu", [128, 2, 8])
    ident = load_ident(P, nc, ident_d)
    P.dma(gbc[:], g[0:1, :].partition_broadcast(128), w=["gbc"])
    for d in range(2):
        P.dma(mut[:, d, :], mu[d].rearrange("(c p) -> p c", p=128), wj=["mut"], allow_slow_non_contiguous=True)
    P.V(lambda: nc.vector.tensor_scalar(out=omu[:], in0=mut[:], scalar1=-1.0, scalar2=1.0, op0=ALU.mult, op1=ALU.add), r=["mut"], w=["omu"])
    for kc in range(8):
        s = stg[kc % 2]; tk = "stg%d" % (kc % 2)
        rows = slice(kc * 128, (kc + 1) * 128)
        P.dma(s[:, 0:2304], w_mix[rows, :], w=[tk])
        for d in range(2):
            P.dma(s[:, 2304 + d * 256:2304 + d * 256 + 64], w1[d, rows, :], wj=[tk])
            P.dma(s[:, 2304 + d * 256 + 128:2304 + d * 256 + 192], a1[d, rows, :], wj=[tk])
        P.dma(s[:, 2816:2944], g1[rows, :], wj=[tk])
        P.A(lambda: nc.scalar.copy(out=Wsb[:, kc, 0:1152], in_=s[:, 0:1152]), r=[tk], wj=["Wsb"])
        P.V(lambda: nc.vector.tensor_copy(out=Wsb[:, kc, 1152:2304], in_=s[:, 1152:2304]), r=[tk], wj=["Wsb"])
        for d in range(2):
            for wa in range(2):
                b0 = 2304 + d * 256 + wa * 128
                P.V(lambda: nc.vector.tensor_scalar(out=Wsb[:, kc, b0 + 64:b0 + 128], in0=s[:, b0:b0 + 64], scalar1=mut[:, d, kc:kc + 1], scalar2=None, op0=ALU.mult),
                    r=[tk, "mut"], wj=["Wsb"])
                P.V(lambda: nc.vector.tensor_scalar(out=Wsb[:, kc, b0:b0 + 64], in0=s[:, b0:b0 + 64], scalar1=omu[:, d, kc:kc + 1], scalar2=None, op0=ALU.mult),
                    r=[tk, "omu"], wj=["Wsb"])
        P.V(lambda: nc.vector.tensor_copy(out=Wsb[:, kc, 2816:2944], in_=s[:, 2816:2944]), r=[tk], wj=["Wsb"])
    xt = [P.sb("xt%d" % i, [128, 1024]) for i in range(2)]
    xnb = P.sb("xnb", [128, 1024], BF16)
    xnT = P.sb("xnT", [128, 8, 128], BF16)
    zt = [P.sb("zt%d" % i, [128, NZ]) for i in range(2)]
    scr = {"sq": P.sb("sq", [128, 1024]), "ss": P.sb("ss", [128, 1])}
    pst = P.ps("pst", [128, 8, 128], BF16)
    psz = [P.ps("psz%d" % i, [128, 512]) for i in range(4)]
    NT = TC // 128
    P.dma(xt[0][:], x[0:128, :], w=["xt0"])
    ci = 0
    for t in range(NT):
        b = t % 2
        if t + 1 < NT:
            P.dma(xt[1 - b][:], x[(t + 1) * 128:(t + 2) * 128, :], w=["xt%d" % (1 - b)])
        rmsnorm_tile(P, nc, xt[b][:], "xt%d" % b, gbc[:], "gbc", xnb[:], "xnb", scr)
        for kc in range(8):
            P.T(lambda: nc.tensor.transpose(pst[:, kc, :], xnb[:, kc * 128:(kc + 1) * 128], ident[:]), r=["xnb", "ident"],
                w=["pst"] if kc == 0 else [], wj=[] if kc == 0 else ["pst"])
        P.V(lambda: nc.vector.tensor_copy(out=xnT[:], in_=pst[:]), r=["pst"], w=["xnT"])
        for n0 in range(0, NZ, 512):
            n1 = min(NZ, n0 + 512); pz = psz[ci % 4]; pk = "psz%d" % (ci % 4)
            for kc in range(8):
                P.T(lambda: nc.tensor.matmul(pz[:, 0:n1 - n0], lhsT=xnT[:, kc, :], rhs=Wsb[:, kc, n0:n1], start=(kc == 0), stop=(kc == 7)),
                    r=["xnT", "Wsb"], w=[pk] if kc == 0 else [], wj=[] if kc == 0 else [pk])
            first = (n0 == 0)
            if ci % 2 == 0:
                P.A(lambda: nc.scalar.copy(out=zt[b][:, n0:n1], in_=pz[:, 0:n1 - n0]), r=[pk], w=["zt%d" % b] if first else [], wj=[] if first else ["zt%d" % b])
            else:
                P.V(lambda: nc.vector.tensor_copy(out=zt[b][:, n0:n1], in_=pz[:, 0:n1 - n0]), r=[pk], w=["zt%d" % b] if first else [], wj=[] if first else ["zt%d" % b])
            ci += 1
        P.dma(z[t * 128:(t + 1) * 128, :], zt[b][:], r=["zt%d" % b], wj=["z"], key="zt%d" % b)
    P.finish(); P.close()
    return nc


def run_P(xf, prm, l, ncores=8):
    T = xf.shape[0]; TC = T // ncores
    nc = build_P(TC)
    com = {"g": prm["norm_mix_g"][l][None, :], "ident": np.eye(128, dtype=np.float32),
           "w_mix": np.ascontiguousarray(prm["w_in"][l][:, :2304]), "w1": prm["rwkv_w1"][l], "a1": prm["rwkv_a1"][l],
           "g1": prm["rwkv_g1"][l], "mu": prm["rwkv_mu_x"][l]}
    maps = [dict(com, x=xf[c * TC:(c + 1) * TC]) for c in range(ncores)]
    res = run_bass_kernel_spmd(nc, maps, core_ids=list(range(ncores)))
    return np.concatenate([r["z"] for r in res.results], axis=0)


def load_ident32(P, nc, ident_d):
    i32 = P.sb("ident32", [128, 128])
    P.dma(i32[:], ident_d[:, :], w=["ident32"])
    return i32


import os
DBG = int(os.environ.get('DBG', '0'))


def build_F2(TC, F, E, moe, final):
    P = Prog(); nc = P.nc
    x = P.dram("x", [TC, 1024]); g = P.dram("g", [1, 1024]); ident_d = P.dram("ident", [128, 128])
    wg = P.dram("wg", [E, 1024, F]); wu = P.dram("wu", [E, 1024, F]); wd = P.dram("wd", [E, F, 1024])
    if moe:
        router = P.dram("router", [1024, 8])
    if final:
        gf = P.dram("gf", [1, 1024])
    y = P.dram("y", [TC, 1024], kind="ExternalOutput")
    ST = min(TC, 1024); NTT = ST // 128; NST = TC // ST; NTH = max(1, ST // 512); TH = min(512, ST)
    NFC = F // 128
    GC = 11 if NFC % 11 == 0 else 7
    NG = NFC // GC
    ident = load_ident32(P, nc, ident_d)
    gbc = P.sb("gbc", [128, 1024]); P.dma(gbc[:], g[0:1, :].partition_broadcast(128), w=["gbc"])
    if final:
        gfbc = P.sb("gfbc", [128, 1024]); P.dma(gfbc[:], gf[0:1, :].partition_broadcast(128), w=["gfbc"])
    if moe:
        rsb = P.sb("rsb", [128, 8, 8])
        if not (DBG & 8):
            P.dma(rsb[:], router.rearrange("(kc p) e -> p kc e", p=128), w=["rsb"])
        xnT32 = P.sb("xnT32", [128, 8, 128])
        wt = P.sb("wt", [128, NTT, 8]); lg = P.sb("lg", [128, 8]); eq1 = P.sb("eq1", [128, 8]); eq2 = P.sb("eq2", [128, 8])
        m1 = P.sb("m1", [128, 1]); m2 = P.sb("m2", [128, 1])
    acc = P.sb("acc", [128, NTT, 1024])
    xnT = P.sb("xnT", [128, 8, ST], BF16)
    xn32 = P.sb("xn32", [128, 1024]); sq = P.sb("sq", [128, 1024]); ss = P.sb("ss", [128, 1])
    hT = P.sb("hT", [128, GC, ST], BF16)
    wdb = P.sb("wdb", [128, GC, 1024], BF16)
    wds = [P.sb("wds%d" % i, [128, 1024]) for i in range(2)]
    wgs = [P.sb("wgs%d" % i, [128, 8, 128]) for i in range(2)]
    wus = [P.sb("wus%d" % i, [128, 8, 128]) for i in range(2)]
    wgb = [P.sb("wgb%d" % i, [128, 8, 128], BF16) for i in range(2)]
    wub = [P.sb("wub%d" % i, [128, 8, 128], BF16) for i in range(2)]
    sg = [P.sb("sg%d" % i, [128, TH]) for i in range(2)]
    pst = P.ps("pst", [128, 8, 128])
    psg = [P.ps("psg%d" % i, [128, 512]) for i in range(2)]
    psu = [P.ps("psu%d" % i, [128, 512]) for i in range(2)]
    pso = P.ps("pso", [128, 1024])
    wi = 0; di = 0; gi = 0
    for st in range(NST):
        t0 = st * ST
        for tt in range(NTT):
            atok = "acc%d" % tt
            P.dma(acc[:, tt, :], x[t0 + tt * 128:t0 + (tt + 1) * 128, :], w=[atok])
            P.A(lambda: nc.scalar.activation(out=sq[:], in_=acc[:, tt, :], func=AF.Square, accum_out=ss[:]), r=[atok], w=["sq", "ss"])
            P.V(lambda: nc.vector.tensor_scalar(out=ss[:], in0=ss[:], scalar1=1.0 / 1024, scalar2=1e-6, op0=ALU.mult, op1=ALU.add), r=["ss"], w=["ss"])
            P.A(lambda: nc.scalar.sqrt(out=ss[:], in_=ss[:]), r=["ss"], w=["ss"])
            P.V(lambda: nc.vector.reciprocal(out=ss[:], in_=ss[:]), r=["ss"], w=["ss"])
            P.V(lambda: nc.vector.scalar_tensor_tensor(out=xn32[:], in0=acc[:, tt, :], scalar=ss[:], in1=gbc[:], op0=ALU.mult, op1=ALU.mult),
                r=[atok, "ss", "gbc"], w=["xn32"])
            for kc in range(8):
                P.T(lambda: nc.tensor.transpose(pst[:, kc, :], xn32[:, kc * 128:(kc + 1) * 128], ident[:]), r=["xn32", "ident32"],
                    w=["pst"] if kc == 0 else [], wj=[] if kc == 0 else ["pst"])
            P.V(lambda: nc.vector.tensor_copy(out=xnT[:, :, tt * 128:(tt + 1) * 128], in_=pst[:]), r=["pst"], w=["xnT%d" % tt])
            if moe:
                if not (DBG & 16):
                    P.V(lambda: nc.vector.tensor_copy(out=xnT32[:], in_=pst[:]), r=["pst"], w=["xnT32"])
                for kc in range(8 if not (DBG & 1) else 0):
                    P.T(lambda: nc.tensor.matmul(pso[:, 0:8], lhsT=xnT32[:, kc, :], rhs=rsb[:, kc, :], start=(kc == 0), stop=(kc == 7)),
                        r=["xnT32", "rsb"], w=["pso"] if kc == 0 else [], wj=[] if kc == 0 else ["pso"])
                if DBG & 2:
                    P.V(lambda: nc.vector.memset(wt[:, tt, :], 0.5), w=["wt%d" % tt])
                    continue
                if DBG & 1:
                    P.V(lambda: nc.vector.memset(lg[:], 0.5), w=["lg"])
                else:
                    P.V(lambda: nc.vector.tensor_copy(out=lg[:], in_=pso[:, 0:8]), r=["pso"], w=["lg"])
                P.V(lambda: nc.vector.tensor_reduce(out=m1[:], in_=lg[:], axis=AX.X, op=ALU.max), r=["lg"], w=["m1"])
                P.V(lambda: nc.vector.tensor_scalar(out=eq1[:], in0=lg[:], scalar1=m1[:], scalar2=None, op0=ALU.is_equal), r=["lg", "m1"], w=["eq1"])
                P.V(lambda: nc.vector.scalar_tensor_tensor(out=lg[:], in0=eq1[:], scalar=-1e30, in1=lg[:], op0=ALU.mult, op1=ALU.add), r=["eq1", "lg"], w=["lg"])
                P.V(lambda: nc.vector.tensor_reduce(out=m2[:], in_=lg[:], axis=AX.X, op=ALU.max), r=["lg"], w=["m2"])
                P.V(lambda: nc.vector.tensor_scalar(out=eq2[:], in0=lg[:], scalar1=m2[:], scalar2=None, op0=ALU.is_equal), r=["lg", "m2"], w=["eq2"])
                P.V(lambda: nc.vector.tensor_tensor(out=m1[:], in0=m1[:], in1=m2[:], op=ALU.subtract), r=["m1", "m2"], w=["m1"])
                P.A(lambda: nc.scalar.activation(out=m1[:], in_=m1[:], func=AF.Sigmoid), r=["m1"], w=["m1"])
                P.V(lambda: nc.vector.tensor_tensor(out=eq1[:], in0=eq1[:], in1=eq2[:], op=ALU.subtract), r=["eq1", "eq2"], w=["eq1"])
                P.V(lambda: nc.vector.scalar_tensor_tensor(out=wt[:, tt, :], in0=eq1[:], scalar=m1[:], in1=eq2[:], op0=ALU.mult, op1=ALU.add),
                    r=["eq1", "eq2", "m1"], w=["wt%d" % tt])
        xtoks = ["xnT%d" % tt for tt in range(NTT)]
        for e in range(E):
            for grp in range(NG):
                for fci in range(GC):
                    fc = grp * GC + fci; b = wi % 2; wi += 1
                    P.dma(wgs[b][:], wg[e, :, fc * 128:(fc + 1) * 128].rearrange("(kc p) f -> p kc f", p=128), w=["wgs%d" % b])
                    P.dma(wus[b][:], wu[e, :, fc * 128:(fc + 1) * 128].rearrange("(kc p) f -> p kc f", p=128), w=["wus%d" % b])
                    P.G(lambda: nc.gpsimd.tensor_copy(out=wgb[b][:], in_=wgs[b][:]), r=["wgs%d" % b], w=["wgb%d" % b])
                    P.G(lambda: nc.gpsimd.tensor_copy(out=wub[b][:], in_=wus[b][:]), r=["wus%d" % b], w=["wub%d" % b])
                    for th in range(NTH):
                        pb = gi % 2; gi += 1
                        cols = slice(th * TH, (th + 1) * TH)
                        ttk = xtoks[th * (TH // 128):(th + 1) * (TH // 128)]
                        for kc in range(8):
                            P.T(lambda: nc.tensor.matmul(psg[pb][:, 0:TH], lhsT=wgb[b][:, kc, :], rhs=xnT[:, kc, cols], start=(kc == 0), stop=(kc == 7)),
                                r=["wgb%d" % b] + ttk, w=["psg%d" % pb] if kc == 0 else [], wj=[] if kc == 0 else ["psg%d" % pb])
                        for kc in range(8):
                            P.T(lambda: nc.tensor.matmul(psu[pb][:, 0:TH], lhsT=wub[b][:, kc, :], rhs=xnT[:, kc, cols], start=(kc == 0), stop=(kc == 7)),
                                r=["wub%d" % b] + ttk, w=["psu%d" % pb] if kc == 0 else [], wj=[] if kc == 0 else ["psu%d" % pb])
                        P.A(lambda: nc.scalar.activation(out=sg[pb][:], in_=psg[pb][:, 0:TH], func=AF.Silu), r=["psg%d" % pb], w=["sg%d" % pb])
                        P.V(lambda: nc.vector.tensor_tensor(out=hT[:, fci, cols], in0=psu[pb][:, 0:TH], in1=sg[pb][:], op=ALU.mult),
                            r=["psu%d" % pb, "sg%d" % pb], w=["hT%d_%d" % (fci, th)])
                for fci in range(GC):
                    fc = grp * GC + fci; b = di % 2; di += 1
                    P.dma(wds[b][:], wd[e, fc * 128:(fc + 1) * 128, :], w=["wds%d" % b])
                    P.G(lambda: nc.gpsimd.tensor_copy(out=wdb[:, fci, :], in_=wds[b][:]), r=["wds%d" % b], w=["wdb%d" % fci])
                for tt in range(NTT):
                    th = (tt * 128) // TH
                    pso_, ptk = (pso[:], "pso") if tt % 2 == 0 else (pst[:].rearrange("p a b -> p (a b)"), "pst")
                    for half in range(2):
                        for fci in range(GC):
                            P.T(lambda: nc.tensor.matmul(pso_[:, half * 512:(half + 1) * 512], lhsT=hT[:, fci, tt * 128:(tt + 1) * 128],
                                                         rhs=wdb[:, fci, half * 512:(half + 1) * 512], start=(fci == 0), stop=(fci == GC - 1)),
                                r=["hT%d_%d" % (fci, th), "wdb%d" % fci], w=[ptk] if (fci == 0 and half == 0) else [], wj=[] if (fci == 0 and half == 0) else [ptk])
                    atok = "acc%d" % tt
                    if moe and not (DBG & 4):
                        P.V(lambda: nc.vector.scalar_tensor_tensor(out=acc[:, tt, :], in0=pso_, scalar=wt[:, tt, e:e + 1], in1=acc[:, tt, :], op0=ALU.mult, op1=ALU.add),
                            r=[ptk, "wt%d" % tt, atok], w=[atok])
                    else:
                        P.V(lambda: nc.vector.tensor_tensor(out=acc[:, tt, :], in0=pso_, in1=acc[:, tt, :], op=ALU.add), r=[ptk, atok], w=[atok])
        for tt in range(NTT):
            atok = "acc%d" % tt
            if final:
                P.A(lambda: nc.scalar.activation(out=sq[:], in_=acc[:, tt, :], func=AF.Square, accum_out=ss[:]), r=[atok], w=["sq", "ss"])
                P.V(lambda: nc.vector.tensor_scalar(out=ss[:], in0=ss[:], scalar1=1.0 / 1024, scalar2=1e-6, op0=ALU.mult, op1=ALU.add), r=["ss"], w=["ss"])
                P.A(lambda: nc.scalar.sqrt(out=ss[:], in_=ss[:]), r=["ss"], w=["ss"])
                P.V(lambda: nc.vector.reciprocal(out=ss[:], in_=ss[:]), r=["ss"], w=["ss"])
                P.V(lambda: nc.vector.scalar_tensor_tensor(out=acc[:, tt, :], in0=acc[:, tt, :], scalar=ss[:], in1=gfbc[:], op0=ALU.mult, op1=ALU.mult),
                    r=[atok, "ss", "gfbc"], w=[atok])
            P.dma(y[t0 + tt * 128:t0 + (tt + 1) * 128, :], acc[:, tt, :], r=[atok], wj=["y"], key=atok)
    P.finish(); P.close()
    return nc


def run_F2(xf, prm, l, ncores=8):
    T = xf.shape[0]; TC = T // ncores
    moe = (l % 2 == 1); final = (l == 1); i = l // 2
    com = {"g": prm["norm_ffn_g"][l][None, :], "ident": np.eye(128, dtype=np.float32)}
    if moe:
        com.update(wg=prm["moe_w_gate"][i], wu=prm["moe_w_up"][i], wd=prm["moe_w_down"][i], router=prm["moe_router"][i])
        E, F = 8, 3584
    else:
        com.update(wg=prm["dense_w_gate"][i][None], wu=prm["dense_w_up"][i][None], wd=prm["dense_w_down"][i][None])
        E, F = 1, 2816
    if final:
        com["gf"] = prm["final_norm_g"][None, :]
    nc = build_F2(TC, F, E, moe, final)
    maps = [dict(com, x=xf[c * TC:(c + 1) * TC]) for c in range(ncores)]
    res = run_bass_kernel_spmd(nc, maps, core_ids=list(range(ncores)))
    return np.concatenate([r["y"] for r in res.results], axis=0)


def prep_qk(P, nc, src, dstT, PADC, S, nrot, cos_d, sin_d, ident, scale, norm_g, pfx, ps_t):
    NT = S // 128
    xt = [P.sb(pfx + "x%d" % i, [128, 64]) for i in range(2)]
    cs = [P.sb(pfx + "c%d" % i, [128, 2, nrot]) for i in range(2)]
    tmp = P.sb(pfx + "tmp", [128, 4, nrot]); sq = P.sb(pfx + "sq", [128, 64]); ss = P.sb(pfx + "ss", [128, 1])
    if norm_g is not None:
        gbc = P.sb(pfx + "gbc", [128, 64]); P.dma(gbc[:], norm_g[0:1, :].partition_broadcast(128), w=[pfx + "gbc"])
    for t in range(NT):
        b = t % 2; xk = pfx + "x%d" % b; ck = pfx + "c%d" % b
        x = xt[b]; c = cs[b]
        P.dma(x[:], src[t * 128:(t + 1) * 128, :], w=[xk])
        P.dma(c[:, 0, :], cos_d[t * 128:(t + 1) * 128, :], w=[ck])
        P.dma(c[:, 1, :], sin_d[t * 128:(t + 1) * 128, :], wj=[ck])
        if norm_g is not None:
            P.A(lambda: nc.scalar.activation(out=sq[:], in_=x[:], func=AF.Square, accum_out=ss[:]), r=[xk], w=[pfx + "sq", pfx + "ss"])
            P.V(lambda: nc.vector.tensor_scalar(out=ss[:], in0=ss[:], scalar1=1.0 / 64, scalar2=1e-6, op0=ALU.mult, op1=ALU.add), r=[pfx + "ss"], w=[pfx + "ss"])
            P.A(lambda: nc.scalar.sqrt(out=ss[:], in_=ss[:]), r=[pfx + "ss"], w=[pfx + "ss"])
            P.V(lambda: nc.vector.reciprocal(out=ss[:], in_=ss[:]), r=[pfx + "ss"], w=[pfx + "ss"])
            P.V(lambda: nc.vector.scalar_tensor_tensor(out=x[:], in0=x[:], scalar=ss[:], in1=gbc[:], op0=ALU.mult, op1=ALU.mult), r=[xk, pfx + "ss", pfx + "gbc"], w=[xk])
        n = nrot
        x1, x2 = x[:, 0:n], x[:, n:2 * n]
        tk = pfx + "tmp"
        P.V(lambda: nc.vector.tensor_tensor(out=tmp[:, 0, :], in0=x1, in1=c[:, 0, :], op=ALU.mult), r=[xk, ck], w=[tk])
        P.V(lambda: nc.vector.tensor_tensor(out=tmp[:, 1, :], in0=x2, in1=c[:, 1, :], op=ALU.mult), r=[xk, ck], wj=[tk])
        P.V(lambda: nc.vector.tensor_tensor(out=tmp[:, 2, :], in0=x2, in1=c[:, 0, :], op=ALU.mult), r=[xk, ck], wj=[tk])
        P.V(lambda: nc.vector.tensor_tensor(out=tmp[:, 3, :], in0=x1, in1=c[:, 1, :], op=ALU.mult), r=[xk, ck], wj=[tk])
        P.V(lambda: nc.vector.tensor_tensor(out=x1, in0=tmp[:, 0, :], in1=tmp[:, 1, :], op=ALU.subtract), r=[tk], w=[xk])
        P.V(lambda: nc.vector.tensor_tensor(out=x2, in0=tmp[:, 2, :], in1=tmp[:, 3, :], op=ALU.add), r=[tk], wj=[xk])
        P.T(lambda: nc.tensor.transpose(ps_t[0:64, 0:128], x[:], ident[:]), r=[xk, "ident32"], w=["ps_t"])
        P.V(lambda: nc.vector.tensor_scalar(out=dstT[:, PADC + t * 128:PADC + (t + 1) * 128], in0=ps_t[0:64, 0:128], scalar1=scale, scalar2=None, op0=ALU.mult),
            r=["ps_t"], wj=[pfx + "T"])


def normalize_blk(P, nc, acc, acctok, W, yT, c0, ones, ps_bc, rl, ob, obtok):
    P.V(lambda: nc.vector.reciprocal(out=rl[64:65, 0:W], in_=acc[64:65, 0:W]), r=[acctok], w=["rl"])
    P.T(lambda: nc.tensor.matmul(ps_bc[0:64, 0:W], lhsT=ones[64:65, 0:64], rhs=rl[64:65, 0:W], start=True, stop=True), r=["rl", "ones"], w=["ps_bc"])
    P.V(lambda: nc.vector.tensor_tensor(out=ob[:, 0:W], in0=acc[0:64, 0:W], in1=ps_bc[0:64, 0:W], op=ALU.mult), r=[acctok, "ps_bc"], w=[obtok])
    P.dma(yT[:, c0:c0 + W], ob[:, 0:W], r=[obtok], wj=["yT"], key=obtok)


def build_MD(S):
    P = Prog(); nc = P.nc
    q = P.dram("q", [S, 64]); k = P.dram("k", [S, 64]); v = P.dram("v", [S, 64])
    qg = P.dram("qg", [1, 64]); kg = P.dram("kg", [1, 64]); cos_d = P.dram("cos", [S, 32]); sin_d = P.dram("sin", [S, 32])
    ident_d = P.dram("ident", [128, 128])
    yT = P.dram("yT", [64, S], kind="ExternalOutput")
    NT = S // 128
    ident = load_ident32(P, nc, ident_d)
    ones = P.sb("ones", [128, 64]); P.V(lambda: nc.vector.memset(ones[:], 1.0), w=["ones"])
    qT = P.sb("qT", [64, S], BF16); kT = P.sb("kT", [64, S], BF16)
    ps_t = P.ps("ps_t", [128, 512])
    prep_qk(P, nc, q, qT, 0, S, 32, cos_d, sin_d, ident, 0.125, qg, "q", ps_t)
    prep_qk(P, nc, k, kT, 0, S, 32, cos_d, sin_d, ident, 1.0, kg, "k", ps_t)
    Vx = P.sb("Vx", [128, NT, 65], BF16)
    P.V(lambda: nc.vector.memset(Vx[:, :, 64:65], 1.0), w=["Vx"])
    VC = min(16, NT)
    vst = [P.sb("vst%d" % i, [128, VC, 64]) for i in range(2)]
    for ci, m0 in enumerate(range(0, NT, VC)):
        vb = ci % 2
        P.dma(vst[vb][:], v[m0 * 128:(m0 + VC) * 128, :].rearrange("(m p) c -> p m c", p=128), w=["vst%d" % vb])
        P.V(lambda: nc.vector.tensor_copy(out=Vx[:, m0:m0 + VC, 0:64], in_=vst[vb][:]), r=["vst%d" % vb], wj=["Vx"])
    acc = [P.sb("acc%d" % i, [65, 512]) for i in range(2)]
    rl = P.sb("rl", [65, 512]); ob = [P.sb("ob%d" % i, [64, 512]) for i in range(2)]
    NB = 3
    ps_s = [P.ps("ps_s%d" % i, [128, 512]) for i in range(NB)]
    pT = [P.sb("pT%d" % i, [128, 512], BF16) for i in range(NB)]
    po = [P.ps("po%d" % i, [128, 512]) for i in range(2)]
    ps_bc = P.ps("ps_bc", [128, 512])
    W = min(512, S)
    its = [(qb, kt) for qb in range(S // W) for kt in range(NT)]
    LA = 2

    def emit_S(i):
        qb, kt = its[i]; b = i % NB
        P.T(lambda: nc.tensor.matmul(ps_s[b][:, 0:W], lhsT=kT[:, kt * 128:(kt + 1) * 128], rhs=qT[:, qb * W:(qb + 1) * W], start=True, stop=True),
            r=["qT", "kT"], w=["ps_s%d" % b])

    for i in range(min(LA, len(its))):
        emit_S(i)
    for i, (qb, kt) in enumerate(its):
        b = i % NB; pb = qb % 2
        if i + LA < len(its):
            emit_S(i + LA)
        P.A(lambda: nc.scalar.activation(out=pT[b][:, 0:W], in_=ps_s[b][:, 0:W], func=AF.Exp), r=["ps_s%d" % b], w=["pT%d" % b])
        P.T(lambda: nc.tensor.matmul(po[pb][0:65, 0:W], lhsT=Vx[:, kt, :], rhs=pT[b][:, 0:W], start=(kt == 0), stop=(kt == NT - 1)),
            r=["Vx", "pT%d" % b], w=["po%d" % pb] if kt == 0 else [], wj=[] if kt == 0 else ["po%d" % pb])
        if kt == NT - 1:
            P.V(lambda: nc.vector.tensor_copy(out=acc[pb][:, 0:W], in_=po[pb][0:65, 0:W]), r=["po%d" % pb], w=["acc%d" % pb])
            normalize_blk(P, nc, acc[pb], "acc%d" % pb, W, yT, qb * W, ones, ps_bc, rl, ob[pb], "ob%d" % pb)
    P.finish(); P.close()
    return nc


def axial_tables(S):
    def ang(pos, n, theta):
        inv = (np.float32(theta) ** (-np.arange(n, dtype=np.float32) / np.float32(n))).astype(np.float32)
        return pos.astype(np.float32)[:, None] * inv[None, :]
    t = np.arange(S)
    a = np.concatenate([ang(t // 64, 16, 10000.0), ang(t % 64, 16, 10000.0)], axis=-1).astype(np.float32)
    return np.cos(a).astype(np.float32), np.sin(a).astype(np.float32)


def rope_tables(S):
    inv = (np.float32(500000.0) ** (-np.arange(8, dtype=np.float32) / np.float32(8))).astype(np.float32)
    a = (np.arange(S).astype(np.float32)[:, None] * inv[None, :]).astype(np.float32)
    return np.cos(a).astype(np.float32), np.sin(a).astype(np.float32)


def run_MD(zq, zk, zv, prm, l):
    B, S, _ = zq.shape
    nc = build_MD(S)
    cos, sin = axial_tables(S)
    com = {"qg": prm["gqa_q_norm"][l][None, :], "kg": prm["gqa_k_norm"][l][None, :], "cos": cos, "sin": sin, "ident": np.eye(128, dtype=np.float32)}
    maps = []
    for c in range(B * 4):
        b, h = c // 4, c % 4
        maps.append(dict(com, q=np.ascontiguousarray(zq[b, :, h * 64:(h + 1) * 64]), k=np.ascontiguousarray(zk[b, :, (h // 2) * 64:(h // 2 + 1) * 64]),
                         v=np.ascontiguousarray(zv[b, :, (h // 2) * 64:(h // 2 + 1) * 64])))
    res = run_bass_kernel_spmd(nc, maps, core_ids=list(range(B * 4)))
    y = np.zeros((B, S, 256), np.float32)
    for c in range(B * 4):
        b, h = c // 4, c % 4
        y[b, :, h * 64:(h + 1) * 64] = res.results[c]["yT"].T
    return y


def build_MA(S):
    P = Prog(); nc = P.nc
    PADC = 1024
    DIL = (1, 4, 16)
    q = P.dram("q", [S, 64]); k = P.dram("k", [S, 64])
    vx = [P.dram("vx%d" % d, [d * (S // d + 128), 65]) for d in DIL]
    cos_d = P.dram("cos", [S, 8]); sin_d = P.dram("sin", [S, 8])
    ident_d = P.dram("ident", [128, 128]); mask_d = P.dram("mask", [128, 256])
    yT = P.dram("yT", [64, S], kind="ExternalOutput")
    ident = load_ident32(P, nc, ident_d)
    ones = P.sb("ones", [128, 64]); P.V(lambda: nc.vector.memset(ones[:], 1.0), w=["ones"])
    m32 = P.sb("m32", [128, 256]); mask = P.sb("maskb", [128, 256], BF16)
    P.dma(m32[:], mask_d[:, :], w=["m32"]); P.V(lambda: nc.vector.tensor_copy(out=mask[:], in_=m32[:]), r=["m32"], w=["mask"])
    qT = P.sb("qT", [64, S + 2 * PADC], BF16); kT = P.sb("kT", [64, S + 2 * PADC], BF16)
    P.V(lambda: nc.vector.memset(kT[:, 0:PADC], 0.0), w=["kT"]); P.V(lambda: nc.vector.memset(kT[:, PADC + S:], 0.0), wj=["kT"])
    P.V(lambda: nc.vector.memset(qT[:, 0:PADC], 0.0), w=["qT"]); P.V(lambda: nc.vector.memset(qT[:, PADC + S:], 0.0), wj=["qT"])
    ps_t = P.ps("ps_t", [128, 512])
    prep_qk(P, nc, q, qT, PADC, S, 8, cos_d, sin_d, ident, 0.125, None, "q", ps_t)
    prep_qk(P, nc, k, kT, PADC, S, 8, cos_d, sin_d, ident, 1.0, None, "k", ps_t)
    SB = 2048
    accT = P.sb("accT", [65, SB])
    NB = 3
    ps_s = [P.ps("ps_s%d" % i, [128, 512]) for i in range(NB)]
    pT = [P.sb("pT%d" % i, [128, 256], BF16) for i in range(NB)]
    pTm = [P.sb("pTm%d" % i, [128, 256], BF16) for i in range(NB)]
    po = [P.ps("po%d" % i, [128, 512]) for i in range(NB)]
    ps_bc = ps_t
    rl = P.sb("rl", [65, 512]); ob = [P.sb("ob%d" % i, [64, 512]) for i in range(2)]
    Vxs = {}
    vst = [P.sb("vst%d" % i, [128, 16, 65]) for i in range(2)]
    ci = 0
    for di, d in enumerate(DIL):
        L = S // d; NKT = L // 128 + 1; NTL = d * NKT
        Vx = P.sb("Vx%d" % d, [128, NTL, 65], BF16)
        for m0 in range(0, NTL, 16):
            m1 = min(NTL, m0 + 16); vb = ci % 2; ci += 1
            P.dma(vst[vb][:, 0:m1 - m0, :], vx[di][m0 * 128:m1 * 128, :].rearrange("(m p) c -> p m c", p=128), w=["vst%d" % vb])
            P.V(lambda: nc.vector.tensor_copy(out=Vx[:, m0:m1, :], in_=vst[vb][:, 0:m1 - m0, :]), r=["vst%d" % vb], wj=["Vx%d" % d])
        Vxs[d] = (Vx, NKT)
    oi = 0
    its = []
    for sbi in range(S // SB):
        for di, d in enumerate(DIL):
            for r in range(d):
                for bl in range(SB // (128 * d)):
                    its.append((sbi, di, d, r, bl))
    LA = 2

    def emit_S(i):
        sbi, di, d, r, bl = its[i]; b = i % NB
        i0 = (sbi * SB // d) + bl * 128
        qs = PADC + r + d * i0
        rhs = qT[:, qs:qs + 127 * d + 1:d]
        for ab in range(2):
            ks = PADC + r + d * (i0 - 64 + 128 * ab)
            P.T(lambda: nc.tensor.matmul(ps_s[b][:, ab * 128:(ab + 1) * 128], lhsT=kT[:, ks:ks + 127 * d + 1:d], rhs=rhs, start=True, stop=True),
                r=["qT", "kT"], w=["ps_s%d" % b] if ab == 0 else [], wj=[] if ab == 0 else ["ps_s%d" % b])

    for i in range(min(LA, len(its))):
        emit_S(i)
    for i, (sbi, di, d, r, bl) in enumerate(its):
        b = i % NB
        Vx, NKT = Vxs[d]
        i0 = (sbi * SB // d) + bl * 128; blk = i0 // 128
        if i + LA < len(its):
            emit_S(i + LA)
        P.A(lambda: nc.scalar.activation(out=pT[b][:], in_=ps_s[b][:, 0:256], func=AF.Exp), r=["ps_s%d" % b], w=["pT%d" % b])
        P.G(lambda: nc.gpsimd.tensor_tensor(out=pTm[b][:], in0=pT[b][:], in1=mask[:], op=ALU.mult), r=["pT%d" % b, "mask"], w=["pTm%d" % b])
        for ab in range(2):
            P.T(lambda: nc.tensor.matmul(po[b][0:65, 0:128], lhsT=Vx[:, r * NKT + blk + ab, :], rhs=pTm[b][:, ab * 128:(ab + 1) * 128], start=(ab == 0), stop=(ab == 1)),
                r=["Vx%d" % d, "pTm%d" % b], w=["po%d" % b] if ab == 0 else [], wj=[] if ab == 0 else ["po%d" % b])
        t0 = r + d * bl * 128
        dst = accT[:, t0:t0 + 127 * d + 1:d]
        if di == 0:
            P.V(lambda: nc.vector.tensor_copy(out=dst, in_=po[b][0:65, 0:128]), r=["po%d" % b], w=["accT"] if (bl == 0) else [], wj=[] if (bl == 0) else ["accT"])
        else:
            P.V(lambda: nc.vector.tensor_tensor(out=dst, in0=dst, in1=po[b][0:65, 0:128], op=ALU.add), r=["po%d" % b, "accT"], w=["accT"])
        last = (i + 1 == len(its)) or (its[i + 1][0] != sbi)
        if last:
            for c0 in range(0, SB, 512):
                o = oi % 2; oi += 1
                normalize_blk(P, nc, accT[:, c0:c0 + 512], "accT", 512, yT, sbi * SB + c0, ones, ps_bc, rl, ob[o], "ob%d" % o)
    P.finish(); P.close()
    return nc


def dil_mask():
    kk = np.arange(128)[:, None]; qq = np.arange(128)[None, :]
    return np.concatenate([(kk >= qq), (kk <= qq)], axis=1).astype(np.float32)


def vext_dilated(v, d):
    S = v.shape[0]; L = S // d
    out = np.zeros((d, L + 128, 65), np.float32)
    vr = v.reshape(L, d, 64).transpose(1, 0, 2)
    out[:, 64:64 + L, :64] = vr
    out[:, 64:64 + L, 64] = 1.0
    return out.reshape(d * (L + 128), 65)


def run_MA(za, prm, l):
    B, S, _ = za.shape
    nc = build_MA(S)
    cos, sin = rope_tables(S)
    com = {"cos": cos, "sin": sin, "ident": np.eye(128, dtype=np.float32), "mask": dil_mask()}
    maps = []
    for c in range(B * 4):
        b, h = c // 4, c % 4
        v = za[b, :, 512 + h * 64:512 + (h + 1) * 64]
        m = dict(com, q=np.ascontiguousarray(za[b, :, h * 64:(h + 1) * 64]), k=np.ascontiguousarray(za[b, :, 256 + h * 64:256 + (h + 1) * 64]))
        for d in (1, 4, 16):
            m["vx%d" % d] = vext_dilated(v, d)
        maps.append(m)
    res = run_bass_kernel_spmd(nc, maps, core_ids=list(range(B * 4)))
    y = np.zeros((B, S, 256), np.float32)
    for c in range(B * 4):
        b, h = c // 4, c % 4
        y[b, :, h * 64:(h + 1) * 64] = res.results[c]["yT"].T
    return y


TWO_PI = 6.283185307179586
PI = 3.141592653589793


def range_reduce(P, nc, r, ki, tok, shape):
    kf = P.sb(tok + "_kf", shape)
    P.V(lambda: nc.vector.tensor_scalar(out=kf[:], in0=r[:], scalar1=1.0 / TWO_PI, scalar2=None, op0=ALU.mult), r=[tok], w=[tok + "kf"])
    P.V(lambda: nc.vector.tensor_copy(out=ki[:], in_=kf[:]), r=[tok + "kf"], w=[tok + "ki"])
    P.V(lambda: nc.vector.tensor_copy(out=kf[:], in_=ki[:]), r=[tok + "ki"], w=[tok + "kf"])
    P.V(lambda: nc.vector.scalar_tensor_tensor(out=r[:], in0=kf[:], scalar=-TWO_PI, in1=r[:], op0=ALU.mult, op1=ALU.add), r=[tok + "kf", tok], w=[tok])
    for (cmp, thr, add) in ((ALU.is_gt, PI, -TWO_PI), (ALU.is_lt, -PI, TWO_PI), (ALU.is_gt, PI, -TWO_PI)):
        P.V(lambda: nc.vector.tensor_scalar(out=kf[:], in0=r[:], scalar1=thr, scalar2=add, op0=cmp, op1=ALU.mult), r=[tok], w=[tok + "kf"])
        P.V(lambda: nc.vector.tensor_tensor(out=r[:], in0=r[:], in1=kf[:], op=ALU.add), r=[tok, tok + "kf"], w=[tok])
    P.V(lambda: nc.vector.tensor_scalar(out=r[:], in0=r[:], scalar1=3.1415925, scalar2=-3.1415925, op0=ALU.min, op1=ALU.max), r=[tok], w=[tok])


def build_MC(S):
    P = Prog(); nc = P.nc
    T = min(512, S); NCH = S // T
    uT_d = [P.dram("uT%d" % d, [64, S]) for d in range(2)]
    a_re = P.dram("a_re", [2, 4, 64]); a_im = P.dram("a_im", [2, 4, 64]); ldt = P.dram("ldt", [2, 4])
    b_re = P.dram("b_re", [4, 64, 16]); b_im = P.dram("b_im", [4, 64, 16])
    c_re = P.dram("c_re", [2, 4, 16, 64]); c_im = P.dram("c_im", [2, 4, 16, 64])
    ident_d = P.dram("ident", [128, 128]); iota_d = P.dram("iota", [1, T + 1])
    yT_d = [P.dram("yT%d" % d, [64, S], kind="ExternalOutput") for d in range(2)]
    ident = load_ident32(P, nc, ident_d)
    iota = P.sb("iota", [128, T + 1]); P.dma(iota[:], iota_d[0:1, :].partition_broadcast(128), w=["iota"])
    uT = []
    UC = min(2048, S)
    ust = [P.sb("ust%d" % i, [64, UC]) for i in range(2)]
    ui = 0
    for d in range(2):
        u = P.sb("uTb%d" % d, [64, S], BF16)
        for c0 in range(0, S, UC):
            ub = ui % 2; ui += 1
            P.dma(ust[ub][:], uT_d[d][:, c0:c0 + UC], w=["ust%d" % ub])
            P.V(lambda: nc.vector.tensor_copy(out=u[:, c0:c0 + UC], in_=ust[ub][:]), r=["ust%d" % ub], wj=["uT%d" % d])
        uT.append(u)
    ps_t = P.ps("ps_t", [128, 512])
    tiles = {}
    for d in range(2):
        for gp in range(2):
            n = "t%d%d" % (d, gp)
            prm = P.sb(n + "prm", [128, 16])
            pk = n + "prm"
            P.dma(prm[:, 0:1], a_re[d, 2 * gp:2 * gp + 2, :].rearrange("g (p o) -> (g p) o", o=1), w=[pk])
            P.dma(prm[:, 1:2], a_im[d, 2 * gp:2 * gp + 2, :].rearrange("g (p o) -> (g p) o", o=1), wj=[pk])
            for g in range(2):
                P.dma(prm[g * 64:(g + 1) * 64, 2:3], ldt[d:d + 1, 2 * gp + g:2 * gp + g + 1].partition_broadcast(64), wj=[pk])
            c = lambda i: prm[:, i:i + 1]
            P.A(lambda: nc.scalar.activation(out=c(2), in_=c(2), func=AF.Exp), r=[pk], w=[pk])
            P.V(lambda: nc.vector.tensor_tensor(out=c(4), in0=c(1), in1=c(2), op=ALU.mult), r=[pk], w=[pk])
            P.V(lambda: nc.vector.tensor_tensor(out=c(3), in0=c(0), in1=c(2), op=ALU.mult), r=[pk], w=[pk])
            P.A(lambda: nc.scalar.activation(out=c(3), in_=c(3), func=AF.Exp), r=[pk], w=[pk])
            cosT = P.sb(n + "cos", [128, T + 1]); sinT = P.sb(n + "sin", [128, T + 1]); ki = P.sb(n + "ki", [128, T + 1], I32)
            P.V(lambda: nc.vector.tensor_scalar(out=sinT[:], in0=iota[:], scalar1=c(4), scalar2=None, op0=ALU.mult), r=["iota", pk], w=[n + "sin"])
            P.V(lambda: nc.vector.tensor_scalar(out=cosT[:], in0=sinT[:], scalar1=PI / 2, scalar2=None, op0=ALU.add), r=[n + "sin"], w=[n + "cos"])
            range_reduce(P, nc, sinT, ki, n + "sin", [128, T + 1])
            range_reduce(P, nc, cosT, ki, n + "cos", [128, T + 1])
            P.A(lambda: nc.scalar.activation(out=sinT[:], in_=sinT[:], func=AF.Sin), r=[n + "sin"], w=[n + "sin"])
            P.A(lambda: nc.scalar.activation(out=cosT[:], in_=cosT[:], func=AF.Sin), r=[n + "cos"], w=[n + "cos"])
            P.V(lambda: nc.vector.tensor_copy(out=c(5), in_=cosT[:, 1:2]), r=[n + "cos", pk], w=[pk])
            P.V(lambda: nc.vector.tensor_copy(out=c(6), in_=sinT[:, 1:2]), r=[n + "sin", pk], w=[pk])
            P.V(lambda: nc.vector.tensor_copy(out=c(13), in_=cosT[:, T:T + 1]), r=[n + "cos", pk], w=[pk])
            P.V(lambda: nc.vector.tensor_copy(out=c(14), in_=sinT[:, T:T + 1]), r=[n + "sin", pk], w=[pk])
            P.V(lambda: nc.vector.tensor_scalar(out=c(15), in0=c(14), scalar1=-1.0, scalar2=None, op0=ALU.mult), r=[pk], w=[pk])
            P.V(lambda: nc.vector.tensor_tensor(out=c(5), in0=c(5), in1=c(3), op=ALU.mult), r=[pk], w=[pk])
            P.V(lambda: nc.vector.tensor_tensor(out=c(6), in0=c(6), in1=c(3), op=ALU.mult), r=[pk], w=[pk])
            P.V(lambda: nc.vector.tensor_scalar(out=c(7), in0=c(5), scalar1=-1.0, scalar2=None, op0=ALU.add), r=[pk], w=[pk])
            P.V(lambda: nc.vector.tensor_tensor(out=c(8), in0=c(0), in1=c(0), op=ALU.mult), r=[pk], w=[pk])
            P.V(lambda: nc.vector.tensor_tensor(out=c(11), in0=c(1), in1=c(1), op=ALU.mult), r=[pk], w=[pk])
            P.V(lambda: nc.vector.tensor_tensor(out=c(8), in0=c(8), in1=c(11), op=ALU.add), r=[pk], w=[pk])
            P.V(lambda: nc.vector.reciprocal(out=c(8), in_=c(8)), r=[pk], w=[pk])
            P.V(lambda: nc.vector.tensor_tensor(out=c(9), in0=c(7), in1=c(0), op=ALU.mult), r=[pk], w=[pk])
            P.V(lambda: nc.vector.tensor_tensor(out=c(11), in0=c(6), in1=c(1), op=ALU.mult), r=[pk], w=[pk])
            P.V(lambda: nc.vector.tensor_tensor(out=c(9), in0=c(9), in1=c(11), op=ALU.add), r=[pk], w=[pk])
            P.V(lambda: nc.vector.tensor_tensor(out=c(9), in0=c(9), in1=c(8), op=ALU.mult), r=[pk], w=[pk])
            P.V(lambda: nc.vector.tensor_tensor(out=c(10), in0=c(6), in1=c(0), op=ALU.mult), r=[pk], w=[pk])
            P.V(lambda: nc.vector.tensor_tensor(out=c(11), in0=c(7), in1=c(1), op=ALU.mult), r=[pk], w=[pk])
            P.V(lambda: nc.vector.tensor_tensor(out=c(10), in0=c(10), in1=c(11), op=ALU.subtract), r=[pk], w=[pk])
            P.V(lambda: nc.vector.tensor_tensor(out=c(10), in0=c(10), in1=c(8), op=ALU.mult), r=[pk], w=[pk])
            P.V(lambda: nc.vector.tensor_scalar(out=c(12), in0=c(10), scalar1=-1.0, scalar2=None, op0=ALU.mult), r=[pk], w=[pk])
            braw = P.sb(n + "braw", [128, 2, 16]); bk = n + "braw"
            P.dma(braw[:, 0, :], b_re[2 * gp:2 * gp + 2].rearrange("g p c -> (g p) c"), w=[bk])
            P.dma(braw[:, 1, :], b_im[2 * gp:2 * gp + 2].rearrange("g p c -> (g p) c"), wj=[bk])
            BD = P.sb(n + "BD", [128, 2, 64]); tb = P.sb(n + "tb", [128, 16])
            P.V(lambda: nc.vector.memset(BD[:], 0.0), w=[n + "BD"])
            for g in range(2):
                rows = slice(g * 64, (g + 1) * 64); cols = slice(32 * gp + 16 * g, 32 * gp + 16 * g + 16)
                P.V(lambda: nc.vector.tensor_scalar(out=tb[rows, :], in0=braw[rows, 1, :], scalar1=prm[rows, 12:13], scalar2=None, op0=ALU.mult), r=[bk, pk], w=[n + "tb"])
                P.V(lambda: nc.vector.scalar_tensor_tensor(out=BD[rows, 0, cols], in0=braw[rows, 0, :], scalar=prm[rows, 9:10], in1=tb[rows, :], op0=ALU.mult, op1=ALU.add),
                    r=[bk, pk, n + "tb"], wj=[n + "BD"])
                P.V(lambda: nc.vector.tensor_scalar(out=tb[rows, :], in0=braw[rows, 0, :], scalar1=prm[rows, 10:11], scalar2=None, op0=ALU.mult), r=[bk, pk], w=[n + "tb"])
                P.V(lambda: nc.vector.scalar_tensor_tensor(out=BD[rows, 1, cols], in0=braw[rows, 1, :], scalar=prm[rows, 9:10], in1=tb[rows, :], op0=ALU.mult, op1=ALU.add),
                    r=[bk, pk, n + "tb"], wj=[n + "BD"])
            BT = P.sb(n + "BT", [64, 2, 128], BF16)
            for ri in range(2):
                P.T(lambda: nc.tensor.transpose(ps_t[0:64, 0:128], BD[:, ri, :], ident[:]), r=[n + "BD", "ident32"], w=["ps_t"])
                P.V(lambda: nc.vector.tensor_copy(out=BT[:, ri, :], in_=ps_t[0:64, 0:128]), r=["ps_t"], wj=[n + "BT"])
            craw = P.sb(n + "craw", [128, 2, 16]); ck = n + "craw"
            for g in range(2):
                P.dma(craw[g * 64:(g + 1) * 64, 0, :], c_re[d, 2 * gp + g].rearrange("c p -> p c"), wj=[ck], allow_slow_non_contiguous=True)
                P.dma(craw[g * 64:(g + 1) * 64, 1, :], c_im[d, 2 * gp + g].rearrange("c p -> p c"), wj=[ck], allow_slow_non_contiguous=True)
            CT = P.sb(n + "CT", [128, 2, 64], BF16)
            P.V(lambda: nc.vector.memset(CT[:], 0.0), w=[n + "CT"])
            for g in range(2):
                rows = slice(g * 64, (g + 1) * 64); cols = slice(32 * gp + 16 * g, 32 * gp + 16 * g + 16)
                P.V(lambda: nc.vector.tensor_copy(out=CT[rows, 0, cols], in_=craw[rows, 0, :]), r=[ck], wj=[n + "CT"])
                P.V(lambda: nc.vector.tensor_scalar(out=CT[rows, 1, cols], in0=craw[rows, 1, :], scalar1=-1.0, scalar2=None, op0=ALU.mult), r=[ck], wj=[n + "CT"])
            rho_t = P.sb(n + "rho", [128, T])
            P.V(lambda: nc.vector.tensor_scalar(out=rho_t[:], in0=iota[:, 0:T], scalar1=0.0, scalar2=prm[:, 3:4], op0=ALU.mult, op1=ALU.add), r=["iota", pk], w=[n + "rho"])
            init = P.sb(n + "init", [128, 2]); P.V(lambda: nc.vector.memset(init[:], 0.0), w=[n + "init"])
            tiles[(d, gp)] = dict(n=n, prm=prm, pk=pk, cosT=cosT, sinT=sinT, BT=BT, CT=CT, rho=rho_t, init=init)
    ps_b = [P.ps("ps_b%d" % i, [128, 512]) for i in range(2)]
    ps_y = P.ps("ps_y", [128, 512])
    m = [P.sb("m%d" % i, [128, T]) for i in range(4)]
    bp = [P.sb("bp%d" % i, [128, T]) for i in range(2)]
    wv = [P.sb("wv%d" % i, [128, T]) for i in range(2)]
    pp = [P.sb("pp%d" % i, [128, T]) for i in range(4)]
    xb = [[P.sb("xb%d_%d" % (gp, i), [128, T], BF16) for i in range(2)] for gp in range(2)]
    yo = [P.sb("yo%d" % i, [64, T]) for i in range(2)]
    tmpc = P.sb("tmpc", [128, 2])
    it = 0
    for d in range(2):
        for ch in range(NCH):
            cols = slice(ch * T, (ch + 1) * T)
            for gp in range(2):
                t = tiles[(d, gp)]; n = t["n"]; cosT, sinT = t["cosT"], t["sinT"]
                for ri in range(2):
                    P.T(lambda: nc.tensor.matmul(ps_b[ri][:, 0:T], lhsT=t["BT"][:, ri, :], rhs=uT[d][:, cols], start=True, stop=True), r=[n + "BT", "uT%d" % d], w=["ps_b%d" % ri])
                P.V(lambda: nc.vector.tensor_tensor(out=m[0][:], in0=ps_b[0][:, 0:T], in1=cosT[:, 0:T], op=ALU.mult), r=["ps_b0", n + "cos"], w=["m0"])
                P.V(lambda: nc.vector.tensor_tensor(out=m[1][:], in0=ps_b[1][:, 0:T], in1=sinT[:, 0:T], op=ALU.mult), r=["ps_b1", n + "sin"], w=["m1"])
                P.V(lambda: nc.vector.tensor_tensor(out=m[2][:], in0=ps_b[1][:, 0:T], in1=cosT[:, 0:T], op=ALU.mult), r=["ps_b1", n + "cos"], w=["m2"])
                P.V(lambda: nc.vector.tensor_tensor(out=m[3][:], in0=ps_b[0][:, 0:T], in1=sinT[:, 0:T], op=ALU.mult), r=["ps_b0", n + "sin"], w=["m3"])
                P.G(lambda: nc.gpsimd.tensor_tensor(out=bp[0][:], in0=m[0][:], in1=m[1][:], op=ALU.add), r=["m0", "m1"], w=["bp0"])
                P.G(lambda: nc.gpsimd.tensor_tensor(out=bp[1][:], in0=m[2][:], in1=m[3][:], op=ALU.subtract), r=["m2", "m3"], w=["bp1"])
                for ri in range(2):
                    P.V(lambda: nc.vector.tensor_tensor_scan(out=wv[ri][:], data0=t["rho"][:], data1=bp[ri][:], initial=t["init"][:, ri:ri + 1], op0=ALU.mult, op1=ALU.add),
                        r=[n + "rho", "bp%d" % ri, n + "init"], w=["wv%d" % ri])
                prm = t["prm"]
                P.V(lambda: nc.vector.tensor_scalar(out=tmpc[:, 0:1], in0=wv[0][:, T - 1:T], scalar1=prm[:, 13:14], scalar2=None, op0=ALU.mult), r=["wv0", t["pk"]], w=["tmpc"])
                P.V(lambda: nc.vector.tensor_scalar(out=tmpc[:, 1:2], in0=wv[0][:, T - 1:T], scalar1=prm[:, 14:15], scalar2=None, op0=ALU.mult), r=["wv0", t["pk"]], wj=["tmpc"])
                P.V(lambda: nc.vector.scalar_tensor_tensor(out=t["init"][:, 0:1], in0=wv[1][:, T - 1:T], scalar=prm[:, 15:16], in1=tmpc[:, 0:1], op0=ALU.mult, op1=ALU.add),
                    r=["wv1", "tmpc", t["pk"]], w=[n + "init"])
                P.V(lambda: nc.vector.scalar_tensor_tensor(out=t["init"][:, 1:2], in0=wv[1][:, T - 1:T], scalar=prm[:, 13:14], in1=tmpc[:, 1:2], op0=ALU.mult, op1=ALU.add),
                    r=["wv1", "tmpc", t["pk"]], wj=[n + "init"])
                P.G(lambda: nc.gpsimd.tensor_tensor(out=pp[0][:], in0=wv[0][:], in1=cosT[:, 0:T], op=ALU.mult), r=["wv0", n + "cos"], w=["pp0"])
                P.G(lambda: nc.gpsimd.tensor_tensor(out=pp[1][:], in0=wv[1][:], in1=sinT[:, 0:T], op=ALU.mult), r=["wv1", n + "sin"], w=["pp1"])
                P.G(lambda: nc.gpsimd.tensor_tensor(out=xb[gp][0][:], in0=pp[0][:], in1=pp[1][:], op=ALU.subtract), r=["pp0", "pp1"], w=["xb%d_0" % gp])
                P.G(lambda: nc.gpsimd.tensor_tensor(out=pp[2][:], in0=wv[0][:], in1=sinT[:, 0:T], op=ALU.mult), r=["wv0", n + "sin"], w=["pp2"])
                P.G(lambda: nc.gpsimd.tensor_tensor(out=pp[3][:], in0=wv[1][:], in1=cosT[:, 0:T], op=ALU.mult), r=["wv1", n + "cos"], w=["pp3"])
                P.G(lambda: nc.gpsimd.tensor_tensor(out=xb[gp][1][:], in0=pp[2][:], in1=pp[3][:], op=ALU.add), r=["pp2", "pp3"], w=["xb%d_1" % gp])
            k = 0
            for gp in range(2):
                t = tiles[(d, gp)]
                for ri in range(2):
                    P.T(lambda: nc.tensor.matmul(ps_y[0:64, 0:T], lhsT=t["CT"][:, ri, :], rhs=xb[gp][ri][:], start=(k == 0), stop=(k == 3)),
                        r=[t["n"] + "CT", "xb%d_%d" % (gp, ri)], w=["ps_y"] if k == 0 else [], wj=[] if k == 0 else ["ps_y"])
                    k += 1
            b = it % 2; it += 1
            P.V(lambda: nc.vector.tensor_copy(out=yo[b][:], in_=ps_y[0:64, 0:T]), r=["ps_y"], w=["yo%d" % b])
            P.dma(yT_d[d][:, cols], yo[b][:], r=["yo%d" % b], wj=["yT%d" % d], key="yo%d" % b)
    P.finish(); P.close()
    return nc


def mc_inputs(zc_b, prm, l, h, S):
    T = min(512, S)
    u = zc_b[:, h * 64:(h + 1) * 64]
    gs = slice(4 * h, 4 * h + 4)
    return {"uT0": np.ascontiguousarray(u.T), "uT1": np.ascontiguousarray(u[::-1].T),
            "a_re": np.ascontiguousarray(prm["s5_a_re"][l][:, gs]), "a_im": np.ascontiguousarray(prm["s5_a_im"][l][:, gs]),
            "ldt": np.ascontiguousarray(prm["s5_log_dt"][l][:, gs]), "b_re": np.ascontiguousarray(prm["s5_b_re"][l][gs]), "b_im": np.ascontiguousarray(prm["s5_b_im"][l][gs]),
            "c_re": np.ascontiguousarray(prm["s5_c_re"][l][:, gs]), "c_im": np.ascontiguousarray(prm["s5_c_im"][l][:, gs]),
            "ident": np.eye(128, dtype=np.float32), "iota": np.arange(T + 1, dtype=np.float32)[None, :]}


def run_MC(zc, prm, l):
    B, S, _ = zc.shape
    nc = build_MC(S)
    maps = [mc_inputs(zc[c // 4], prm, l, c % 4, S) for c in range(B * 4)]
    res = run_bass_kernel_spmd(nc, maps, core_ids=list(range(B * 4)))
    yf = np.zeros((B, S, 256), np.float32); yb = np.zeros((B, S, 256), np.float32)
    for c in range(B * 4):
        b, h = c // 4, c % 4
        yf[b, :, h * 64:(h + 1) * 64] = res.results[c]["yT0"].T
        yb[b, :, h * 64:(h + 1) * 64] = res.results[c]["yT1"].T[::-1]
    return yf, yb


EM05 = 0.6065306597126334


def build_MB(S):
    P = Prog(); nc = P.nc
    NT = S // 128
    rkv = [P.dram("rkv%d" % d, [S, 192]) for d in range(2)]
    h1 = [[P.dram("h%s1_%d" % (n, d), [64, S]) for d in range(2)] for n in "wa"]
    h2 = [[P.dram("h%s2_%d" % (n, d), [64, S]) for d in range(2)] for n in "wa"]
    w2 = P.dram("w2", [2, 64, 64]); a2 = P.dram("a2", [2, 64, 64]); w0 = P.dram("w0", [2, 64]); a0 = P.dram("a0", [2, 64])
    mu = P.dram("mu", [2, 2, 192]); kka = P.dram("kka", [3, 64])
    ident_d = P.dram("ident", [128, 128]); zsel_d = P.dram("zsel", [64, 32 * 128])
    y_o = P.dram("y", [2, S, 64], kind="ExternalOutput"); bonus_o = P.dram("bonus", [2, S, 64], kind="ExternalOutput")
    pkd = [P.dram("pkd%d" % d, [S, 256], BF16, kind="Internal") for d in range(2)]
    dkd = [P.dram("dkd%d" % d, [S, 64], F32, kind="Internal") for d in range(2)]
    vTs = P.dram("vTs", [128, S], F32, kind="Internal")
    ident = load_ident32(P, nc, ident_d)
    z32 = P.sb("z32", [64, 32 * 128]); zb = P.sb("zb", [64, 32 * 128], BF16)
    P.dma(z32[:], zsel_d[:, :], w=["z32"]); P.V(lambda: nc.vector.tensor_copy(out=zb[:], in_=z32[:]), r=["z32"], w=["zb"])
    def bc(name, src, n):
        t = P.sb(name, [128, n]); P.dma(t[:], src.partition_broadcast(128), w=[name]); return t
    kk_bc = bc("kk_bc", kka[0:1, :], 64); ka_bc = bc("ka_bc", kka[1:2, :], 64); rk_bc = bc("rk_bc", kka[2:3, :], 64)
    ps_t = P.ps("ps_t", [128, 512]); ps_l = P.ps("ps_l", [128, 512])
    thT = P.sb("thT", [64, S], BF16); haT = P.sb("haT", [64, S], BF16)
    CW = min(2048, S)
    ha = P.sb("ha_", [64, CW]); hb = P.sb("hb_", [64, CW])
    vstage = P.sb("vstage", [128, 128]); P.V(lambda: nc.vector.memset(vstage[:], 0.0), w=["vstage"])
    zrow = P.sb("zrow", [1, 64], BF16); P.V(lambda: nc.vector.memset(zrow[:], 0.0), w=["zrow"])
    for d in range(2):
        for wi, dst in enumerate((thT, haT)):
            dk_ = "thT" if wi == 0 else "haT"
            for ci, c0 in enumerate(range(0, S, CW)):
                P.dma(ha[:], h1[wi][d][:, c0:c0 + CW], w=["ha"])
                if c0 == 0:
                    P.V(lambda: nc.vector.memset(hb[:, 0:1], 0.0), w=["hb"])
                    P.dma(hb[:, 1:CW], h2[wi][d][:, 0:CW - 1], wj=["hb"])
                else:
                    P.dma(hb[:], h2[wi][d][:, c0 - 1:c0 + CW - 1], w=["hb"])
                P.V(lambda: nc.vector.tensor_tensor(out=ha[:], in0=ha[:], in1=hb[:], op=ALU.add), r=["ha", "hb"], w=["ha"])
                first = (ci == 0)
                if wi == 0:
                    P.A(lambda: nc.scalar.activation(out=dst[:, c0:c0 + CW], in_=ha[:], func=AF.Tanh), r=["ha"], w=[dk_] if first else [], wj=[] if first else [dk_])
                else:
                    P.V(lambda: nc.vector.tensor_copy(out=dst[:, c0:c0 + CW], in_=ha[:]), r=["ha"], w=[dk_] if first else [], wj=[] if first else [dk_])
        w2s = P.sb("w2s%d" % d, [64, 2, 64]); w2b = P.sb("w2b%d" % d, [64, 2, 64], BF16)
        P.dma(w2s[:, 0, :], w2[d], w=["w2s%d" % d]); P.dma(w2s[:, 1, :], a2[d], wj=["w2s%d" % d])
        P.V(lambda: nc.vector.tensor_copy(out=w2b[:], in_=w2s[:]), r=["w2s%d" % d], w=["w2b%d" % d])
        w0_bc = bc("w0_bc%d" % d, w0[d:d + 1, :], 64); a0_bc = bc("a0_bc%d" % d, a0[d:d + 1, :], 64)
        mu0_bc = bc("mu0_bc%d" % d, mu[d, 0:1, :], 192); mu1_bc = bc("mu1_bc%d" % d, mu[d, 1:2, :], 192)
        cur = [P.sb("cur%d_%d" % (d, i), [128, 192]) for i in range(2)]
        prv = [P.sb("prv%d_%d" % (d, i), [128, 192]) for i in range(2)]
        nxt = [P.sb("nxt%d_%d" % (d, i), [128, 192]) for i in range(2)]
        d0 = P.sb("d0_%d" % d, [128, 192]); d1 = P.sb("d1_%d" % d, [128, 192]); mix = P.sb("mix%d" % d, [128, 192])
        dec = [P.sb("dec%d_%d" % (d, i), [128, 64]) for i in range(2)]
        pack = [P.sb("pack%d_%d" % (d, i), [128, 256], BF16) for i in range(2)]
        bon = [P.sb("bon%d_%d" % (d, i), [128, 64]) for i in range(2)]
        vto = [P.sb("vto%d_%d" % (d, i), [128, 128]) for i in range(2)]
        icl = P.sb("icl%d" % d, [128, 64]); kkt = P.sb("kkt%d" % d, [128, 64]); kap = P.sb("kap%d" % d, [128, 64]); kd = P.sb("kd%d" % d, [128, 64])
        t1 = P.sb("t1_%d" % d, [128, 64]); sq = P.sb("sq%d" % d, [128, 64]); ss = P.sb("ss%d" % d, [128, 1]); sb_ = P.sb("sb%d" % d, [128, 1])
        for t in range(NT):
            b = t % 2; r0 = t * 128
            ck, pk_, nk = "cur%d" % b, "prv%d" % b, "nxt%d" % b
            P.dma(cur[b][:], rkv[d][r0:r0 + 128, :], w=[ck])
            if t == 0:
                P.V(lambda: nc.vector.memset(prv[b][:], 0.0), w=[pk_])
                P.dma(prv[b][1:128, :], rkv[d][0:127, :], wj=[pk_])
            else:
                P.dma(prv[b][:], rkv[d][r0 - 1:r0 + 127, :], w=[pk_])
            if t == NT - 1:
                P.V(lambda: nc.vector.memset(nxt[b][:], 0.0), w=[nk])
                P.dma(nxt[b][0:127, :], rkv[d][r0 + 1:r0 + 128, :], wj=[nk])
            else:
                P.dma(nxt[b][:], rkv[d][r0 + 1:r0 + 129, :], w=[nk])
            P.T(lambda: nc.tensor.matmul(ps_l[:, 0:64], lhsT=thT[:, r0:r0 + 128], rhs=w2b[:, 0, :], start=True, stop=True), r=["thT", "w2b%d" % d], w=["ps_l"])
            P.T(lambda: nc.tensor.matmul(ps_l[:, 64:128], lhsT=haT[:, r0:r0 + 128], rhs=w2b[:, 1, :], start=True, stop=True), r=["haT", "w2b%d" % d], wj=["ps_l"])
            P.V(lambda: nc.vector.tensor_tensor(out=d0[:], in0=prv[b][:], in1=cur[b][:], op=ALU.subtract), r=[pk_, ck], w=["d0"])
            P.V(lambda: nc.vector.tensor_tensor(out=d0[:], in0=d0[:], in1=mu0_bc[:], op=ALU.mult), r=["d0", "mu0_bc%d" % d], w=["d0"])
            P.V(lambda: nc.vector.tensor_tensor(out=d1[:], in0=nxt[b][:], in1=cur[b][:], op=ALU.subtract), r=[nk, ck], w=["d1"])
            P.V(lambda: nc.vector.tensor_tensor(out=d1[:], in0=d1[:], in1=mu1_bc[:], op=ALU.mult), r=["d1", "mu1_bc%d" % d], w=["d1"])
            P.V(lambda: nc.vector.tensor_tensor(out=d0[:], in0=d0[:], in1=d1[:], op=ALU.add), r=["d0", "d1"], w=["d0"])
            P.V(lambda: nc.vector.tensor_tensor(out=mix[:], in0=cur[b][:], in1=d0[:], op=ALU.add), r=[ck, "d0"], w=["mix"])
            rr, kp, vp = mix[:, 0:64], mix[:, 64:128], mix[:, 128:192]
            P.V(lambda: nc.vector.tensor_tensor(out=t1[:], in0=ps_l[:, 0:64], in1=w0_bc[:], op=ALU.add), r=["ps_l", "w0_bc%d" % d], w=["t1"])
            P.A(lambda: nc.scalar.activation(out=t1[:], in_=t1[:], func=AF.Sigmoid), r=["t1"], w=["t1"])
            P.A(lambda: nc.scalar.activation(out=dec[b][:], in_=t1[:], func=AF.Exp, scale=-EM05), r=["t1"], w=["dec%d" % b])
            P.V(lambda: nc.vector.tensor_tensor(out=icl[:], in0=ps_l[:, 64:128], in1=a0_bc[:], op=ALU.add), r=["ps_l", "a0_bc%d" % d], w=["icl"])
            P.A(lambda: nc.scalar.activation(out=icl[:], in_=icl[:], func=AF.Sigmoid), r=["icl"], w=["icl"])
            P.V(lambda: nc.vector.tensor_tensor(out=kkt[:], in0=kp, in1=kk_bc[:], op=ALU.mult), r=["mix", "kk_bc"], w=["kkt"])
            P.A(lambda: nc.scalar.activation(out=sq[:], in_=kkt[:], func=AF.Square, accum_out=ss[:]), r=["kkt"], w=["sq", "ss"])
            P.V(lambda: nc.vector.tensor_scalar(out=ss[:], in0=ss[:], scalar1=1e-12, scalar2=None, op0=ALU.add), r=["ss"], w=["ss"])
            P.A(lambda: nc.scalar.sqrt(out=ss[:], in_=ss[:]), r=["ss"], w=["ss"])
            P.V(lambda: nc.vector.reciprocal(out=ss[:], in_=ss[:]), r=["ss"], w=["ss"])
            P.V(lambda: nc.vector.tensor_scalar(out=kap[:], in0=kkt[:], scalar1=ss[:], scalar2=None, op0=ALU.mult), r=["kkt", "ss"], w=["kap"])
            P.V(lambda: nc.vector.tensor_tensor(out=t1[:], in0=icl[:], in1=ka_bc[:], op=ALU.mult), r=["icl", "ka_bc"], w=["t1"])
            P.V(lambda: nc.vector.scalar_tensor_tensor(out=t1[:], in0=t1[:], scalar=1.0, in1=ka_bc[:], op0=ALU.add, op1=ALU.subtract), r=["t1", "ka_bc"], w=["t1"])
            P.V(lambda: nc.vector.tensor_tensor(out=kd[:], in0=kp, in1=t1[:], op=ALU.mult), r=["mix", "t1"], w=["kd"])
            pkk = "pack%d" % b
            P.V(lambda: nc.vector.tensor_copy(out=pack[b][:, 0:64], in_=rr), r=["mix"], w=[pkk])
            P.V(lambda: nc.vector.tensor_copy(out=pack[b][:, 64:128], in_=kap[:]), r=["kap"], wj=[pkk])
            P.V(lambda: nc.vector.scalar_tensor_tensor(out=pack[b][:, 128:192], in0=icl[:], scalar=-1.0, in1=kap[:], op0=ALU.mult, op1=ALU.mult), r=["icl", "kap"], wj=[pkk])
            P.V(lambda: nc.vector.tensor_copy(out=pack[b][:, 192:256], in_=kd[:]), r=["kd"], wj=[pkk])
            P.dma(pkd[d][r0:r0 + 128, 0:64], pack[b][:, 0:64], r=[pkk], wj=["scr"], key=pkk)
            P.dma(pkd[d][r0:r0 + 128, 128:256], pack[b][:, 128:256], r=[pkk], wj=["scr"], key=pkk)
            if t == 0:
                P.dma(pkd[d][0:127, 64:128], pack[b][1:128, 64:128], r=[pkk], wj=["scr"], key=pkk)
            else:
                P.dma(pkd[d][r0 - 1:r0 + 127, 64:128], pack[b][:, 64:128], r=[pkk], wj=["scr"], key=pkk)
            if t == NT - 1:
                P.dma(pkd[d][S - 1:S, 64:128], zrow[0:1, :], r=["zrow"], wj=["scr"], key="zrow")
            P.dma(dkd[d][r0:r0 + 128, :], dec[b][:], r=["dec%d" % b], wj=["scr"], key="dec%d" % b)
            P.V(lambda: nc.vector.tensor_tensor(out=t1[:], in0=rr, in1=kd[:], op=ALU.mult), r=["mix", "kd"], w=["t1"])
            P.V(lambda: nc.vector.scalar_tensor_tensor(out=sq[:], in0=t1[:], scalar=1.0, in1=rk_bc[:], op0=ALU.mult, op1=ALU.mult, accum_out=sb_[:]),
                r=["t1", "rk_bc"], w=["sq", "sb_"])
            P.V(lambda: nc.vector.tensor_scalar(out=bon[b][:], in0=vp, scalar1=sb_[:], scalar2=None, op0=ALU.mult), r=["mix", "sb_"], w=["bon%d" % b])
            P.dma(bonus_o[d, r0:r0 + 128, :], bon[b][:], r=["bon%d" % b], wj=["bonus"], key="bon%d" % b)
            P.V(lambda: nc.vector.tensor_copy(out=vstage[:, 64 * d:64 * d + 64], in_=vp), r=["mix"], w=["vstage"])
            P.T(lambda: nc.tensor.transpose(ps_t[:, 0:128], vstage[:], ident[:]), r=["vstage", "ident32"], w=["ps_t"])
            P.V(lambda: nc.vector.tensor_copy(out=vto[b][64 * d:64 * d + 64, :], in_=ps_t[64 * d:64 * d + 64, 0:128]), r=["ps_t"], w=["vto%d" % b])
            P.dma(vTs[64 * d:64 * d + 64, r0:r0 + 128], vto[b][64 * d:64 * d + 64, :], r=["vto%d" % b], wj=["scr"], key="vto%d" % b)
    SEG = min(1024, S); NB = 4
    St = P.sb("St", [128, 64]); P.V(lambda: nc.vector.memset(St[:], 0.0), w=["S"])
    prod = P.sb("prod", [128, 2, 64])
    zcol = P.sb("zcol", [128, 1]); P.V(lambda: nc.vector.memset(zcol[:], 0.0), w=["zcol"])
    bcp = [P.ps("bcp%d" % i, [128, 512]) for i in range(NB)]
    pkt = [P.sb("pkt%d" % i, [64, 4, 256], BF16) for i in range(2)]
    dkt = [P.sb("dkt%d" % i, [64, 4, 64]) for i in range(2)]
    vseg = [P.sb("vseg%d" % i, [128, SEG]) for i in range(2)]
    yseg = [P.sb("yseg%d" % i, [128, SEG, 2]) for i in range(2)]
    yo = [P.sb("yo%d" % i, [128, 128]) for i in range(2)]
    St_b = St[:].unsqueeze(1).to_broadcast([128, 2, 64])
    step = 0; oi = 0
    sk_ap, sk_tok = zcol[:], "zcol"
    for sg in range(S // SEG):
        sb2 = sg % 2; vk = "vseg%d" % sb2; yk = "yseg%d" % sb2
        P.dma(vseg[sb2][:], vTs[:, sg * SEG:(sg + 1) * SEG], r=["scr"], w=[vk])
        for blk in range(SEG // 128):
            s0 = sg * SEG + blk * 128; bb = (s0 // 128) % 2
            pk_, dk_ = "pkt%d" % bb, "dkt%d" % bb
            for d in range(2):
                P.dma(pkt[bb][32 * d:32 * d + 32, :, :], pkd[d][s0:s0 + 128, :].rearrange("(g q) c -> q g c", q=32), r=["scr"], w=[pk_] if d == 0 else [], wj=[] if d == 0 else [pk_])
                P.dma(dkt[bb][32 * d:32 * d + 32, :, :], dkd[d][s0:s0 + 128, :].rearrange("(g q) c -> q g c", q=32), r=["scr"], w=[dk_] if d == 0 else [], wj=[] if d == 0 else [dk_])
            for g in range(4):
                for j in range(32):
                    sl = step % NB; step += 1; bk = "bcp%d" % sl
                    col = blk * 128 + g * 32 + j
                    P.T(lambda: nc.tensor.matmul(bcp[sl][:, 0:256], lhsT=zb[:, j * 128:(j + 1) * 128], rhs=pkt[bb][:, g, :], start=True, stop=True), r=["zb", pk_], w=[bk])
                    P.T(lambda: nc.tensor.matmul(bcp[sl][:, 256:320], lhsT=z32[:, j * 128:(j + 1) * 128], rhs=dkt[bb][:, g, :], start=True, stop=True), r=["z32", dk_], wj=[bk])
                    P.ses = bool(os.environ.get("FORCE_SES"))
                    nbb, kdb, wb = bcp[sl][:, 128:192], bcp[sl][:, 192:256], bcp[sl][:, 256:320]
                    rk2 = bcp[sl][:, 0:128].rearrange("p (a n) -> p a n", a=2)
                    P.V(lambda: nc.vector.tensor_tensor(out=St[:], in0=St[:], in1=wb, op=ALU.mult), r=["S", bk], w=["S"])
                    P.V(lambda: nc.vector.scalar_tensor_tensor(out=St[:], in0=nbb, scalar=sk_ap, in1=St[:], op0=ALU.mult, op1=ALU.add), r=["S", bk, sk_tok], w=["S"])
                    P.V(lambda: nc.vector.scalar_tensor_tensor(out=St[:], in0=kdb, scalar=vseg[sb2][:, col:col + 1], in1=St[:], op0=ALU.mult, op1=ALU.add), r=["S", bk, vk], w=["S"])
                    P.V(lambda: nc.vector.tensor_tensor(out=prod[:], in0=St_b, in1=rk2, op=ALU.mult), r=["S", bk], w=["prod"])
                    P.V(lambda: nc.vector.tensor_reduce(out=yseg[sb2][:, col, :], in_=prod[:], axis=AX.X, op=ALU.add), r=["prod"], wj=[yk])
                    P.ses = True
                    sk_ap, sk_tok = yseg[sb2][:, col, 1:2], yk
        for blk in range(SEG // 128):
            ob = oi % 2; oi += 1; r0 = sg * SEG + blk * 128
            P.T(lambda: nc.tensor.transpose(ps_t[:, 0:128], yseg[sb2][:, blk * 128:(blk + 1) * 128, 0], ident[:]), r=[yk, "ident32"], w=["ps_t"])
            P.V(lambda: nc.vector.tensor_copy(out=yo[ob][:], in_=ps_t[:, 0:128]), r=["ps_t"], w=["yo%d" % ob])
            for d in range(2):
                P.dma(y_o[d, r0:r0 + 128, :], yo[ob][:, 64 * d:64 * d + 64], r=["yo%d" % ob], wj=["y"], key="yo%d" % ob)
        P.V(lambda: nc.vector.memset(zcol[:], 0.0), r=[yk], w=["zcol2"])
    P.finish(); P.close()
    return nc


def zsel_const():
    z = np.zeros((64, 32, 128), np.float32)
    for j in range(32):
        z[j, j, 0:64] = 1.0
        z[32 + j, j, 64:128] = 1.0
    return z.reshape(64, 32 * 128)


def mb_inputs(zb_b, zl_b, prm, l, h):
    hs = slice(h * 64, (h + 1) * 64)
    rkvh = np.concatenate([zb_b[:, h * 64:(h + 1) * 64], zb_b[:, 256 + h * 64:256 + (h + 1) * 64], zb_b[:, 512 + h * 64:512 + (h + 1) * 64]], axis=1)
    m = {"ident": np.eye(128, dtype=np.float32), "zsel": zsel_const(),
         "w2": np.ascontiguousarray(prm["rwkv_w2"][l][:, :, hs]), "a2": np.ascontiguousarray(prm["rwkv_a2"][l][:, :, hs]),
         "w0": np.ascontiguousarray(prm["rwkv_w0"][l][:, hs]), "a0": np.ascontiguousarray(prm["rwkv_a0"][l][:, hs]),
         "kka": np.stack([prm["rwkv_k_k"][l][hs], prm["rwkv_k_a"][l][hs], prm["rwkv_r_k"][l][hs]])}
    mur = prm["rwkv_mu_rkv"][l]
    muh = np.stack([np.concatenate([mur[i, j, hs] for j in range(3)]) for i in range(2)])
    m["mu"] = np.stack([muh, muh[::-1]])
    for d in range(2):
        o = lambda a: np.ascontiguousarray(a if d == 0 else a[::-1])
        m["rkv%d" % d] = o(rkvh)
        base = d * 256
        for wi, nm in enumerate("wa"):
            m["h%s1_%d" % (nm, d)] = np.ascontiguousarray(o(zl_b[:, base + wi * 128:base + wi * 128 + 64]).T)
            m["h%s2_%d" % (nm, d)] = np.ascontiguousarray(o(zl_b[:, base + wi * 128 + 64:base + wi * 128 + 128]).T)
    return m


def run_MB(zb, zl, prm, l):
    B, S, _ = zb.shape
    nc = build_MB(S)
    maps = [mb_inputs(zb[c // 4], zl[c // 4], prm, l, c % 4) for c in range(B * 4)]
    res = run_bass_kernel_spmd(nc, maps, core_ids=list(range(B * 4)))
    outs = [np.zeros((B, S, 256), np.float32) for _ in range(4)]
    for c in range(B * 4):
        b, h = c // 4, c % 4
        r = res.results[c]
        outs[0][b, :, h * 64:(h + 1) * 64] = r["y"][0]
        outs[1][b, :, h * 64:(h + 1) * 64] = r["y"][1][::-1]
        outs[2][b, :, h * 64:(h + 1) * 64] = r["bonus"][0]
        outs[3][b, :, h * 64:(h + 1) * 64] = r["bonus"][1][::-1]
    return outs


def gen_MD(P0, S):
    P = NS(P0, "d_"); nc = P.nc
    q = P.dram("q", [S, 64]); k = P.dram("k", [S, 64]); v = P.dram("v", [S, 64])
    qg = P.dram("qg", [1, 64]); kg = P.dram("kg", [1, 64]); cos_d = P.dram("cos", [S, 32]); sin_d = P.dram("sin", [S, 32])
    ident_d = P.dram("ident", [128, 128])
    yT = P.dram("yT", [64, S], kind="ExternalOutput")
    NT = S // 128
    ident = load_ident32(P, nc, ident_d)
    ones = P.sb("ones", [128, 64]); P.V(lambda: nc.vector.memset(ones[:], 1.0), w=["ones"])
    qT = P.sb("qT", [64, S], BF16); kT = P.sb("kT", [64, S], BF16)
    ps_t = P.ps("ps_t", [128, 512])
    prep_qk(P, nc, q, qT, 0, S, 32, cos_d, sin_d, ident, 0.125, qg, "q", ps_t)
    prep_qk(P, nc, k, kT, 0, S, 32, cos_d, sin_d, ident, 1.0, kg, "k", ps_t)
    Vx = P.sb("Vx", [128, NT, 65], BF16)
    P.V(lambda: nc.vector.memset(Vx[:, :, 64:65], 1.0), w=["Vx"])
    VC = min(16, NT)
    vst = [P.sb("vst%d" % i, [128, VC, 64]) for i in range(2)]
    for ci, m0 in enumerate(range(0, NT, VC)):
        vb = ci % 2
        P.dma(vst[vb][:], v[m0 * 128:(m0 + VC) * 128, :].rearrange("(m p) c -> p m c", p=128), w=["vst%d" % vb])
        P.V(lambda: nc.vector.tensor_copy(out=Vx[:, m0:m0 + VC, 0:64], in_=vst[vb][:]), r=["vst%d" % vb], wj=["Vx"])
    acc = [P.sb("acc%d" % i, [65, 512]) for i in range(2)]
    rl = P.sb("rl", [65, 512]); ob = [P.sb("ob%d" % i, [64, 512]) for i in range(2)]
    NB = 2
    ps_s = [P.ps("ps_s%d" % i, [128, 512]) for i in range(NB)]
    pT = [P.sb("pT%d" % i, [128, 512], BF16) for i in range(NB)]
    po = [P.ps("po0", [128, 512])]
    ps_bc = ps_t
    W = min(512, S)
    its = [(qb, kt) for qb in range(S // W) for kt in range(NT)]
    LA = 1

    def emit_S(i):
        qb, kt = its[i]; b = i % NB
        P.T(lambda: nc.tensor.matmul(ps_s[b][:, 0:W], lhsT=kT[:, kt * 128:(kt + 1) * 128], rhs=qT[:, qb * W:(qb + 1) * W], start=True, stop=True),
            r=["qT", "kT"], w=["ps_s%d" % b])

    for i in range(min(LA, len(its))):
        emit_S(i)
    for i, (qb, kt) in enumerate(its):
        b = i % NB; pb = qb % 2; pq = 0
        if i + LA < len(its):
            emit_S(i + LA)
        P.A(lambda: nc.scalar.activation(out=pT[b][:, 0:W], in_=ps_s[b][:, 0:W], func=AF.Exp), r=["ps_s%d" % b], w=["pT%d" % b])
        P.T(lambda: nc.tensor.matmul(po[pq][0:65, 0:W], lhsT=Vx[:, kt, :], rhs=pT[b][:, 0:W], start=(kt == 0), stop=(kt == NT - 1)),
            r=["Vx", "pT%d" % b], w=["po%d" % pq] if kt == 0 else [], wj=[] if kt == 0 else ["po%d" % pq])
        if kt == NT - 1:
            P.V(lambda: nc.vector.tensor_copy(out=acc[pb][:, 0:W], in_=po[pq][0:65, 0:W]), r=["po%d" % pq], w=["acc%d" % pb])
            normalize_blk(P, nc, acc[pb], "acc%d" % pb, W, yT, qb * W, ones, ps_bc, rl, ob[pb], "ob%d" % pb)
        yield 1
    P.close_ns()
    yield 'done'


def gen_MA(P0, S):
    P = NS(P0, "a_"); nc = P.nc
    PADC = 1024
    DIL = (1, 4, 16)
    q = P.dram("q", [S, 64]); k = P.dram("k", [S, 64])
    vx = [P.dram("vx%d" % d, [d * (S // d + 128), 65]) for d in DIL]
    cos_d = P.dram("cos", [S, 8]); sin_d = P.dram("sin", [S, 8])
    ident_d = P.dram("ident", [128, 128]); mask_d = P.dram("mask", [128, 256])
    yT = P.dram("yT", [64, S], kind="ExternalOutput")
    ident = load_ident32(P, nc, ident_d)
    ones = P.sb("ones", [128, 64]); P.V(lambda: nc.vector.memset(ones[:], 1.0), w=["ones"])
    m32 = P.sb("m32", [128, 256]); mask = P.sb("maskb", [128, 256], BF16)
    P.dma(m32[:], mask_d[:, :], w=["m32"]); P.V(lambda: nc.vector.tensor_copy(out=mask[:], in_=m32[:]), r=["m32"], w=["mask"])
    qT = P.sb("qT", [64, S + 2 * PADC], BF16); kT = P.sb("kT", [64, S + 2 * PADC], BF16)
    P.V(lambda: nc.vector.memset(kT[:, 0:PADC], 0.0), w=["kT"]); P.V(lambda: nc.vector.memset(kT[:, PADC + S:], 0.0), wj=["kT"])
    P.V(lambda: nc.vector.memset(qT[:, 0:PADC], 0.0), w=["qT"]); P.V(lambda: nc.vector.memset(qT[:, PADC + S:], 0.0), wj=["qT"])
    ps_t = P.ps("ps_t", [128, 512])
    prep_qk(P, nc, q, qT, PADC, S, 8, cos_d, sin_d, ident, 0.125, None, "q", ps_t)
    prep_qk(P, nc, k, kT, PADC, S, 8, cos_d, sin_d, ident, 1.0, None, "k", ps_t)
    SB = 2048
    accT = P.sb("accT", [65, SB])
    NB = 2
    ps_s = [P.ps("ps_s%d" % i, [128, 512]) for i in range(NB)]
    pT = [P.sb("pT%d" % i, [128, 256], BF16) for i in range(NB)]
    pTm = [P.sb("pTm%d" % i, [128, 256], BF16) for i in range(NB)]
    po = [P.ps("po%d" % i, [128, 512]) for i in range(NB)]
    ps_bc = ps_t
    rl = P.sb("rl", [65, 512]); ob = [P.sb("ob%d" % i, [64, 512]) for i in range(2)]
    Vxs = {}
    vst = [P.sb("vst%d" % i, [128, 16, 65]) for i in range(2)]
    ci = 0
    for di, d in enumerate(DIL):
        L = S // d; NKT = L // 128 + 1; NTL = d * NKT
        Vx = P.sb("Vx%d" % d, [128, NTL, 65], BF16)
        for m0 in range(0, NTL, 16):
            m1 = min(NTL, m0 + 16); vb = ci % 2; ci += 1
            P.dma(vst[vb][:, 0:m1 - m0, :], vx[di][m0 * 128:m1 * 128, :].rearrange("(m p) c -> p m c", p=128), w=["vst%d" % vb])
            P.V(lambda: nc.vector.tensor_copy(out=Vx[:, m0:m1, :], in_=vst[vb][:, 0:m1 - m0, :]), r=["vst%d" % vb], wj=["Vx%d" % d])
        Vxs[d] = (Vx, NKT)
    oi = 0
    its = []
    for sbi in range(S // SB):
        for di, d in enumerate(DIL):
            for r in range(d):
                for bl in range(SB // (128 * d)):
                    its.append((sbi, di, d, r, bl))
    LA = 1

    def emit_S(i):
        sbi, di, d, r, bl = its[i]; b = i % NB
        i0 = (sbi * SB // d) + bl * 128
        qs = PADC + r + d * i0
        rhs = qT[:, qs:qs + 127 * d + 1:d]
        for ab in range(2):
            ks = PADC + r + d * (i0 - 64 + 128 * ab)
            P.T(lambda: nc.tensor.matmul(ps_s[b][:, ab * 128:(ab + 1) * 128], lhsT=kT[:, ks:ks + 127 * d + 1:d], rhs=rhs, start=True, stop=True),
                r=["qT", "kT"], w=["ps_s%d" % b] if ab == 0 else [], wj=[] if ab == 0 else ["ps_s%d" % b])

    for i in range(min(LA, len(its))):
        emit_S(i)
    for i, (sbi, di, d, r, bl) in enumerate(its):
        b = i % NB
        Vx, NKT = Vxs[d]
        i0 = (sbi * SB // d) + bl * 128; blk = i0 // 128
        if i + LA < len(its):
            emit_S(i + LA)
        P.A(lambda: nc.scalar.activation(out=pT[b][:], in_=ps_s[b][:, 0:256], func=AF.Exp), r=["ps_s%d" % b], w=["pT%d" % b])
        P.G(lambda: nc.gpsimd.tensor_tensor(out=pTm[b][:], in0=pT[b][:], in1=mask[:], op=ALU.mult), r=["pT%d" % b, "mask"], w=["pTm%d" % b])
        for ab in range(2):
            P.T(lambda: nc.tensor.matmul(po[b][0:65, 0:128], lhsT=Vx[:, r * NKT + blk + ab, :], rhs=pTm[b][:, ab * 128:(ab + 1) * 128], start=(ab == 0), stop=(ab == 1)),
                r=["Vx%d" % d, "pTm%d" % b], w=["po%d" % b] if ab == 0 else [], wj=[] if ab == 0 else ["po%d" % b])
        t0 = r + d * bl * 128
        dst = accT[:, t0:t0 + 127 * d + 1:d]
        if di == 0:
            P.V(lambda: nc.vector.tensor_copy(out=dst, in_=po[b][0:65, 0:128]), r=["po%d" % b], w=["accT"] if (bl == 0) else [], wj=[] if (bl == 0) else ["accT"])
        else:
            P.V(lambda: nc.vector.tensor_tensor(out=dst, in0=dst, in1=po[b][0:65, 0:128], op=ALU.add), r=["po%d" % b, "accT"], w=["accT"])
        last = (i + 1 == len(its)) or (its[i + 1][0] != sbi)
        if last:
            for c0 in range(0, SB, 512):
                o = oi % 2; oi += 1
                normalize_blk(P, nc, accT[:, c0:c0 + 512], "accT", 512, yT, sbi * SB + c0, ones, ps_bc, rl, ob[o], "ob%d" % o)
        yield 1
    P.close_ns()
    yield 'done'


def gen_MC(P0, S):
    P = NS(P0, "c_"); nc = P.nc
    T = min(512, S); NCH = S // T
    uT_d = [P.dram("uT%d" % d, [64, S]) for d in range(2)]
    a_re = P.dram("a_re", [2, 4, 64]); a_im = P.dram("a_im", [2, 4, 64]); ldt = P.dram("ldt", [2, 4])
    b_re = P.dram("b_re", [4, 64, 16]); b_im = P.dram("b_im", [4, 64, 16])
    c_re = P.dram("c_re", [2, 4, 16, 64]); c_im = P.dram("c_im", [2, 4, 16, 64])
    ident_d = P.dram("ident", [128, 128]); iota_d = P.dram("iota", [1, T + 1])
    yT_d = [P.dram("yT%d" % d, [64, S], kind="ExternalOutput") for d in range(2)]
    ident = load_ident32(P, nc, ident_d)
    iota = P.sb("iota", [128, T + 1]); P.dma(iota[:], iota_d[0:1, :].partition_broadcast(128), w=["iota"])
    uT = []
    UC = min(2048, S)
    ust = [P.sb("ust%d" % i, [64, UC]) for i in range(2)]
    ui = 0
    for d in range(2):
        u = P.sb("uTb%d" % d, [64, S], BF16)
        for c0 in range(0, S, UC):
            ub = ui % 2; ui += 1
            P.dma(ust[ub][:], uT_d[d][:, c0:c0 + UC], w=["ust%d" % ub])
            P.V(lambda: nc.vector.tensor_copy(out=u[:, c0:c0 + UC], in_=ust[ub][:]), r=["ust%d" % ub], wj=["uT%d" % d])
        uT.append(u)
    ps_t = P.ps("ps_t", [128, 512])
    tiles = {}
    for d in range(2):
        for gp in range(2):
            n = "t%d%d" % (d, gp)
            prm = P.sb(n + "prm", [128, 16])
            pk = n + "prm"
            P.dma(prm[:, 0:1], a_re[d, 2 * gp:2 * gp + 2, :].rearrange("g (p o) -> (g p) o", o=1), w=[pk])
            P.dma(prm[:, 1:2], a_im[d, 2 * gp:2 * gp + 2, :].rearrange("g (p o) -> (g p) o", o=1), wj=[pk])
            for g in range(2):
                P.dma(prm[g * 64:(g + 1) * 64, 2:3], ldt[d:d + 1, 2 * gp + g:2 * gp + g + 1].partition_broadcast(64), wj=[pk])
            c = lambda i: prm[:, i:i + 1]
            P.A(lambda: nc.scalar.activation(out=c(2), in_=c(2), func=AF.Exp), r=[pk], w=[pk])
            P.V(lambda: nc.vector.tensor_tensor(out=c(4), in0=c(1), in1=c(2), op=ALU.mult), r=[pk], w=[pk])
            P.V(lambda: nc.vector.tensor_tensor(out=c(3), in0=c(0), in1=c(2), op=ALU.mult), r=[pk], w=[pk])
            P.A(lambda: nc.scalar.activation(out=c(3), in_=c(3), func=AF.Exp), r=[pk], w=[pk])
            cosT = P.sb(n + "cos", [128, T + 1]); sinT = P.sb(n + "sin", [128, T + 1]); ki = P.sb(n + "ki", [128, T + 1], I32)
            P.V(lambda: nc.vector.tensor_scalar(out=sinT[:], in0=iota[:], scalar1=c(4), scalar2=None, op0=ALU.mult), r=["iota", pk], w=[n + "sin"])
            P.V(lambda: nc.vector.tensor_scalar(out=cosT[:], in0=sinT[:], scalar1=PI / 2, scalar2=None, op0=ALU.add), r=[n + "sin"], w=[n + "cos"])
            range_reduce(P, nc, sinT, ki, n + "sin", [128, T + 1])
            range_reduce(P, nc, cosT, ki, n + "cos", [128, T + 1])
            P.A(lambda: nc.scalar.activation(out=sinT[:], in_=sinT[:], func=AF.Sin), r=[n + "sin"], w=[n + "sin"])
            P.A(lambda: nc.scalar.activation(out=cosT[:], in_=cosT[:], func=AF.Sin), r=[n + "cos"], w=[n + "cos"])
            P.V(lambda: nc.vector.tensor_copy(out=c(5), in_=cosT[:, 1:2]), r=[n + "cos", pk], w=[pk])
            P.V(lambda: nc.vector.tensor_copy(out=c(6), in_=sinT[:, 1:2]), r=[n + "sin", pk], w=[pk])
            P.V(lambda: nc.vector.tensor_copy(out=c(13), in_=cosT[:, T:T + 1]), r=[n + "cos", pk], w=[pk])
            P.V(lambda: nc.vector.tensor_copy(out=c(14), in_=sinT[:, T:T + 1]), r=[n + "sin", pk], w=[pk])
            P.V(lambda: nc.vector.tensor_scalar(out=c(15), in0=c(14), scalar1=-1.0, scalar2=None, op0=ALU.mult), r=[pk], w=[pk])
            P.V(lambda: nc.vector.tensor_tensor(out=c(5), in0=c(5), in1=c(3), op=ALU.mult), r=[pk], w=[pk])
            P.V(lambda: nc.vector.tensor_tensor(out=c(6), in0=c(6), in1=c(3), op=ALU.mult), r=[pk], w=[pk])
            P.V(lambda: nc.vector.tensor_scalar(out=c(7), in0=c(5), scalar1=-1.0, scalar2=None, op0=ALU.add), r=[pk], w=[pk])
            P.V(lambda: nc.vector.tensor_tensor(out=c(8), in0=c(0), in1=c(0), op=ALU.mult), r=[pk], w=[pk])
            P.V(lambda: nc.vector.tensor_tensor(out=c(11), in0=c(1), in1=c(1), op=ALU.mult), r=[pk], w=[pk])
            P.V(lambda: nc.vector.tensor_tensor(out=c(8), in0=c(8), in1=c(11), op=ALU.add), r=[pk], w=[pk])
            P.V(lambda: nc.vector.reciprocal(out=c(8), in_=c(8)), r=[pk], w=[pk])
            P.V(lambda: nc.vector.tensor_tensor(out=c(9), in0=c(7), in1=c(0), op=ALU.mult), r=[pk], w=[pk])
            P.V(lambda: nc.vector.tensor_tensor(out=c(11), in0=c(6), in1=c(1), op=ALU.mult), r=[pk], w=[pk])
            P.V(lambda: nc.vector.tensor_tensor(out=c(9), in0=c(9), in1=c(11), op=ALU.add), r=[pk], w=[pk])
            P.V(lambda: nc.vector.tensor_tensor(out=c(9), in0=c(9), in1=c(8), op=ALU.mult), r=[pk], w=[pk])
            P.V(lambda: nc.vector.tensor_tensor(out=c(10), in0=c(6), in1=c(0), op=ALU.mult), r=[pk], w=[pk])
            P.V(lambda: nc.vector.tensor_tensor(out=c(11), in0=c(7), in1=c(1), op=ALU.mult), r=[pk], w=[pk])
            P.V(lambda: nc.vector.tensor_tensor(out=c(10), in0=c(10), in1=c(11), op=ALU.subtract), r=[pk], w=[pk])
            P.V(lambda: nc.vector.tensor_tensor(out=c(10), in0=c(10), in1=c(8), op=ALU.mult), r=[pk], w=[pk])
            P.V(lambda: nc.vector.tensor_scalar(out=c(12), in0=c(10), scalar1=-1.0, scalar2=None, op0=ALU.mult), r=[pk], w=[pk])
            braw = P.sb(n + "braw", [128, 2, 16]); bk = n + "braw"
            P.dma(braw[:, 0, :], b_re[2 * gp:2 * gp + 2].rearrange("g p c -> (g p) c"), w=[bk])
            P.dma(braw[:, 1, :], b_im[2 * gp:2 * gp + 2].rearrange("g p c -> (g p) c"), wj=[bk])
            BD = P.sb(n + "BD", [128, 2, 64]); tb = P.sb(n + "tb", [128, 16])
            P.V(lambda: nc.vector.memset(BD[:], 0.0), w=[n + "BD"])
            for g in range(2):
                rows = slice(g * 64, (g + 1) * 64); cols = slice(32 * gp + 16 * g, 32 * gp + 16 * g + 16)
                P.V(lambda: nc.vector.tensor_scalar(out=tb[rows, :], in0=braw[rows, 1, :], scalar1=prm[rows, 12:13], scalar2=None, op0=ALU.mult), r=[bk, pk], w=[n + "tb"])
                P.V(lambda: nc.vector.scalar_tensor_tensor(out=BD[rows, 0, cols], in0=braw[rows, 0, :], scalar=prm[rows, 9:10], in1=tb[rows, :], op0=ALU.mult, op1=ALU.add),
                    r=[bk, pk, n + "tb"], wj=[n + "BD"])
                P.V(lambda: nc.vector.tensor_scalar(out=tb[rows, :], in0=braw[rows, 0, :], scalar1=prm[rows, 10:11], scalar2=None, op0=ALU.mult), r=[bk, pk], w=[n + "tb"])
                P.V(lambda: nc.vector.scalar_tensor_tensor(out=BD[rows, 1, cols], in0=braw[rows, 1, :], scalar=prm[rows, 9:10], in1=tb[rows, :], op0=ALU.mult, op1=ALU.add),
                    r=[bk, pk, n + "tb"], wj=[n + "BD"])
            BT = P.sb(n + "BT", [64, 2, 128], BF16)
            for ri in range(2):
                P.T(lambda: nc.tensor.transpose(ps_t[0:64, 0:128], BD[:, ri, :], ident[:]), r=[n + "BD", "ident32"], w=["ps_t"])
                P.V(lambda: nc.vector.tensor_copy(out=BT[:, ri, :], in_=ps_t[0:64, 0:128]), r=["ps_t"], wj=[n + "BT"])
            craw = P.sb(n + "craw", [128, 2, 16]); ck = n + "craw"
            for g in range(2):
                P.dma(craw[g * 64:(g + 1) * 64, 0, :], c_re[d, 2 * gp + g].rearrange("c p -> p c"), wj=[ck], allow_slow_non_contiguous=True)
                P.dma(craw[g * 64:(g + 1) * 64, 1, :], c_im[d, 2 * gp + g].rearrange("c p -> p c"), wj=[ck], allow_slow_non_contiguous=True)
            CT = P.sb(n + "CT", [128, 2, 64], BF16)
            P.V(lambda: nc.vector.memset(CT[:], 0.0), w=[n + "CT"])
            for g in range(2):
                rows = slice(g * 64, (g + 1) * 64); cols = slice(32 * gp + 16 * g, 32 * gp + 16 * g + 16)
                P.V(lambda: nc.vector.tensor_copy(out=CT[rows, 0, cols], in_=craw[rows, 0, :]), r=[ck], wj=[n + "CT"])
                P.V(lambda: nc.vector.tensor_scalar(out=CT[rows, 1, cols], in0=craw[rows, 1, :], scalar1=-1.0, scalar2=None, op0=ALU.mult), r=[ck], wj=[n + "CT"])
            rho_t = P.sb(n + "rho", [128, T])
            P.V(lambda: nc.vector.tensor_scalar(out=rho_t[:], in0=iota[:, 0:T], scalar1=0.0, scalar2=prm[:, 3:4], op0=ALU.mult, op1=ALU.add), r=["iota", pk], w=[n + "rho"])
            init = P.sb(n + "init", [128, 2]); P.V(lambda: nc.vector.memset(init[:], 0.0), w=[n + "init"])
            tiles[(d, gp)] = dict(n=n, prm=prm, pk=pk, cosT=cosT, sinT=sinT, BT=BT, CT=CT, rho=rho_t, init=init)
    ps_b = [P.ps("ps_b%d" % i, [128, 512]) for i in range(2)]
    ps_y = P.ps("ps_y", [128, 512])
    m = [P.sb("m%d" % i, [128, T]) for i in range(4)]
    bp = [P.sb("bp%d" % i, [128, T]) for i in range(2)]
    wv = [P.sb("wv%d" % i, [128, T]) for i in range(2)]
    pp = [P.sb("pp%d" % i, [128, T]) for i in range(4)]
    xb = [[P.sb("xb%d_%d" % (gp, i), [128, T], BF16) for i in range(2)] for gp in range(2)]
    yo = [P.sb("yo%d" % i, [64, T]) for i in range(2)]
    tmpc = P.sb("tmpc", [128, 2])
    it = 0
    for d in range(2):
        for ch in range(NCH):
            cols = slice(ch * T, (ch + 1) * T)
            for gp in range(2):
                t = tiles[(d, gp)]; n = t["n"]; cosT, sinT = t["cosT"], t["sinT"]
                for ri in range(2):
                    P.T(lambda: nc.tensor.matmul(ps_b[ri][:, 0:T], lhsT=t["BT"][:, ri, :], rhs=uT[d][:, cols], start=True, stop=True), r=[n + "BT", "uT%d" % d], w=["ps_b%d" % ri])
                P.V(lambda: nc.vector.tensor_tensor(out=m[0][:], in0=ps_b[0][:, 0:T], in1=cosT[:, 0:T], op=ALU.mult), r=["ps_b0", n + "cos"], w=["m0"])
                P.V(lambda: nc.vector.tensor_tensor(out=m[1][:], in0=ps_b[1][:, 0:T], in1=sinT[:, 0:T], op=ALU.mult), r=["ps_b1", n + "sin"], w=["m1"])
                P.V(lambda: nc.vector.tensor_tensor(out=m[2][:], in0=ps_b[1][:, 0:T], in1=cosT[:, 0:T], op=ALU.mult), r=["ps_b1", n + "cos"], w=["m2"])
                P.V(lambda: nc.vector.tensor_tensor(out=m[3][:], in0=ps_b[0][:, 0:T], in1=sinT[:, 0:T], op=ALU.mult), r=["ps_b0", n + "sin"], w=["m3"])
                P.G(lambda: nc.gpsimd.tensor_tensor(out=bp[0][:], in0=m[0][:], in1=m[1][:], op=ALU.add), r=["m0", "m1"], w=["bp0"])
                P.G(lambda: nc.gpsimd.tensor_tensor(out=bp[1][:], in0=m[2][:], in1=m[3][:], op=ALU.subtract), r=["m2", "m3"], w=["bp1"])
                for ri in range(2):
                    P.V(lambda: nc.vector.tensor_tensor_scan(out=wv[ri][:], data0=t["rho"][:], data1=bp[ri][:], initial=t["init"][:, ri:ri + 1], op0=ALU.mult, op1=ALU.add),
                        r=[n + "rho", "bp%d" % ri, n + "init"], w=["wv%d" % ri])
                prm = t["prm"]
                P.V(lambda: nc.vector.tensor_scalar(out=tmpc[:, 0:1], in0=wv[0][:, T - 1:T], scalar1=prm[:, 13:14], scalar2=None, op0=ALU.mult), r=["wv0", t["pk"]], w=["tmpc"])
                P.V(lambda: nc.vector.tensor_scalar(out=tmpc[:, 1:2], in0=wv[0][:, T - 1:T], scalar1=prm[:, 14:15], scalar2=None, op0=ALU.mult), r=["wv0", t["pk"]], wj=["tmpc"])
                P.V(lambda: nc.vector.scalar_tensor_tensor(out=t["init"][:, 0:1], in0=wv[1][:, T - 1:T], scalar=prm[:, 15:16], in1=tmpc[:, 0:1], op0=ALU.mult, op1=ALU.add),
                    r=["wv1", "tmpc", t["pk"]], w=[n + "init"])
                P.V(lambda: nc.vector.scalar_tensor_tensor(out=t["init"][:, 1:2], in0=wv[1][:, T - 1:T], scalar=prm[:, 13:14], in1=tmpc[:, 1:2], op0=ALU.mult, op1=ALU.add),
                    r=["wv1", "tmpc", t["pk"]], wj=[n + "init"])
                P.G(lambda: nc.gpsimd.tensor_tensor(out=pp[0][:], in0=wv[0][:], in1=cosT[:, 0:T], op=ALU.mult), r=["wv0", n + "cos"], w=["pp0"])
                P.G(lambda: nc.gpsimd.tensor_tensor(out=pp[1][:], in0=wv[1][:], in1=sinT[:, 0:T], op=ALU.mult), r=["wv1", n + "sin"], w=["pp1"])
                P.G(lambda: nc.gpsimd.tensor_tensor(out=xb[gp][0][:], in0=pp[0][:], in1=pp[1][:], op=ALU.subtract), r=["pp0", "pp1"], w=["xb%d_0" % gp])
                P.G(lambda: nc.gpsimd.tensor_tensor(out=pp[2][:], in0=wv[0][:], in1=sinT[:, 0:T], op=ALU.mult), r=["wv0", n + "sin"], w=["pp2"])
                P.G(lambda: nc.gpsimd.tensor_tensor(out=pp[3][:], in0=wv[1][:], in1=cosT[:, 0:T], op=ALU.mult), r=["wv1", n + "cos"], w=["pp3"])
                P.G(lambda: nc.gpsimd.tensor_tensor(out=xb[gp][1][:], in0=pp[2][:], in1=pp[3][:], op=ALU.add), r=["pp2", "pp3"], w=["xb%d_1" % gp])
                yield 1
            k = 0
            for gp in range(2):
                t = tiles[(d, gp)]
                for ri in range(2):
                    P.T(lambda: nc.tensor.matmul(ps_y[0:64, 0:T], lhsT=t["CT"][:, ri, :], rhs=xb[gp][ri][:], start=(k == 0), stop=(k == 3)),
                        r=[t["n"] + "CT", "xb%d_%d" % (gp, ri)], w=["ps_y"] if k == 0 else [], wj=[] if k == 0 else ["ps_y"])
                    k += 1
            b = it % 2; it += 1
            P.V(lambda: nc.vector.tensor_copy(out=yo[b][:], in_=ps_y[0:64, 0:T]), r=["ps_y"], w=["yo%d" % b])
            P.dma(yT_d[d][:, cols], yo[b][:], r=["yo%d" % b], wj=["yT%d" % d], key="yo%d" % b)
    P.close_ns()
    yield 'done'


def gen_MB(P0, S):
    Pp = NS(P0, "b_"); P = Pp; nc = P0.nc
    Ps = NS(P0, "b_")
    NT = S // 128
    rkv = [P.dram("rkv%d" % d, [S, 192]) for d in range(2)]
    h1 = [[P.dram("h%s1_%d" % (n, d), [64, S]) for d in range(2)] for n in "wa"]
    h2 = [[P.dram("h%s2_%d" % (n, d), [64, S]) for d in range(2)] for n in "wa"]
    w2 = P.dram("w2", [2, 64, 64]); a2 = P.dram("a2", [2, 64, 64]); w0 = P.dram("w0", [2, 64]); a0 = P.dram("a0", [2, 64])
    mu = P.dram("mu", [2, 2, 192]); kka = P.dram("kka", [3, 64])
    ident_d = P.dram("ident", [128, 128]); zsel_d = P.dram("zsel", [64, 32 * 128])
    y_o = P.dram("y", [2, S, 64], kind="ExternalOutput"); bonus_o = P.dram("bonus", [2, S, 64], kind="ExternalOutput")
    pkd = [P.dram("pkd%d" % d, [S, 384], BF16, kind="Internal") for d in range(2)]
    vTs = P.dram("vTs", [128, S], F32, kind="Internal")
    i32 = Ps.sb("ident32", [128, 128]); P.dma(i32[:], ident_d[:, :], w=["ident32"]); ident = i32
    zb = Ps.sb("zb", [64, 32 * 128], BF16)
    SEG = min(512, S); NB = 2
    St = Ps.sb("St", [128, 64]); prod = Ps.sb("prod", [128, 2, 64]); zcol = Ps.sb("zcol", [128, 1])
    bcp = [Ps.ps("bcp%d" % i, [128, 512]) for i in range(NB)]
    pkt = [Ps.sb("pkt%d" % i, [64, 4, 384], BF16) for i in range(2)]
    vseg = [Ps.sb("vseg%d" % i, [128, SEG]) for i in range(2)]
    yseg = [Ps.sb("yseg%d" % i, [128, SEG, 2]) for i in range(2)]
    yo = [Ps.sb("yo%d" % i, [128, 128]) for i in range(2)]
    ps_t = Ps.ps("ps_t", [128, 512])
    z32 = Pp.sb("z32", [64, 32 * 128])
    P.dma(z32[:], zsel_d[:, :], w=["z32"]); P.V(lambda: nc.vector.tensor_copy(out=zb[:], in_=z32[:]), r=["z32"], w=["zb"])
    def bc(name, src, n):
        t = Pp.sb(name, [128, n]); P.dma(t[:], src.partition_broadcast(128), w=[name]); return t
    kk_bc = bc("kk_bc", kka[0:1, :], 64); ka_bc = bc("ka_bc", kka[1:2, :], 64); rk_bc = bc("rk_bc", kka[2:3, :], 64)
    ps_l = ps_t[:, 256:512]
    thT = P.sb("thT", [64, S], BF16); haT = P.sb("haT", [64, S], BF16)
    CW = min(2048, S)
    ha = P.sb("ha_", [64, CW]); hb = P.sb("hb_", [64, CW])
    vstage = P.sb("vstage", [128, 128]); P.V(lambda: nc.vector.memset(vstage[:], 0.0), w=["vstage"])
    zrow = P.sb("zrow", [1, 64], BF16); P.V(lambda: nc.vector.memset(zrow[:], 0.0), w=["zrow"])
    for d in range(2):
        for wi, dst in enumerate((thT, haT)):
            dk_ = "thT" if wi == 0 else "haT"
            for ci, c0 in enumerate(range(0, S, CW)):
                P.dma(ha[:], h1[wi][d][:, c0:c0 + CW], w=["ha"])
                if c0 == 0:
                    P.V(lambda: nc.vector.memset(hb[:, 0:1], 0.0), w=["hb"])
                    P.dma(hb[:, 1:CW], h2[wi][d][:, 0:CW - 1], wj=["hb"])
                else:
                    P.dma(hb[:], h2[wi][d][:, c0 - 1:c0 + CW - 1], w=["hb"])
                P.V(lambda: nc.vector.tensor_tensor(out=ha[:], in0=ha[:], in1=hb[:], op=ALU.add), r=["ha", "hb"], w=["ha"])
                first = (ci == 0)
                if wi == 0:
                    P.A(lambda: nc.scalar.activation(out=dst[:, c0:c0 + CW], in_=ha[:], func=AF.Tanh), r=["ha"], w=[dk_] if first else [], wj=[] if first else [dk_])
                else:
                    P.V(lambda: nc.vector.tensor_copy(out=dst[:, c0:c0 + CW], in_=ha[:]), r=["ha"], w=[dk_] if first else [], wj=[] if first else [dk_])
                yield 1
        w2s = P.sb("w2s%d" % d, [64, 2, 64]); w2b = P.sb("w2b%d" % d, [64, 2, 64], BF16)
        P.dma(w2s[:, 0, :], w2[d], w=["w2s%d" % d]); P.dma(w2s[:, 1, :], a2[d], wj=["w2s%d" % d])
        P.V(lambda: nc.vector.tensor_copy(out=w2b[:], in_=w2s[:]), r=["w2s%d" % d], w=["w2b%d" % d])
        w0_bc = bc("w0_bc%d" % d, w0[d:d + 1, :], 64); a0_bc = bc("a0_bc%d" % d, a0[d:d + 1, :], 64)
        mu0_bc = bc("mu0_bc%d" % d, mu[d, 0:1, :], 192); mu1_bc = bc("mu1_bc%d" % d, mu[d, 1:2, :], 192)
        if d == 0:
            cur = [P.sb("cur%d" % i, [128, 192]) for i in range(2)]
            prv = [P.sb("prv%d" % i, [128, 192]) for i in range(2)]
            nxt = [P.sb("nxt%d" % i, [128, 192]) for i in range(2)]
            d0 = P.sb("d0", [128, 192]); d1 = P.sb("d1", [128, 192]); mix = P.sb("mix", [128, 192])
            dec = P.sb("dec", [128, 64]); hif = P.sb("hif", [128, 64])
            pack = [P.sb("pack%d" % i, [128, 384], BF16) for i in range(2)]
            bon = [P.sb("bon%d" % i, [128, 64]) for i in range(2)]
            vto = [P.sb("vto%d" % i, [128, 128]) for i in range(2)]
            icl = P.sb("icl", [128, 64]); kkt = P.sb("kkt", [128, 64]); kap = P.sb("kap", [128, 64]); kd = P.sb("kd", [128, 64])
            t1 = P.sb("t1", [128, 64]); sq = P.sb("sq", [128, 64]); ss = P.sb("ss", [128, 1]); sb_ = P.sb("sb_", [128, 1])
        for t in range(NT):
            b = t % 2; r0 = t * 128
            ck, pk_, nk = "cur%d" % b, "prv%d" % b, "nxt%d" % b
            P.dma(cur[b][:], rkv[d][r0:r0 + 128, :], w=[ck])
            if t == 0:
                P.V(lambda: nc.vector.memset(prv[b][:], 0.0), w=[pk_])
                P.dma(prv[b][1:128, :], rkv[d][0:127, :], wj=[pk_])
            else:
                P.dma(prv[b][:], rkv[d][r0 - 1:r0 + 127, :], w=[pk_])
            if t == NT - 1:
                P.V(lambda: nc.vector.memset(nxt[b][:], 0.0), w=[nk])
                P.dma(nxt[b][0:127, :], rkv[d][r0 + 1:r0 + 128, :], wj=[nk])
            else:
                P.dma(nxt[b][:], rkv[d][r0 + 1:r0 + 129, :], w=[nk])
            P.T(lambda: nc.tensor.matmul(ps_l[:, 0:64], lhsT=thT[:, r0:r0 + 128], rhs=w2b[:, 0, :], start=True, stop=True), r=["thT", "w2b%d" % d], w=["ps_t"])
            P.T(lambda: nc.tensor.matmul(ps_l[:, 64:128], lhsT=haT[:, r0:r0 + 128], rhs=w2b[:, 1, :], start=True, stop=True), r=["haT", "w2b%d" % d], wj=["ps_t"])
            P.V(lambda: nc.vector.tensor_tensor(out=d0[:], in0=prv[b][:], in1=cur[b][:], op=ALU.subtract), r=[pk_, ck], w=["d0"])
            P.V(lambda: nc.vector.tensor_tensor(out=d0[:], in0=d0[:], in1=mu0_bc[:], op=ALU.mult), r=["d0", "mu0_bc%d" % d], w=["d0"])
            P.V(lambda: nc.vector.tensor_tensor(out=d1[:], in0=nxt[b][:], in1=cur[b][:], op=ALU.subtract), r=[nk, ck], w=["d1"])
            P.V(lambda: nc.vector.tensor_tensor(out=d1[:], in0=d1[:], in1=mu1_bc[:], op=ALU.mult), r=["d1", "mu1_bc%d" % d], w=["d1"])
            P.V(lambda: nc.vector.tensor_tensor(out=d0[:], in0=d0[:], in1=d1[:], op=ALU.add), r=["d0", "d1"], w=["d0"])
            P.V(lambda: nc.vector.tensor_tensor(out=mix[:], in0=cur[b][:], in1=d0[:], op=ALU.add), r=[ck, "d0"], w=["mix"])
            rr, kp, vp = mix[:, 0:64], mix[:, 64:128], mix[:, 128:192]
            pkk = "pack%d" % b
            P.V(lambda: nc.vector.tensor_tensor(out=t1[:], in0=ps_l[:, 0:64], in1=w0_bc[:], op=ALU.add), r=["ps_t", "w0_bc%d" % d], w=["t1"])
            P.A(lambda: nc.scalar.activation(out=t1[:], in_=t1[:], func=AF.Sigmoid), r=["t1"], w=["t1"])
            P.A(lambda: nc.scalar.activation(out=dec[:], in_=t1[:], func=AF.Exp, scale=-EM05), r=["t1"], w=["dec"])
            P.V(lambda: nc.vector.tensor_copy(out=pack[b][:, 256:320], in_=dec[:]), r=["dec"], w=[pkk])
            P.V(lambda: nc.vector.tensor_copy(out=hif[:], in_=pack[b][:, 256:320]), r=[pkk], w=["hif"])
            P.V(lambda: nc.vector.tensor_tensor(out=hif[:], in0=dec[:], in1=hif[:], op=ALU.subtract), r=["dec", "hif"], w=["hif"])
            P.V(lambda: nc.vector.tensor_copy(out=pack[b][:, 320:384], in_=hif[:]), r=["hif"], wj=[pkk])
            P.V(lambda: nc.vector.tensor_tensor(out=icl[:], in0=ps_l[:, 64:128], in1=a0_bc[:], op=ALU.add), r=["ps_t", "a0_bc%d" % d], w=["icl"])
            P.A(lambda: nc.scalar.activation(out=icl[:], in_=icl[:], func=AF.Sigmoid), r=["icl"], w=["icl"])
            P.V(lambda: nc.vector.tensor_tensor(out=kkt[:], in0=kp, in1=kk_bc[:], op=ALU.mult), r=["mix", "kk_bc"], w=["kkt"])
            P.A(lambda: nc.scalar.activation(out=sq[:], in_=kkt[:], func=AF.Square, accum_out=ss[:]), r=["kkt"], w=["sq", "ss"])
            P.V(lambda: nc.vector.tensor_scalar(out=ss[:], in0=ss[:], scalar1=1e-12, scalar2=None, op0=ALU.add), r=["ss"], w=["ss"])
            P.A(lambda: nc.scalar.sqrt(out=ss[:], in_=ss[:]), r=["ss"], w=["ss"])
            P.V(lambda: nc.vector.reciprocal(out=ss[:], in_=ss[:]), r=["ss"], w=["ss"])
            P.V(lambda: nc.vector.tensor_scalar(out=kap[:], in0=kkt[:], scalar1=ss[:], scalar2=None, op0=ALU.mult), r=["kkt", "ss"], w=["kap"])
            P.V(lambda: nc.vector.tensor_tensor(out=t1[:], in0=icl[:], in1=ka_bc[:], op=ALU.mult), r=["icl", "ka_bc"], w=["t1"])
            P.V(lambda: nc.vector.scalar_tensor_tensor(out=t1[:], in0=t1[:], scalar=1.0, in1=ka_bc[:], op0=ALU.add, op1=ALU.subtract), r=["t1", "ka_bc"], w=["t1"])
            P.V(lambda: nc.vector.tensor_tensor(out=kd[:], in0=kp, in1=t1[:], op=ALU.mult), r=["mix", "t1"], w=["kd"])
            P.V(lambda: nc.vector.tensor_copy(out=pack[b][:, 0:64], in_=rr), r=["mix"], wj=[pkk])
            P.V(lambda: nc.vector.tensor_copy(out=pack[b][:, 64:128], in_=kap[:]), r=["kap"], wj=[pkk])
            P.V(lambda: nc.vector.scalar_tensor_tensor(out=pack[b][:, 128:192], in0=icl[:], scalar=-1.0, in1=kap[:], op0=ALU.mult, op1=ALU.mult), r=["icl", "kap"], wj=[pkk])
            P.V(lambda: nc.vector.tensor_copy(out=pack[b][:, 192:256], in_=kd[:]), r=["kd"], wj=[pkk])
            P.dma(pkd[d][r0:r0 + 128, 0:64], pack[b][:, 0:64], r=[pkk], wj=["scr"], key=pkk)
            P.dma(pkd[d][r0:r0 + 128, 128:384], pack[b][:, 128:384], r=[pkk], wj=["scr"], key=pkk)
            if t == 0:
                P.dma(pkd[d][0:127, 64:128], pack[b][1:128, 64:128], r=[pkk], wj=["scr"], key=pkk)
            else:
                P.dma(pkd[d][r0 - 1:r0 + 127, 64:128], pack[b][:, 64:128], r=[pkk], wj=["scr"], key=pkk)
            if t == NT - 1:
                P.dma(pkd[d][S - 1:S, 64:128], zrow[0:1, :], r=["zrow"], wj=["scr"], key="zrow")
            P.V(lambda: nc.vector.tensor_tensor(out=t1[:], in0=rr, in1=kd[:], op=ALU.mult), r=["mix", "kd"], w=["t1"])
            P.V(lambda: nc.vector.scalar_tensor_tensor(out=sq[:], in0=t1[:], scalar=1.0, in1=rk_bc[:], op0=ALU.mult, op1=ALU.mult, accum_out=sb_[:]),
                r=["t1", "rk_bc"], w=["sq", "sb_"])
            P.V(lambda: nc.vector.tensor_scalar(out=bon[b][:], in0=vp, scalar1=sb_[:], scalar2=None, op0=ALU.mult), r=["mix", "sb_"], w=["bon%d" % b])
            P.dma(bonus_o[d, r0:r0 + 128, :], bon[b][:], r=["bon%d" % b], wj=["bonus"], key="bon%d" % b)
            P.V(lambda: nc.vector.tensor_copy(out=vstage[:, 64 * d:64 * d + 64], in_=vp), r=["mix"], w=["vstage"])
            P.T(lambda: nc.tensor.transpose(ps_t[:, 0:128], vstage[:], ident[:]), r=["vstage", "ident32"], w=["ps_t"])
            P.V(lambda: nc.vector.tensor_copy(out=vto[b][64 * d:64 * d + 64, :], in_=ps_t[64 * d:64 * d + 64, 0:128]), r=["ps_t"], w=["vto%d" % b])
            P.dma(vTs[64 * d:64 * d + 64, r0:r0 + 128], vto[b][64 * d:64 * d + 64, :], r=["vto%d" % b], wj=["scr"], key="vto%d" % b)
            yield 1
    Pp.close_ns()
    yield 'prep_done'
    P = Ps
    P.V(lambda: nc.vector.memset(St[:], 0.0), w=["S"])
    P.V(lambda: nc.vector.memset(zcol[:], 0.0), w=["zcol"])
    St_b = St[:].unsqueeze(1).to_broadcast([128, 2, 64])
    step = 0; oi = 0
    sk_ap, sk_tok = zcol[:], "zcol"
    for sg in range(S // SEG):
        sb2 = sg % 2; vk = "vseg%d" % sb2; yk = "yseg%d" % sb2
        P.dma(vseg[sb2][:], vTs[:, sg * SEG:(sg + 1) * SEG], r=["scr"], w=[vk])
        for blk in range(SEG // 128):
            s0 = sg * SEG + blk * 128; bb = (s0 // 128) % 2
            pk_ = "pkt%d" % bb
            for d in range(2):
                P.dma(pkt[bb][32 * d:32 * d + 32, :, :], pkd[d][s0:s0 + 128, :].rearrange("(g q) c -> q g c", q=32), r=["scr"], w=[pk_] if d == 0 else [], wj=[] if d == 0 else [pk_])
            for g in range(4):
                for j in range(32):
                    sl = step % NB; step += 1; bk = "bcp%d" % sl
                    col = blk * 128 + g * 32 + j
                    sel = zb[:, j * 128:(j + 1) * 128]
                    P.T(lambda: nc.tensor.matmul(bcp[sl][:, 0:256], lhsT=sel, rhs=pkt[bb][:, g, 0:256], start=True, stop=True), r=["zb", pk_], w=[bk])
                    P.T(lambda: nc.tensor.matmul(bcp[sl][:, 256:320], lhsT=sel, rhs=pkt[bb][:, g, 256:320], start=True, stop=False), r=["zb", pk_], wj=[bk])
                    P.T(lambda: nc.tensor.matmul(bcp[sl][:, 256:320], lhsT=sel, rhs=pkt[bb][:, g, 320:384], start=False, stop=True), r=["zb", pk_], wj=[bk])
                    P.ses = bool(os.environ.get("FORCE_SES"))
                    nbb, kdb, wb = bcp[sl][:, 128:192], bcp[sl][:, 192:256], bcp[sl][:, 256:320]
                    rk2 = bcp[sl][:, 0:128].rearrange("p (a n) -> p a n", a=2)
                    P.V(lambda: nc.vector.tensor_tensor(out=St[:], in0=St[:], in1=wb, op=ALU.mult), r=["S", bk], w=["S"])
                    P.V(lambda: nc.vector.scalar_tensor_tensor(out=St[:], in0=nbb, scalar=sk_ap, in1=St[:], op0=ALU.mult, op1=ALU.add), r=["S", bk, sk_tok], w=["S"])
                    P.V(lambda: nc.vector.scalar_tensor_tensor(out=St[:], in0=kdb, scalar=vseg[sb2][:, col:col + 1], in1=St[:], op0=ALU.mult, op1=ALU.add), r=["S", bk, vk], w=["S"])
                    P.V(lambda: nc.vector.tensor_tensor(out=prod[:], in0=St_b, in1=rk2, op=ALU.mult), r=["S", bk], w=["prod"])
                    P.V(lambda: nc.vector.tensor_reduce(out=yseg[sb2][:, col, :], in_=prod[:], axis=AX.X, op=ALU.add), r=["prod"], wj=[yk])
                    P.ses = True
                    sk_ap, sk_tok = yseg[sb2][:, col, 1:2], yk
                    yield 2
        for blk in range(SEG // 128):
            ob = oi % 2; oi += 1; r0 = sg * SEG + blk * 128
            P.T(lambda: nc.tensor.transpose(ps_t[:, 0:128], yseg[sb2][:, blk * 128:(blk + 1) * 128, 0], ident[:]), r=[yk, "ident32"], w=["ps_t"])
            P.V(lambda: nc.vector.tensor_copy(out=yo[ob][:], in_=ps_t[:, 0:128]), r=["ps_t"], w=["yo%d" % ob])
            for d in range(2):
                P.dma(y_o[d, r0:r0 + 128, :], yo[ob][:, 64 * d:64 * d + 64], r=["yo%d" % ob], wj=["y"], key="yo%d" % ob)
        P.V(lambda: nc.vector.memset(zcol[:], 0.0), r=[yk], w=["zcol2"])
    Ps.close_ns()
    yield 'done'


def build_M(S):
    P0 = Prog()
    gB = gen_MB(P0, S)
    for v in gB:
        if v == 'prep_done':
            break
    others = [(gen_MD, 3), (gen_MA, 2), (gen_MC, 10)]
    b_done = False
    for gfn, ratio in others:
        g = gfn(P0, S)
        for v in g:
            if v == 'done':
                break
            if not b_done:
                for _ in range(ratio):
                    if next(gB) == 'done':
                        b_done = True
                        break
    if not b_done:
        for v in gB:
            pass
    P0.finish(); P0.close()
    return P0.nc


def m_inputs(z_b, prm, l, h, S):
    m = {}
    za = z_b[:, 0:768]
    cosr, sinr = rope_tables(S); cosa, sina = axial_tables(S)
    ident = np.eye(128, dtype=np.float32)
    ma = {"cos": cosr, "sin": sinr, "ident": ident, "mask": dil_mask(), "q": np.ascontiguousarray(za[:, h * 64:(h + 1) * 64]), "k": np.ascontiguousarray(za[:, 256 + h * 64:256 + (h + 1) * 64])}
    v = za[:, 512 + h * 64:512 + (h + 1) * 64]
    for d in (1, 4, 16):
        ma["vx%d" % d] = vext_dilated(v, d)
    for k_, v_ in ma.items(): m["a_" + k_] = v_
    for k_, v_ in mb_inputs(z_b[:, 768:1536], z_b[:, 2304:2944], prm, l, h).items(): m["b_" + k_] = v_
    for k_, v_ in mc_inputs(z_b[:, 1536:1792], prm, l, h, S).items(): m["c_" + k_] = v_
    kv = h // 2
    md = {"qg": prm["gqa_q_norm"][l][None, :], "kg": prm["gqa_k_norm"][l][None, :], "cos": cosa, "sin": sina, "ident": ident,
          "q": np.ascontiguousarray(z_b[:, 1792 + h * 64:1792 + (h + 1) * 64]), "k": np.ascontiguousarray(z_b[:, 2048 + kv * 64:2048 + (kv + 1) * 64]),
          "v": np.ascontiguousarray(z_b[:, 2176 + kv * 64:2176 + (kv + 1) * 64])}
    for k_, v_ in md.items(): m["d_" + k_] = v_
    return m


def run_M(z, prm, l):
    B, S, _ = z.shape
    nc = build_M(S)
    maps = [m_inputs(z[c // 4], prm, l, c % 4, S) for c in range(B * 4)]
    res = run_bass_kernel_spmd(nc, maps, core_ids=list(range(B * 4)))
    names = ["ya", "yd", "rwf", "rwb", "bnf", "bnb", "cf", "cb"]
    out = {n: np.zeros((B, S, 256), np.float32) for n in names}
    for c in range(B * 4):
        b, h = c // 4, c % 4
        r = res.results[c]; hs = slice(h * 64, (h + 1) * 64)
        out["ya"][b, :, hs] = r["a_yT"].T
        out["yd"][b, :, hs] = r["d_yT"].T
        out["rwf"][b, :, hs] = r["b_y"][0]; out["rwb"][b, :, hs] = r["b_y"][1][::-1]
        out["bnf"][b, :, hs] = r["b_bonus"][0]; out["bnb"][b, :, hs] = r["b_bonus"][1][::-1]
        out["cf"][b, :, hs] = r["c_yT0"].T; out["cb"][b, :, hs] = r["c_yT1"].T[::-1]
    return out


def build_F1(TC):
    P = Prog(); nc = P.nc
    x = P.dram("x", [TC, 1024]); g = P.dram("g", [1, 1024]); ident_d = P.dram("ident", [128, 128])
    br = {n: P.dram(n, [TC, 256]) for n in ("ya", "yd", "rwf", "rwb", "bnf", "bnb", "cf", "cb", "u")}
    hg = P.dram("hg", [TC, 128])
    wgate = P.dram("wgate", [1024, 4096]); gate_b = P.dram("gate_b", [1, 4096]); w_branch = P.dram("w_branch", [4, 256, 1024]); w_out = P.dram("w_out", [1024, 1024])
    glu_w = P.dram("glu_w", [256, 512]); glu_b = P.dram("glu_b", [1, 512]); g2 = P.dram("g2", [128, 256])
    vecs = P.dram("vecs", [3, 256])
    y = P.dram("y", [TC, 1024], kind="ExternalOutput")
    ident = load_ident32(P, nc, ident_d)
    def bc(name, src, n):
        t = P.sb(name, [128, n]); P.dma(t[:], src.partition_broadcast(128), w=[name]); return t
    gbc = bc("gbc", g[0:1, :], 1024); gb_bc = bc("gb_bc", gate_b[0:1, :], 4096); glub_bc = bc("glub_bc", glu_b[0:1, :], 512)
    lnw_bc = bc("lnw_bc", vecs[0:1, :], 256); lnb_bc = bc("lnb_bc", vecs[1:2, :], 256); d_bc = bc("d_bc", vecs[2:3, :], 256)
    Wg = P.sb("Wg", [128, 8, 4096], BF16); Wbr = P.sb("Wbr", [128, 8, 1024], BF16); Wo = P.sb("Wo", [128, 8, 1024], BF16)
    glub = P.sb("glub", [128, 2, 512], BF16); g2b = P.sb("g2b", [128, 256], BF16)
    stg = [P.sb("stg%d" % i, [128, 1024]) for i in range(2)]
    si = 0
    def load_cast(dst, src, n, tok):
        nonlocal si
        b = si % 2; si += 1
        P.dma(stg[b][:, 0:n], src, w=["stg%d" % b])
        if b == 0:
            P.V(lambda: nc.vector.tensor_copy(out=dst, in_=stg[b][:, 0:n]), r=["stg%d" % b], wj=[tok])
        else:
            P.G(lambda: nc.gpsimd.tensor_copy(out=dst, in_=stg[b][:, 0:n]), r=["stg%d" % b], wj=[tok])
    for kc in range(8):
        rows = slice(kc * 128, (kc + 1) * 128)
        for q in range(4):
            load_cast(Wg[:, kc, q * 1024:(q + 1) * 1024], wgate[rows, q * 1024:(q + 1) * 1024], 1024, "Wg")
        load_cast(Wo[:, kc, :], w_out[rows, :], 1024, "Wo")
        load_cast(Wbr[:, kc, :], w_branch[kc // 2, (kc % 2) * 128:(kc % 2 + 1) * 128, :], 1024, "Wbr")
    for kc in range(2):
        load_cast(glub[:, kc, :], glu_w[kc * 128:(kc + 1) * 128, :], 512, "glub")
    load_cast(g2b[:], g2[:, :], 256, "g2b")
    xt = [P.sb("xt%d" % i, [128, 1024]) for i in range(2)]
    xn32 = P.sb("xn32", [128, 1024]); ss = P.sb("ss", [128, 1])
    xnT = P.sb("xnT", [128, 8, 128], BF16); mT = P.sb("mT", [128, 8, 128], BF16)
    gate = P.sb("gate", [128, 1024]); merged = P.sb("merged", [128, 1024]); tmpm = P.sb("tmpm", [128, 1024])
    inp = {n: P.sb("i_" + n, [128, 256]) for n in br}
    hgt = P.sb("hgt", [128, 128]); hgT = P.sb("hgT", [128, 128], BF16)
    ys = P.sb("ys", [128, 256]); sqh = P.sb("sqh", [128, 64]); st = P.sb("st", [128, 12])
    ybf = P.sb("ybf", [128, 256]); ycf = P.sb("ycf", [128, 256]); t256 = P.sb("t256", [128, 256]); h512 = P.sb("h512", [128, 512])
    yT = P.sb("yT", [128, 2, 128], BF16)
    pst = P.ps("pst", [128, 8, 128]); pbr = P.ps("pbr", [128, 1024]); pg = P.ps("pg", [128, 1024]); po = P.ps("po", [128, 1024])
    NT = TC // 128
    for t in range(NT):
        b = t % 2; r0 = t * 128; xk = "xt%d" % b
        P.dma(xt[b][:], x[r0:r0 + 128, :], w=[xk])
        for n in br:
            P.dma(inp[n][:], br[n][r0:r0 + 128, :], w=["i_" + n])
        P.dma(hgt[:], hg[r0:r0 + 128, :], w=["hgt"])
        P.A(lambda: nc.scalar.activation(out=xn32[:], in_=xt[b][:], func=AF.Square, accum_out=ss[:]), r=[xk], w=["xn32", "ss"])
        P.V(lambda: nc.vector.tensor_scalar(out=ss[:], in0=ss[:], scalar1=1.0 / 1024, scalar2=1e-6, op0=ALU.mult, op1=ALU.add), r=["ss"], w=["ss"])
        P.A(lambda: nc.scalar.sqrt(out=ss[:], in_=ss[:]), r=["ss"], w=["ss"])
        P.V(lambda: nc.vector.reciprocal(out=ss[:], in_=ss[:]), r=["ss"], w=["ss"])
        P.V(lambda: nc.vector.scalar_tensor_tensor(out=xn32[:], in0=xt[b][:], scalar=ss[:], in1=gbc[:], op0=ALU.mult, op1=ALU.mult), r=[xk, "ss", "gbc"], w=["xn32"])
        for kc in range(8):
            P.T(lambda: nc.tensor.transpose(pst[:, kc, :], xn32[:, kc * 128:(kc + 1) * 128], ident[:]), r=["xn32", "ident32"], w=["pst"] if kc == 0 else [], wj=[] if kc == 0 else ["pst"])
        P.V(lambda: nc.vector.tensor_copy(out=xnT[:], in_=pst[:]), r=["pst"], w=["xnT"])
        P.V(lambda: nc.vector.tensor_tensor(out=ys[:], in0=inp["rwf"][:], in1=inp["rwb"][:], op=ALU.add), r=["i_rwf", "i_rwb"], w=["ys"])
        P.V(lambda: nc.vector.tensor_reduce(out=st[:, 0:4], in_=ys[:].rearrange("p (h n) -> p h n", h=4), axis=AX.X, op=ALU.add), r=["ys"], w=["st"])
        for h in range(4):
            P.A(lambda: nc.scalar.activation(out=sqh[:], in_=ys[:, h * 64:(h + 1) * 64], func=AF.Square, accum_out=st[:, 4 + h:5 + h]), r=["ys", "st"], w=["sqh", "st"])
        P.V(lambda: nc.vector.tensor_scalar(out=st[:, 0:8], in0=st[:, 0:8], scalar1=1.0 / 64, scalar2=None, op0=ALU.mult), r=["st"], w=["st"])
        P.V(lambda: nc.vector.tensor_tensor(out=st[:, 8:12], in0=st[:, 0:4], in1=st[:, 0:4], op=ALU.mult), r=["st"], w=["st"])
        P.V(lambda: nc.vector.tensor_tensor(out=st[:, 4:8], in0=st[:, 4:8], in1=st[:, 8:12], op=ALU.subtract), r=["st"], w=["st"])
        P.V(lambda: nc.vector.tensor_scalar(out=st[:, 4:8], in0=st[:, 4:8], scalar1=64e-5, scalar2=None, op0=ALU.add), r=["st"], w=["st"])
        P.A(lambda: nc.scalar.sqrt(out=st[:, 4:8], in_=st[:, 4:8]), r=["st"], w=["st"])
        P.V(lambda: nc.vector.reciprocal(out=st[:, 4:8], in_=st[:, 4:8]), r=["st"], w=["st"])
        for h in range(4):
            P.V(lambda: nc.vector.tensor_scalar(out=ybf[:, h * 64:(h + 1) * 64], in0=ys[:, h * 64:(h + 1) * 64], scalar1=st[:, h:h + 1], scalar2=st[:, 4 + h:5 + h], op0=ALU.subtract, op1=ALU.mult),
                r=["ys", "st"], w=["ybf"] if h == 0 else [], wj=[] if h == 0 else ["ybf"])
        P.V(lambda: nc.vector.tensor_tensor(out=ybf[:], in0=ybf[:], in1=lnw_bc[:], op=ALU.mult), r=["ybf", "lnw_bc"], w=["ybf"])
        P.V(lambda: nc.vector.tensor_tensor(out=ybf[:], in0=ybf[:], in1=lnb_bc[:], op=ALU.add), r=["ybf", "lnb_bc"], w=["ybf"])
        P.V(lambda: nc.vector.tensor_tensor(out=ybf[:], in0=ybf[:], in1=inp["bnf"][:], op=ALU.add), r=["ybf", "i_bnf"], w=["ybf"])
        P.V(lambda: nc.vector.tensor_tensor(out=ybf[:], in0=ybf[:], in1=inp["bnb"][:], op=ALU.add), r=["ybf", "i_bnb"], w=["ybf"])
        P.A(lambda: nc.scalar.activation(out=hgt[:], in_=hgt[:], func=AF.Sigmoid), r=["hgt"], w=["hgt"])
        P.T(lambda: nc.tensor.transpose(po[:, 0:128], hgt[:], ident[:]), r=["hgt", "ident32"], w=["po"])
        P.V(lambda: nc.vector.tensor_copy(out=hgT[:], in_=po[:, 0:128]), r=["po"], w=["hgT"])
        P.T(lambda: nc.tensor.matmul(po[:, 0:256], lhsT=hgT[:], rhs=g2b[:], start=True, stop=True), r=["hgT", "g2b"], w=["po"])
        P.V(lambda: nc.vector.tensor_tensor(out=ybf[:], in0=ybf[:], in1=po[:, 0:256], op=ALU.mult), r=["ybf", "po"], w=["ybf"])
        P.V(lambda: nc.vector.tensor_tensor(out=ycf[:], in0=inp["u"][:], in1=d_bc[:], op=ALU.mult), r=["i_u", "d_bc"], w=["ycf"])
        P.V(lambda: nc.vector.tensor_tensor(out=ycf[:], in0=ycf[:], in1=inp["cf"][:], op=ALU.add), r=["ycf", "i_cf"], w=["ycf"])
        P.V(lambda: nc.vector.tensor_tensor(out=ycf[:], in0=ycf[:], in1=inp["cb"][:], op=ALU.add), r=["ycf", "i_cb"], w=["ycf"])
        P.V(lambda: nc.vector.tensor_tensor(out=t256[:], in0=ycf[:], in1=ycf[:], op=ALU.mult), r=["ycf"], w=["t256"])
        P.V(lambda: nc.vector.tensor_scalar(out=t256[:], in0=t256[:], scalar1=0.044715, scalar2=1.0, op0=ALU.mult, op1=ALU.add), r=["t256"], w=["t256"])
        P.V(lambda: nc.vector.tensor_tensor(out=t256[:], in0=t256[:], in1=ycf[:], op=ALU.mult), r=["t256", "ycf"], w=["t256"])
        P.A(lambda: nc.scalar.activation(out=t256[:], in_=t256[:], func=AF.Sigmoid, scale=1.5957691216057308), r=["t256"], w=["t256"])
        P.V(lambda: nc.vector.tensor_tensor(out=ycf[:], in0=ycf[:], in1=t256[:], op=ALU.mult), r=["ycf", "t256"], w=["ycf"])
        for kc in range(2):
            P.T(lambda: nc.tensor.transpose(pst[:, kc, :], ycf[:, kc * 128:(kc + 1) * 128], ident[:]), r=["ycf", "ident32"], w=["pst"] if kc == 0 else [], wj=[] if kc == 0 else ["pst"])
        P.V(lambda: nc.vector.tensor_copy(out=yT[:], in_=pst[:, 0:2, :]), r=["pst"], w=["yT"])
        for kc in range(2):
            P.T(lambda: nc.tensor.matmul(po[:, 0:512], lhsT=yT[:, kc, :], rhs=glub[:, kc, :], start=(kc == 0), stop=(kc == 1)), r=["yT", "glub"], w=["po"] if kc == 0 else [], wj=[] if kc == 0 else ["po"])
        P.V(lambda: nc.vector.tensor_tensor(out=h512[:], in0=po[:, 0:512], in1=glub_bc[:], op=ALU.add), r=["po", "glub_bc"], w=["h512"])
        P.A(lambda: nc.scalar.activation(out=t256[:], in_=h512[:, 256:512], func=AF.Sigmoid), r=["h512"], w=["t256"])
        P.V(lambda: nc.vector.tensor_tensor(out=ycf[:], in0=h512[:, 0:256], in1=t256[:], op=ALU.mult), r=["h512", "t256"], w=["ycf"])
        srcs = [(inp["ya"], "i_ya"), (ybf, "ybf"), (ycf, "ycf"), (inp["yd"], "i_yd")]
        for i, (src, stok) in enumerate(srcs):
            for kc in range(2):
                P.T(lambda: nc.tensor.transpose(pst[:, kc, :], src[:, kc * 128:(kc + 1) * 128], ident[:]), r=[stok, "ident32"], w=["pst"] if kc == 0 else [], wj=[] if kc == 0 else ["pst"])
            P.V(lambda: nc.vector.tensor_copy(out=yT[:], in_=pst[:, 0:2, :]), r=["pst"], w=["yT"])
            for half in range(2):
                cs = slice(half * 512, (half + 1) * 512)
                for kc in range(2):
                    P.T(lambda: nc.tensor.matmul(pbr[:, cs], lhsT=yT[:, kc, :], rhs=Wbr[:, i * 2 + kc, cs], start=(kc == 0), stop=(kc == 1)),
                        r=["yT", "Wbr"], w=["pbr"] if (kc == 0 and half == 0) else [], wj=[] if (kc == 0 and half == 0) else ["pbr"])
                for kc in range(8):
                    P.T(lambda: nc.tensor.matmul(pg[:, cs], lhsT=xnT[:, kc, :], rhs=Wg[:, kc, i * 1024 + half * 512:i * 1024 + (half + 1) * 512], start=(kc == 0), stop=(kc == 7)),
                        r=["xnT", "Wg"], w=["pg"] if (kc == 0 and half == 0) else [], wj=[] if (kc == 0 and half == 0) else ["pg"])
            P.V(lambda: nc.vector.tensor_tensor(out=gate[:], in0=pg[:], in1=gb_bc[:, i * 1024:(i + 1) * 1024], op=ALU.add), r=["pg", "gb_bc"], w=["gate"])
            P.A(lambda: nc.scalar.activation(out=gate[:], in_=gate[:], func=AF.Sigmoid), r=["gate"], w=["gate"])
            if i == 0:
                P.V(lambda: nc.vector.tensor_tensor(out=merged[:], in0=pbr[:], in1=gate[:], op=ALU.mult), r=["pbr", "gate"], w=["merged"])
            else:
                P.V(lambda: nc.vector.tensor_tensor(out=tmpm[:], in0=pbr[:], in1=gate[:], op=ALU.mult), r=["pbr", "gate"], w=["tmpm"])
                P.G(lambda: nc.gpsimd.tensor_tensor(out=merged[:], in0=merged[:], in1=tmpm[:], op=ALU.add), r=["merged", "tmpm"], w=["merged"])
        for kc in range(8):
            P.T(lambda: nc.tensor.transpose(pst[:, kc, :], merged[:, kc * 128:(kc + 1) * 128], ident[:]), r=["merged", "ident32"], w=["pst"] if kc == 0 else [], wj=[] if kc == 0 else ["pst"])
        P.V(lambda: nc.vector.tensor_copy(out=mT[:], in_=pst[:]), r=["pst"], w=["mT"])
        for half in range(2):
            cs = slice(half * 512, (half + 1) * 512)
            for kc in range(8):
                P.T(lambda: nc.tensor.matmul(po[:, cs], lhsT=mT[:, kc, :], rhs=Wo[:, kc, cs], start=(kc == 0), stop=(kc == 7)),
                    r=["mT", "Wo"], w=["po"] if (kc == 0 and half == 0) else [], wj=[] if (kc == 0 and half == 0) else ["po"])
        P.V(lambda: nc.vector.tensor_tensor(out=xt[b][:], in0=xt[b][:], in1=po[:], op=ALU.add), r=[xk, "po"], w=[xk])
        P.dma(y[r0:r0 + 128, :], xt[b][:], r=[xk], wj=["y"], key=xk)
    P.finish(); P.close()
    return nc


def run_F1(xf, brs, hg, prm, l, ncores=8):
    T = xf.shape[0]; TC = T // ncores
    nc = build_F1(TC)
    com = {"g": prm["norm_mix_g"][l][None, :], "ident": np.eye(128, dtype=np.float32),
           "wgate": np.ascontiguousarray(prm["w_in"][l][:, 2304:]), "gate_b": prm["gate_b"][l].reshape(1, 4096), "w_branch": prm["w_branch"][l], "w_out": prm["w_out"][l],
           "glu_w": prm["s5_glu_w"][l], "glu_b": prm["s5_glu_b"][l][None, :], "g2": prm["rwkv_g2"][l],
           "vecs": np.stack([prm["rwkv_ln_w"][l], prm["rwkv_ln_b"][l], prm["s5_d"][l]])}
    maps = []
    for c in range(ncores):
        sl = slice(c * TC, (c + 1) * TC)
        m = dict(com, x=xf[sl], hg=np.ascontiguousarray(hg[sl]))
        for n, a in brs.items():
            m[n] = np.ascontiguousarray(a[sl])
        maps.append(m)
    res = run_bass_kernel_spmd(nc, maps, core_ids=list(range(ncores)))
    return np.concatenate([r["y"] for r in res.results], axis=0)


def kernel(**inp):
    prm = {k: np.ascontiguousarray(np.asarray(v, dtype=np.float32)) for k, v in inp.items()}
    x = prm["x"]
    B, S, D = x.shape
    xf = np.ascontiguousarray(x.reshape(B * S, D))
    f2 = lambda a: np.ascontiguousarray(a.reshape(B * S, a.shape[-1]))
    for l in range(2):
        zP = run_P(xf, prm, l)
        z = zP.reshape(B, S, NZ)
        brs = {n: f2(a) for n, a in run_M(z, prm, l).items()}
        brs["u"] = f2(z[:, :, 1536:1792])
        xmid = run_F1(xf, brs, np.ascontiguousarray(zP[:, 2816:2944]), prm, l)
        del zP, z, brs
        xf = run_F2(xmid, prm, l)
    return xf.reshape(B, S, D).astype(np.float32)
```

```python
import os
from concourse.bass_utils import run_bass_kernel_spmd

import os
from contextlib import ExitStack
import numpy as np
import concourse.bass as bass
import concourse.mybir as mybir

F32 = mybir.dt.float32
BF16 = mybir.dt.bfloat16
I32 = mybir.dt.int32
ALU = mybir.AluOpType
AF = mybir.ActivationFunctionType
AX = mybir.AxisListType

COMPUTE = ("tensor", "vector", "scalar", "gpsimd")


class Prog:
    def __init__(self, same_engine_sync=True):
        self.nc = bass.Bass("TRN2", target_bir_lowering=False)
        self.es = ExitStack()
        self.eng = {"tensor": self.nc.tensor, "vector": self.nc.vector, "scalar": self.nc.scalar,
                    "gpsimd": self.nc.gpsimd, "sync": self.nc.sync}
        self.esem = {e: self.es.enter_context(self.nc.semaphore("s_" + e)) for e in COMPUTE}
        self.ecount = {e: 0 for e in COMPUTE}
        self.waited = {}
        self.tsem = {}
        self.writers = {}
        self.readers = {}
        self.gen = {}
        self.ses = True
        self.excl = {}
        self.same_engine_sync = same_engine_sync
        self.ses_engines = ("vector", "scalar", "gpsimd")
        self.n_inst = 0

    def dram(self, name, shape, dtype=F32, kind="ExternalInput"):
        return self.nc.dram_tensor(name, list(shape), dtype, kind=kind).ap()

    def sb(self, name, shape, dtype=F32):
        return self.es.enter_context(self.nc.sbuf_tensor("sb_" + name, list(shape), dtype))

    def ps(self, name, shape, dtype=F32):
        return self.es.enter_context(self.nc.psum_tensor("ps_" + name, list(shape), dtype))

    def _wait(self, eng, ev):
        kind, a, b = ev
        if kind == "e":
            if a == eng and not (self.ses and (eng in self.ses_engines)):
                return
            sem, val, key = self.esem[a], b, (eng, "e" + a)
        else:
            sem, val, key = self.tsem[a][0], b, (eng, "d", a)
        if self.waited.get(key, -1) >= val:
            return
        self.waited[key] = val
        self.eng[eng].wait_ge(sem, val)

    def _deps(self, eng, r, w, wj=()):
        evs = []
        for t in r:
            evs += self.writers.get(t, [])
        for t in w:
            prior = list(self.writers.get(t, ())) + list(self.readers.get(t, ()))
            evs += prior
            self.gen[t] = prior
        for t in wj:
            evs += self.gen.get(t, [])
            evs += self.readers.get(t, [])
            if t in self.excl:
                evs.append(self.excl[t])
        mx = {}
        for (k, a, b) in evs:
            if mx.get((k, a), -1) < b:
                mx[(k, a)] = b
        for (k, a), b in mx.items():
            self._wait(eng, (k, a, b))

    def _commit(self, ev, r, w, wj=()):
        for t in r:
            self.readers.setdefault(t, []).append(ev)
        for t in w:
            self.writers[t] = [ev]
            self.readers[t] = []
            self.excl[t] = ev
        for t in wj:
            self.writers.setdefault(t, []).append(ev)

    def op(self, eng, fn, r=(), w=(), wj=()):
        self._deps(eng, r, w, wj)
        ins = fn()
        self.ecount[eng] += 1
        ins.then_inc(self.esem[eng], 1)
        self._commit(("e", eng, self.ecount[eng]), r, w, wj)
        self.n_inst += 1
        return ins

    def T(self, fn, r=(), w=(), wj=()): return self.op("tensor", fn, r, w, wj)
    def V(self, fn, r=(), w=(), wj=()): return self.op("vector", fn, r, w, wj)
    def A(self, fn, r=(), w=(), wj=()): return self.op("scalar", fn, r, w, wj)
    def G(self, fn, r=(), w=(), wj=()): return self.op("gpsimd", fn, r, w, wj)

    def dma(self, out, in_, r=(), w=(), wj=(), q="sync", key=None, **kw):
        if key is None:
            key = (list(w) + list(wj) + list(r))[0]
        self._deps(q, r, w, wj)
        if key not in self.tsem:
            self.tsem[key] = [self.es.enter_context(self.nc.semaphore("d%d" % len(self.tsem))), 0]
        ent = self.tsem[key]
        ent[1] += 16
        self.eng[q].dma_start(out=out, in_=in_, **kw).then_inc(ent[0], 16)
        self._commit(("d", key, ent[1]), r, w, wj)
        self.n_inst += 1

    def finish(self, q="sync"):
        for key, ent in self.tsem.items():
            self._wait(q, ("d", key, ent[1]))
        return self.nc

    def close(self):
        self.es.close()


class NS:
    def __init__(self, P, pfx):
        self.P = P; self.pfx = pfx; self.nc = P.nc; self.es = ExitStack()

    @property
    def ses(self): return self.P.ses

    @ses.setter
    def ses(self, v): self.P.ses = v

    def _t(self, toks): return [self.pfx + x for x in toks]

    def dram(self, name, shape, dtype=F32, kind="ExternalInput"):
        return self.P.dram(self.pfx + name, shape, dtype, kind)

    def sb(self, name, shape, dtype=F32):
        return self.es.enter_context(self.nc.sbuf_tensor("sb_" + self.pfx + name, list(shape), dtype))

    def ps(self, name, shape, dtype=F32):
        return self.es.enter_context(self.nc.psum_tensor("ps_" + self.pfx + name, list(shape), dtype))

    def op(self, eng, fn, r=(), w=(), wj=()): return self.P.op(eng, fn, self._t(r), self._t(w), self._t(wj))
    def T(self, fn, r=(), w=(), wj=()): return self.op("tensor", fn, r, w, wj)
    def V(self, fn, r=(), w=(), wj=()): return self.op("vector", fn, r, w, wj)
    def A(self, fn, r=(), w=(), wj=()): return self.op("scalar", fn, r, w, wj)
    def G(self, fn, r=(), w=(), wj=()): return self.op("gpsimd", fn, r, w, wj)

    def dma(self, out, in_, r=(), w=(), wj=(), q="sync", key=None, **kw):
        return self.P.dma(out, in_, self._t(r), self._t(w), self._t(wj), q, None if key is None else self.pfx + key, **kw)

    def close_ns(self, barrier=True):
        if barrier:
            P = self.P
            evs = []
            for d_ in (P.writers, P.readers):
                for t, lst in d_.items():
                    if isinstance(t, str) and t.startswith(self.pfx):
                        evs += lst
            mx = {}
            for (k, a, b) in evs:
                if mx.get((k, a), -1) < b:
                    mx[(k, a)] = b
            for eng in list(COMPUTE) + ["sync"]:
                for (k, a), b in mx.items():
                    P._wait(eng, (k, a, b))
        self.es.close()


NZ = 2944


def rmsnorm_tile(P, nc, xt, xtok, gbc, gtok, out_bf, otok, scr, eps=1e-6, D=1024):
    sq, ss = scr["sq"], scr["ss"]
    P.A(lambda: nc.scalar.activation(out=sq[:], in_=xt, func=AF.Square, accum_out=ss[:]), r=[xtok], w=["sq", "ss"])
    P.V(lambda: nc.vector.tensor_scalar(out=ss[:], in0=ss[:], scalar1=1.0 / D, scalar2=eps, op0=ALU.mult, op1=ALU.add), r=["ss"], w=["ss"])
    P.A(lambda: nc.scalar.sqrt(out=ss[:], in_=ss[:]), r=["ss"], w=["ss"])
    P.V(lambda: nc.vector.reciprocal(out=ss[:], in_=ss[:]), r=["ss"], w=["ss"])
    P.V(lambda: nc.vector.scalar_tensor_tensor(out=out_bf, in0=xt, scalar=ss[:], in1=gbc, op0=ALU.mult, op1=ALU.mult),
        r=[xtok, "ss", gtok], w=[otok])


def load_ident(P, nc, ident_d):
    i32 = P.sb("ident32", [128, 128]); ib = P.sb("identb", [128, 128], BF16)
    P.dma(i32[:], ident_d[:, :], w=["ident32"])
    P.V(lambda: nc.vector.tensor_copy(out=ib[:], in_=i32[:]), r=["ident32"], w=["ident"])
    return ib


def build_P(TC):
    P = Prog(); nc = P.nc
    x = P.dram("x", [TC, 1024]); g = P.dram("g", [1, 1024]); ident_d = P.dram("ident", [128, 128])
    w_mix = P.dram("w_mix", [1024, 2304]); w1 = P.dram("w1", [2, 1024, 64]); a1 = P.dram("a1", [2, 1024, 64])
    g1 = P.dram("g1", [1024, 128]); mu = P.dram("mu", [2, 1024])
    z = P.dram("z", [TC, NZ], kind="ExternalOutput")
    Wsb = P.sb("Wsb", [128, 8, NZ], BF16)
    stg = [P.sb("stg%d" % i, [128, NZ]) for i in range(2)]
    gbc = P.sb("gbc", [128, 1024]); mut = P.sb("mut", [128, 2, 8]); omu = P.sb("omu", [128, 2, 8])
    ident = load_ident(P, nc, ident_d)
    P.dma(gbc[:], g[0:1, :].partition_broadcast(128), w=["gbc"])
    for d in range(2):
        P.dma(mut[:, d, :], mu[d].rearrange("(c p) -> p c", p=128), wj=["mut"], allow_slow_non_contiguous=True)
    P.V(lambda: nc.vector.tensor_scalar(out=omu[:], in0=mut[:], scalar1=-1.0, scalar2=1.0, op0=ALU.mult, op1=ALU.add), r=["mut"], w=["omu"])
    for kc in range(8):
        s = stg[kc % 2]; tk = "stg%d" % (kc % 2)
        rows = slice(kc * 128, (kc + 1) * 128)
        P.dma(s[:, 0:2304], w_mix[rows, :], w=[tk])
        for d in range(2):
            P.dma(s[:, 2304 + d * 256:2304 + d * 256 + 64], w1[d, rows, :], wj=[tk])
            P.dma(s[:, 2304 + d * 256 + 128:2304 + d * 256 + 192], a1[d, rows, :], wj=[tk])
        P.dma(s[:, 2816:2944], g1[rows, :], wj=[tk])
        P.A(lambda: nc.scalar.copy(out=Wsb[:, kc, 0:1152], in_=s[:, 0:1152]), r=[tk], wj=["Wsb"])
        P.V(lambda: nc.vector.tensor_copy(out=Wsb[:, kc, 1152:2304], in_=s[:, 1152:2304]), r=[tk], wj=["Wsb"])
        for d in range(2):
            for wa in range(2):
                b0 = 2304 + d * 256 + wa * 128
                P.V(lambda: nc.vector.tensor_scalar(out=Wsb[:, kc, b0 + 64:b0 + 128], in0=s[:, b0:b0 + 64], scalar1=mut[:, d, kc:kc + 1], scalar2=None, op0=ALU.mult),
                    r=[tk, "mut"], wj=["Wsb"])
                P.V(lambda: nc.vector.tensor_scalar(out=Wsb[:, kc, b0:b0 + 64], in0=s[:, b0:b0 + 64], scalar1=omu[:, d, kc:kc + 1], scalar2=None, op0=ALU.mult),
                    r=[tk, "omu"], wj=["Wsb"])
        P.V(lambda: nc.vector.tensor_copy(out=Wsb[:, kc, 2816:2944], in_=s[:, 2816:2944]), r=[tk], wj=["Wsb"])
    xt = [P.sb("xt%d" % i, [128, 1024]) for i in range(2)]
    xnb = P.sb("xnb", [128, 1024], BF16)
    xnT = P.sb("xnT", [128, 8, 128], BF16)
    zt = [P.sb("zt%d" % i, [128, NZ]) for i in range(2)]
    scr = {"sq": P.sb("sq", [128, 1024]), "ss": P.sb("ss", [128, 1])}
    pst = P.ps("pst", [128, 8, 128], BF16)
    psz = [P.ps("psz%d" % i, [128, 512]) for i in range(4)]
    NT = TC // 128
    P.dma(xt[0][:], x[0:128, :], w=["xt0"])
    ci = 0
    for t in range(NT):
        b = t % 2
        if t + 1 < NT:
            P.dma(xt[1 - b][:], x[(t + 1) * 128:(t + 2) * 128, :], w=["xt%d" % (1 - b)])
        rmsnorm_tile(P, nc, xt[b][:], "xt%d" % b, gbc[:], "gbc", xnb[:], "xnb", scr)
        for kc in range(8):
            P.T(lambda: nc.tensor.transpose(pst[:, kc, :], xnb[:, kc * 128:(kc + 1) * 128], ident[:]), r=["xnb", "ident"],
                w=["pst"] if kc == 0 else [], wj=[] if kc == 0 else ["pst"])
        P.V(lambda: nc.vector.tensor_copy(out=xnT[:], in_=pst[:]), r=["pst"], w=["xnT"])
        for n0 in range(0, NZ, 512):
            n1 = min(NZ, n0 + 512); pz = psz[ci % 4]; pk = "psz%d" % (ci % 4)
            for kc in range(8):
                P.T(lambda: nc.tensor.matmul(pz[:, 0:n1 - n0], lhsT=xnT[:, kc, :], rhs=Wsb[:, kc, n0:n1], start=(kc == 0), stop=(kc == 7)),
                    r=["xnT", "Wsb"], w=[pk] if kc == 0 else [], wj=[] if kc == 0 else [pk])
            first = (n0 == 0)
            if ci % 2 == 0:
                P.A(lambda: nc.scalar.copy(out=zt[b][:, n0:n1], in_=pz[:, 0:n1 - n0]), r=[pk], w=["zt%d" % b] if first else [], wj=[] if first else ["zt%d" % b])
            else:
                P.V(lambda: nc.vector.tensor_copy(out=zt[b][:, n0:n1], in_=pz[:, 0:n1 - n0]), r=[pk], w=["zt%d" % b] if first else [], wj=[] if first else ["zt%d" % b])
            ci += 1
        P.dma(z[t * 128:(t + 1) * 128, :], zt[b][:], r=["zt%d" % b], wj=["z"], key="zt%d" % b)
    P.finish(); P.close()
    return nc


def run_P(xf, prm, l, ncores=8):
    T = xf.shape[0]; TC = T // ncores
    nc = build_P(TC)
    com = {"g": prm["norm_mix_g"][l][None, :], "ident": np.eye(128, dtype=np.float32),
           "w_mix": np.ascontiguousarray(prm["w_in"][l][:, :2304]), "w1": prm["rwkv_w1"][l], "a1": prm["rwkv_a1"][l],
           "g1": prm["rwkv_g1"][l], "mu": prm["rwkv_mu_x"][l]}
    maps = [dict(com, x=xf[c * TC:(c + 1) * TC]) for c in range(ncores)]
    res = run_bass_kernel_spmd(nc, maps, core_ids=list(range(ncores)))
    return np.concatenate([r["z"] for r in res.results], axis=0)


def load_ident32(P, nc, ident_d):
    i32 = P.sb("ident32", [128, 128])
    P.dma(i32[:], ident_d[:, :], w=["ident32"])
    return i32


import os
DBG = int(os.environ.get('DBG', '0'))


def build_F2(TC, F, E, moe, final):
    P = Prog(); nc = P.nc
    x = P.dram("x", [TC, 1024]); g = P.dram("g", [1, 1024]); ident_d = P.dram("ident", [128, 128])
    wg = P.dram("wg", [E, 1024, F]); wu = P.dram("wu", [E, 1024, F]); wd = P.dram("wd", [E, F, 1024])
    if moe:
        router = P.dram("router", [1024, 8])
    if final:
        gf = P.dram("gf", [1, 1024])
    y = P.dram("y", [TC, 1024], kind="ExternalOutput")
    ST = min(TC, 1024); NTT = ST // 128; NST = TC // ST; NTH = max(1, ST // 512); TH = min(512, ST)
    NFC = F // 128
    GC = 11 if NFC % 11 == 0 else 7
    NG = NFC // GC
    ident = load_ident32(P, nc, ident_d)
    gbc = P.sb("gbc", [128, 1024]); P.dma(gbc[:], g[0:1, :].partition_broadcast(128), w=["gbc"])
    if final:
        gfbc = P.sb("gfbc", [128, 1024]); P.dma(gfbc[:], gf[0:1, :].partition_broadcast(128), w=["gfbc"])
    if moe:
        rsb = P.sb("rsb", [128, 8, 8])
        if not (DBG & 8):
            P.dma(rsb[:], router.rearrange("(kc p) e -> p kc e", p=128), w=["rsb"])
        xnT32 = P.sb("xnT32", [128, 8, 128])
        wt = P.sb("wt", [128, NTT, 8]); lg = P.sb("lg", [128, 8]); eq1 = P.sb("eq1", [128, 8]); eq2 = P.sb("eq2", [128, 8])
        m1 = P.sb("m1", [128, 1]); m2 = P.sb("m2", [128, 1])
    acc = P.sb("acc", [128, NTT, 1024])
    xnT = P.sb("xnT", [128, 8, ST], BF16)
    xn32 = P.sb("xn32", [128, 1024]); sq = P.sb("sq", [128, 1024]); ss = P.sb("ss", [128, 1])
    hT = P.sb("hT", [128, GC, ST], BF16)
    wdb = P.sb("wdb", [128, GC, 1024], BF16)
    wds = [P.sb("wds%d" % i, [128, 1024]) for i in range(2)]
    wgs = [P.sb("wgs%d" % i, [128, 8, 128]) for i in range(2)]
    wus = [P.sb("wus%d" % i, [128, 8, 128]) for i in range(2)]
    wgb = [P.sb("wgb%d" % i, [128, 8, 128], BF16) for i in range(2)]
    wub = [P.sb("wub%d" % i, [128, 8, 128], BF16) for i in range(2)]
    sg = [P.sb("sg%d" % i, [128, TH]) for i in range(2)]
    pst = P.ps("pst", [128, 8, 128])
    psg = [P.ps("psg%d" % i, [128, 512]) for i in range(2)]
    psu = [P.ps("psu%d" % i, [128, 512]) for i in range(2)]
    pso = P.ps("pso", [128, 1024])
    wi = 0; di = 0; gi = 0
    for st in range(NST):
        t0 = st * ST
        for tt in range(NTT):
            atok = "acc%d" % tt
            P.dma(acc[:, tt, :], x[t0 + tt * 128:t0 + (tt + 1) * 128, :], w=[atok])
            P.A(lambda: nc.scalar.activation(out=sq[:], in_=acc[:, tt, :], func=AF.Square, accum_out=ss[:]), r=[atok], w=["sq", "ss"])
            P.V(lambda: nc.vector.tensor_scalar(out=ss[:], in0=ss[:], scalar1=1.0 / 1024, scalar2=1e-6, op0=ALU.mult, op1=ALU.add), r=["ss"], w=["ss"])
            P.A(lambda: nc.scalar.sqrt(out=ss[:], in_=ss[:]), r=["ss"], w=["ss"])
            P.V(lambda: nc.vector.reciprocal(out=ss[:], in_=ss[:]), r=["ss"], w=["ss"])
            P.V(lambda: nc.vector.scalar_tensor_tensor(out=xn32[:], in0=acc[:, tt, :], scalar=ss[:], in1=gbc[:], op0=ALU.mult, op1=ALU.mult),
                r=[atok, "ss", "gbc"], w=["xn32"])
            for kc in range(8):
                P.T(lambda: nc.tensor.transpose(pst[:, kc, :], xn32[:, kc * 128:(kc + 1) * 128], ident[:]), r=["xn32", "ident32"],
                    w=["pst"] if kc == 0 else [], wj=[] if kc == 0 else ["pst"])
            P.V(lambda: nc.vector.tensor_copy(out=xnT[:, :, tt * 128:(tt + 1) * 128], in_=pst[:]), r=["pst"], w=["xnT%d" % tt])
            if moe:
                if not (DBG & 16):
                    P.V(lambda: nc.vector.tensor_copy(out=xnT32[:], in_=pst[:]), r=["pst"], w=["xnT32"])
                for kc in range(8 if not (DBG & 1) else 0):
                    P.T(lambda: nc.tensor.matmul(pso[:, 0:8], lhsT=xnT32[:, kc, :], rhs=rsb[:, kc, :], start=(kc == 0), stop=(kc == 7)),
                        r=["xnT32", "rsb"], w=["pso"] if kc == 0 else [], wj=[] if kc == 0 else ["pso"])
                if DBG & 2:
                    P.V(lambda: nc.vector.memset(wt[:, tt, :], 0.5), w=["wt%d" % tt])
                    continue
                if DBG & 1:
                    P.V(lambda: nc.vector.memset(lg[:], 0.5), w=["lg"])
                else:
                    P.V(lambda: nc.vector.tensor_copy(out=lg[:], in_=pso[:, 0:8]), r=["pso"], w=["lg"])
                P.V(lambda: nc.vector.tensor_reduce(out=m1[:], in_=lg[:], axis=AX.X, op=ALU.max), r=["lg"], w=["m1"])
                P.V(lambda: nc.vector.tensor_scalar(out=eq1[:], in0=lg[:], scalar1=m1[:], scalar2=None, op0=ALU.is_equal), r=["lg", "m1"], w=["eq1"])
                P.V(lambda: nc.vector.scalar_tensor_tensor(out=lg[:], in0=eq1[:], scalar=-1e30, in1=lg[:], op0=ALU.mult, op1=ALU.add), r=["eq1", "lg"], w=["lg"])
                P.V(lambda: nc.vector.tensor_reduce(out=m2[:], in_=lg[:], axis=AX.X, op=ALU.max), r=["lg"], w=["m2"])
                P.V(lambda: nc.vector.tensor_scalar(out=eq2[:], in0=lg[:], scalar1=m2[:], scalar2=None, op0=ALU.is_equal), r=["lg", "m2"], w=["eq2"])
                P.V(lambda: nc.vector.tensor_tensor(out=m1[:], in0=m1[:], in1=m2[:], op=ALU.subtract), r=["m1", "m2"], w=["m1"])
                P.A(lambda: nc.scalar.activation(out=m1[:], in_=m1[:], func=AF.Sigmoid), r=["m1"], w=["m1"])
                P.V(lambda: nc.vector.tensor_tensor(out=eq1[:], in0=eq1[:], in1=eq2[:], op=ALU.subtract), r=["eq1", "eq2"], w=["eq1"])
                P.V(lambda: nc.vector.scalar_tensor_tensor(out=wt[:, tt, :], in0=eq1[:], scalar=m1[:], in1=eq2[:], op0=ALU.mult, op1=ALU.add),
                    r=["eq1", "eq2", "m1"], w=["wt%d" % tt])
        xtoks = ["xnT%d" % tt for tt in range(NTT)]
        for e in range(E):
            for grp in range(NG):
                for fci in range(GC):
                    fc = grp * GC + fci; b = wi % 2; wi += 1
                    P.dma(wgs[b][:], wg[e, :, fc * 128:(fc + 1) * 128].rearrange("(kc p) f -> p kc f", p=128), w=["wgs%d" % b])
                    P.dma(wus[b][:], wu[e, :, fc * 128:(fc + 1) * 128].rearrange("(kc p) f -> p kc f", p=128), w=["wus%d" % b])
                    P.G(lambda: nc.gpsimd.tensor_copy(out=wgb[b][:], in_=wgs[b][:]), r=["wgs%d" % b], w=["wgb%d" % b])
                    P.G(lambda: nc.gpsimd.tensor_copy(out=wub[b][:], in_=wus[b][:]), r=["wus%d" % b], w=["wub%d" % b])
                    for th in range(NTH):
                        pb = gi % 2; gi += 1
                        cols = slice(th * TH, (th + 1) * TH)
                        ttk = xtoks[th * (TH // 128):(th + 1) * (TH // 128)]
                        for kc in range(8):
                            P.T(lambda: nc.tensor.matmul(psg[pb][:, 0:TH], lhsT=wgb[b][:, kc, :], rhs=xnT[:, kc, cols], start=(kc == 0), stop=(kc == 7)),
                                r=["wgb%d" % b] + ttk, w=["psg%d" % pb] if kc == 0 else [], wj=[] if kc == 0 else ["psg%d" % pb])
                        for kc in range(8):
                            P.T(lambda: nc.tensor.matmul(psu[pb][:, 0:TH], lhsT=wub[b][:, kc, :], rhs=xnT[:, kc, cols], start=(kc == 0), stop=(kc == 7)),
                                r=["wub%d" % b] + ttk, w=["psu%d" % pb] if kc == 0 else [], wj=[] if kc == 0 else ["psu%d" % pb])
                        P.A(lambda: nc.scalar.activation(out=sg[pb][:], in_=psg[pb][:, 0:TH], func=AF.Silu), r=["psg%d" % pb], w=["sg%d" % pb])
                        P.V(lambda: nc.vector.tensor_tensor(out=hT[:, fci, cols], in0=psu[pb][:, 0:TH], in1=sg[pb][:], op=ALU.mult),
                            r=["psu%d" % pb, "sg%d" % pb], w=["hT%d_%d" % (fci, th)])
                for fci in range(GC):
                    fc = grp * GC + fci; b = di % 2; di += 1
                    P.dma(wds[b][:], wd[e, fc * 128:(fc + 1) * 128, :], w=["wds%d" % b])
                    P.G(lambda: nc.gpsimd.tensor_copy(out=wdb[:, fci, :], in_=wds[b][:]), r=["wds%d" % b], w=["wdb%d" % fci])
                for tt in range(NTT):
                    th = (tt * 128) // TH
                    pso_, ptk = (pso[:], "pso") if tt % 2 == 0 else (pst[:].rearrange("p a b -> p (a b)"), "pst")
                    for half in range(2):
                        for fci in range(GC):
                            P.T(lambda: nc.tensor.matmul(pso_[:, half * 512:(half + 1) * 512], lhsT=hT[:, fci, tt * 128:(tt + 1) * 128],
                                                         rhs=wdb[:, fci, half * 512:(half + 1) * 512], start=(fci == 0), stop=(fci == GC - 1)),
                                r=["hT%d_%d" % (fci, th), "wdb%d" % fci], w=[ptk] if (fci == 0 and half == 0) else [], wj=[] if (fci == 0 and half == 0) else [ptk])
                    atok = "acc%d" % tt
                    if moe and not (DBG & 4):
                        P.V(lambda: nc.vector.scalar_tensor_tensor(out=acc[:, tt, :], in0=pso_, scalar=wt[:, tt, e:e + 1], in1=acc[:, tt, :], op0=ALU.mult, op1=ALU.add),
                            r=[ptk, "wt%d" % tt, atok], w=[atok])
                    else:
                        P.V(lambda: nc.vector.tensor_tensor(out=acc[:, tt, :], in0=pso_, in1=acc[:, tt, :], op=ALU.add), r=[ptk, atok], w=[atok])
        for tt in range(NTT):
            atok = "acc%d" % tt
            if final:
                P.A(lambda: nc.scalar.activation(out=sq[:], in_=acc[:, tt, :], func=AF.Square, accum_out=ss[:]), r=[atok], w=["sq", "ss"])
                P.V(lambda: nc.vector.tensor_scalar(out=ss[:], in0=ss[:], scalar1=1.0 / 1024, scalar2=1e-6, op0=ALU.mult, op1=ALU.add), r=["ss"], w=["ss"])
                P.A(lambda: nc.scalar.sqrt(out=ss[:], in_=ss[:]), r=["ss"], w=["ss"])
                P.V(lambda: nc.vector.reciprocal(out=ss[:], in_=ss[:]), r=["ss"], w=["ss"])
                P.V(lambda: nc.vector.scalar_tensor_tensor(out=acc[:, tt, :], in0=acc[:, tt, :], scalar=ss[:], in1=gfbc[:], op0=ALU.mult, op1=ALU.mult),
                    r=[atok, "ss", "gfbc"], w=[atok])
            P.dma(y[t0 + tt * 128:t0 + (tt + 1) * 128, :], acc[:, tt, :], r=[atok], wj=["y"], key=atok)
    P.finish(); P.close()
    return nc


def run_F2(xf, prm, l, ncores=8):
    T = xf.shape[0]; TC = T // ncores
    moe = (l % 2 == 1); final = (l == 1); i = l // 2
    com = {"g": prm["norm_ffn_g"][l][None, :], "ident": np.eye(128, dtype=np.float32)}
    if moe:
        com.update(wg=prm["moe_w_gate"][i], wu=prm["moe_w_up"][i], wd=prm["moe_w_down"][i], router=prm["moe_router"][i])
        E, F = 8, 3584
    else:
        com.update(wg=prm["dense_w_gate"][i][None], wu=prm["dense_w_up"][i][None], wd=prm["dense_w_down"][i][None])
        E, F = 1, 2816
    if final:
        com["gf"] = prm["final_norm_g"][None, :]
    nc = build_F2(TC, F, E, moe, final)
    maps = [dict(com, x=xf[c * TC:(c + 1) * TC]) for c in range(ncores)]
    res = run_bass_kernel_spmd(nc, maps, core_ids=list(range(ncores)))
    return np.concatenate([r["y"] for r in res.results], axis=0)


def prep_qk(P, nc, src, dstT, PADC, S, nrot, cos_d, sin_d, ident, scale, norm_g, pfx, ps_t):
    NT = S // 128
    xt = [P.sb(pfx + "x%d" % i, [128, 64]) for i in range(2)]
    cs = [P.sb(pfx + "c%d" % i, [128, 2, nrot]) for i in range(2)]
    tmp = P.sb(pfx + "tmp", [128, 4, nrot]); sq = P.sb(pfx + "sq", [128, 64]); ss = P.sb(pfx + "ss", [128, 1])
    if norm_g is not None:
        gbc = P.sb(pfx + "gbc", [128, 64]); P.dma(gbc[:], norm_g[0:1, :].partition_broadcast(128), w=[pfx + "gbc"])
    for t in range(NT):
        b = t % 2; xk = pfx + "x%d" % b; ck = pfx + "c%d" % b
        x = xt[b]; c = cs[b]
        P.dma(x[:], src[t * 128:(t + 1) * 128, :], w=[xk])
        P.dma(c[:, 0, :], cos_d[t * 128:(t + 1) * 128, :], w=[ck])
        P.dma(c[:, 1, :], sin_d[t * 128:(t + 1) * 128, :], wj=[ck])
        if norm_g is not None:
            P.A(lambda: nc.scalar.activation(out=sq[:], in_=x[:], func=AF.Square, accum_out=ss[:]), r=[xk], w=[pfx + "sq", pfx + "ss"])
            P.V(lambda: nc.vector.tensor_scalar(out=ss[:], in0=ss[:], scalar1=1.0 / 64, scalar2=1e-6, op0=ALU.mult, op1=ALU.add), r=[pfx + "ss"], w=[pfx + "ss"])
            P.A(lambda: nc.scalar.sqrt(out=ss[:], in_=ss[:]), r=[pfx + "ss"], w=[pfx + "ss"])
            P.V(lambda: nc.vector.reciprocal(out=ss[:], in_=ss[:]), r=[pfx + "ss"], w=[pfx + "ss"])
            P.V(lambda: nc.vector.scalar_tensor_tensor(out=x[:], in0=x[:], scalar=ss[:], in1=gbc[:], op0=ALU.mult, op1=ALU.mult), r=[xk, pfx + "ss", pfx + "gbc"], w=[xk])
        n = nrot
        x1, x2 = x[:, 0:n], x[:, n:2 * n]
        tk = pfx + "tmp"
        P.V(lambda: nc.vector.tensor_tensor(out=tmp[:, 0, :], in0=x1, in1=c[:, 0, :], op=ALU.mult), r=[xk, ck], w=[tk])
        P.V(lambda: nc.vector.tensor_tensor(out=tmp[:, 1, :], in0=x2, in1=c[:, 1, :], op=ALU.mult), r=[xk, ck], wj=[tk])
        P.V(lambda: nc.vector.tensor_tensor(out=tmp[:, 2, :], in0=x2, in1=c[:, 0, :], op=ALU.mult), r=[xk, ck], wj=[tk])
        P.V(lambda: nc.vector.tensor_tensor(out=tmp[:, 3, :], in0=x1, in1=c[:, 1, :], op=ALU.mult), r=[xk, ck], wj=[tk])
        P.V(lambda: nc.vector.tensor_tensor(out=x1, in0=tmp[:, 0, :], in1=tmp[:, 1, :], op=ALU.subtract), r=[tk], w=[xk])
        P.V(lambda: nc.vector.tensor_tensor(out=x2, in0=tmp[:, 2, :], in1=tmp[:, 3, :], op=ALU.add), r=[tk], wj=[xk])
        P.T(lambda: nc.tensor.transpose(ps_t[0:64, 0:128], x[:], ident[:]), r=[xk, "ident32"], w=["ps_t"])
        P.V(lambda: nc.vector.tensor_scalar(out=dstT[:, PADC + t * 128:PADC + (t + 1) * 128], in0=ps_t[0:64, 0:128], scalar1=scale, scalar2=None, op0=ALU.mult),
            r=["ps_t"], wj=[pfx + "T"])


def normalize_blk(P, nc, acc, acctok, W, yT, c0, ones, ps_bc, rl, ob, obtok):
    P.V(lambda: nc.vector.reciprocal(out=rl[64:65, 0:W], in_=acc[64:65, 0:W]), r=[acctok], w=["rl"])
    P.T(lambda: nc.tensor.matmul(ps_bc[0:64, 0:W], lhsT=ones[64:65, 0:64], rhs=rl[64:65, 0:W], start=True, stop=True), r=["rl", "ones"], w=["ps_bc"])
    P.V(lambda: nc.vector.tensor_tensor(out=ob[:, 0:W], in0=acc[0:64, 0:W], in1=ps_bc[0:64, 0:W], op=ALU.mult), r=[acctok, "ps_bc"], w=[obtok])
    P.dma(yT[:, c0:c0 + W], ob[:, 0:W], r=[obtok], wj=["yT"], key=obtok)


def build_MD(S):
    P = Prog(); nc = P.nc
    q = P.dram("q", [S, 64]); k = P.dram("k", [S, 64]); v = P.dram("v", [S, 64])
    qg = P.dram("qg", [1, 64]); kg = P.dram("kg", [1, 64]); cos_d = P.dram("cos", [S, 32]); sin_d = P.dram("sin", [S, 32])
    ident_d = P.dram("ident", [128, 128])
    yT = P.dram("yT", [64, S], kind="ExternalOutput")
    NT = S // 128
    ident = load_ident32(P, nc, ident_d)
    ones = P.sb("ones", [128, 64]); P.V(lambda: nc.vector.memset(ones[:], 1.0), w=["ones"])
    qT = P.sb("qT", [64, S], BF16); kT = P.sb("kT", [64, S], BF16)
    ps_t = P.ps("ps_t", [128, 512])
    prep_qk(P, nc, q, qT, 0, S, 32, cos_d, sin_d, ident, 0.125, qg, "q", ps_t)
    prep_qk(P, nc, k, kT, 0, S, 32, cos_d, sin_d, ident, 1.0, kg, "k", ps_t)
    Vx = P.sb("Vx", [128, NT, 65], BF16)
    P.V(lambda: nc.vector.memset(Vx[:, :, 64:65], 1.0), w=["Vx"])
    VC = min(16, NT)
    vst = [P.sb("vst%d" % i, [128, VC, 64]) for i in range(2)]
    for ci, m0 in enumerate(range(0, NT, VC)):
        vb = ci % 2
        P.dma(vst[vb][:], v[m0 * 128:(m0 + VC) * 128, :].rearrange("(m p) c -> p m c", p=128), w=["vst%d" % vb])
        P.V(lambda: nc.vector.tensor_copy(out=Vx[:, m0:m0 + VC, 0:64], in_=vst[vb][:]), r=["vst%d" % vb], wj=["Vx"])
    acc = [P.sb("acc%d" % i, [65, 512]) for i in range(2)]
    rl = P.sb("rl", [65, 512]); ob = [P.sb("ob%d" % i, [64, 512]) for i in range(2)]
    NB = 3
    ps_s = [P.ps("ps_s%d" % i, [128, 512]) for i in range(NB)]
    pT = [P.sb("pT%d" % i, [128, 512], BF16) for i in range(NB)]
    po = [P.ps("po%d" % i, [128, 512]) for i in range(2)]
    ps_bc = P.ps("ps_bc", [128, 512])
    W = min(512, S)
    its = [(qb, kt) for qb in range(S // W) for kt in range(NT)]
    LA = 2

    def emit_S(i):
        qb, kt = its[i]; b = i % NB
        P.T(lambda: nc.tensor.matmul(ps_s[b][:, 0:W], lhsT=kT[:, kt * 128:(kt + 1) * 128], rhs=qT[:, qb * W:(qb + 1) * W], start=True, stop=True),
            r=["qT", "kT"], w=["ps_s%d" % b])

    for i in range(min(LA, len(its))):
        emit_S(i)
    for i, (qb, kt) in enumerate(its):
        b = i % NB; pb = qb % 2
        if i + LA < len(its):
            emit_S(i + LA)
        P.A(lambda: nc.scalar.activation(out=pT[b][:, 0:W], in_=ps_s[b][:, 0:W], func=AF.Exp), r=["ps_s%d" % b], w=["pT%d" % b])
        P.T(lambda: nc.tensor.matmul(po[pb][0:65, 0:W], lhsT=Vx[:, kt, :], rhs=pT[b][:, 0:W], start=(kt == 0), stop=(kt == NT - 1)),
            r=["Vx", "pT%d" % b], w=["po%d" % pb] if kt == 0 else [], wj=[] if kt == 0 else ["po%d" % pb])
        if kt == NT - 1:
            P.V(lambda: nc.vector.tensor_copy(out=acc[pb][:, 0:W], in_=po[pb][0:65, 0:W]), r=["po%d" % pb], w=["acc%d" % pb])
            normalize_blk(P, nc, acc[pb], "acc%d" % pb, W, yT, qb * W, ones, ps_bc, rl, ob[pb], "ob%d" % pb)
    P.finish(); P.close()
    return nc


def axial_tables(S):
    def ang(pos, n, theta):
        inv = (np.float32(theta) ** (-np.arange(n, dtype=np.float32) / np.float32(n))).astype(np.float32)
        return pos.astype(np.float32)[:, None] * inv[None, :]
    t = np.arange(S)
    a = np.concatenate([ang(t // 64, 16, 10000.0), ang(t % 64, 16, 10000.0)], axis=-1).astype(np.float32)
    return np.cos(a).astype(np.float32), np.sin(a).astype(np.float32)


def rope_tables(S):
    inv = (np.float32(500000.0) ** (-np.arange(8, dtype=np.float32) / np.float32(8))).astype(np.float32)
    a = (np.arange(S).astype(np.float32)[:, None] * inv[None, :]).astype(np.float32)
    return np.cos(a).astype(np.float32), np.sin(a).astype(np.float32)


def run_MD(zq, zk, zv, prm, l):
    B, S, _ = zq.shape
    nc = build_MD(S)
    cos, sin = axial_tables(S)
    com = {"qg": prm["gqa_q_norm"][l][None, :], "kg": prm["gqa_k_norm"][l][None, :], "cos": cos, "sin": sin, "ident": np.eye(128, dtype=np.float32)}
    maps = []
    for c in range(B * 4):
        b, h = c // 4, c % 4
        maps.append(dict(com, q=np.ascontiguousarray(zq[b, :, h * 64:(h + 1) * 64]), k=np.ascontiguousarray(zk[b, :, (h // 2) * 64:(h // 2 + 1) * 64]),
                         v=np.ascontiguousarray(zv[b, :, (h // 2) * 64:(h // 2 + 1) * 64])))
    res = run_bass_kernel_spmd(nc, maps, core_ids=list(range(B * 4)))
    y = np.zeros((B, S, 256), np.float32)
    for c in range(B * 4):
        b, h = c // 4, c % 4
        y[b, :, h * 64:(h + 1) * 64] = res.results[c]["yT"].T
    return y


def build_MA(S):
    P = Prog(); nc = P.nc
    PADC = 1024
    DIL = (1, 4, 16)
    q = P.dram("q", [S, 64]); k = P.dram("k", [S, 64])
    vx = [P.dram("vx%d" % d, [d * (S // d + 128), 65]) for d in DIL]
    cos_d = P.dram("cos", [S, 8]); sin_d = P.dram("sin", [S, 8])
    ident_d = P.dram("ident", [128, 128]); mask_d = P.dram("mask", [128, 256])
    yT = P.dram("yT", [64, S], kind="ExternalOutput")
    ident = load_ident32(P, nc, ident_d)
    ones = P.sb("ones", [128, 64]); P.V(lambda: nc.vector.memset(ones[:], 1.0), w=["ones"])
    m32 = P.sb("m32", [128, 256]); mask = P.sb("maskb", [128, 256], BF16)
    P.dma(m32[:], mask_d[:, :], w=["m32"]); P.V(lambda: nc.vector.tensor_copy(out=mask[:], in_=m32[:]), r=["m32"], w=["mask"])
    qT = P.sb("qT", [64, S + 2 * PADC], BF16); kT = P.sb("kT", [64, S + 2 * PADC], BF16)
    P.V(lambda: nc.vector.memset(kT[:, 0:PADC], 0.0), w=["kT"]); P.V(lambda: nc.vector.memset(kT[:, PADC + S:], 0.0), wj=["kT"])
    P.V(lambda: nc.vector.memset(qT[:, 0:PADC], 0.0), w=["qT"]); P.V(lambda: nc.vector.memset(qT[:, PADC + S:], 0.0), wj=["qT"])
    ps_t = P.ps("ps_t", [128, 512])
    prep_qk(P, nc, q, qT, PADC, S, 8, cos_d, sin_d, ident, 0.125, None, "q", ps_t)
    prep_qk(P, nc, k, kT, PADC, S, 8, cos_d, sin_d, ident, 1.0, None, "k", ps_t)
    SB = 2048
    accT = P.sb("accT", [65, SB])
    NB = 3
    ps_s = [P.ps("ps_s%d" % i, [128, 512]) for i in range(NB)]
    pT = [P.sb("pT%d" % i, [128, 256], BF16) for i in range(NB)]
    pTm = [P.sb("pTm%d" % i, [128, 256], BF16) for i in range(NB)]
    po = [P.ps("po%d" % i, [128, 512]) for i in range(NB)]
    ps_bc = ps_t
    rl = P.sb("rl", [65, 512]); ob = [P.sb("ob%d" % i, [64, 512]) for i in range(2)]
    Vxs = {}
    vst = [P.sb("vst%d" % i, [128, 16, 65]) for i in range(2)]
    ci = 0
    for di, d in enumerate(DIL):
        L = S // d; NKT = L // 128 + 1; NTL = d * NKT
        Vx = P.sb("Vx%d" % d, [128, NTL, 65], BF16)
        for m0 in range(0, NTL, 16):
            m1 = min(NTL, m0 + 16); vb = ci % 2; ci += 1
            P.dma(vst[vb][:, 0:m1 - m0, :], vx[di][m0 * 128:m1 * 128, :].rearrange("(m p) c -> p m c", p=128), w=["vst%d" % vb])
            P.V(lambda: nc.vector.tensor_copy(out=Vx[:, m0:m1, :], in_=vst[vb][:, 0:m1 - m0, :]), r=["vst%d" % vb], wj=["Vx%d" % d])
        Vxs[d] = (Vx, NKT)
    oi = 0
    its = []
    for sbi in range(S // SB):
        for di, d in enumerate(DIL):
            for r in range(d):
                for bl in range(SB // (128 * d)):
                    its.append((sbi, di, d, r, bl))
    LA = 2

    def emit_S(i):
        sbi, di, d, r, bl = its[i]; b = i % NB
        i0 = (sbi * SB // d) + bl * 128
        qs = PADC + r + d * i0
        rhs = qT[:, qs:qs + 127 * d + 1:d]
        for ab in range(2):
            ks = PADC + r + d * (i0 - 64 + 128 * ab)
            P.T(lambda: nc.tensor.matmul(ps_s[b][:, ab * 128:(ab + 1) * 128], lhsT=kT[:, ks:ks + 127 * d + 1:d], rhs=rhs, start=True, stop=True),
                r=["qT", "kT"], w=["ps_s%d" % b] if ab == 0 else [], wj=[] if ab == 0 else ["ps_s%d" % b])

    for i in range(min(LA, len(its))):
        emit_S(i)
    for i, (sbi, di, d, r, bl) in enumerate(its):
        b = i % NB
        Vx, NKT = Vxs[d]
        i0 = (sbi * SB // d) + bl * 128; blk = i0 // 128
        if i + LA < len(its):
            emit_S(i + LA)
        P.A(lambda: nc.scalar.activation(out=pT[b][:], in_=ps_s[b][:, 0:256], func=AF.Exp), r=["ps_s%d" % b], w=["pT%d" % b])
        P.G(lambda: nc.gpsimd.tensor_tensor(out=pTm[b][:], in0=pT[b][:], in1=mask[:], op=ALU.mult), r=["pT%d" % b, "mask"], w=["pTm%d" % b])
        for ab in range(2):
            P.T(lambda: nc.tensor.matmul(po[b][0:65, 0:128], lhsT=Vx[:, r * NKT + blk + ab, :], rhs=pTm[b][:, ab * 128:(ab + 1) * 128], start=(ab == 0), stop=(ab == 1)),
                r=["Vx%d" % d, "pTm%d" % b], w=["po%d" % b] if ab == 0 else [], wj=[] if ab == 0 else ["po%d" % b])
        t0 = r + d * bl * 128
        dst = accT[:, t0:t0 + 127 * d + 1:d]
        if di == 0:
            P.V(lambda: nc.vector.tensor_copy(out=dst, in_=po[b][0:65, 0:128]), r=["po%d" % b], w=["accT"] if (bl == 0) else [], wj=[] if (bl == 0) else ["accT"])
        else:
            P.V(lambda: nc.vector.tensor_tensor(out=dst, in0=dst, in1=po[b][0:65, 0:128], op=ALU.add), r=["po%d" % b, "accT"], w=["accT"])
        last = (i + 1 == len(its)) or (its[i + 1][0] != sbi)
        if last:
            for c0 in range(0, SB, 512):
                o = oi % 2; oi += 1
                normalize_blk(P, nc, accT[:, c0:c0 + 512], "accT", 512, yT, sbi * SB + c0, ones, ps_bc, rl, ob[o], "ob%d" % o)
    P.finish(); P.close()
    return nc


def dil_mask():
    kk = np.arange(128)[:, None]; qq = np.arange(128)[None, :]
    return np.concatenate([(kk >= qq), (kk <= qq)], axis=1).astype(np.float32)


def vext_dilated(v, d):
    S = v.shape[0]; L = S // d
    out = np.zeros((d, L + 128, 65), np.float32)
    vr = v.reshape(L, d, 64).transpose(1, 0, 2)
    out[:, 64:64 + L, :64] = vr
    out[:, 64:64 + L, 64] = 1.0
    return out.reshape(d * (L + 128), 65)


def run_MA(za, prm, l):
    B, S, _ = za.shape
    nc = build_MA(S)
    cos, sin = rope_tables(S)
    com = {"cos": cos, "sin": sin, "ident": np.eye(128, dtype=np.float32), "mask": dil_mask()}
    maps = []
    for c in range(B * 4):
        b, h = c // 4, c % 4
        v = za[b, :, 512 + h * 64:512 + (h + 1) * 64]
        m = dict(com, q=np.ascontiguousarray(za[b, :, h * 64:(h + 1) * 64]), k=np.ascontiguousarray(za[b, :, 256 + h * 64:256 + (h + 1) * 64]))
        for d in (1, 4, 16):
            m["vx%d" % d] = vext_dilated(v, d)
        maps.append(m)
    res = run_bass_kernel_spmd(nc, maps, core_ids=list(range(B * 4)))
    y = np.zeros((B, S, 256), np.float32)
    for c in range(B * 4):
        b, h = c // 4, c % 4
        y[b, :, h * 64:(h + 1) * 64] = res.results[c]["yT"].T
    return y


TWO_PI = 6.283185307179586
PI = 3.141592653589793


def range_reduce(P, nc, r, ki, tok, shape):
    kf = P.sb(tok + "_kf", shape)
    P.V(lambda: nc.vector.tensor_scalar(out=kf[:], in0=r[:], scalar1=1.0 / TWO_PI, scalar2=None, op0=ALU.mult), r=[tok], w=[tok + "kf"])
    P.V(lambda: nc.vector.tensor_copy(out=ki[:], in_=kf[:]), r=[tok + "kf"], w=[tok + "ki"])
    P.V(lambda: nc.vector.tensor_copy(out=kf[:], in_=ki[:]), r=[tok + "ki"], w=[tok + "kf"])
    P.V(lambda: nc.vector.scalar_tensor_tensor(out=r[:], in0=kf[:], scalar=-TWO_PI, in1=r[:], op0=ALU.mult, op1=ALU.add), r=[tok + "kf", tok], w=[tok])
    for (cmp, thr, add) in ((ALU.is_gt, PI, -TWO_PI), (ALU.is_lt, -PI, TWO_PI), (ALU.is_gt, PI, -TWO_PI)):
        P.V(lambda: nc.vector.tensor_scalar(out=kf[:], in0=r[:], scalar1=thr, scalar2=add, op0=cmp, op1=ALU.mult), r=[tok], w=[tok + "kf"])
        P.V(lambda: nc.vector.tensor_tensor(out=r[:], in0=r[:], in1=kf[:], op=ALU.add), r=[tok, tok + "kf"], w=[tok])
    P.V(lambda: nc.vector.tensor_scalar(out=r[:], in0=r[:], scalar1=3.1415925, scalar2=-3.1415925, op0=ALU.min, op1=ALU.max), r=[tok], w=[tok])


def build_MC(S):
    P = Prog(); nc = P.nc
    T = min(512, S); NCH = S // T
    uT_d = [P.dram("uT%d" % d, [64, S]) for d in range(2)]
    a_re = P.dram("a_re", [2, 4, 64]); a_im = P.dram("a_im", [2, 4, 64]); ldt = P.dram("ldt", [2, 4])
    b_re = P.dram("b_re", [4, 64, 16]); b_im = P.dram("b_im", [4, 64, 16])
    c_re = P.dram("c_re", [2, 4, 16, 64]); c_im = P.dram("c_im", [2, 4, 16, 64])
    ident_d = P.dram("ident", [128, 128]); iota_d = P.dram("iota", [1, T + 1])
    yT_d = [P.dram("yT%d" % d, [64, S], kind="ExternalOutput") for d in range(2)]
    ident = load_ident32(P, nc, ident_d)
    iota = P.sb("iota", [128, T + 1]); P.dma(iota[:], iota_d[0:1, :].partition_broadcast(128), w=["iota"])
    uT = []
    UC = min(2048, S)
    ust = [P.sb("ust%d" % i, [64, UC]) for i in range(2)]
    ui = 0
    for d in range(2):
        u = P.sb("uTb%d" % d, [64, S], BF16)
        for c0 in range(0, S, UC):
            ub = ui % 2; ui += 1
            P.dma(ust[ub][:], uT_d[d][:, c0:c0 + UC], w=["ust%d" % ub])
            P.V(lambda: nc.vector.tensor_copy(out=u[:, c0:c0 + UC], in_=ust[ub][:]), r=["ust%d" % ub], wj=["uT%d" % d])
        uT.append(u)
    ps_t = P.ps("ps_t", [128, 512])
    tiles = {}
    for d in range(2):
        for gp in range(2):
            n = "t%d%d" % (d, gp)
            prm = P.sb(n + "prm", [128, 16])
            pk = n + "prm"
            P.dma(prm[:, 0:1], a_re[d, 2 * gp:2 * gp + 2, :].rearrange("g (p o) -> (g p) o", o=1), w=[pk])
            P.dma(prm[:, 1:2], a_im[d, 2 * gp:2 * gp + 2, :].rearrange("g (p o) -> (g p) o", o=1), wj=[pk])
            for g in range(2):
                P.dma(prm[g * 64:(g + 1) * 64, 2:3], ldt[d:d + 1, 2 * gp + g:2 * gp + g + 1].partition_broadcast(64), wj=[pk])
            c = lambda i: prm[:, i:i + 1]
            P.A(lambda: nc.scalar.activation(out=c(2), in_=c(2), func=AF.Exp), r=[pk], w=[pk])
            P.V(lambda: nc.vector.tensor_tensor(out=c(4), in0=c(1), in1=c(2), op=ALU.mult), r=[pk], w=[pk])
            P.V(lambda: nc.vector.tensor_tensor(out=c(3), in0=c(0), in1=c(2), op=ALU.mult), r=[pk], w=[pk])
            P.A(lambda: nc.scalar.activation(out=c(3), in_=c(3), func=AF.Exp), r=[pk], w=[pk])
            cosT = P.sb(n + "cos", [128, T + 1]); sinT = P.sb(n + "sin", [128, T + 1]); ki = P.sb(n + "ki", [128, T + 1], I32)
            P.V(lambda: nc.vector.tensor_scalar(out=sinT[:], in0=iota[:], scalar1=c(4), scalar2=None, op0=ALU.mult), r=["iota", pk], w=[n + "sin"])
            P.V(lambda: nc.vector.tensor_scalar(out=cosT[:], in0=sinT[:], scalar1=PI / 2, scalar2=None, op0=ALU.add), r=[n + "sin"], w=[n + "cos"])
            range_reduce(P, nc, sinT, ki, n + "sin", [128, T + 1])
            range_reduce(P, nc, cosT, ki, n + "cos", [128, T + 1])
            P.A(lambda: nc.scalar.activation(out=sinT[:], in_=sinT[:], func=AF.Sin), r=[n + "sin"], w=[n + "sin"])
            P.A(lambda: nc.scalar.activation(out=cosT[:], in_=cosT[:], func=AF.Sin), r=[n + "cos"], w=[n + "cos"])
            P.V(lambda: nc.vector.tensor_copy(out=c(5), in_=cosT[:, 1:2]), r=[n + "cos", pk], w=[pk])
            P.V(lambda: nc.vector.tensor_copy(out=c(6), in_=sinT[:, 1:2]), r=[n + "sin", pk], w=[pk])
            P.V(lambda: nc.vector.tensor_copy(out=c(13), in_=cosT[:, T:T + 1]), r=[n + "cos", pk], w=[pk])
            P.V(lambda: nc.vector.tensor_copy(out=c(14), in_=sinT[:, T:T + 1]), r=[n + "sin", pk], w=[pk])
            P.V(lambda: nc.vector.tensor_scalar(out=c(15), in0=c(14), scalar1=-1.0, scalar2=None, op0=ALU.mult), r=[pk], w=[pk])
            P.V(lambda: nc.vector.tensor_tensor(out=c(5), in0=c(5), in1=c(3), op=ALU.mult), r=[pk], w=[pk])
            P.V(lambda: nc.vector.tensor_tensor(out=c(6), in0=c(6), in1=c(3), op=ALU.mult), r=[pk], w=[pk])
            P.V(lambda: nc.vector.tensor_scalar(out=c(7), in0=c(5), scalar1=-1.0, scalar2=None, op0=ALU.add), r=[pk], w=[pk])
            P.V(lambda: nc.vector.tensor_tensor(out=c(8), in0=c(0), in1=c(0), op=ALU.mult), r=[pk], w=[pk])
            P.V(lambda: nc.vector.tensor_tensor(out=c(11), in0=c(1), in1=c(1), op=ALU.mult), r=[pk], w=[pk])
            P.V(lambda: nc.vector.tensor_tensor(out=c(8), in0=c(8), in1=c(11), op=ALU.add), r=[pk], w=[pk])
            P.V(lambda: nc.vector.reciprocal(out=c(8), in_=c(8)), r=[pk], w=[pk])
            P.V(lambda: nc.vector.tensor_tensor(out=c(9), in0=c(7), in1=c(0), op=ALU.mult), r=[pk], w=[pk])
            P.V(lambda: nc.vector.tensor_tensor(out=c(11), in0=c(6), in1=c(1), op=ALU.mult), r=[pk], w=[pk])
            P.V(lambda: nc.vector.tensor_tensor(out=c(9), in0=c(9), in1=c(11), op=ALU.add), r=[pk], w=[pk])
            P.V(lambda: nc.vector.tensor_tensor(out=c(9), in0=c(9), in1=c(8), op=ALU.mult), r=[pk], w=[pk])
            P.V(lambda: nc.vector.tensor_tensor(out=c(10), in0=c(6), in1=c(0), op=ALU.mult), r=[pk], w=[pk])
            P.V(lambda: nc.vector.tensor_tensor(out=c(11), in0=c(7), in1=c(1), op=ALU.mult), r=[pk], w=[pk])
            P.V(lambda: nc.vector.tensor_tensor(out=c(10), in0=c(10), in1=c(11), op=ALU.subtract), r=[pk], w=[pk])
            P.V(lambda: nc.vector.tensor_tensor(out=c(10), in0=c(10), in1=c(8), op=ALU.mult), r=[pk], w=[pk])
            P.V(lambda: nc.vector.tensor_scalar(out=c(12), in0=c(10), scalar1=-1.0, scalar2=None, op0=ALU.mult), r=[pk], w=[pk])
            braw = P.sb(n + "braw", [128, 2, 16]); bk = n + "braw"
            P.dma(braw[:, 0, :], b_re[2 * gp:2 * gp + 2].rearrange("g p c -> (g p) c"), w=[bk])
            P.dma(braw[:, 1, :], b_im[2 * gp:2 * gp + 2].rearrange("g p c -> (g p) c"), wj=[bk])
            BD = P.sb(n + "BD", [128, 2, 64]); tb = P.sb(n + "tb", [128, 16])
            P.V(lambda: nc.vector.memset(BD[:], 0.0), w=[n + "BD"])
            for g in range(2):
                rows = slice(g * 64, (g + 1) * 64); cols = slice(32 * gp + 16 * g, 32 * gp + 16 * g + 16)
                P.V(lambda: nc.vector.tensor_scalar(out=tb[rows, :], in0=braw[rows, 1, :], scalar1=prm[rows, 12:13], scalar2=None, op0=ALU.mult), r=[bk, pk], w=[n + "tb"])
                P.V(lambda: nc.vector.scalar_tensor_tensor(out=BD[rows, 0, cols], in0=braw[rows, 0, :], scalar=prm[rows, 9:10], in1=tb[rows, :], op0=ALU.mult, op1=ALU.add),
                    r=[bk, pk, n + "tb"], wj=[n + "BD"])
                P.V(lambda: nc.vector.tensor_scalar(out=tb[rows, :], in0=braw[rows, 0, :], scalar1=prm[rows, 10:11], scalar2=None, op0=ALU.mult), r=[bk, pk], w=[n + "tb"])
                P.V(lambda: nc.vector.scalar_tensor_tensor(out=BD[rows, 1, cols], in0=braw[rows, 1, :], scalar=prm[rows, 9:10], in1=tb[rows, :], op0=ALU.mult, op1=ALU.add),
                    r=[bk, pk, n + "tb"], wj=[n + "BD"])
            BT = P.sb(n + "BT", [64, 2, 128], BF16)
            for ri in range(2):
                P.T(lambda: nc.tensor.transpose(ps_t[0:64, 0:128], BD[:, ri, :], ident[:]), r=[n + "BD", "ident32"], w=["ps_t"])
                P.V(lambda: nc.vector.tensor_copy(out=BT[:, ri, :], in_=ps_t[0:64, 0:128]), r=["ps_t"], wj=[n + "BT"])
            craw = P.sb(n + "craw", [128, 2, 16]); ck = n + "craw"
            for g in range(2):
                P.dma(craw[g * 64:(g + 1) * 64, 0, :], c_re[d, 2 * gp + g].rearrange("c p -> p c"), wj=[ck], allow_slow_non_contiguous=True)
                P.dma(craw[g * 64:(g + 1) * 64, 1, :], c_im[d, 2 * gp + g].rearrange("c p -> p c"), wj=[ck], allow_slow_non_contiguous=True)
            CT = P.sb(n + "CT", [128, 2, 64], BF16)
            P.V(lambda: nc.vector.memset(CT[:], 0.0), w=[n + "CT"])
            for g in range(2):
                rows = slice(g * 64, (g + 1) * 64); cols = slice(32 * gp + 16 * g, 32 * gp + 16 * g + 16)
                P.V(lambda: nc.vector.tensor_copy(out=CT[rows, 0, cols], in_=craw[rows, 0, :]), r=[ck], wj=[n + "CT"])
                P.V(lambda: nc.vector.tensor_scalar(out=CT[rows, 1, cols], in0=craw[rows, 1, :], scalar1=-1.0, scalar2=None, op0=ALU.mult), r=[ck], wj=[n + "CT"])
            rho_t = P.sb(n + "rho", [128, T])
            P.V(lambda: nc.vector.tensor_scalar(out=rho_t[:], in0=iota[:, 0:T], scalar1=0.0, scalar2=prm[:, 3:4], op0=ALU.mult, op1=ALU.add), r=["iota", pk], w=[n + "rho"])
            init = P.sb(n + "init", [128, 2]); P.V(lambda: nc.vector.memset(init[:], 0.0), w=[n + "init"])
            tiles[(d, gp)] = dict(n=n, prm=prm, pk=pk, cosT=cosT, sinT=sinT, BT=BT, CT=CT, rho=rho_t, init=init)
    ps_b = [P.ps("ps_b%d" % i, [128, 512]) for i in range(2)]
    ps_y = P.ps("ps_y", [128, 512])
    m = [P.sb("m%d" % i, [128, T]) for i in range(4)]
    bp = [P.sb("bp%d" % i, [128, T]) for i in range(2)]
    wv = [P.sb("wv%d" % i, [128, T]) for i in range(2)]
    pp = [P.sb("pp%d" % i, [128, T]) for i in range(4)]
    xb = [[P.sb("xb%d_%d" % (gp, i), [128, T], BF16) for i in range(2)] for gp in range(2)]
    yo = [P.sb("yo%d" % i, [64, T]) for i in range(2)]
    tmpc = P.sb("tmpc", [128, 2])
    it = 0
    for d in range(2):
        for ch in range(NCH):
            cols = slice(ch * T, (ch + 1) * T)
            for gp in range(2):
                t = tiles[(d, gp)]; n = t["n"]; cosT, sinT = t["cosT"], t["sinT"]
                for ri in range(2):
                    P.T(lambda: nc.tensor.matmul(ps_b[ri][:, 0:T], lhsT=t["BT"][:, ri, :], rhs=uT[d][:, cols], start=True, stop=True), r=[n + "BT", "uT%d" % d], w=["ps_b%d" % ri])
                P.V(lambda: nc.vector.tensor_tensor(out=m[0][:], in0=ps_b[0][:, 0:T], in1=cosT[:, 0:T], op=ALU.mult), r=["ps_b0", n + "cos"], w=["m0"])
                P.V(lambda: nc.vector.tensor_tensor(out=m[1][:], in0=ps_b[1][:, 0:T], in1=sinT[:, 0:T], op=ALU.mult), r=["ps_b1", n + "sin"], w=["m1"])
                P.V(lambda: nc.vector.tensor_tensor(out=m[2][:], in0=ps_b[1][:, 0:T], in1=cosT[:, 0:T], op=ALU.mult), r=["ps_b1", n + "cos"], w=["m2"])
                P.V(lambda: nc.vector.tensor_tensor(out=m[3][:], in0=ps_b[0][:, 0:T], in1=sinT[:, 0:T], op=ALU.mult), r=["ps_b0", n + "sin"], w=["m3"])
                P.G(lambda: nc.gpsimd.tensor_tensor(out=bp[0][:], in0=m[0][:], in1=m[1][:], op=ALU.add), r=["m0", "m1"], w=["bp0"])
                P.G(lambda: nc.gpsimd.tensor_tensor(out=bp[1][:], in0=m[2][:], in1=m[3][:], op=ALU.subtract), r=["m2", "m3"], w=["bp1"])
                for ri in range(2):
                    P.V(lambda: nc.vector.tensor_tensor_scan(out=wv[ri][:], data0=t["rho"][:], data1=bp[ri][:], initial=t["init"][:, ri:ri + 1], op0=ALU.mult, op1=ALU.add),
                        r=[n + "rho", "bp%d" % ri, n + "init"], w=["wv%d" % ri])
                prm = t["prm"]
                P.V(lambda: nc.vector.tensor_scalar(out=tmpc[:, 0:1], in0=wv[0][:, T - 1:T], scalar1=prm[:, 13:14], scalar2=None, op0=ALU.mult), r=["wv0", t["pk"]], w=["tmpc"])
                P.V(lambda: nc.vector.tensor_scalar(out=tmpc[:, 1:2], in0=wv[0][:, T - 1:T], scalar1=prm[:, 14:15], scalar2=None, op0=ALU.mult), r=["wv0", t["pk"]], wj=["tmpc"])
                P.V(lambda: nc.vector.scalar_tensor_tensor(out=t["init"][:, 0:1], in0=wv[1][:, T - 1:T], scalar=prm[:, 15:16], in1=tmpc[:, 0:1], op0=ALU.mult, op1=ALU.add),
                    r=["wv1", "tmpc", t["pk"]], w=[n + "init"])
                P.V(lambda: nc.vector.scalar_tensor_tensor(out=t["init"][:, 1:2], in0=wv[1][:, T - 1:T], scalar=prm[:, 13:14], in1=tmpc[:, 1:2], op0=ALU.mult, op1=ALU.add),
                    r=["wv1", "tmpc", t["pk"]], wj=[n + "init"])
                P.G(lambda: nc.gpsimd.tensor_tensor(out=pp[0][:], in0=wv[0][:], in1=cosT[:, 0:T], op=ALU.mult), r=["wv0", n + "cos"], w=["pp0"])
                P.G(lambda: nc.gpsimd.tensor_tensor(out=pp[1][:], in0=wv[1][:], in1=sinT[:, 0:T], op=ALU.mult), r=["wv1", n + "sin"], w=["pp1"])
                P.G(lambda: nc.gpsimd.tensor_tensor(out=xb[gp][0][:], in0=pp[0][:], in1=pp[1][:], op=ALU.subtract), r=["pp0", "pp1"], w=["xb%d_0" % gp])
                P.G(lambda: nc.gpsimd.tensor_tensor(out=pp[2][:], in0=wv[0][:], in1=sinT[:, 0:T], op=ALU.mult), r=["wv0", n + "sin"], w=["pp2"])
                P.G(lambda: nc.gpsimd.tensor_tensor(out=pp[3][:], in0=wv[1][:], in1=cosT[:, 0:T], op=ALU.mult), r=["wv1", n + "cos"], w=["pp3"])
                P.G(lambda: nc.gpsimd.tensor_tensor(out=xb[gp][1][:], in0=pp[2][:], in1=pp[3][:], op=ALU.add), r=["pp2", "pp3"], w=["xb%d_1" % gp])
            k = 0
            for gp in range(2):
                t = tiles[(d, gp)]
                for ri in range(2):
                    P.T(lambda: nc.tensor.matmul(ps_y[0:64, 0:T], lhsT=t["CT"][:, ri, :], rhs=xb[gp][ri][:], start=(k == 0), stop=(k == 3)),
                        r=[t["n"] + "CT", "xb%d_%d" % (gp, ri)], w=["ps_y"] if k == 0 else [], wj=[] if k == 0 else ["ps_y"])
                    k += 1
            b = it % 2; it += 1
            P.V(lambda: nc.vector.tensor_copy(out=yo[b][:], in_=ps_y[0:64, 0:T]), r=["ps_y"], w=["yo%d" % b])
            P.dma(yT_d[d][:, cols], yo[b][:], r=["yo%d" % b], wj=["yT%d" % d], key="yo%d" % b)
    P.finish(); P.close()
    return nc


def mc_inputs(zc_b, prm, l, h, S):
    T = min(512, S)
    u = zc_b[:, h * 64:(h + 1) * 64]
    gs = slice(4 * h, 4 * h + 4)
    return {"uT0": np.ascontiguousarray(u.T), "uT1": np.ascontiguousarray(u[::-1].T),
            "a_re": np.ascontiguousarray(prm["s5_a_re"][l][:, gs]), "a_im": np.ascontiguousarray(prm["s5_a_im"][l][:, gs]),
            "ldt": np.ascontiguousarray(prm["s5_log_dt"][l][:, gs]), "b_re": np.ascontiguousarray(prm["s5_b_re"][l][gs]), "b_im": np.ascontiguousarray(prm["s5_b_im"][l][gs]),
            "c_re": np.ascontiguousarray(prm["s5_c_re"][l][:, gs]), "c_im": np.ascontiguousarray(prm["s5_c_im"][l][:, gs]),
            "ident": np.eye(128, dtype=np.float32), "iota": np.arange(T + 1, dtype=np.float32)[None, :]}


def run_MC(zc, prm, l):
    B, S, _ = zc.shape
    nc = build_MC(S)
    maps = [mc_inputs(zc[c // 4], prm, l, c % 4, S) for c in range(B * 4)]
    res = run_bass_kernel_spmd(nc, maps, core_ids=list(range(B * 4)))
    yf = np.zeros((B, S, 256), np.float32); yb = np.zeros((B, S, 256), np.float32)
    for c in range(B * 4):
        b, h = c // 4, c % 4
        yf[b, :, h * 64:(h + 1) * 64] = res.results[c]["yT0"].T
        yb[b, :, h * 64:(h + 1) * 64] = res.results[c]["yT1"].T[::-1]
    return yf, yb


EM05 = 0.6065306597126334


def build_MB(S):
    P = Prog(); nc = P.nc
    NT = S // 128
    rkv = [P.dram("rkv%d" % d, [S, 192]) for d in range(2)]
    h1 = [[P.dram("h%s1_%d" % (n, d), [64, S]) for d in range(2)] for n in "wa"]
    h2 = [[P.dram("h%s2_%d" % (n, d), [64, S]) for d in range(2)] for n in "wa"]
    w2 = P.dram("w2", [2, 64, 64]); a2 = P.dram("a2", [2, 64, 64]); w0 = P.dram("w0", [2, 64]); a0 = P.dram("a0", [2, 64])
    mu = P.dram("mu", [2, 2, 192]); kka = P.dram("kka", [3, 64])
    ident_d = P.dram("ident", [128, 128]); zsel_d = P.dram("zsel", [64, 32 * 128])
    y_o = P.dram("y", [2, S, 64], kind="ExternalOutput"); bonus_o = P.dram("bonus", [2, S, 64], kind="ExternalOutput")
    pkd = [P.dram("pkd%d" % d, [S, 256], BF16, kind="Internal") for d in range(2)]
    dkd = [P.dram("dkd%d" % d, [S, 64], F32, kind="Internal") for d in range(2)]
    vTs = P.dram("vTs", [128, S], F32, kind="Internal")
    ident = load_ident32(P, nc, ident_d)
    z32 = P.sb("z32", [64, 32 * 128]); zb = P.sb("zb", [64, 32 * 128], BF16)
    P.dma(z32[:], zsel_d[:, :], w=["z32"]); P.V(lambda: nc.vector.tensor_copy(out=zb[:], in_=z32[:]), r=["z32"], w=["zb"])
    def bc(name, src, n):
        t = P.sb(name, [128, n]); P.dma(t[:], src.partition_broadcast(128), w=[name]); return t
    kk_bc = bc("kk_bc", kka[0:1, :], 64); ka_bc = bc("ka_bc", kka[1:2, :], 64); rk_bc = bc("rk_bc", kka[2:3, :], 64)
    ps_t = P.ps("ps_t", [128, 512]); ps_l = P.ps("ps_l", [128, 512])
    thT = P.sb("thT", [64, S], BF16); haT = P.sb("haT", [64, S], BF16)
    CW = min(2048, S)
    ha = P.sb("ha_", [64, CW]); hb = P.sb("hb_", [64, CW])
    vstage = P.sb("vstage", [128, 128]); P.V(lambda: nc.vector.memset(vstage[:], 0.0), w=["vstage"])
    zrow = P.sb("zrow", [1, 64], BF16); P.V(lambda: nc.vector.memset(zrow[:], 0.0), w=["zrow"])
    for d in range(2):
        for wi, dst in enumerate((thT, haT)):
            dk_ = "thT" if wi == 0 else "haT"
            for ci, c0 in enumerate(range(0, S, CW)):
                P.dma(ha[:], h1[wi][d][:, c0:c0 + CW], w=["ha"])
                if c0 == 0:
                    P.V(lambda: nc.vector.memset(hb[:, 0:1], 0.0), w=["hb"])
                    P.dma(hb[:, 1:CW], h2[wi][d][:, 0:CW - 1], wj=["hb"])
                else:
                    P.dma(hb[:], h2[wi][d][:, c0 - 1:c0 + CW - 1], w=["hb"])
                P.V(lambda: nc.vector.tensor_tensor(out=ha[:], in0=ha[:], in1=hb[:], op=ALU.add), r=["ha", "hb"], w=["ha"])
                first = (ci == 0)
                if wi == 0:
                    P.A(lambda: nc.scalar.activation(out=dst[:, c0:c0 + CW], in_=ha[:], func=AF.Tanh), r=["ha"], w=[dk_] if first else [], wj=[] if first else [dk_])
                else:
                    P.V(lambda: nc.vector.tensor_copy(out=dst[:, c0:c0 + CW], in_=ha[:]), r=["ha"], w=[dk_] if first else [], wj=[] if first else [dk_])
        w2s = P.sb("w2s%d" % d, [64, 2, 64]); w2b = P.sb("w2b%d" % d, [64, 2, 64], BF16)
        P.dma(w2s[:, 0, :], w2[d], w=["w2s%d" % d]); P.dma(w2s[:, 1, :], a2[d], wj=["w2s%d" % d])
        P.V(lambda: nc.vector.tensor_copy(out=w2b[:], in_=w2s[:]), r=["w2s%d" % d], w=["w2b%d" % d])
        w0_bc = bc("w0_bc%d" % d, w0[d:d + 1, :], 64); a0_bc = bc("a0_bc%d" % d, a0[d:d + 1, :], 64)
        mu0_bc = bc("mu0_bc%d" % d, mu[d, 0:1, :], 192); mu1_bc = bc("mu1_bc%d" % d, mu[d, 1:2, :], 192)
        cur = [P.sb("cur%d_%d" % (d, i), [128, 192]) for i in range(2)]
        prv = [P.sb("prv%d_%d" % (d, i), [128, 192]) for i in range(2)]
        nxt = [P.sb("nxt%d_%d" % (d, i), [128, 192]) for i in range(2)]
        d0 = P.sb("d0_%d" % d, [128, 192]); d1 = P.sb("d1_%d" % d, [128, 192]); mix = P.sb("mix%d" % d, [128, 192])
        dec = [P.sb("dec%d_%d" % (d, i), [128, 64]) for i in range(2)]
        pack = [P.sb("pack%d_%d" % (d, i), [128, 256], BF16) for i in range(2)]
        bon = [P.sb("bon%d_%d" % (d, i), [128, 64]) for i in range(2)]
        vto = [P.sb("vto%d_%d" % (d, i), [128, 128]) for i in range(2)]
        icl = P.sb("icl%d" % d, [128, 64]); kkt = P.sb("kkt%d" % d, [128, 64]); kap = P.sb("kap%d" % d, [128, 64]); kd = P.sb("kd%d" % d, [128, 64])
        t1 = P.sb("t1_%d" % d, [128, 64]); sq = P.sb("sq%d" % d, [128, 64]); ss = P.sb("ss%d" % d, [128, 1]); sb_ = P.sb("sb%d" % d, [128, 1])
        for t in range(NT):
            b = t % 2; r0 = t * 128
            ck, pk_, nk = "cur%d" % b, "prv%d" % b, "nxt%d" % b
            P.dma(cur[b][:], rkv[d][r0:r0 + 128, :], w=[ck])
            if t == 0:
                P.V(lambda: nc.vector.memset(prv[b][:], 0.0), w=[pk_])
                P.dma(prv[b][1:128, :], rkv[d][0:127, :], wj=[pk_])
            else:
                P.dma(prv[b][:], rkv[d][r0 - 1:r0 + 127, :], w=[pk_])
            if t == NT - 1:
                P.V(lambda: nc.vector.memset(nxt[b][:], 0.0), w=[nk])
                P.dma(nxt[b][0:127, :], rkv[d][r0 + 1:r0 + 128, :], wj=[nk])
            else:
                P.dma(nxt[b][:], rkv[d][r0 + 1:r0 + 129, :], w=[nk])
            P.T(lambda: nc.tensor.matmul(ps_l[:, 0:64], lhsT=thT[:, r0:r0 + 128], rhs=w2b[:, 0, :], start=True, stop=True), r=["thT", "w2b%d" % d], w=["ps_l"])
            P.T(lambda: nc.tensor.matmul(ps_l[:, 64:128], lhsT=haT[:, r0:r0 + 128], rhs=w2b[:, 1, :], start=True, stop=True), r=["haT", "w2b%d" % d], wj=["ps_l"])
            P.V(lambda: nc.vector.tensor_tensor(out=d0[:], in0=prv[b][:], in1=cur[b][:], op=ALU.subtract), r=[pk_, ck], w=["d0"])
            P.V(lambda: nc.vector.tensor_tensor(out=d0[:], in0=d0[:], in1=mu0_bc[:], op=ALU.mult), r=["d0", "mu0_bc%d" % d], w=["d0"])
            P.V(lambda: nc.vector.tensor_tensor(out=d1[:], in0=nxt[b][:], in1=cur[b][:], op=ALU.subtract), r=[nk, ck], w=["d1"])
            P.V(lambda: nc.vector.tensor_tensor(out=d1[:], in0=d1[:], in1=mu1_bc[:], op=ALU.mult), r=["d1", "mu1_bc%d" % d], w=["d1"])
            P.V(lambda: nc.vector.tensor_tensor(out=d0[:], in0=d0[:], in1=d1[:], op=ALU.add), r=["d0", "d1"], w=["d0"])
            P.V(lambda: nc.vector.tensor_tensor(out=mix[:], in0=cur[b][:], in1=d0[:], op=ALU.add), r=[ck, "d0"], w=["mix"])
            rr, kp, vp = mix[:, 0:64], mix[:, 64:128], mix[:, 128:192]
            P.V(lambda: nc.vector.tensor_tensor(out=t1[:], in0=ps_l[:, 0:64], in1=w0_bc[:], op=ALU.add), r=["ps_l", "w0_bc%d" % d], w=["t1"])
            P.A(lambda: nc.scalar.activation(out=t1[:], in_=t1[:], func=AF.Sigmoid), r=["t1"], w=["t1"])
            P.A(lambda: nc.scalar.activation(out=dec[b][:], in_=t1[:], func=AF.Exp, scale=-EM05), r=["t1"], w=["dec%d" % b])
            P.V(lambda: nc.vector.tensor_tensor(out=icl[:], in0=ps_l[:, 64:128], in1=a0_bc[:], op=ALU.add), r=["ps_l", "a0_bc%d" % d], w=["icl"])
            P.A(lambda: nc.scalar.activation(out=icl[:], in_=icl[:], func=AF.Sigmoid), r=["icl"], w=["icl"])
            P.V(lambda: nc.vector.tensor_tensor(out=kkt[:], in0=kp, in1=kk_bc[:], op=ALU.mult), r=["mix", "kk_bc"], w=["kkt"])
            P.A(lambda: nc.scalar.activation(out=sq[:], in_=kkt[:], func=AF.Square, accum_out=ss[:]), r=["kkt"], w=["sq", "ss"])
            P.V(lambda: nc.vector.tensor_scalar(out=ss[:], in0=ss[:], scalar1=1e-12, scalar2=None, op0=ALU.add), r=["ss"], w=["ss"])
            P.A(lambda: nc.scalar.sqrt(out=ss[:], in_=ss[:]), r=["ss"], w=["ss"])
            P.V(lambda: nc.vector.reciprocal(out=ss[:], in_=ss[:]), r=["ss"], w=["ss"])
            P.V(lambda: nc.vector.tensor_scalar(out=kap[:], in0=kkt[:], scalar1=ss[:], scalar2=None, op0=ALU.mult), r=["kkt", "ss"], w=["kap"])
            P.V(lambda: nc.vector.tensor_tensor(out=t1[:], in0=icl[:], in1=ka_bc[:], op=ALU.mult), r=["icl", "ka_bc"], w=["t1"])
            P.V(lambda: nc.vector.scalar_tensor_tensor(out=t1[:], in0=t1[:], scalar=1.0, in1=ka_bc[:], op0=ALU.add, op1=ALU.subtract), r=["t1", "ka_bc"], w=["t1"])
            P.V(lambda: nc.vector.tensor_tensor(out=kd[:], in0=kp, in1=t1[:], op=ALU.mult), r=["mix", "t1"], w=["kd"])
            pkk = "pack%d" % b
            P.V(lambda: nc.vector.tensor_copy(out=pack[b][:, 0:64], in_=rr), r=["mix"], w=[pkk])
            P.V(lambda: nc.vector.tensor_copy(out=pack[b][:, 64:128], in_=kap[:]), r=["kap"], wj=[pkk])
            P.V(lambda: nc.vector.scalar_tensor_tensor(out=pack[b][:, 128:192], in0=icl[:], scalar=-1.0, in1=kap[:], op0=ALU.mult, op1=ALU.mult), r=["icl", "kap"], wj=[pkk])
            P.V(lambda: nc.vector.tensor_copy(out=pack[b][:, 192:256], in_=kd[:]), r=["kd"], wj=[pkk])
            P.dma(pkd[d][r0:r0 + 128, 0:64], pack[b][:, 0:64], r=[pkk], wj=["scr"], key=pkk)
            P.dma(pkd[d][r0:r0 + 128, 128:256], pack[b][:, 128:256], r=[pkk], wj=["scr"], key=pkk)
            if t == 0:
                P.dma(pkd[d][0:127, 64:128], pack[b][1:128, 64:128], r=[pkk], wj=["scr"], key=pkk)
            else:
                P.dma(pkd[d][r0 - 1:r0 + 127, 64:128], pack[b][:, 64:128], r=[pkk], wj=["scr"], key=pkk)
            if t == NT - 1:
                P.dma(pkd[d][S - 1:S, 64:128], zrow[0:1, :], r=["zrow"], wj=["scr"], key="zrow")
            P.dma(dkd[d][r0:r0 + 128, :], dec[b][:], r=["dec%d" % b], wj=["scr"], key="dec%d" % b)
            P.V(lambda: nc.vector.tensor_tensor(out=t1[:], in0=rr, in1=kd[:], op=ALU.mult), r=["mix", "kd"], w=["t1"])
            P.V(lambda: nc.vector.scalar_tensor_tensor(out=sq[:], in0=t1[:], scalar=1.0, in1=rk_bc[:], op0=ALU.mult, op1=ALU.mult, accum_out=sb_[:]),
                r=["t1", "rk_bc"], w=["sq", "sb_"])
            P.V(lambda: nc.vector.tensor_scalar(out=bon[b][:], in0=vp, scalar1=sb_[:], scalar2=None, op0=ALU.mult), r=["mix", "sb_"], w=["bon%d" % b])
            P.dma(bonus_o[d, r0:r0 + 128, :], bon[b][:], r=["bon%d" % b], wj=["bonus"], key="bon%d" % b)
            P.V(lambda: nc.vector.tensor_copy(out=vstage[:, 64 * d:64 * d + 64], in_=vp), r=["mix"], w=["vstage"])
            P.T(lambda: nc.tensor.transpose(ps_t[:, 0:128], vstage[:], ident[:]), r=["vstage", "ident32"], w=["ps_t"])
            P.V(lambda: nc.vector.tensor_copy(out=vto[b][64 * d:64 * d + 64, :], in_=ps_t[64 * d:64 * d + 64, 0:128]), r=["ps_t"], w=["vto%d" % b])
            P.dma(vTs[64 * d:64 * d + 64, r0:r0 + 128], vto[b][64 * d:64 * d + 64, :], r=["vto%d" % b], wj=["scr"], key="vto%d" % b)
    SEG = min(1024, S); NB = 4
    St = P.sb("St", [128, 64]); P.V(lambda: nc.vector.memset(St[:], 0.0), w=["S"])
    prod = P.sb("prod", [128, 2, 64])
    zcol = P.sb("zcol", [128, 1]); P.V(lambda: nc.vector.memset(zcol[:], 0.0), w=["zcol"])
    bcp = [P.ps("bcp%d" % i, [128, 512]) for i in range(NB)]
    pkt = [P.sb("pkt%d" % i, [64, 4, 256], BF16) for i in range(2)]
    dkt = [P.sb("dkt%d" % i, [64, 4, 64]) for i in range(2)]
    vseg = [P.sb("vseg%d" % i, [128, SEG]) for i in range(2)]
    yseg = [P.sb("yseg%d" % i, [128, SEG, 2]) for i in range(2)]
    yo = [P.sb("yo%d" % i, [128, 128]) for i in range(2)]
    St_b = St[:].unsqueeze(1).to_broadcast([128, 2, 64])
    step = 0; oi = 0
    sk_ap, sk_tok = zcol[:], "zcol"
    for sg in range(S // SEG):
        sb2 = sg % 2; vk = "vseg%d" % sb2; yk = "yseg%d" % sb2
        P.dma(vseg[sb2][:], vTs[:, sg * SEG:(sg + 1) * SEG], r=["scr"], w=[vk])
        for blk in range(SEG // 128):
            s0 = sg * SEG + blk * 128; bb = (s0 // 128) % 2
            pk_, dk_ = "pkt%d" % bb, "dkt%d" % bb
            for d in range(2):
                P.dma(pkt[bb][32 * d:32 * d + 32, :, :], pkd[d][s0:s0 + 128, :].rearrange("(g q) c -> q g c", q=32), r=["scr"], w=[pk_] if d == 0 else [], wj=[] if d == 0 else [pk_])
                P.dma(dkt[bb][32 * d:32 * d + 32, :, :], dkd[d][s0:s0 + 128, :].rearrange("(g q) c -> q g c", q=32), r=["scr"], w=[dk_] if d == 0 else [], wj=[] if d == 0 else [dk_])
            for g in range(4):
                for j in range(32):
                    sl = step % NB; step += 1; bk = "bcp%d" % sl
                    col = blk * 128 + g * 32 + j
                    P.T(lambda: nc.tensor.matmul(bcp[sl][:, 0:256], lhsT=zb[:, j * 128:(j + 1) * 128], rhs=pkt[bb][:, g, :], start=True, stop=True), r=["zb", pk_], w=[bk])
                    P.T(lambda: nc.tensor.matmul(bcp[sl][:, 256:320], lhsT=z32[:, j * 128:(j + 1) * 128], rhs=dkt[bb][:, g, :], start=True, stop=True), r=["z32", dk_], wj=[bk])
                    P.ses = bool(os.environ.get("FORCE_SES"))
                    nbb, kdb, wb = bcp[sl][:, 128:192], bcp[sl][:, 192:256], bcp[sl][:, 256:320]
                    rk2 = bcp[sl][:, 0:128].rearrange("p (a n) -> p a n", a=2)
                    P.V(lambda: nc.vector.tensor_tensor(out=St[:], in0=St[:], in1=wb, op=ALU.mult), r=["S", bk], w=["S"])
                    P.V(lambda: nc.vector.scalar_tensor_tensor(out=St[:], in0=nbb, scalar=sk_ap, in1=St[:], op0=ALU.mult, op1=ALU.add), r=["S", bk, sk_tok], w=["S"])
                    P.V(lambda: nc.vector.scalar_tensor_tensor(out=St[:], in0=kdb, scalar=vseg[sb2][:, col:col + 1], in1=St[:], op0=ALU.mult, op1=ALU.add), r=["S", bk, vk], w=["S"])
                    P.V(lambda: nc.vector.tensor_tensor(out=prod[:], in0=St_b, in1=rk2, op=ALU.mult), r=["S", bk], w=["prod"])
                    P.V(lambda: nc.vector.tensor_reduce(out=yseg[sb2][:, col, :], in_=prod[:], axis=AX.X, op=ALU.add), r=["prod"], wj=[yk])
                    P.ses = True
                    sk_ap, sk_tok = yseg[sb2][:, col, 1:2], yk
        for blk in range(SEG // 128):
            ob = oi % 2; oi += 1; r0 = sg * SEG + blk * 128
            P.T(lambda: nc.tensor.transpose(ps_t[:, 0:128], yseg[sb2][:, blk * 128:(blk + 1) * 128, 0], ident[:]), r=[yk, "ident32"], w=["ps_t"])
            P.V(lambda: nc.vector.tensor_copy(out=yo[ob][:], in_=ps_t[:, 0:128]), r=["ps_t"], w=["yo%d" % ob])
            for d in range(2):
                P.dma(y_o[d, r0:r0 + 128, :], yo[ob][:, 64 * d:64 * d + 64], r=["yo%d" % ob], wj=["y"], key="yo%d" % ob)
        P.V(lambda: nc.vector.memset(zcol[:], 0.0), r=[yk], w=["zcol2"])
    P.finish(); P.close()
    return nc


def zsel_const():
    z = np.zeros((64, 32, 128), np.float32)
    for j in range(32):
        z[j, j, 0:64] = 1.0
        z[32 + j, j, 64:128] = 1.0
    return z.reshape(64, 32 * 128)


def mb_inputs(zb_b, zl_b, prm, l, h):
    hs = slice(h * 64, (h + 1) * 64)
    rkvh = np.concatenate([zb_b[:, h * 64:(h + 1) * 64], zb_b[:, 256 + h * 64:256 + (h + 1) * 64], zb_b[:, 512 + h * 64:512 + (h + 1) * 64]], axis=1)
    m = {"ident": np.eye(128, dtype=np.float32), "zsel": zsel_const(),
         "w2": np.ascontiguousarray(prm["rwkv_w2"][l][:, :, hs]), "a2": np.ascontiguousarray(prm["rwkv_a2"][l][:, :, hs]),
         "w0": np.ascontiguousarray(prm["rwkv_w0"][l][:, hs]), "a0": np.ascontiguousarray(prm["rwkv_a0"][l][:, hs]),
         "kka": np.stack([prm["rwkv_k_k"][l][hs], prm["rwkv_k_a"][l][hs], prm["rwkv_r_k"][l][hs]])}
    mur = prm["rwkv_mu_rkv"][l]
    muh = np.stack([np.concatenate([mur[i, j, hs] for j in range(3)]) for i in range(2)])
    m["mu"] = np.stack([muh, muh[::-1]])
    for d in range(2):
        o = lambda a: np.ascontiguousarray(a if d == 0 else a[::-1])
        m["rkv%d" % d] = o(rkvh)
        base = d * 256
        for wi, nm in enumerate("wa"):
            m["h%s1_%d" % (nm, d)] = np.ascontiguousarray(o(zl_b[:, base + wi * 128:base + wi * 128 + 64]).T)
            m["h%s2_%d" % (nm, d)] = np.ascontiguousarray(o(zl_b[:, base + wi * 128 + 64:base + wi * 128 + 128]).T)
    return m


def run_MB(zb, zl, prm, l):
    B, S, _ = zb.shape
    nc = build_MB(S)
    maps = [mb_inputs(zb[c // 4], zl[c // 4], prm, l, c % 4) for c in range(B * 4)]
    res = run_bass_kernel_spmd(nc, maps, core_ids=list(range(B * 4)))
    outs = [np.zeros((B, S, 256), np.float32) for _ in range(4)]
    for c in range(B * 4):
        b, h = c // 4, c % 4
        r = res.results[c]
        outs[0][b, :, h * 64:(h + 1) * 64] = r["y"][0]
        outs[1][b, :, h * 64:(h + 1) * 64] = r["y"][1][::-1]
        outs[2][b, :, h * 64:(h + 1) * 64] = r["bonus"][0]
        outs[3][b, :, h * 64:(h + 1) * 64] = r["bonus"][1][::-1]
    return outs


def gen_MD(P0, S):
    P = NS(P0, "d_"); nc = P.nc
    q = P.dram("q", [S, 64]); k = P.dram("k", [S, 64]); v = P.dram("v", [S, 64])
    qg = P.dram("qg", [1, 64]); kg = P.dram("kg", [1, 64]); cos_d = P.dram("cos", [S, 32]); sin_d = P.dram("sin", [S, 32])
    ident_d = P.dram("ident", [128, 128])
    yT = P.dram("yT", [64, S], kind="ExternalOutput")
    NT = S // 128
    ident = load_ident32(P, nc, ident_d)
    ones = P.sb("ones", [128, 64]); P.V(lambda: nc.vector.memset(ones[:], 1.0), w=["ones"])
    qT = P.sb("qT", [64, S], BF16); kT = P.sb("kT", [64, S], BF16)
    ps_t = P.ps("ps_t", [128, 512])
    prep_qk(P, nc, q, qT, 0, S, 32, cos_d, sin_d, ident, 0.125, qg, "q", ps_t)
    prep_qk(P, nc, k, kT, 0, S, 32, cos_d, sin_d, ident, 1.0, kg, "k", ps_t)
    Vx = P.sb("Vx", [128, NT, 65], BF16)
    P.V(lambda: nc.vector.memset(Vx[:, :, 64:65], 1.0), w=["Vx"])
    VC = min(16, NT)
    vst = [P.sb("vst%d" % i, [128, VC, 64]) for i in range(2)]
    for ci, m0 in enumerate(range(0, NT, VC)):
        vb = ci % 2
        P.dma(vst[vb][:], v[m0 * 128:(m0 + VC) * 128, :].rearrange("(m p) c -> p m c", p=128), w=["vst%d" % vb])
        P.V(lambda: nc.vector.tensor_copy(out=Vx[:, m0:m0 + VC, 0:64], in_=vst[vb][:]), r=["vst%d" % vb], wj=["Vx"])
    acc = [P.sb("acc%d" % i, [65, 512]) for i in range(2)]
    rl = P.sb("rl", [65, 512]); ob = [P.sb("ob%d" % i, [64, 512]) for i in range(2)]
    NB = 2
    ps_s = [P.ps("ps_s%d" % i, [128, 512]) for i in range(NB)]
    pT = [P.sb("pT%d" % i, [128, 512], BF16) for i in range(NB)]
    po = [P.ps("po0", [128, 512])]
    ps_bc = ps_t
    W = min(512, S)
    its = [(qb, kt) for qb in range(S // W) for kt in range(NT)]
    LA = 1

    def emit_S(i):
        qb, kt = its[i]; b = i % NB
        P.T(lambda: nc.tensor.matmul(ps_s[b][:, 0:W], lhsT=kT[:, kt * 128:(kt + 1) * 128], rhs=qT[:, qb * W:(qb + 1) * W], start=True, stop=True),
            r=["qT", "kT"], w=["ps_s%d" % b])

    for i in range(min(LA, len(its))):
        emit_S(i)
    for i, (qb, kt) in enumerate(its):
        b = i % NB; pb = qb % 2; pq = 0
        if i + LA < len(its):
            emit_S(i + LA)
        P.A(lambda: nc.scalar.activation(out=pT[b][:, 0:W], in_=ps_s[b][:, 0:W], func=AF.Exp), r=["ps_s%d" % b], w=["pT%d" % b])
        P.T(lambda: nc.tensor.matmul(po[pq][0:65, 0:W], lhsT=Vx[:, kt, :], rhs=pT[b][:, 0:W], start=(kt == 0), stop=(kt == NT - 1)),
            r=["Vx", "pT%d" % b], w=["po%d" % pq] if kt == 0 else [], wj=[] if kt == 0 else ["po%d" % pq])
        if kt == NT - 1:
            P.V(lambda: nc.vector.tensor_copy(out=acc[pb][:, 0:W], in_=po[pq][0:65, 0:W]), r=["po%d" % pq], w=["acc%d" % pb])
            normalize_blk(P, nc, acc[pb], "acc%d" % pb, W, yT, qb * W, ones, ps_bc, rl, ob[pb], "ob%d" % pb)
        yield 1
    P.close_ns()
    yield 'done'


def gen_MA(P0, S):
    P = NS(P0, "a_"); nc = P.nc
    PADC = 1024
    DIL = (1, 4, 16)
    q = P.dram("q", [S, 64]); k = P.dram("k", [S, 64])
    vx = [P.dram("vx%d" % d, [d * (S // d + 128), 65]) for d in DIL]
    cos_d = P.dram("cos", [S, 8]); sin_d = P.dram("sin", [S, 8])
    ident_d = P.dram("ident", [128, 128]); mask_d = P.dram("mask", [128, 256])
    yT = P.dram("yT", [64, S], kind="ExternalOutput")
    ident = load_ident32(P, nc, ident_d)
    ones = P.sb("ones", [128, 64]); P.V(lambda: nc.vector.memset(ones[:], 1.0), w=["ones"])
    m32 = P.sb("m32", [128, 256]); mask = P.sb("maskb", [128, 256], BF16)
    P.dma(m32[:], mask_d[:, :], w=["m32"]); P.V(lambda: nc.vector.tensor_copy(out=mask[:], in_=m32[:]), r=["m32"], w=["mask"])
    qT = P.sb("qT", [64, S + 2 * PADC], BF16); kT = P.sb("kT", [64, S + 2 * PADC], BF16)
    P.V(lambda: nc.vector.memset(kT[:, 0:PADC], 0.0), w=["kT"]); P.V(lambda: nc.vector.memset(kT[:, PADC + S:], 0.0), wj=["kT"])
    P.V(lambda: nc.vector.memset(qT[:, 0:PADC], 0.0), w=["qT"]); P.V(lambda: nc.vector.memset(qT[:, PADC + S:], 0.0), wj=["qT"])
    ps_t = P.ps("ps_t", [128, 512])
    prep_qk(P, nc, q, qT, PADC, S, 8, cos_d, sin_d, ident, 0.125, None, "q", ps_t)
    prep_qk(P, nc, k, kT, PADC, S, 8, cos_d, sin_d, ident, 1.0, None, "k", ps_t)
    SB = 2048
    accT = P.sb("accT", [65, SB])
    NB = 2
    ps_s = [P.ps("ps_s%d" % i, [128, 512]) for i in range(NB)]
    pT = [P.sb("pT%d" % i, [128, 256], BF16) for i in range(NB)]
    pTm = [P.sb("pTm%d" % i, [128, 256], BF16) for i in range(NB)]
    po = [P.ps("po%d" % i, [128, 512]) for i in range(NB)]
    ps_bc = ps_t
    rl = P.sb("rl", [65, 512]); ob = [P.sb("ob%d" % i, [64, 512]) for i in range(2)]
    Vxs = {}
    vst = [P.sb("vst%d" % i, [128, 16, 65]) for i in range(2)]
    ci = 0
    for di, d in enumerate(DIL):
        L = S // d; NKT = L // 128 + 1; NTL = d * NKT
        Vx = P.sb("Vx%d" % d, [128, NTL, 65], BF16)
        for m0 in range(0, NTL, 16):
            m1 = min(NTL, m0 + 16); vb = ci % 2; ci += 1
            P.dma(vst[vb][:, 0:m1 - m0, :], vx[di][m0 * 128:m1 * 128, :].rearrange("(m p) c -> p m c", p=128), w=["vst%d" % vb])
            P.V(lambda: nc.vector.tensor_copy(out=Vx[:, m0:m1, :], in_=vst[vb][:, 0:m1 - m0, :]), r=["vst%d" % vb], wj=["Vx%d" % d])
        Vxs[d] = (Vx, NKT)
    oi = 0
    its = []
    for sbi in range(S // SB):
        for di, d in enumerate(DIL):
            for r in range(d):
                for bl in range(SB // (128 * d)):
                    its.append((sbi, di, d, r, bl))
    LA = 1

    def emit_S(i):
        sbi, di, d, r, bl = its[i]; b = i % NB
        i0 = (sbi * SB // d) + bl * 128
        qs = PADC + r + d * i0
        rhs = qT[:, qs:qs + 127 * d + 1:d]
        for ab in range(2):
            ks = PADC + r + d * (i0 - 64 + 128 * ab)
            P.T(lambda: nc.tensor.matmul(ps_s[b][:, ab * 128:(ab + 1) * 128], lhsT=kT[:, ks:ks + 127 * d + 1:d], rhs=rhs, start=True, stop=True),
                r=["qT", "kT"], w=["ps_s%d" % b] if ab == 0 else [], wj=[] if ab == 0 else ["ps_s%d" % b])

    for i in range(min(LA, len(its))):
        emit_S(i)
    for i, (sbi, di, d, r, bl) in enumerate(its):
        b = i % NB
        Vx, NKT = Vxs[d]
        i0 = (sbi * SB // d) + bl * 128; blk = i0 // 128
        if i + LA < len(its):
            emit_S(i + LA)
        P.A(lambda: nc.scalar.activation(out=pT[b][:], in_=ps_s[b][:, 0:256], func=AF.Exp), r=["ps_s%d" % b], w=["pT%d" % b])
        P.G(lambda: nc.gpsimd.tensor_tensor(out=pTm[b][:], in0=pT[b][:], in1=mask[:], op=ALU.mult), r=["pT%d" % b, "mask"], w=["pTm%d" % b])
        for ab in range(2):
            P.T(lambda: nc.tensor.matmul(po[b][0:65, 0:128], lhsT=Vx[:, r * NKT + blk + ab, :], rhs=pTm[b][:, ab * 128:(ab + 1) * 128], start=(ab == 0), stop=(ab == 1)),
                r=["Vx%d" % d, "pTm%d" % b], w=["po%d" % b] if ab == 0 else [], wj=[] if ab == 0 else ["po%d" % b])
        t0 = r + d * bl * 128
        dst = accT[:, t0:t0 + 127 * d + 1:d]
        if di == 0:
            P.V(lambda: nc.vector.tensor_copy(out=dst, in_=po[b][0:65, 0:128]), r=["po%d" % b], w=["accT"] if (bl == 0) else [], wj=[] if (bl == 0) else ["accT"])
        else:
            P.V(lambda: nc.vector.tensor_tensor(out=dst, in0=dst, in1=po[b][0:65, 0:128], op=ALU.add), r=["po%d" % b, "accT"], w=["accT"])
        last = (i + 1 == len(its)) or (its[i + 1][0] != sbi)
        if last:
            for c0 in range(0, SB, 512):
                o = oi % 2; oi += 1
                normalize_blk(P, nc, accT[:, c0:c0 + 512], "accT", 512, yT, sbi * SB + c0, ones, ps_bc, rl, ob[o], "ob%d" % o)
        yield 1
    P.close_ns()
    yield 'done'


def gen_MC(P0, S):
    P = NS(P0, "c_"); nc = P.nc
    T = min(512, S); NCH = S // T
    uT_d = [P.dram("uT%d" % d, [64, S]) for d in range(2)]
    a_re = P.dram("a_re", [2, 4, 64]); a_im = P.dram("a_im", [2, 4, 64]); ldt = P.dram("ldt", [2, 4])
    b_re = P.dram("b_re", [4, 64, 16]); b_im = P.dram("b_im", [4, 64, 16])
    c_re = P.dram("c_re", [2, 4, 16, 64]); c_im = P.dram("c_im", [2, 4, 16, 64])
    ident_d = P.dram("ident", [128, 128]); iota_d = P.dram("iota", [1, T + 1])
    yT_d = [P.dram("yT%d" % d, [64, S], kind="ExternalOutput") for d in range(2)]
    ident = load_ident32(P, nc, ident_d)
    iota = P.sb("iota", [128, T + 1]); P.dma(iota[:], iota_d[0:1, :].partition_broadcast(128), w=["iota"])
    uT = []
    UC = min(2048, S)
    ust = [P.sb("ust%d" % i, [64, UC]) for i in range(2)]
    ui = 0
    for d in range(2):
        u = P.sb("uTb%d" % d, [64, S], BF16)
        for c0 in range(0, S, UC):
            ub = ui % 2; ui += 1
            P.dma(ust[ub][:], uT_d[d][:, c0:c0 + UC], w=["ust%d" % ub])
            P.V(lambda: nc.vector.tensor_copy(out=u[:, c0:c0 + UC], in_=ust[ub][:]), r=["ust%d" % ub], wj=["uT%d" % d])
        uT.append(u)
    ps_t = P.ps("ps_t", [128, 512])
    tiles = {}
    for d in range(2):
        for gp in range(2):
            n = "t%d%d" % (d, gp)
            prm = P.sb(n + "prm", [128, 16])
            pk = n + "prm"
            P.dma(prm[:, 0:1], a_re[d, 2 * gp:2 * gp + 2, :].rearrange("g (p o) -> (g p) o", o=1), w=[pk])
            P.dma(prm[:, 1:2], a_im[d, 2 * gp:2 * gp + 2, :].rearrange("g (p o) -> (g p) o", o=1), wj=[pk])
            for g in range(2):
                P.dma(prm[g * 64:(g + 1) * 64, 2:3], ldt[d:d + 1, 2 * gp + g:2 * gp + g + 1].partition_broadcast(64), wj=[pk])
            c = lambda i: prm[:, i:i + 1]
            P.A(lambda: nc.scalar.activation(out=c(2), in_=c(2), func=AF.Exp), r=[pk], w=[pk])
            P.V(lambda: nc.vector.tensor_tensor(out=c(4), in0=c(1), in1=c(2), op=ALU.mult), r=[pk], w=[pk])
            P.V(lambda: nc.vector.tensor_tensor(out=c(3), in0=c(0), in1=c(2), op=ALU.mult), r=[pk], w=[pk])
            P.A(lambda: nc.scalar.activation(out=c(3), in_=c(3), func=AF.Exp), r=[pk], w=[pk])
            cosT = P.sb(n + "cos", [128, T + 1]); sinT = P.sb(n + "sin", [128, T + 1]); ki = P.sb(n + "ki", [128, T + 1], I32)
            P.V(lambda: nc.vector.tensor_scalar(out=sinT[:], in0=iota[:], scalar1=c(4), scalar2=None, op0=ALU.mult), r=["iota", pk], w=[n + "sin"])
            P.V(lambda: nc.vector.tensor_scalar(out=cosT[:], in0=sinT[:], scalar1=PI / 2, scalar2=None, op0=ALU.add), r=[n + "sin"], w=[n + "cos"])
            range_reduce(P, nc, sinT, ki, n + "sin", [128, T + 1])
            range_reduce(P, nc, cosT, ki, n + "cos", [128, T + 1])
            P.A(lambda: nc.scalar.activation(out=sinT[:], in_=sinT[:], func=AF.Sin), r=[n + "sin"], w=[n + "sin"])
            P.A(lambda: nc.scalar.activation(out=cosT[:], in_=cosT[:], func=AF.Sin), r=[n + "cos"], w=[n + "cos"])
            P.V(lambda: nc.vector.tensor_copy(out=c(5), in_=cosT[:, 1:2]), r=[n + "cos", pk], w=[pk])
            P.V(lambda: nc.vector.tensor_copy(out=c(6), in_=sinT[:, 1:2]), r=[n + "sin", pk], w=[pk])
            P.V(lambda: nc.vector.tensor_copy(out=c(13), in_=cosT[:, T:T + 1]), r=[n + "cos", pk], w=[pk])
            P.V(lambda: nc.vector.tensor_copy(out=c(14), in_=sinT[:, T:T + 1]), r=[n + "sin", pk], w=[pk])
            P.V(lambda: nc.vector.tensor_scalar(out=c(15), in0=c(14), scalar1=-1.0, scalar2=None, op0=ALU.mult), r=[pk], w=[pk])
            P.V(lambda: nc.vector.tensor_tensor(out=c(5), in0=c(5), in1=c(3), op=ALU.mult), r=[pk], w=[pk])
            P.V(lambda: nc.vector.tensor_tensor(out=c(6), in0=c(6), in1=c(3), op=ALU.mult), r=[pk], w=[pk])
            P.V(lambda: nc.vector.tensor_scalar(out=c(7), in0=c(5), scalar1=-1.0, scalar2=None, op0=ALU.add), r=[pk], w=[pk])
            P.V(lambda: nc.vector.tensor_tensor(out=c(8), in0=c(0), in1=c(0), op=ALU.mult), r=[pk], w=[pk])
            P.V(lambda: nc.vector.tensor_tensor(out=c(11), in0=c(1), in1=c(1), op=ALU.mult), r=[pk], w=[pk])
            P.V(lambda: nc.vector.tensor_tensor(out=c(8), in0=c(8), in1=c(11), op=ALU.add), r=[pk], w=[pk])
            P.V(lambda: nc.vector.reciprocal(out=c(8), in_=c(8)), r=[pk], w=[pk])
            P.V(lambda: nc.vector.tensor_tensor(out=c(9), in0=c(7), in1=c(0), op=ALU.mult), r=[pk], w=[pk])
            P.V(lambda: nc.vector.tensor_tensor(out=c(11), in0=c(6), in1=c(1), op=ALU.mult), r=[pk], w=[pk])
            P.V(lambda: nc.vector.tensor_tensor(out=c(9), in0=c(9), in1=c(11), op=ALU.add), r=[pk], w=[pk])
            P.V(lambda: nc.vector.tensor_tensor(out=c(9), in0=c(9), in1=c(8), op=ALU.mult), r=[pk], w=[pk])
            P.V(lambda: nc.vector.tensor_tensor(out=c(10), in0=c(6), in1=c(0), op=ALU.mult), r=[pk], w=[pk])
            P.V(lambda: nc.vector.tensor_tensor(out=c(11), in0=c(7), in1=c(1), op=ALU.mult), r=[pk], w=[pk])
            P.V(lambda: nc.vector.tensor_tensor(out=c(10), in0=c(10), in1=c(11), op=ALU.subtract), r=[pk], w=[pk])
            P.V(lambda: nc.vector.tensor_tensor(out=c(10), in0=c(10), in1=c(8), op=ALU.mult), r=[pk], w=[pk])
            P.V(lambda: nc.vector.tensor_scalar(out=c(12), in0=c(10), scalar1=-1.0, scalar2=None, op0=ALU.mult), r=[pk], w=[pk])
            braw = P.sb(n + "braw", [128, 2, 16]); bk = n + "braw"
            P.dma(braw[:, 0, :], b_re[2 * gp:2 * gp + 2].rearrange("g p c -> (g p) c"), w=[bk])
            P.dma(braw[:, 1, :], b_im[2 * gp:2 * gp + 2].rearrange("g p c -> (g p) c"), wj=[bk])
            BD = P.sb(n + "BD", [128, 2, 64]); tb = P.sb(n + "tb", [128, 16])
            P.V(lambda: nc.vector.memset(BD[:], 0.0), w=[n + "BD"])
            for g in range(2):
                rows = slice(g * 64, (g + 1) * 64); cols = slice(32 * gp + 16 * g, 32 * gp + 16 * g + 16)
                P.V(lambda: nc.vector.tensor_scalar(out=tb[rows, :], in0=braw[rows, 1, :], scalar1=prm[rows, 12:13], scalar2=None, op0=ALU.mult), r=[bk, pk], w=[n + "tb"])
                P.V(lambda: nc.vector.scalar_tensor_tensor(out=BD[rows, 0, cols], in0=braw[rows, 0, :], scalar=prm[rows, 9:10], in1=tb[rows, :], op0=ALU.mult, op1=ALU.add),
                    r=[bk, pk, n + "tb"], wj=[n + "BD"])
                P.V(lambda: nc.vector.tensor_scalar(out=tb[rows, :], in0=braw[rows, 0, :], scalar1=prm[rows, 10:11], scalar2=None, op0=ALU.mult), r=[bk, pk], w=[n + "tb"])
                P.V(lambda: nc.vector.scalar_tensor_tensor(out=BD[rows, 1, cols], in0=braw[rows, 1, :], scalar=prm[rows, 9:10], in1=tb[rows, :], op0=ALU.mult, op1=ALU.add),
                    r=[bk, pk, n + "tb"], wj=[n + "BD"])
            BT = P.sb(n + "BT", [64, 2, 128], BF16)
            for ri in range(2):
                P.T(lambda: nc.tensor.transpose(ps_t[0:64, 0:128], BD[:, ri, :], ident[:]), r=[n + "BD", "ident32"], w=["ps_t"])
                P.V(lambda: nc.vector.tensor_copy(out=BT[:, ri, :], in_=ps_t[0:64, 0:128]), r=["ps_t"], wj=[n + "BT"])
            craw = P.sb(n + "craw", [128, 2, 16]); ck = n + "craw"
            for g in range(2):
                P.dma(craw[g * 64:(g + 1) * 64, 0, :], c_re[d, 2 * gp + g].rearrange("c p -> p c"), wj=[ck], allow_slow_non_contiguous=True)
                P.dma(craw[g * 64:(g + 1) * 64, 1, :], c_im[d, 2 * gp + g].rearrange("c p -> p c"), wj=[ck], allow_slow_non_contiguous=True)
            CT = P.sb(n + "CT", [128, 2, 64], BF16)
            P.V(lambda: nc.vector.memset(CT[:], 0.0), w=[n + "CT"])
            for g in range(2):
                rows = slice(g * 64, (g + 1) * 64); cols = slice(32 * gp + 16 * g, 32 * gp + 16 * g + 16)
                P.V(lambda: nc.vector.tensor_copy(out=CT[rows, 0, cols], in_=craw[rows, 0, :]), r=[ck], wj=[n + "CT"])
                P.V(lambda: nc.vector.tensor_scalar(out=CT[rows, 1, cols], in0=craw[rows, 1, :], scalar1=-1.0, scalar2=None, op0=ALU.mult), r=[ck], wj=[n + "CT"])
            rho_t = P.sb(n + "rho", [128, T])
            P.V(lambda: nc.vector.tensor_scalar(out=rho_t[:], in0=iota[:, 0:T], scalar1=0.0, scalar2=prm[:, 3:4], op0=ALU.mult, op1=ALU.add), r=["iota", pk], w=[n + "rho"])
            init = P.sb(n + "init", [128, 2]); P.V(lambda: nc.vector.memset(init[:], 0.0), w=[n + "init"])
            tiles[(d, gp)] = dict(n=n, prm=prm, pk=pk, cosT=cosT, sinT=sinT, BT=BT, CT=CT, rho=rho_t, init=init)
    ps_b = [P.ps("ps_b%d" % i, [128, 512]) for i in range(2)]
    ps_y = P.ps("ps_y", [128, 512])
    m = [P.sb("m%d" % i, [128, T]) for i in range(4)]
    bp = [P.sb("bp%d" % i, [128, T]) for i in range(2)]
    wv = [P.sb("wv%d" % i, [128, T]) for i in range(2)]
    pp = [P.sb("pp%d" % i, [128, T]) for i in range(4)]
    xb = [[P.sb("xb%d_%d" % (gp, i), [128, T], BF16) for i in range(2)] for gp in range(2)]
    yo = [P.sb("yo%d" % i, [64, T]) for i in range(2)]
    tmpc = P.sb("tmpc", [128, 2])
    it = 0
    for d in range(2):
        for ch in range(NCH):
            cols = slice(ch * T, (ch + 1) * T)
            for gp in range(2):
                t = tiles[(d, gp)]; n = t["n"]; cosT, sinT = t["cosT"], t["sinT"]
                for ri in range(2):
                    P.T(lambda: nc.tensor.matmul(ps_b[ri][:, 0:T], lhsT=t["BT"][:, ri, :], rhs=uT[d][:, cols], start=True, stop=True), r=[n + "BT", "uT%d" % d], w=["ps_b%d" % ri])
                P.V(lambda: nc.vector.tensor_tensor(out=m[0][:], in0=ps_b[0][:, 0:T], in1=cosT[:, 0:T], op=ALU.mult), r=["ps_b0", n + "cos"], w=["m0"])
                P.V(lambda: nc.vector.tensor_tensor(out=m[1][:], in0=ps_b[1][:, 0:T], in1=sinT[:, 0:T], op=ALU.mult), r=["ps_b1", n + "sin"], w=["m1"])
                P.V(lambda: nc.vector.tensor_tensor(out=m[2][:], in0=ps_b[1][:, 0:T], in1=cosT[:, 0:T], op=ALU.mult), r=["ps_b1", n + "cos"], w=["m2"])
                P.V(lambda: nc.vector.tensor_tensor(out=m[3][:], in0=ps_b[0][:, 0:T], in1=sinT[:, 0:T], op=ALU.mult), r=["ps_b0", n + "sin"], w=["m3"])
                P.G(lambda: nc.gpsimd.tensor_tensor(out=bp[0][:], in0=m[0][:], in1=m[1][:], op=ALU.add), r=["m0", "m1"], w=["bp0"])
                P.G(lambda: nc.gpsimd.tensor_tensor(out=bp[1][:], in0=m[2][:], in1=m[3][:], op=ALU.subtract), r=["m2", "m3"], w=["bp1"])
                for ri in range(2):
                    P.V(lambda: nc.vector.tensor_tensor_scan(out=wv[ri][:], data0=t["rho"][:], data1=bp[ri][:], initial=t["init"][:, ri:ri + 1], op0=ALU.mult, op1=ALU.add),
                        r=[n + "rho", "bp%d" % ri, n + "init"], w=["wv%d" % ri])
                prm = t["prm"]
                P.V(lambda: nc.vector.tensor_scalar(out=tmpc[:, 0:1], in0=wv[0][:, T - 1:T], scalar1=prm[:, 13:14], scalar2=None, op0=ALU.mult), r=["wv0", t["pk"]], w=["tmpc"])
                P.V(lambda: nc.vector.tensor_scalar(out=tmpc[:, 1:2], in0=wv[0][:, T - 1:T], scalar1=prm[:, 14:15], scalar2=None, op0=ALU.mult), r=["wv0", t["pk"]], wj=["tmpc"])
                P.V(lambda: nc.vector.scalar_tensor_tensor(out=t["init"][:, 0:1], in0=wv[1][:, T - 1:T], scalar=prm[:, 15:16], in1=tmpc[:, 0:1], op0=ALU.mult, op1=ALU.add),
                    r=["wv1", "tmpc", t["pk"]], w=[n + "init"])
                P.V(lambda: nc.vector.scalar_tensor_tensor(out=t["init"][:, 1:2], in0=wv[1][:, T - 1:T], scalar=prm[:, 13:14], in1=tmpc[:, 1:2], op0=ALU.mult, op1=ALU.add),
                    r=["wv1", "tmpc", t["pk"]], wj=[n + "init"])
                P.G(lambda: nc.gpsimd.tensor_tensor(out=pp[0][:], in0=wv[0][:], in1=cosT[:, 0:T], op=ALU.mult), r=["wv0", n + "cos"], w=["pp0"])
                P.G(lambda: nc.gpsimd.tensor_tensor(out=pp[1][:], in0=wv[1][:], in1=sinT[:, 0:T], op=ALU.mult), r=["wv1", n + "sin"], w=["pp1"])
                P.G(lambda: nc.gpsimd.tensor_tensor(out=xb[gp][0][:], in0=pp[0][:], in1=pp[1][:], op=ALU.subtract), r=["pp0", "pp1"], w=["xb%d_0" % gp])
                P.G(lambda: nc.gpsimd.tensor_tensor(out=pp[2][:], in0=wv[0][:], in1=sinT[:, 0:T], op=ALU.mult), r=["wv0", n + "sin"], w=["pp2"])
                P.G(lambda: nc.gpsimd.tensor_tensor(out=pp[3][:], in0=wv[1][:], in1=cosT[:, 0:T], op=ALU.mult), r=["wv1", n + "cos"], w=["pp3"])
                P.G(lambda: nc.gpsimd.tensor_tensor(out=xb[gp][1][:], in0=pp[2][:], in1=pp[3][:], op=ALU.add), r=["pp2", "pp3"], w=["xb%d_1" % gp])
                yield 1
            k = 0
            for gp in range(2):
                t = tiles[(d, gp)]
                for ri in range(2):
                    P.T(lambda: nc.tensor.matmul(ps_y[0:64, 0:T], lhsT=t["CT"][:, ri, :], rhs=xb[gp][ri][:], start=(k == 0), stop=(k == 3)),
                        r=[t["n"] + "CT", "xb%d_%d" % (gp, ri)], w=["ps_y"] if k == 0 else [], wj=[] if k == 0 else ["ps_y"])
                    k += 1
            b = it % 2; it += 1
            P.V(lambda: nc.vector.tensor_copy(out=yo[b][:], in_=ps_y[0:64, 0:T]), r=["ps_y"], w=["yo%d" % b])
            P.dma(yT_d[d][:, cols], yo[b][:], r=["yo%d" % b], wj=["yT%d" % d], key="yo%d" % b)
    P.close_ns()
    yield 'done'


def gen_MB(P0, S):
    Pp = NS(P0, "b_"); P = Pp; nc = P0.nc
    Ps = NS(P0, "b_")
    NT = S // 128
    rkv = [P.dram("rkv%d" % d, [S, 192]) for d in range(2)]
    h1 = [[P.dram("h%s1_%d" % (n, d), [64, S]) for d in range(2)] for n in "wa"]
    h2 = [[P.dram("h%s2_%d" % (n, d), [64, S]) for d in range(2)] for n in "wa"]
    w2 = P.dram("w2", [2, 64, 64]); a2 = P.dram("a2", [2, 64, 64]); w0 = P.dram("w0", [2, 64]); a0 = P.dram("a0", [2, 64])
    mu = P.dram("mu", [2, 2, 192]); kka = P.dram("kka", [3, 64])
    ident_d = P.dram("ident", [128, 128]); zsel_d = P.dram("zsel", [64, 32 * 128])
    y_o = P.dram("y", [2, S, 64], kind="ExternalOutput"); bonus_o = P.dram("bonus", [2, S, 64], kind="ExternalOutput")
    pkd = [P.dram("pkd%d" % d, [S, 384], BF16, kind="Internal") for d in range(2)]
    vTs = P.dram("vTs", [128, S], F32, kind="Internal")
    i32 = Ps.sb("ident32", [128, 128]); P.dma(i32[:], ident_d[:, :], w=["ident32"]); ident = i32
    zb = Ps.sb("zb", [64, 32 * 128], BF16)
    SEG = min(512, S); NB = 2
    St = Ps.sb("St", [128, 64]); prod = Ps.sb("prod", [128, 2, 64]); zcol = Ps.sb("zcol", [128, 1])
    bcp = [Ps.ps("bcp%d" % i, [128, 512]) for i in range(NB)]
    pkt = [Ps.sb("pkt%d" % i, [64, 4, 384], BF16) for i in range(2)]
    vseg = [Ps.sb("vseg%d" % i, [128, SEG]) for i in range(2)]
    yseg = [Ps.sb("yseg%d" % i, [128, SEG, 2]) for i in range(2)]
    yo = [Ps.sb("yo%d" % i, [128, 128]) for i in range(2)]
    ps_t = Ps.ps("ps_t", [128, 512])
    ps_t2 = Pp.ps("ps_t2", [128, 512])
    z32 = Pp.sb("z32", [64, 32 * 128])
    P.dma(z32[:], zsel_d[:, :], w=["z32"]); P.V(lambda: nc.vector.tensor_copy(out=zb[:], in_=z32[:]), r=["z32"], w=["zb"])
    def bc(name, src, n):
        t = Pp.sb(name, [128, n]); P.dma(t[:], src.partition_broadcast(128), w=[name]); return t
    kk_bc = bc("kk_bc", kka[0:1, :], 64); ka_bc = bc("ka_bc", kka[1:2, :], 64); rk_bc = bc("rk_bc", kka[2:3, :], 64)
    ps_l = ps_t2[:, 0:512]
    thT = P.sb("thT", [64, S], BF16); haT = P.sb("haT", [64, S], BF16)
    CW = min(2048, S)
    ha = P.sb("ha_", [64, CW]); hb = P.sb("hb_", [64, CW])
    vstage = P.sb("vstage", [128, 128]); P.V(lambda: nc.vector.memset(vstage[:], 0.0), w=["vstage"])
    zrow = P.sb("zrow", [1, 64], BF16); P.V(lambda: nc.vector.memset(zrow[:], 0.0), w=["zrow"])
    for d in range(2):
        for wi, dst in enumerate((thT, haT)):
            dk_ = "thT" if wi == 0 else "haT"
            for ci, c0 in enumerate(range(0, S, CW)):
                P.dma(ha[:], h1[wi][d][:, c0:c0 + CW], w=["ha"])
                if c0 == 0:
                    P.V(lambda: nc.vector.memset(hb[:, 0:1], 0.0), w=["hb"])
                    P.dma(hb[:, 1:CW], h2[wi][d][:, 0:CW - 1], wj=["hb"])
                else:
                    P.dma(hb[:], h2[wi][d][:, c0 - 1:c0 + CW - 1], w=["hb"])
                P.V(lambda: nc.vector.tensor_tensor(out=ha[:], in0=ha[:], in1=hb[:], op=ALU.add), r=["ha", "hb"], w=["ha"])
                first = (ci == 0)
                if wi == 0:
                    P.A(lambda: nc.scalar.activation(out=dst[:, c0:c0 + CW], in_=ha[:], func=AF.Tanh), r=["ha"], w=[dk_] if first else [], wj=[] if first else [dk_])
                else:
                    P.V(lambda: nc.vector.tensor_copy(out=dst[:, c0:c0 + CW], in_=ha[:]), r=["ha"], w=[dk_] if first else [], wj=[] if first else [dk_])
                yield 1
        w2s = P.sb("w2s%d" % d, [64, 2, 64]); w2b = P.sb("w2b%d" % d, [64, 2, 64], BF16)
        P.dma(w2s[:, 0, :], w2[d], w=["w2s%d" % d]); P.dma(w2s[:, 1, :], a2[d], wj=["w2s%d" % d])
        P.V(lambda: nc.vector.tensor_copy(out=w2b[:], in_=w2s[:]), r=["w2s%d" % d], w=["w2b%d" % d])
        w0_bc = bc("w0_bc%d" % d, w0[d:d + 1, :], 64); a0_bc = bc("a0_bc%d" % d, a0[d:d + 1, :], 64)
        mu0_bc = bc("mu0_bc%d" % d, mu[d, 0:1, :], 192); mu1_bc = bc("mu1_bc%d" % d, mu[d, 1:2, :], 192)
        J = 4; NBT = NT // J
        def b3(t, n):
            return t[:].unsqueeze(1).to_broadcast([128, J, n])
        def s3(t):
            return t[:].unsqueeze(2).to_broadcast([128, J, 64])
        if d == 0:
            cur = [P.sb("cur%d" % i, [128, J, 192]) for i in range(2)]
            prv = [P.sb("prv%d" % i, [128, J, 192]) for i in range(2)]
            nxt = [P.sb("nxt%d" % i, [128, J, 192]) for i in range(2)]
            d0 = P.sb("d0", [128, J, 192]); d1 = P.sb("d1", [128, J, 192]); mix = P.sb("mix", [128, J, 192])
            dec = P.sb("dec", [128, J, 64]); hif = P.sb("hif", [128, J, 64])
            pack = [P.sb("pack%d" % i, [128, J, 384], BF16) for i in range(2)]
            bon = [P.sb("bon%d" % i, [128, J, 64]) for i in range(2)]
            vto = [P.sb("vto%d" % i, [128, J, 128]) for i in range(2)]
            icl = P.sb("icl", [128, J, 64]); kkt = P.sb("kkt", [128, J, 64]); kap = P.sb("kap", [128, J, 64]); kd = P.sb("kd", [128, J, 64])
            t1 = P.sb("t1", [128, J, 64]); sq = P.sb("sq", [128, J, 64]); ss = P.sb("ss", [128, J]); sb_ = P.sb("sb_", [128, J])
        for bt in range(NBT):
            b = bt % 2; r0 = bt * 128 * J; R = 128 * J
            ck, pk_, nk = "cur%d" % b, "prv%d" % b, "nxt%d" % b
            P.dma(cur[b][:], rkv[d][r0:r0 + R, :].rearrange("(j p) c -> p j c", p=128), w=[ck])
            if bt == 0:
                P.V(lambda: nc.vector.memset(prv[b][:, 0, :], 0.0), w=[pk_])
                P.dma(prv[b][1:128, 0, :], rkv[d][0:127, :], wj=[pk_])
                P.dma(prv[b][:, 1:J, :], rkv[d][127:127 + 128 * (J - 1), :].rearrange("(j p) c -> p j c", p=128), wj=[pk_])
            else:
                P.dma(prv[b][:], rkv[d][r0 - 1:r0 - 1 + R, :].rearrange("(j p) c -> p j c", p=128), w=[pk_])
            if bt == NBT - 1:
                P.V(lambda: nc.vector.memset(nxt[b][:, J - 1, :], 0.0), w=[nk])
                P.dma(nxt[b][0:127, J - 1, :], rkv[d][S - 127:S, :], wj=[nk])
                P.dma(nxt[b][:, 0:J - 1, :], rkv[d][r0 + 1:r0 + 1 + 128 * (J - 1), :].rearrange("(j p) c -> p j c", p=128), wj=[nk])
            else:
                P.dma(nxt[b][:], rkv[d][r0 + 1:r0 + 1 + R, :].rearrange("(j p) c -> p j c", p=128), w=[nk])
            for j in range(J):
                c0_ = r0 + j * 128
                P.T(lambda: nc.tensor.matmul(ps_l[:, j * 128:j * 128 + 64], lhsT=thT[:, c0_:c0_ + 128], rhs=w2b[:, 0, :], start=True, stop=True), r=["thT", "w2b%d" % d],
                    w=["ps_t"] if j == 0 else [], wj=[] if j == 0 else ["ps_t"])
                P.T(lambda: nc.tensor.matmul(ps_l[:, j * 128 + 64:j * 128 + 128], lhsT=haT[:, c0_:c0_ + 128], rhs=w2b[:, 1, :], start=True, stop=True), r=["haT", "w2b%d" % d], wj=["ps_t"])
            pl3 = ps_l.rearrange("p (j c) -> p j c", j=J)
            P.V(lambda: nc.vector.tensor_tensor(out=d0[:], in0=prv[b][:], in1=cur[b][:], op=ALU.subtract), r=[pk_, ck], w=["d0"])
            P.V(lambda: nc.vector.tensor_tensor(out=d0[:], in0=d0[:], in1=b3(mu0_bc, 192), op=ALU.mult), r=["d0", "mu0_bc%d" % d], w=["d0"])
            P.V(lambda: nc.vector.tensor_tensor(out=d1[:], in0=nxt[b][:], in1=cur[b][:], op=ALU.subtract), r=[nk, ck], w=["d1"])
            P.V(lambda: nc.vector.tensor_tensor(out=d1[:], in0=d1[:], in1=b3(mu1_bc, 192), op=ALU.mult), r=["d1", "mu1_bc%d" % d], w=["d1"])
            P.V(lambda: nc.vector.tensor_tensor(out=d0[:], in0=d0[:], in1=d1[:], op=ALU.add), r=["d0", "d1"], w=["d0"])
            P.V(lambda: nc.vector.tensor_tensor(out=mix[:], in0=cur[b][:], in1=d0[:], op=ALU.add), r=[ck, "d0"], w=["mix"])
            rr, kp, vp = mix[:, :, 0:64], mix[:, :, 64:128], mix[:, :, 128:192]
            pkk = "pack%d" % b
            P.V(lambda: nc.vector.tensor_tensor(out=t1[:], in0=pl3[:, :, 0:64], in1=b3(w0_bc, 64), op=ALU.add), r=["ps_t", "w0_bc%d" % d], w=["t1"])
            P.A(lambda: nc.scalar.activation(out=t1[:], in_=t1[:], func=AF.Sigmoid), r=["t1"], w=["t1"])
            P.A(lambda: nc.scalar.activation(out=dec[:], in_=t1[:], func=AF.Exp, scale=-EM05), r=["t1"], w=["dec"])
            P.V(lambda: nc.vector.tensor_copy(out=pack[b][:, :, 256:320], in_=dec[:]), r=["dec"], w=[pkk])
            P.V(lambda: nc.vector.tensor_copy(out=hif[:], in_=pack[b][:, :, 256:320]), r=[pkk], w=["hif"])
            P.V(lambda: nc.vector.tensor_tensor(out=hif[:], in0=dec[:], in1=hif[:], op=ALU.subtract), r=["dec", "hif"], w=["hif"])
            P.V(lambda: nc.vector.tensor_copy(out=pack[b][:, :, 320:384], in_=hif[:]), r=["hif"], wj=[pkk])
            P.V(lambda: nc.vector.tensor_tensor(out=icl[:], in0=pl3[:, :, 64:128], in1=b3(a0_bc, 64), op=ALU.add), r=["ps_t", "a0_bc%d" % d], w=["icl"])
            P.A(lambda: nc.scalar.activation(out=icl[:], in_=icl[:], func=AF.Sigmoid), r=["icl"], w=["icl"])
            P.V(lambda: nc.vector.tensor_tensor(out=kkt[:], in0=kp, in1=b3(kk_bc, 64), op=ALU.mult), r=["mix", "kk_bc"], w=["kkt"])
            P.V(lambda: nc.vector.tensor_tensor(out=sq[:], in0=kkt[:], in1=kkt[:], op=ALU.mult), r=["kkt"], w=["sq"])
            P.V(lambda: nc.vector.tensor_reduce(out=ss[:], in_=sq[:], axis=AX.X, op=ALU.add), r=["sq"], w=["ss"])
            P.V(lambda: nc.vector.tensor_scalar(out=ss[:], in0=ss[:], scalar1=1e-12, scalar2=None, op0=ALU.add), r=["ss"], w=["ss"])
            P.A(lambda: nc.scalar.sqrt(out=ss[:], in_=ss[:]), r=["ss"], w=["ss"])
            P.V(lambda: nc.vector.reciprocal(out=ss[:], in_=ss[:]), r=["ss"], w=["ss"])
            P.V(lambda: nc.vector.tensor_tensor(out=kap[:], in0=kkt[:], in1=s3(ss), op=ALU.mult), r=["kkt", "ss"], w=["kap"])
            P.V(lambda: nc.vector.tensor_tensor(out=t1[:], in0=icl[:], in1=b3(ka_bc, 64), op=ALU.mult), r=["icl", "ka_bc"], w=["t1"])
            P.V(lambda: nc.vector.scalar_tensor_tensor(out=t1[:], in0=t1[:], scalar=1.0, in1=b3(ka_bc, 64), op0=ALU.add, op1=ALU.subtract), r=["t1", "ka_bc"], w=["t1"])
            P.V(lambda: nc.vector.tensor_tensor(out=kd[:], in0=kp, in1=t1[:], op=ALU.mult), r=["mix", "t1"], w=["kd"])
            P.V(lambda: nc.vector.tensor_copy(out=pack[b][:, :, 0:64], in_=rr), r=["mix"], wj=[pkk])
            P.V(lambda: nc.vector.tensor_copy(out=pack[b][:, :, 64:128], in_=kap[:]), r=["kap"], wj=[pkk])
            P.V(lambda: nc.vector.scalar_tensor_tensor(out=pack[b][:, :, 128:192], in0=icl[:], scalar=-1.0, in1=kap[:], op0=ALU.mult, op1=ALU.mult), r=["icl", "kap"], wj=[pkk])
            P.V(lambda: nc.vector.tensor_copy(out=pack[b][:, :, 192:256], in_=kd[:]), r=["kd"], wj=[pkk])
            rows3 = lambda a, lo, hi: a[lo:hi, :].rearrange("(j p) c -> p j c", p=128)
            P.dma(rows3(pkd[d][:, 0:64], r0, r0 + R), pack[b][:, :, 0:64], r=[pkk], wj=["scr"], key=pkk)
            P.dma(rows3(pkd[d][:, 128:384], r0, r0 + R), pack[b][:, :, 128:384], r=[pkk], wj=["scr"], key=pkk)
            if bt == 0:
                P.dma(pkd[d][0:127, 64:128], pack[b][1:128, 0, 64:128], r=[pkk], wj=["scr"], key=pkk)
                P.dma(rows3(pkd[d][:, 64:128], 127, 127 + 128 * (J - 1)), pack[b][:, 1:J, 64:128], r=[pkk], wj=["scr"], key=pkk)
            else:
                P.dma(rows3(pkd[d][:, 64:128], r0 - 1, r0 - 1 + R), pack[b][:, :, 64:128], r=[pkk], wj=["scr"], key=pkk)
            if bt == NBT - 1:
                P.dma(pkd[d][S - 1:S, 64:128], zrow[0:1, :], r=["zrow"], wj=["scr"], key="zrow")
            P.V(lambda: nc.vector.tensor_tensor(out=t1[:], in0=rr, in1=kd[:], op=ALU.mult), r=["mix", "kd"], w=["t1"])
            P.V(lambda: nc.vector.tensor_tensor(out=t1[:], in0=t1[:], in1=b3(rk_bc, 64), op=ALU.mult), r=["t1", "rk_bc"], w=["t1"])
            P.V(lambda: nc.vector.tensor_reduce(out=sb_[:], in_=t1[:], axis=AX.X, op=ALU.add), r=["t1"], w=["sb_"])
            P.V(lambda: nc.vector.tensor_tensor(out=bon[b][:], in0=vp, in1=s3(sb_), op=ALU.mult), r=["mix", "sb_"], w=["bon%d" % b])
            P.dma(bonus_o[d, r0:r0 + R, :].rearrange("(j p) c -> p j c", p=128), bon[b][:], r=["bon%d" % b], wj=["bonus"], key="bon%d" % b)
            for j in range(J):
                P.V(lambda: nc.vector.tensor_copy(out=vstage[:, 64 * d:64 * d + 64], in_=mix[:, j, 128:192]), r=["mix"], w=["vstage"])
                P.T(lambda: nc.tensor.transpose(ps_t[:, 0:128], vstage[:], ident[:]), r=["vstage", "ident32", "t1", "icl"], w=["ps_t"])
                P.V(lambda: nc.vector.tensor_copy(out=vto[b][64 * d:64 * d + 64, j, :], in_=ps_t[64 * d:64 * d + 64, 0:128]), r=["ps_t"], w=["vto%d" % b] if j == 0 else [], wj=[] if j == 0 else ["vto%d" % b])
            P.dma(vTs[64 * d:64 * d + 64, r0:r0 + R], vto[b][64 * d:64 * d + 64, :, :].rearrange("p j c -> p (j c)"), r=["vto%d" % b], wj=["scr"], key="vto%d" % b)
            yield 1
    Pp.close_ns()
    yield 'prep_done'
    P = Ps
    P.V(lambda: nc.vector.memset(St[:], 0.0), w=["S"])
    P.V(lambda: nc.vector.memset(zcol[:], 0.0), w=["zcol"])
    St_b = St[:].unsqueeze(1).to_broadcast([128, 2, 64])
    step = 0; oi = 0
    sk_ap, sk_tok = zcol[:], "zcol"
    for sg in range(S // SEG):
        sb2 = sg % 2; vk = "vseg%d" % sb2; yk = "yseg%d" % sb2
        P.dma(vseg[sb2][:], vTs[:, sg * SEG:(sg + 1) * SEG], r=["scr"], w=[vk])
        for blk in range(SEG // 128):
            s0 = sg * SEG + blk * 128; bb = (s0 // 128) % 2
            pk_ = "pkt%d" % bb
            for d in range(2):
                P.dma(pkt[bb][32 * d:32 * d + 32, :, :], pkd[d][s0:s0 + 128, :].rearrange("(g q) c -> q g c", q=32), r=["scr"], w=[pk_] if d == 0 else [], wj=[] if d == 0 else [pk_])
            for g in range(4):
                for j in range(32):
                    sl = step % NB; step += 1; bk = "bcp%d" % sl
                    col = blk * 128 + g * 32 + j
                    sel = zb[:, j * 128:(j + 1) * 128]
                    P.T(lambda: nc.tensor.matmul(bcp[sl][:, 0:256], lhsT=sel, rhs=pkt[bb][:, g, 0:256], start=True, stop=True), r=["zb", pk_], w=[bk])
                    P.T(lambda: nc.tensor.matmul(bcp[sl][:, 256:320], lhsT=sel, rhs=pkt[bb][:, g, 256:320], start=True, stop=False), r=["zb", pk_], wj=[bk])
                    P.T(lambda: nc.tensor.matmul(bcp[sl][:, 256:320], lhsT=sel, rhs=pkt[bb][:, g, 320:384], start=False, stop=True), r=["zb", pk_], wj=[bk])
                    P.ses = bool(os.environ.get("FORCE_SES"))
                    nbb, kdb, wb = bcp[sl][:, 128:192], bcp[sl][:, 192:256], bcp[sl][:, 256:320]
                    rk2 = bcp[sl][:, 0:128].rearrange("p (a n) -> p a n", a=2)
                    P.V(lambda: nc.vector.tensor_tensor(out=St[:], in0=St[:], in1=wb, op=ALU.mult), r=["S", bk], w=["S"])
                    P.V(lambda: nc.vector.scalar_tensor_tensor(out=St[:], in0=nbb, scalar=sk_ap, in1=St[:], op0=ALU.mult, op1=ALU.add), r=["S", bk, sk_tok], w=["S"])
                    P.V(lambda: nc.vector.scalar_tensor_tensor(out=St[:], in0=kdb, scalar=vseg[sb2][:, col:col + 1], in1=St[:], op0=ALU.mult, op1=ALU.add), r=["S", bk, vk], w=["S"])
                    P.V(lambda: nc.vector.tensor_tensor(out=prod[:], in0=St_b, in1=rk2, op=ALU.mult), r=["S", bk], w=["prod"])
                    P.V(lambda: nc.vector.tensor_reduce(out=yseg[sb2][:, col, :], in_=prod[:], axis=AX.X, op=ALU.add), r=["prod"], wj=[yk])
                    P.ses = True
                    sk_ap, sk_tok = yseg[sb2][:, col, 1:2], yk
                    yield 2
        for blk in range(SEG // 128):
            ob = oi % 2; oi += 1; r0 = sg * SEG + blk * 128
            P.T(lambda: nc.tensor.transpose(ps_t[:, 0:128], yseg[sb2][:, blk * 128:(blk + 1) * 128, 0], ident[:]), r=[yk, "ident32"], w=["ps_t"])
            P.V(lambda: nc.vector.tensor_copy(out=yo[ob][:], in_=ps_t[:, 0:128]), r=["ps_t"], w=["yo%d" % ob])
            for d in range(2):
                P.dma(y_o[d, r0:r0 + 128, :], yo[ob][:, 64 * d:64 * d + 64], r=["yo%d" % ob], wj=["y"], key="yo%d" % ob)
        P.V(lambda: nc.vector.memset(zcol[:], 0.0), r=[yk], w=["zcol2"])
    Ps.close_ns()
    yield 'done'


def build_M(S):
    P0 = Prog()
    gB = gen_MB(P0, S)
    for v in gB:
        if v == 'prep_done':
            break
    others = [(gen_MD, 3), (gen_MA, 2), (gen_MC, 10)]
    b_done = False
    for gfn, ratio in others:
        g = gfn(P0, S)
        for v in g:
            if v == 'done':
                break
            if not b_done:
                for _ in range(ratio):
                    if next(gB) == 'done':
                        b_done = True
                        break
    if not b_done:
        for v in gB:
            pass
    P0.finish(); P0.close()
    return P0.nc


def m_inputs(z_b, prm, l, h, S):
    m = {}
    za = z_b[:, 0:768]
    cosr, sinr = rope_tables(S); cosa, sina = axial_tables(S)
    ident = np.eye(128, dtype=np.float32)
    ma = {"cos": cosr, "sin": sinr, "ident": ident, "mask": dil_mask(), "q": np.ascontiguousarray(za[:, h * 64:(h + 1) * 64]), "k": np.ascontiguousarray(za[:, 256 + h * 64:256 + (h + 1) * 64])}
    v = za[:, 512 + h * 64:512 + (h + 1) * 64]
    for d in (1, 4, 16):
        ma["vx%d" % d] = vext_dilated(v, d)
    for k_, v_ in ma.items(): m["a_" + k_] = v_
    for k_, v_ in mb_inputs(z_b[:, 768:1536], z_b[:, 2304:2944], prm, l, h).items(): m["b_" + k_] = v_
    for k_, v_ in mc_inputs(z_b[:, 1536:1792], prm, l, h, S).items(): m["c_" + k_] = v_
    kv = h // 2
    md = {"qg": prm["gqa_q_norm"][l][None, :], "kg": prm["gqa_k_norm"][l][None, :], "cos": cosa, "sin": sina, "ident": ident,
          "q": np.ascontiguousarray(z_b[:, 1792 + h * 64:1792 + (h + 1) * 64]), "k": np.ascontiguousarray(z_b[:, 2048 + kv * 64:2048 + (kv + 1) * 64]),
          "v": np.ascontiguousarray(z_b[:, 2176 + kv * 64:2176 + (kv + 1) * 64])}
    for k_, v_ in md.items(): m["d_" + k_] = v_
    return m


def run_M(z, prm, l):
    B, S, _ = z.shape
    nc = build_M(S)
    maps = [m_inputs(z[c // 4], prm, l, c % 4, S) for c in range(B * 4)]
    res = run_bass_kernel_spmd(nc, maps, core_ids=list(range(B * 4)))
    names = ["ya", "yd", "rwf", "rwb", "bnf", "bnb", "cf", "cb"]
    out = {n: np.zeros((B, S, 256), np.float32) for n in names}
    for c in range(B * 4):
        b, h = c // 4, c % 4
        r = res.results[c]; hs = slice(h * 64, (h + 1) * 64)
        out["ya"][b, :, hs] = r["a_yT"].T
        out["yd"][b, :, hs] = r["d_yT"].T
        out["rwf"][b, :, hs] = r["b_y"][0]; out["rwb"][b, :, hs] = r["b_y"][1][::-1]
        out["bnf"][b, :, hs] = r["b_bonus"][0]; out["bnb"][b, :, hs] = r["b_bonus"][1][::-1]
        out["cf"][b, :, hs] = r["c_yT0"].T; out["cb"][b, :, hs] = r["c_yT1"].T[::-1]
    return out


def build_F1(TC):
    P = Prog(); nc = P.nc
    x = P.dram("x", [TC, 1024]); g = P.dram("g", [1, 1024]); ident_d = P.dram("ident", [128, 128])
    br = {n: P.dram(n, [TC, 256]) for n in ("ya", "yd", "rwf", "rwb", "bnf", "bnb", "cf", "cb", "u")}
    hg = P.dram("hg", [TC, 128])
    wgate = P.dram("wgate", [1024, 4096]); gate_b = P.dram("gate_b", [1, 4096]); w_branch = P.dram("w_branch", [4, 256, 1024]); w_out = P.dram("w_out", [1024, 1024])
    glu_w = P.dram("glu_w", [256, 512]); glu_b = P.dram("glu_b", [1, 512]); g2 = P.dram("g2", [128, 256])
    vecs = P.dram("vecs", [3, 256])
    y = P.dram("y", [TC, 1024], kind="ExternalOutput")
    ident = load_ident32(P, nc, ident_d)
    def bc(name, src, n):
        t = P.sb(name, [128, n]); P.dma(t[:], src.partition_broadcast(128), w=[name]); return t
    gbc = bc("gbc", g[0:1, :], 1024); gb_bc = bc("gb_bc", gate_b[0:1, :], 4096); glub_bc = bc("glub_bc", glu_b[0:1, :], 512)
    lnw_bc = bc("lnw_bc", vecs[0:1, :], 256); lnb_bc = bc("lnb_bc", vecs[1:2, :], 256); d_bc = bc("d_bc", vecs[2:3, :], 256)
    Wg = P.sb("Wg", [128, 8, 4096], BF16); Wbr = P.sb("Wbr", [128, 8, 1024], BF16); Wo = P.sb("Wo", [128, 8, 1024], BF16)
    glub = P.sb("glub", [128, 2, 512], BF16); g2b = P.sb("g2b", [128, 256], BF16)
    stg = [P.sb("stg%d" % i, [128, 1024]) for i in range(2)]
    si = 0
    def load_cast(dst, src, n, tok):
        nonlocal si
        b = si % 2; si += 1
        P.dma(stg[b][:, 0:n], src, w=["stg%d" % b])
        if b == 0:
            P.V(lambda: nc.vector.tensor_copy(out=dst, in_=stg[b][:, 0:n]), r=["stg%d" % b], wj=[tok])
        else:
            P.G(lambda: nc.gpsimd.tensor_copy(out=dst, in_=stg[b][:, 0:n]), r=["stg%d" % b], wj=[tok])
    for kc in range(8):
        rows = slice(kc * 128, (kc + 1) * 128)
        for q in range(4):
            load_cast(Wg[:, kc, q * 1024:(q + 1) * 1024], wgate[rows, q * 1024:(q + 1) * 1024], 1024, "Wg")
        load_cast(Wo[:, kc, :], w_out[rows, :], 1024, "Wo")
        load_cast(Wbr[:, kc, :], w_branch[kc // 2, (kc % 2) * 128:(kc % 2 + 1) * 128, :], 1024, "Wbr")
    for kc in range(2):
        load_cast(glub[:, kc, :], glu_w[kc * 128:(kc + 1) * 128, :], 512, "glub")
    load_cast(g2b[:], g2[:, :], 256, "g2b")
    xt = [P.sb("xt%d" % i, [128, 1024]) for i in range(2)]
    xn32 = P.sb("xn32", [128, 1024]); ss = P.sb("ss", [128, 1])
    xnT = P.sb("xnT", [128, 8, 128], BF16); mT = P.sb("mT", [128, 8, 128], BF16)
    gate = P.sb("gate", [128, 1024]); merged = P.sb("merged", [128, 1024]); tmpm = P.sb("tmpm", [128, 1024])
    inp = {n: P.sb("i_" + n, [128, 256]) for n in br}
    hgt = P.sb("hgt", [128, 128]); hgT = P.sb("hgT", [128, 128], BF16)
    ys = P.sb("ys", [128, 256]); sqh = P.sb("sqh", [128, 64]); st = P.sb("st", [128, 12])
    ybf = P.sb("ybf", [128, 256]); ycf = P.sb("ycf", [128, 256]); t256 = P.sb("t256", [128, 256]); h512 = P.sb("h512", [128, 512])
    yT = P.sb("yT", [128, 2, 128], BF16)
    pst = P.ps("pst", [128, 8, 128]); pbr = P.ps("pbr", [128, 1024]); pg = P.ps("pg", [128, 1024]); po = P.ps("po", [128, 1024])
    NT = TC // 128
    for t in range(NT):
        b = t % 2; r0 = t * 128; xk = "xt%d" % b
        P.dma(xt[b][:], x[r0:r0 + 128, :], w=[xk])
        for n in br:
            P.dma(inp[n][:], br[n][r0:r0 + 128, :], w=["i_" + n])
        P.dma(hgt[:], hg[r0:r0 + 128, :], w=["hgt"])
        P.A(lambda: nc.scalar.activation(out=xn32[:], in_=xt[b][:], func=AF.Square, accum_out=ss[:]), r=[xk], w=["xn32", "ss"])
        P.V(lambda: nc.vector.tensor_scalar(out=ss[:], in0=ss[:], scalar1=1.0 / 1024, scalar2=1e-6, op0=ALU.mult, op1=ALU.add), r=["ss"], w=["ss"])
        P.A(lambda: nc.scalar.sqrt(out=ss[:], in_=ss[:]), r=["ss"], w=["ss"])
        P.V(lambda: nc.vector.reciprocal(out=ss[:], in_=ss[:]), r=["ss"], w=["ss"])
        P.V(lambda: nc.vector.scalar_tensor_tensor(out=xn32[:], in0=xt[b][:], scalar=ss[:], in1=gbc[:], op0=ALU.mult, op1=ALU.mult), r=[xk, "ss", "gbc"], w=["xn32"])
        for kc in range(8):
            P.T(lambda: nc.tensor.transpose(pst[:, kc, :], xn32[:, kc * 128:(kc + 1) * 128], ident[:]), r=["xn32", "ident32"], w=["pst"] if kc == 0 else [], wj=[] if kc == 0 else ["pst"])
        P.V(lambda: nc.vector.tensor_copy(out=xnT[:], in_=pst[:]), r=["pst"], w=["xnT"])
        P.V(lambda: nc.vector.tensor_tensor(out=ys[:], in0=inp["rwf"][:], in1=inp["rwb"][:], op=ALU.add), r=["i_rwf", "i_rwb"], w=["ys"])
        P.V(lambda: nc.vector.tensor_reduce(out=st[:, 0:4], in_=ys[:].rearrange("p (h n) -> p h n", h=4), axis=AX.X, op=ALU.add), r=["ys"], w=["st"])
        for h in range(4):
            P.A(lambda: nc.scalar.activation(out=sqh[:], in_=ys[:, h * 64:(h + 1) * 64], func=AF.Square, accum_out=st[:, 4 + h:5 + h]), r=["ys", "st"], w=["sqh", "st"])
        P.V(lambda: nc.vector.tensor_scalar(out=st[:, 0:8], in0=st[:, 0:8], scalar1=1.0 / 64, scalar2=None, op0=ALU.mult), r=["st"], w=["st"])
        P.V(lambda: nc.vector.tensor_tensor(out=st[:, 8:12], in0=st[:, 0:4], in1=st[:, 0:4], op=ALU.mult), r=["st"], w=["st"])
        P.V(lambda: nc.vector.tensor_tensor(out=st[:, 4:8], in0=st[:, 4:8], in1=st[:, 8:12], op=ALU.subtract), r=["st"], w=["st"])
        P.V(lambda: nc.vector.tensor_scalar(out=st[:, 4:8], in0=st[:, 4:8], scalar1=64e-5, scalar2=None, op0=ALU.add), r=["st"], w=["st"])
        P.A(lambda: nc.scalar.sqrt(out=st[:, 4:8], in_=st[:, 4:8]), r=["st"], w=["st"])
        P.V(lambda: nc.vector.reciprocal(out=st[:, 4:8], in_=st[:, 4:8]), r=["st"], w=["st"])
        for h in range(4):
            P.V(lambda: nc.vector.tensor_scalar(out=ybf[:, h * 64:(h + 1) * 64], in0=ys[:, h * 64:(h + 1) * 64], scalar1=st[:, h:h + 1], scalar2=st[:, 4 + h:5 + h], op0=ALU.subtract, op1=ALU.mult),
                r=["ys", "st"], w=["ybf"] if h == 0 else [], wj=[] if h == 0 else ["ybf"])
        P.V(lambda: nc.vector.tensor_tensor(out=ybf[:], in0=ybf[:], in1=lnw_bc[:], op=ALU.mult), r=["ybf", "lnw_bc"], w=["ybf"])
        P.V(lambda: nc.vector.tensor_tensor(out=ybf[:], in0=ybf[:], in1=lnb_bc[:], op=ALU.add), r=["ybf", "lnb_bc"], w=["ybf"])
        P.V(lambda: nc.vector.tensor_tensor(out=ybf[:], in0=ybf[:], in1=inp["bnf"][:], op=ALU.add), r=["ybf", "i_bnf"], w=["ybf"])
        P.V(lambda: nc.vector.tensor_tensor(out=ybf[:], in0=ybf[:], in1=inp["bnb"][:], op=ALU.add), r=["ybf", "i_bnb"], w=["ybf"])
        P.A(lambda: nc.scalar.activation(out=hgt[:], in_=hgt[:], func=AF.Sigmoid), r=["hgt"], w=["hgt"])
        P.T(lambda: nc.tensor.transpose(po[:, 0:128], hgt[:], ident[:]), r=["hgt", "ident32"], w=["po"])
        P.V(lambda: nc.vector.tensor_copy(out=hgT[:], in_=po[:, 0:128]), r=["po"], w=["hgT"])
        P.T(lambda: nc.tensor.matmul(po[:, 0:256], lhsT=hgT[:], rhs=g2b[:], start=True, stop=True), r=["hgT", "g2b"], w=["po"])
        P.V(lambda: nc.vector.tensor_tensor(out=ybf[:], in0=ybf[:], in1=po[:, 0:256], op=ALU.mult), r=["ybf", "po"], w=["ybf"])
        P.V(lambda: nc.vector.tensor_tensor(out=ycf[:], in0=inp["u"][:], in1=d_bc[:], op=ALU.mult), r=["i_u", "d_bc"], w=["ycf"])
        P.V(lambda: nc.vector.tensor_tensor(out=ycf[:], in0=ycf[:], in1=inp["cf"][:], op=ALU.add), r=["ycf", "i_cf"], w=["ycf"])
        P.V(lambda: nc.vector.tensor_tensor(out=ycf[:], in0=ycf[:], in1=inp["cb"][:], op=ALU.add), r=["ycf", "i_cb"], w=["ycf"])
        P.V(lambda: nc.vector.tensor_tensor(out=t256[:], in0=ycf[:], in1=ycf[:], op=ALU.mult), r=["ycf"], w=["t256"])
        P.V(lambda: nc.vector.tensor_scalar(out=t256[:], in0=t256[:], scalar1=0.044715, scalar2=1.0, op0=ALU.mult, op1=ALU.add), r=["t256"], w=["t256"])
        P.V(lambda: nc.vector.tensor_tensor(out=t256[:], in0=t256[:], in1=ycf[:], op=ALU.mult), r=["t256", "ycf"], w=["t256"])
        P.A(lambda: nc.scalar.activation(out=t256[:], in_=t256[:], func=AF.Sigmoid, scale=1.5957691216057308), r=["t256"], w=["t256"])
        P.V(lambda: nc.vector.tensor_tensor(out=ycf[:], in0=ycf[:], in1=t256[:], op=ALU.mult), r=["ycf", "t256"], w=["ycf"])
        for kc in range(2):
            P.T(lambda: nc.tensor.transpose(pst[:, kc, :], ycf[:, kc * 128:(kc + 1) * 128], ident[:]), r=["ycf", "ident32"], w=["pst"] if kc == 0 else [], wj=[] if kc == 0 else ["pst"])
        P.V(lambda: nc.vector.tensor_copy(out=yT[:], in_=pst[:, 0:2, :]), r=["pst"], w=["yT"])
        for kc in range(2):
            P.T(lambda: nc.tensor.matmul(po[:, 0:512], lhsT=yT[:, kc, :], rhs=glub[:, kc, :], start=(kc == 0), stop=(kc == 1)), r=["yT", "glub"], w=["po"] if kc == 0 else [], wj=[] if kc == 0 else ["po"])
        P.V(lambda: nc.vector.tensor_tensor(out=h512[:], in0=po[:, 0:512], in1=glub_bc[:], op=ALU.add), r=["po", "glub_bc"], w=["h512"])
        P.A(lambda: nc.scalar.activation(out=t256[:], in_=h512[:, 256:512], func=AF.Sigmoid), r=["h512"], w=["t256"])
        P.V(lambda: nc.vector.tensor_tensor(out=ycf[:], in0=h512[:, 0:256], in1=t256[:], op=ALU.mult), r=["h512", "t256"], w=["ycf"])
        srcs = [(inp["ya"], "i_ya"), (ybf, "ybf"), (ycf, "ycf"), (inp["yd"], "i_yd")]
        for i, (src, stok) in enumerate(srcs):
            for kc in range(2):
                P.T(lambda: nc.tensor.transpose(pst[:, kc, :], src[:, kc * 128:(kc + 1) * 128], ident[:]), r=[stok, "ident32"], w=["pst"] if kc == 0 else [], wj=[] if kc == 0 else ["pst"])
            P.V(lambda: nc.vector.tensor_copy(out=yT[:], in_=pst[:, 0:2, :]), r=["pst"], w=["yT"])
            for half in range(2):
                cs = slice(half * 512, (half + 1) * 512)
                for kc in range(2):
                    P.T(lambda: nc.tensor.matmul(pbr[:, cs], lhsT=yT[:, kc, :], rhs=Wbr[:, i * 2 + kc, cs], start=(kc == 0), stop=(kc == 1)),
                        r=["yT", "Wbr"], w=["pbr"] if (kc == 0 and half == 0) else [], wj=[] if (kc == 0 and half == 0) else ["pbr"])
                for kc in range(8):
                    P.T(lambda: nc.tensor.matmul(pg[:, cs], lhsT=xnT[:, kc, :], rhs=Wg[:, kc, i * 1024 + half * 512:i * 1024 + (half + 1) * 512], start=(kc == 0), stop=(kc == 7)),
                        r=["xnT", "Wg"], w=["pg"] if (kc == 0 and half == 0) else [], wj=[] if (kc == 0 and half == 0) else ["pg"])
            P.V(lambda: nc.vector.tensor_tensor(out=gate[:], in0=pg[:], in1=gb_bc[:, i * 1024:(i + 1) * 1024], op=ALU.add), r=["pg", "gb_bc"], w=["gate"])
            P.A(lambda: nc.scalar.activation(out=gate[:], in_=gate[:], func=AF.Sigmoid), r=["gate"], w=["gate"])
            if i == 0:
                P.V(lambda: nc.vector.tensor_tensor(out=merged[:], in0=pbr[:], in1=gate[:], op=ALU.mult), r=["pbr", "gate"], w=["merged"])
            else:
                P.V(lambda: nc.vector.tensor_tensor(out=tmpm[:], in0=pbr[:], in1=gate[:], op=ALU.mult), r=["pbr", "gate"], w=["tmpm"])
                P.G(lambda: nc.gpsimd.tensor_tensor(out=merged[:], in0=merged[:], in1=tmpm[:], op=ALU.add), r=["merged", "tmpm"], w=["merged"])
        for kc in range(8):
            P.T(lambda: nc.tensor.transpose(pst[:, kc, :], merged[:, kc * 128:(kc + 1) * 128], ident[:]), r=["merged", "ident32"], w=["pst"] if kc == 0 else [], wj=[] if kc == 0 else ["pst"])
        P.V(lambda: nc.vector.tensor_copy(out=mT[:], in_=pst[:]), r=["pst"], w=["mT"])
        for half in range(2):
            cs = slice(half * 512, (half + 1) * 512)
            for kc in range(8):
                P.T(lambda: nc.tensor.matmul(po[:, cs], lhsT=mT[:, kc, :], rhs=Wo[:, kc, cs], start=(kc == 0), stop=(kc == 7)),
                    r=["mT", "Wo"], w=["po"] if (kc == 0 and half == 0) else [], wj=[] if (kc == 0 and half == 0) else ["po"])
        P.V(lambda: nc.vector.tensor_tensor(out=xt[b][:], in0=xt[b][:], in1=po[:], op=ALU.add), r=[xk, "po"], w=[xk])
        P.dma(y[r0:r0 + 128, :], xt[b][:], r=[xk], wj=["y"], key=xk)
    P.finish(); P.close()
    return nc


def run_F1(xf, brs, hg, prm, l, ncores=8):
    T = xf.shape[0]; TC = T // ncores
    nc = build_F1(TC)
    com = {"g": prm["norm_mix_g"][l][None, :], "ident": np.eye(128, dtype=np.float32),
           "wgate": np.ascontiguousarray(prm["w_in"][l][:, 2304:]), "gate_b": prm["gate_b"][l].reshape(1, 4096), "w_branch": prm["w_branch"][l], "w_out": prm["w_out"][l],
           "glu_w": prm["s5_glu_w"][l], "glu_b": prm["s5_glu_b"][l][None, :], "g2": prm["rwkv_g2"][l],
           "vecs": np.stack([prm["rwkv_ln_w"][l], prm["rwkv_ln_b"][l], prm["s5_d"][l]])}
    maps = []
    for c in range(ncores):
        sl = slice(c * TC, (c + 1) * TC)
        m = dict(com, x=xf[sl], hg=np.ascontiguousarray(hg[sl]))
        for n, a in brs.items():
            m[n] = np.ascontiguousarray(a[sl])
        maps.append(m)
    res = run_bass_kernel_spmd(nc, maps, core_ids=list(range(ncores)))
    return np.concatenate([r["y"] for r in res.results], axis=0)


def kernel(**inp):
    prm = {k: np.ascontiguousarray(np.asarray(v, dtype=np.float32)) for k, v in inp.items()}
    x = prm["x"]
    B, S, D = x.shape
    xf = np.ascontiguousarray(x.reshape(B * S, D))
    f2 = lambda a: np.ascontiguousarray(a.reshape(B * S, a.shape[-1]))
    for l in range(2):
        zP = run_P(xf, prm, l)
        z = zP.reshape(B, S, NZ)
        brs = {n: f2(a) for n, a in run_M(z, prm, l).items()}
        brs["u"] = f2(z[:, :, 1536:1792])
        xmid = run_F1(xf, brs, np.ascontiguousarray(zP[:, 2816:2944]), prm, l)
        del zP, z, brs
        xf = run_F2(xmid, prm, l)
    return xf.reshape(B, S, D).astype(np.float32)
```
